# Optimizing a Trainium2 kernel written in Bass

```python
import jax
import jax.numpy as jnp
from jax import lax
import numpy as np

D_MODEL = 2048
BATCH = 2
SEQ = 8192
DEPTH = 1

CHUNK = 64
EPS = 1e-6
NEG_INF = -1e30
GMLP_BLOCK = 128
GMLP_GROUPS = 8
GMLP_WIDTH = 1024
GMLP_GROUP_DIM = GMLP_WIDTH // GMLP_GROUPS
ATT_HEADS = 16
ATT_HEAD_DIM = 64
ATT_WIDTH = ATT_HEADS * ATT_HEAD_DIM
LEFT_CHUNKS = 8
BAND = LEFT_CHUNKS + 1
MAX_REL = 256
N_BRANCH = 2
IN_COLS = 2 * GMLP_WIDTH + 3 * ATT_WIDTH + N_BRANCH * D_MODEL
N_GROUPS = 4
EXPERTS_PER_GROUP = 8
TOP_K = 2
D_EXPERT = 512
N_MOD = 6

kernel_name = 'hybrid_gmlp_bandattn_hmoe_adaln'


def _rmsnorm(x, g):
    xf = x.astype(jnp.float32)
    y = xf * lax.rsqrt(jnp.mean(xf * xf, axis=-1, keepdims=True) + EPS)
    return (y * g.astype(jnp.float32)).astype(x.dtype)


def _layernorm(x, g, b):
    xf = x.astype(jnp.float32)
    mu = jnp.mean(xf, axis=-1, keepdims=True)
    var = jnp.mean(jnp.square(xf - mu), axis=-1, keepdims=True)
    y = (xf - mu) * lax.rsqrt(var + EPS)
    return (y * g.astype(jnp.float32) + b.astype(jnp.float32)).astype(x.dtype)


def _gmlp_mask():
    pos = np.arange(GMLP_BLOCK)
    return (pos[None, :] // CHUNK) <= (pos[:, None] // CHUNK)


def _rel_index():
    i = np.arange(CHUNK)[:, None, None]
    j = np.arange(BAND)[None, :, None]
    l = np.arange(CHUNK)[None, None, :]
    dist = (LEFT_CHUNKS - j) * CHUNK + i - l
    return (np.clip(dist, -MAX_REL, MAX_REL) + MAX_REL).reshape(CHUNK, BAND * CHUNK).astype(np.int32)


def _spatial_gating(u, v, w_s, b_s, ln_g, ln_b):
    b, s, _ = v.shape
    nb = s // GMLP_BLOCK
    v = _layernorm(v, ln_g, ln_b).reshape(b, nb, GMLP_BLOCK, GMLP_GROUPS, GMLP_GROUP_DIM)
    w = jnp.where(jnp.asarray(_gmlp_mask())[None], w_s, jnp.zeros_like(w_s))
    mixed = jnp.einsum('gts,bnsgc->bntgc', w, v) + b_s.T[:, :, None]
    return u * mixed.reshape(b, s, GMLP_WIDTH)


def _band_attention(q, k, v, rel_table):
    b, s, h, dh = q.shape
    nc = s // CHUNK
    q = q.reshape(b, nc, CHUNK, h, dh)
    pad = ((0, 0), (LEFT_CHUNKS, 0), (0, 0), (0, 0), (0, 0))
    kp = jnp.pad(k.reshape(b, nc, CHUNK, h, dh), pad)
    vp = jnp.pad(v.reshape(b, nc, CHUNK, h, dh), pad)
    scores = jnp.concatenate(
        [jnp.einsum('bcqhd,bckhd->bhcqk', q, kp[:, j:j + nc]) for j in range(BAND)],
        axis=-1).astype(jnp.float32)
    bias = rel_table.astype(jnp.float32)[:, jnp.asarray(_rel_index())]
    scores = scores * (dh ** -0.5) + bias[None, :, None]
    key_chunk = jnp.arange(nc)[:, None] + jnp.arange(BAND)[None, :] - LEFT_CHUNKS
    key_valid = jnp.repeat(key_chunk >= 0, CHUNK, axis=1)
    scores = jnp.where(key_valid[None, None, :, None, :], scores, NEG_INF)
    probs = jax.nn.softmax(scores, axis=-1).astype(v.dtype).reshape(b, h, nc, CHUNK, BAND, CHUNK)
    out = jnp.einsum('bhcqk,bckhd->bcqhd', probs[:, :, :, :, 0], vp[:, 0:nc])
    for j in range(1, BAND):
        out = out + jnp.einsum('bhcqk,bckhd->bcqhd', probs[:, :, :, :, j], vp[:, j:j + nc])
    return out.reshape(b, s, h * dh)


def _hierarchical_moe(h, w_group, w_expert, w1, w3, w2):
    g_logits = (h @ w_group).astype(jnp.float32)
    g_probs = jax.nn.softmax(g_logits, axis=-1)
    _, g_idx = lax.top_k(g_logits, 1)
    g_onehot = jax.nn.one_hot(g_idx[:, 0], N_GROUPS, dtype=jnp.float32)
    g_weight = jnp.sum(g_probs * g_onehot, axis=-1, keepdims=True)
    e_logits = jnp.einsum('nd,gde->nge', h, w_expert).astype(jnp.float32)
    e_sel = jnp.einsum('nge,ng->ne', e_logits, g_onehot)
    e_top, e_idx = lax.top_k(e_sel, TOP_K)
    e_w = jax.nn.softmax(e_top, axis=-1)
    e_weight = jnp.einsum('nk,nke->ne', e_w, jax.nn.one_hot(e_idx, EXPERTS_PER_GROUP, dtype=jnp.float32))
    combine = ((g_weight * g_onehot)[:, :, None] * e_weight[:, None, :]).astype(h.dtype)
    out = jnp.zeros_like(h)
    for g in range(N_GROUPS):
        a = jnp.einsum('nd,edf->nef', h, w1[g])
        bgate = jnp.einsum('nd,edf->nef', h, w3[g])
        act = jax.nn.silu(a) * bgate * combine[:, g, :, None]
        out = out + jnp.einsum('nef,efd->nd', act, w2[g])
    return out


def setup_inputs(seed: int = 0) -> dict:
    key = jax.random.key(seed)
    ks = jax.random.split(key, 24)
    f32 = jnp.float32
    L, D = DEPTH, D_MODEL

    def nrm(k, shape, scale):
        return jax.random.normal(k, shape, f32) * scale

    return {
        'x': nrm(ks[0], (BATCH, SEQ, D), 1.0),
        'c': nrm(ks[1], (BATCH, D), 1.0),
        'ada_w': nrm(ks[2], (L, D, N_MOD * D), 0.5 * D ** -0.5),
        'ada_b': nrm(ks[3], (L, N_MOD * D), 0.02),
        'norm1_g': 1.0 + nrm(ks[4], (L, D), 0.1),
        'w_in': nrm(ks[5], (L, D, IN_COLS), D ** -0.5),
        'gmlp_ln_g': 1.0 + nrm(ks[6], (L, GMLP_WIDTH), 0.1),
        'gmlp_ln_b': nrm(ks[7], (L, GMLP_WIDTH), 0.02),
        'gmlp_w_s': nrm(ks[8], (L, GMLP_GROUPS, GMLP_BLOCK, GMLP_BLOCK), GMLP_BLOCK ** -0.5),
        'gmlp_b_s': 1.0 + nrm(ks[9], (L, GMLP_GROUPS, GMLP_BLOCK), 0.1),
        'rel_bias': nrm(ks[10], (L, ATT_HEADS, 2 * MAX_REL + 1), 0.5),
        'w_branch_a': nrm(ks[11], (L, GMLP_WIDTH, D), GMLP_WIDTH ** -0.5),
        'w_branch_b': nrm(ks[12], (L, ATT_WIDTH, D), ATT_WIDTH ** -0.5),
        'w_out': nrm(ks[13], (L, D, D), D ** -0.5),
        'norm2_g': 1.0 + nrm(ks[14], (L, D), 0.1),
        'w_group': nrm(ks[15], (L, D, N_GROUPS), D ** -0.5),
        'w_expert': nrm(ks[16], (L, N_GROUPS, D, EXPERTS_PER_GROUP), D ** -0.5),
        'w1': nrm(ks[17], (L, N_GROUPS, EXPERTS_PER_GROUP, D, D_EXPERT), D ** -0.5),
        'w3': nrm(ks[18], (L, N_GROUPS, EXPERTS_PER_GROUP, D, D_EXPERT), D ** -0.5),
        'w2': nrm(ks[19], (L, N_GROUPS, EXPERTS_PER_GROUP, D_EXPERT, D), D_EXPERT ** -0.5),
        'final_g': 1.0 + nrm(ks[20], (D,), 0.1),
    }


def reference(x, c, ada_w, ada_b, norm1_g, w_in, gmlp_ln_g, gmlp_ln_b, gmlp_w_s, gmlp_b_s,
              rel_bias, w_branch_a, w_branch_b, w_out, norm2_g, w_group, w_expert, w1, w3, w2,
              final_g):
    b, s, d = x.shape
    cut = [GMLP_WIDTH, 2 * GMLP_WIDTH, 2 * GMLP_WIDTH + ATT_WIDTH,
           2 * GMLP_WIDTH + 2 * ATT_WIDTH, 2 * GMLP_WIDTH + 3 * ATT_WIDTH]
    for layer in range(DEPTH):
        mod = jax.nn.silu(c) @ ada_w[layer] + ada_b[layer]
        shift1, scale1, gate1, shift2, scale2, gate2 = jnp.split(mod[:, None, :], N_MOD, axis=-1)

        h = _rmsnorm(x, norm1_g[layer]) * (1 + scale1) + shift1
        proj = h @ w_in[layer]
        u_a, v_a, q, k, v_b, gates = jnp.split(proj, cut, axis=-1)
        y_a = _spatial_gating(jax.nn.gelu(u_a, approximate=False), jax.nn.gelu(v_a, approximate=False),
                              gmlp_w_s[layer], gmlp_b_s[layer], gmlp_ln_g[layer], gmlp_ln_b[layer])
        y_a = y_a @ w_branch_a[layer]
        hs = (b, s, ATT_HEADS, ATT_HEAD_DIM)
        y_b = _band_attention(q.reshape(hs), k.reshape(hs), v_b.reshape(hs), rel_bias[layer])
        y_b = y_b @ w_branch_b[layer]
        gate_a, gate_b = jnp.split(jax.nn.sigmoid(gates), N_BRANCH, axis=-1)
        mixed = (gate_a * y_a + gate_b * y_b) @ w_out[layer]
        x = x + gate1 * mixed

        h = _rmsnorm(x, norm2_g[layer]) * (1 + scale2) + shift2
        y = _hierarchical_moe(h.reshape(b * s, d), w_group[layer], w_expert[layer],
                              w1[layer], w3[layer], w2[layer]).reshape(b, s, d)
        x = x + gate2 * y
    return _rmsnorm(x, final_g)
```

```python
import contextlib
import os
import numpy as np
import concourse.bass as bass
import concourse.mybir as mybir
from concourse.bass_utils import run_bass_kernel_spmd

F32 = mybir.dt.float32
BF16 = mybir.dt.bfloat16
I32 = mybir.dt.int32
AF = mybir.ActivationFunctionType
ALU = mybir.AluOpType
AX = mybir.AxisListType

D = 2048
TOKC = 2048
TP = 1024
HALO = 512
NPASS = 2
EPS = 1e-6
NSLOT = 5
SLOT_B = 16384
NEG = -30000.0
EVAC_SPLIT = os.environ.get('K_EVAC', '0') == '1'
ADA_DVE = os.environ.get('K_ADA', '1') == '1'
PREFETCH = os.environ.get('K_PRE', '1') == '1'
CONTIG = os.environ.get('K_CONTIG', '1') == '1'
NE = 32
CAP = 2048
MSLOT = 8
BIGIDX = 1 << 24


class Sem:
    def __init__(self, h, shared=False):
        self.h = h
        self.n = 0
        self.shared = shared


class Eng:
    def __init__(self, name, sem, selfwait=True):
        self.name = name
        self.sem = sem
        self.prog = []
        self.waited = {}
        self.selfwait = selfwait


class Prog:
    def __init__(self, target, handle):
        self.state = {}
        self.floor = {}
        self.nofloor = set()
        self.target = target
        self.handle = handle
        self.inc_log = []
        self.alias = {}
        self.engines = []

    def fence(self):
        for k, st in self.state.items():
            if isinstance(k, tuple) and k[0] == "ring":
                continue
            evs = ([st[0]] if st[0] else []) + list(st[1].items())
            for s, v in evs:
                if s.shared:
                    v = s.n
                if self.floor.get(s, 0) < v:
                    self.floor[s] = v

    def _expand(self, keys):
        out = []
        for k in keys:
            out.extend(self.alias.get(k, (k,)) if isinstance(k, tuple) else (k,))
        return out

    def _deps(self, reads, writes, eng=None):
        reads = self._expand(reads)
        writes = self._expand(writes)
        w = {}

        def add(ev):
            if ev is None:
                return
            s, v = ev
            if s.shared:
                v = s.n
            if w.get(s, 0) < v:
                w[s] = v

        if eng is not None and eng.name not in self.nofloor:
            for s, v in self.floor.items():
                add((s, v))

        for k in reads:
            st = self.state.get(k)
            if st:
                add(st[0])
        for k in writes:
            st = self.state.get(k)
            if st:
                add(st[0])
                for s, v in st[1].items():
                    add((s, v))
        return w

    def _commit(self, reads, writes, ev):
        reads = self._expand(reads)
        writes = self._expand(writes)
        s, v = ev
        for k in reads:
            st = self.state.setdefault(k, [None, {}])
            if st[1].get(s, 0) < v:
                st[1][s] = v
        for k in writes:
            self.state[k] = [ev, {}]

    def op(self, eng, reads, writes, emit):
        w = self._deps(reads, writes, eng)
        wl = []
        for s, v in w.items():
            if s is eng.sem and not eng.selfwait:
                continue
            if eng.waited.get(s, 0) < v:
                eng.waited[s] = v
                wl.append((s, v))
        self.inc_log.append((eng.name, eng.sem, 1))
        eng.sem.n += 1
        ev = (eng.sem, eng.sem.n)
        sem = eng.sem

        if eng.name == self.target:
            e = self.handle
            for s, v in wl:
                e.wait_ge(s.h, v)
            emit(e).then_inc(sem.h, 1)
        self._commit(reads, writes, ev)

    def dma(self, eng, reads, writes, emit, dsem):
        w = self._deps(reads, writes, eng)
        if dsem.n > 0 and w.get(dsem, 0) < dsem.n:
            w[dsem] = dsem.n
        wl = []
        for s, v in w.items():
            if eng.waited.get(s, 0) < v:
                eng.waited[s] = v
                wl.append((s, v))
        self.inc_log.append((eng.name, dsem, 16))
        dsem.n += 16
        ev = (dsem, dsem.n)

        if eng.name == self.target:
            e = self.handle
            for s, v in wl:
                e.wait_ge(s.h, v)
            emit(e).then_inc(dsem.h, 16)
        self._commit(reads, writes, ev)

    def raw(self, eng, reads, emit):
        w = self._deps(reads, [], eng)
        wl = []
        for s, v in w.items():
            if s is eng.sem and not eng.selfwait:
                continue
            if eng.waited.get(s, 0) < v:
                eng.waited[s] = v
                wl.append((s, v))
        if eng.name == self.target:
            for s, v in wl:
                self.handle.wait_ge(s.h, v)
            return emit(self.handle)
        return None

    @contextlib.contextmanager
    def guard(self, cond_fn):
        snap_waited = {e.name: dict(e.waited) for e in self.engines}
        log0 = len(self.inc_log)
        before = {}
        ctx = None
        if self.target is not None:
            ctx = self.handle.If(cond_fn(self.handle))
            ctx.__enter__()
        sems_seen = {}
        n_at_entry = lambda sem: sems_seen.setdefault(id(sem), sem.n)
        entry_vals = {}
        class _Peek:
            pass
        all_sems = set()
        for (_, sem, _) in self.inc_log:
            all_sems.add(sem)
        entry_vals = {sem: sem.n for sem in all_sems}
        try:
            yield
        finally:
            if ctx is not None:
                ctx.__exit__(None, None, None)
                comp = {}
                order = []
                for (en, sem, amt) in self.inc_log[log0:]:
                    if en == self.target:
                        if sem not in comp:
                            comp[sem] = 0
                            order.append(sem)
                        comp[sem] += amt
                ectx = self.handle.Else()
                ectx.__enter__()
                for sem in order:
                    v0 = entry_vals.get(sem, 0)
                    if v0 > 0:
                        self.handle.wait_ge(sem.h, v0)
                    self.handle.sem_inc(sem.h, comp[sem])
                ectx.__exit__(None, None, None)
            for e in self.engines:
                e.waited = snap_waited[e.name]

    def final_wait(self, eng, keys):
        w = self._deps(keys, keys)
        wl = list(w.items())

        if eng.name == self.target:
            for s, v in wl:
                self.handle.wait_ge(s.h, v)


def build(debug=None):
    nc = bass.Bass("TRN2", target_bir_lowering=False)

    def din(name, shape):
        return nc.dram_tensor(name, list(shape), F32, kind="ExternalInput").ap()

    xe = din("xe", [TOKC + HALO, D])
    cT = din("cT", [128, 16])
    flags_d = din("flags", [128, 2])
    ident_d = din("ident", [128, 128])
    ada_w = din("ada_w", [D, 6 * D])
    ada_b = din("ada_b", [1, 6 * D])
    g1T_d = din("g1T", [128, 16])
    g2T_d = din("g2T", [128, 16])
    lnG = din("lnG", [1, 1024])
    lnB = din("lnB", [1, 1024])
    bS = din("bS", [1, 1024])
    wS = din("wS", [8, 128, 128])
    biasT = din("biasT", [128, 16 * 384])
    tabc_d = din("tabc", [1, 16])
    w_in = din("w_in", [D, 9216])
    wbrA = din("wbrA", [1024, D])
    wbrB = din("wbrB", [1024, D])
    wout = din("wout", [D, D])
    wr_d = din("wr", [D, 36])
    w1 = din("w1", [NE, D, 512])
    w3 = din("w3", [NE, D, 512])
    w2 = din("w2", [NE, 512, D])
    fg = din("fg", [1, D])
    g2row = din("g2row", [1, D])
    g1row = din("g1row", [1, D])
    Utri_d = din("Utri", [128, 128])
    Ones_d = din("OnesM", [128, 128])
    ecap_d = din("ecap", [128, 32])
    vals_d = nc.dram_tensor("vals", [128, 128], I32, kind="ExternalInput").ap()
    fillv_d = nc.dram_tensor("fillv", [128, 2048], I32, kind="ExternalInput").ap()
    h2d = nc.dram_tensor("h2d", [TOKC, D], BF16, kind="Internal").ap()
    table = nc.dram_tensor("table", [NE * CAP, 4], I32, kind="Internal").ap()
    cntd = nc.dram_tensor("cntd", [1, NE], I32, kind="Internal").ap()
    Ybuf4 = nc.dram_tensor("Ybuf", [4 * TOKC + 256, D // 2], F32, kind="Internal").ap()
    Ybuf = Ybuf4.rearrange("(r h) c -> r (h c)", h=2)
    y = nc.dram_tensor("y", [TOKC, D], F32, kind="ExternalOutput").ap()
    x1s = nc.dram_tensor("x1s", [TOKC, D], F32, kind="Internal").ap()
    modd = nc.dram_tensor("modd", [1, 6 * D], F32, kind="Internal").ap()
    dbg_out = None
    if debug is not None:
        dbg_out = nc.dram_tensor("dbg", [128, debug["words"]], F32, kind="ExternalOutput").ap()

    es = contextlib.ExitStack()
    with es:
        es.enter_context(nc.allow_low_precision("bf16 matmul operands by design"))
        ARENA_W = 53200
        arena = es.enter_context(nc.sbuf_tensor("arena", [128, ARENA_W], F32))
        PS = [es.enter_context(nc.psum_tensor(f"ps{i}", [128, 1024], F32)) for i in range(4)]

        SEMH = {}
        for _n in (["s_pe", "s_act", "s_dve", "s_pool", "s_sp", "s_const", "s_misc"] + [f"s_ring{i}" for i in range(NSLOT)] + [f"s_ringb{i}" for i in range(NSLOT)] + [f"s_adb{i}" for i in range(3)] +
                   [f"s_x{i}" for i in range(2)] + [f"s_o{i}" for i in range(2)] + [f"s_ad{i}" for i in range(3)] +
                   [f"s_mr{i}" for i in range(MSLOT)] + [f"s_ix{i}" for i in range(5)] + [f"s_ga{i}" for i in range(5)] +
                   [f"s_sc{i}" for i in range(8)] + [f"s_h{i}" for i in range(2)] + [f"s_y{i}" for i in range(4)] + ["s_tab"] +
                   [f"s_cp{i}" for i in range(8)] + [f"s_f{i}" for i in range(12)] + [f"s_tp{i}" for i in range(4)] + [f"s_ab{i}" for i in range(2)] + [f"s_md{i}" for i in range(2)]):
            SEMH[_n] = es.enter_context(nc.semaphore(_n))

        def program(target, handle):
            def newsem(name):
                return Sem(SEMH[name])

            PE = Eng("pe", newsem("s_pe"), selfwait=False)
            ACT = Eng("act", newsem("s_act"))
            DVE = Eng("dve", newsem("s_dve"))
            POOL = Eng("pool", newsem("s_pool"))
            SP = Eng("sp", newsem("s_sp"))
            ring_sem = [newsem(f"s_ring{i}") for i in range(NSLOT)]
            ring_semb = [newsem(f"s_ringb{i}") for i in range(NSLOT)]
            adsemb = [newsem(f"s_adb{i}") for i in range(3)]
            xsem = [newsem(f"s_x{i}") for i in range(2)]
            osem = [newsem(f"s_o{i}") for i in range(2)]
            csem = newsem("s_const")
            csem.shared = True
            msem = newsem("s_misc")
            msem.shared = True
            adsem = [newsem(f"s_ad{i}") for i in range(3)]
            mr_sem = [newsem(f"s_mr{i}") for i in range(MSLOT)]
            ixsem = [newsem(f"s_ix{i}") for i in range(5)]
            gasem = [newsem(f"s_ga{i}") for i in range(5)]
            scsem = [newsem(f"s_sc{i}") for i in range(8)]

            class SemPool:
                def __init__(self, sems):
                    self.sems = sems
                    self.i = 0

                def nxt(self):
                    sm = self.sems[self.i % len(self.sems)]
                    self.i += 1
                    return sm

            cpool = SemPool([newsem(f"s_cp{i}") for i in range(8)])
            tpool = SemPool([newsem(f"s_tp{i}") for i in range(4)])
            absem = [newsem(f"s_ab{i}") for i in range(2)]
            fsem = [newsem(f"s_f{i}") for i in range(12)]
            mdsem = [newsem(f"s_md{i}") for i in range(2)]
            hsem = [newsem(f"s_h{i}") for i in range(2)]
            ysem = [newsem(f"s_y{i}") for i in range(4)]
            tabsem = newsem("s_tab")
            tabsem.shared = True
            P = Prog(target, handle)
            P.nofloor.add("pool")
            P.engines = [PE, ACT, DVE, POOL, SP]

            def sb(off, shape, dt=F32, parts=(0, 128)):
                esz = 4 if dt in (F32, I32) else 2
                n = int(np.prod(shape))
                nbytes = n * esz
                assert off % 4 == 0 and nbytes % 4 == 0
                assert off + nbytes <= ARENA_W * 4, (off, nbytes)
                ap = arena[parts[0]:parts[1], off // 4:(off + nbytes) // 4]
                if dt != F32:
                    ap = ap.bitcast(dt)
                if len(shape) == 2:
                    ap = ap.rearrange("p (a b) -> p a b", a=shape[0], b=shape[1])
                elif len(shape) == 3:
                    ap = ap.rearrange("p (a b c) -> p a b c", a=shape[0], b=shape[1], c=shape[2])
                return ap

            def pbank(b, n=512, off=0):
                return PS[b // 2][:, (b % 2) * 512 + off:(b % 2) * 512 + off + n]

            def pkey(b):
                return ("ps", b)

            C0 = 0
            o = C0
            identF = sb(o, [128]); o += 512
            identB = sb(o, [128], BF16); o += 256
            modT = sb(o, [96]); o += 384
            effT = sb(o, [32]); o += 128
            g1T = sb(o, [16]); o += 64
            g2T = sb(o, [16]); o += 64
            tabc = sb(o, [16]); o += 64
            flags = sb(o, [2]); o += 8
            ones8 = sb(o, [8]); o += 32
            wr = sb(o, [16, 36]); o += 2304
            WsT = sb(o, [8, 128], BF16); o += 2048
            small = sb(o, [256]); o += 1024
            rt = sb(o, [256]); o += 1024
            urow = sb(o, [128], BF16, parts=(0, 1)); o += 256
            vrow = sb(o, [128], BF16, parts=(0, 1)); o += 256
            sel0 = sb(o, [16, 32]); o += 2048
            sel1 = sb(o, [16, 32]); o += 2048
            wsl = sb(o, [16, 2]); o += 128
            ecap = sb(o, [32]); o += 128
            vals = sb(o, [128], I32); o += 512
            CONST_END = o
            RING0 = (CONST_END + 63) // 64 * 64
            PH0 = RING0 + NSLOT * SLOT_B
            PH_END = ARENA_W * 4

            def ring_view(slot, a, b):
                return sb(RING0 + slot * SLOT_B, [a, b], BF16)

            class WS:
                units = []
                released = []
                nloaded = 0

            def w_add(src, a, b):
                WS.units.append((src, a, b))
                WS.released.append(False)
                return len(WS.units) - 1

            def w_pump():
                while WS.nloaded < len(WS.units):
                    u = WS.nloaded
                    if u >= NSLOT and not WS.released[u - NSLOT]:
                        break
                    src, a, b = WS.units[u]
                    slot = u % NSLOT
                    dst = ring_view(slot, a, b)
                    P.alias[("ring", slot)] = (("ring", slot, 0), ("ring", slot, 1))
                    h_ = a // 2
                    P.dma(POOL, [], [("ring", slot, 0)],
                          lambda e, dst=dst, src=src: e.dma_start(out=dst[:, 0:h_, :], in_=src[:, 0:h_, :]),
                          ring_sem[slot])
                    P.dma(POOL, [], [("ring", slot, 1)],
                          lambda e, dst=dst, src=src: e.dma_start(out=dst[:, h_:a, :], in_=src[:, h_:a, :]),
                          ring_semb[slot])
                    WS.nloaded += 1

            def w_get(u):
                w_pump()
                assert u < WS.nloaded, ("weight unit not loadable", u, WS.nloaded)
                src, a, b = WS.units[u]
                slot = u % NSLOT
                return ring_view(slot, a, b), ("ring", slot)

            def w_rel(u):
                WS.released[u] = True
                w_pump()

            def win_blk(c0):
                return w_in[:, c0:c0 + 512].rearrange("(c p) n -> p c n", p=128)

            plan = []
            for p in range(NPASS):
                pl = {}
                pl["k"] = [None, None]
                pl["vb"] = [None, None]
                pl["q"] = [None, None]
                for hh in range(2):
                    pl["k"][hh] = w_add(win_blk(3072 + hh * 512), 16, 512)
                    pl["vb"][hh] = w_add(win_blk(4096 + hh * 512), 16, 512)
                    pl["q"][hh] = w_add(win_blk(2048 + hh * 512), 16, 512)
                pl["u"] = [w_add(win_blk(0 + i * 512), 16, 512) for i in range(2)]
                pl["v"] = [w_add(win_blk(1024 + i * 512), 16, 512) for i in range(2)]
                pl["gA"] = [None] * 4
                pl["gB"] = [None] * 4
                pl["brA"] = [None] * 2
                pl["brB"] = [None] * 2
                for q in range(4):
                    pl["gA"][q] = w_add(win_blk(5120 + q * 512), 16, 512)
                    pl["gB"][q] = w_add(win_blk(7168 + q * 512), 16, 512)
                    if q % 2 == 0:
                        hf = q // 2
                        pl["brA"][hf] = w_add(wbrA[:, hf * 1024:(hf + 1) * 1024].rearrange("(c p) n -> p c n", p=128), 8, 1024)
                        pl["brB"][hf] = w_add(wbrB[:, hf * 1024:(hf + 1) * 1024].rearrange("(c p) n -> p c n", p=128), 8, 1024)
                pl["wo"] = [w_add(wout[:, cu * 512:(cu + 1) * 512].rearrange("(c p) n -> p c n", p=128), 16, 512) for cu in range(4)]
                plan.append(pl)

            def act_fn(out, in_, func, reads, writes, scale=1.0, bias=0.0, accum=None):
                def emit(e):
                    kw = {}
                    if accum is not None:
                        kw["accum_out"] = accum
                    return e.activation(out=out, in_=in_, func=func, bias=bias, scale=scale, **kw)
                P.op(ACT, reads, writes, emit)

            def dve(reads, writes, emit):
                P.op(DVE, reads, writes, emit)

            def pe_group(reads, writes, emit):
                P.op(PE, reads, writes, emit)

            def mm_acc(e, out, pairs):
                n = len(pairs)
                ins = None
                for i, (l, r) in enumerate(pairs):
                    ins = e.matmul(out, l, r, start=(i == 0), stop=(i == n - 1))
                return ins

            def sp_load(dst, src, writes, sem, reads=()):
                P.dma(SP, list(reads), list(writes), lambda e: e.dma_start(out=dst, in_=src), sem)

            sp_load(identF, ident_d, ["identF"], cpool.nxt())
            sp_load(flags, flags_d, ["flags"], cpool.nxt())
            sp_load(g1T, g1T_d, ["g1T"], cpool.nxt())
            sp_load(g2T, g2T_d, ["g2T"], cpool.nxt())
            sp_load(tabc, tabc_d[0].partition_broadcast(128), ["tabc"], cpool.nxt())
            sp_load(wr, wr_d.rearrange("(c p) n -> p c n", p=128), ["wr"], cpool.nxt())
            sp_load(ecap, ecap_d, ["ecap"], cpool.nxt())
            sp_load(vals, vals_d, ["vals"], cpool.nxt())
            scT = sb(PH0, [16])
            cTs = sb(PH0 + 64, [16])
            one11 = sb(PH0 + 128, [1])
            sp_load(cTs, cT, ["cTs"], cpool.nxt())
            dve(["identF"], ["identB"], lambda e: e.tensor_copy(out=identB, in_=identF))
            dve([], ["ones8"], lambda e: e.memset(ones8, 1.0))
            dve([], ["uvrow"], lambda e: e.memset(urow[:, 0:64], NEG))
            dve([], ["uvrow"], lambda e: e.memset(urow[:, 64:128], 0.0))
            dve([], ["uvrow"], lambda e: e.memset(vrow[:, 0:64], 0.0))
            dve([], ["uvrow"], lambda e: e.memset(vrow[:, 64:128], 1.0))
            dve([], ["one11"], lambda e: e.memset(one11, 1.0))
            act_fn(scT, cTs, AF.Silu, ["cTs"], ["scT"])

            wsf = sb(PH0 + 1024, [8, 128])
            P.dma(SP, [], ["wsf"], lambda e: e.dma_start(out=wsf, in_=wS.rearrange("g t s -> t g s")), cpool.nxt())
            dve(["wsf"], ["wsf"], lambda e: e.memset(wsf[0:64, :, 64:128], 0.0))
            for hb in range(2):
                def emit(e, hb=hb):
                    ins = None
                    for g in range(4):
                        gg = hb * 4 + g
                        ins = e.transpose(pbank(hb, 128, g * 128), wsf[:, gg, :], identF)
                    return ins
                pe_group(["wsf", "identF"], [pkey(hb)], emit)
                dve([pkey(hb)], ["WsT"], lambda e, hb=hb: e.tensor_copy(
                    out=WsT[:, hb * 4:(hb + 1) * 4, :], in_=pbank(hb).rearrange("p (g t) -> p g t", g=4)))

            AD0 = PH0 + 5120
            NAD = 3
            adbuf = [sb(AD0 + i * 32768, [16, 512]) for i in range(NAD)]
            abrow = sb(AD0 + NAD * 32768, [512], parts=(0, 1))
            abrow2 = sb(AD0 + NAD * 32768 + 2048, [512], parts=(0, 1))
            mrow = [sb(AD0 + NAD * 32768 + 4096 + i * 2048, [512], parts=(0, 1)) for i in range(2)]
            accb = [sb(AD0 + NAD * 32768 + 8192 + i * 2048, [512]) for i in range(2)]
            NB = 24
            MTB = 7
            def ada_load(blk):
                bi = blk % NAD
                P.alias[("adbuf", bi)] = (("adbuf", bi, 0), ("adbuf", bi, 1))
                srcv = ada_w[:, blk * 512:(blk + 1) * 512].rearrange("(c p) n -> p c n", p=128)
                P.dma(SP, [], [("adbuf", bi, 0)], lambda e: e.dma_start(out=adbuf[bi][:, 0:8, :], in_=srcv[:, 0:8, :]), adsem[bi])
                P.dma(SP, [], [("adbuf", bi, 1)], lambda e: e.dma_start(out=adbuf[bi][:, 8:16, :], in_=srcv[:, 8:16, :]), adsemb[bi])

            for blk in range(min(NAD - 1, NB)):
                ada_load(blk)
            for blk in range(NB):
                bi = blk % NAD
                if blk + NAD - 1 < NB:
                    ada_load(blk + NAD - 1)
                ab = abrow if blk % 2 == 0 else abrow2
                abk = ("abrow", blk % 2)
                P.dma(SP, [], [abk], lambda e, ab=ab, blk=blk: e.dma_start(out=ab, in_=ada_b[:, blk * 512:(blk + 1) * 512]), absem[blk % 2])
                pb_ = blk % 2

                ac = accb[blk % 2]
                ack = ("accb", blk % 2)
                NDV = 10
                pe_group([("adbuf", bi), "scT"], [pkey(pb_)], lambda e, bi=bi, pb_=pb_: [
                    e.matmul(pbank(pb_)[0:1, :], scT[:, dc:dc + 1], adbuf[bi][:, dc, :], start=(dc == NDV), stop=False)
                    for dc in range(NDV, 16)][-1])
                dve([("adbuf", bi), "scT"], [ack], lambda e, bi=bi, ac=ac: e.tensor_scalar(
                    out=ac, in0=adbuf[bi][:, 0, :], scalar1=scT[:, 0:1], scalar2=None, op0=ALU.mult))
                for dc in range(1, NDV):
                    dve([("adbuf", bi), "scT", ack], [ack], lambda e, bi=bi, ac=ac, dc=dc: e.scalar_tensor_tensor(
                        out=ac, in0=adbuf[bi][:, dc, :], scalar=scT[:, dc:dc + 1], in1=ac, op0=ALU.mult, op1=ALU.add))
                pe_group([ack, "ones8"], [pkey(pb_)], lambda e, ac=ac, pb_=pb_: e.matmul(
                    pbank(pb_)[0:1, :], ones8[:, 0:1], ac, start=False, stop=True))
                mr = mrow[blk % 2]
                mrk = ("mrow", blk % 2)
                dve([pkey(pb_), abk], [mrk], lambda e, mr=mr, ab=ab, pb_=pb_: e.tensor_tensor(
                    out=mr, in0=pbank(pb_)[0:1, :], in1=ab, op=ALU.add))
                P.dma(SP, [mrk], [("modd", blk)], lambda e, mr=mr, blk=blk: e.dma_start(
                    out=modd[:, blk * 512:(blk + 1) * 512], in_=mr), mdsem[blk % 2])

                def emit2(e, mr=mr, blk=blk):
                    ins = None
                    for k in range(4):
                        ins = e.matmul(pbank(MTB)[:, blk * 4 + k:blk * 4 + k + 1], mr[0:1, k * 128:(k + 1) * 128], one11[0:1, 0:1],
                                       start=True, stop=True)
                    return ins
                pe_group([mrk, "one11"], [("mtb", blk)], emit2)
            dve([("mtb", b) for b in range(NB)], ["modT"], lambda e: e.tensor_copy(out=modT, in_=pbank(MTB)[:, 0:96]))
            dve(["modT", "g1T"], ["effT"], lambda e: e.scalar_tensor_tensor(
                out=effT[:, 0:16], in0=modT[:, 16:32], scalar=1.0, in1=g1T, op0=ALU.add, op1=ALU.mult))
            dve(["modT", "g2T"], ["effT"], lambda e: e.scalar_tensor_tensor(
                out=effT[:, 16:32], in0=modT[:, 64:80], scalar=1.0, in1=g2T, op0=ALU.add, op1=ALU.mult))
            shift1 = modT[:, 0:16]
            shift2 = modT[:, 48:64]
            setup_keys = ["modT", "effT", "identF", "identB", "flags", "tabc", "wr", "WsT", "ones8"]
            setup_scratch = ["scT", "cTs", "one11", "wsf"] + [("adbuf", i) for i in range(3)] + \
                [("abrow", i) for i in range(2)] + [("mrow", i) for i in range(2)]
            modd_keys = [("modd", b) for b in range(NB)]

            dbg_done = [False]

            def dbg_dump(items):
                off = 0
                for ap, nw, keys in items:
                    P.dma(SP, list(keys), [("dbg", off)], lambda e, ap=ap, off=off, nw=nw: e.dma_start(
                        out=dbg_out[:, off:off + nw], in_=ap), msem)
                    off += nw
                dbg_done[0] = True

            def rms_rstd(ss_ap, rstd_ap, key_ss, key_rstd):
                act_fn(rstd_ap, ss_ap, AF.Sqrt, [key_ss], [key_rstd], scale=1.0 / D, bias=EPS)
                dve([key_rstd], [key_rstd], lambda e: e.reciprocal(out=rstd_ap, in_=rstd_ap))

            for p in range(NPASS if debug is None else debug.get("npass", NPASS)):
                pl = plan[p]
                row0 = p * TP
                fl = flags[:, p:p + 1]
                o = PH0
                h1own = sb(o, [16, 1024], BF16); o += 32768
                ybT = sb(o, [8, 1024], BF16); o += 16384
                GM0 = o
                h1halo = sb(o, [16, 512], BF16); o += 16384
                MIX0 = o
                xt = [sb(MIX0 + i * 8192, [2048]) for i in range(2)]
                junk = sb(MIX0 + 16384, [2048], BF16)
                P.fence()
                effB1 = sb(MIX0 + 20480, [2048])
                shiftB1 = sb(MIX0 + 28672, [2048])
                g1b = sb(PH0 + 32768, [2048])
                h1tok = [sb(MIX0 + 36864 + i * 4096, [2048], BF16) for i in range(2)]
                sp_load(g1b, g1row[0].partition_broadcast(128), ["g1b"], cpool.nxt())
                sp_load(effB1, modd[0, D:2 * D].partition_broadcast(128), ["effB1"], cpool.nxt(), reads=modd_keys)
                sp_load(shiftB1, modd[0, 0:D].partition_broadcast(128), ["shiftB1"], cpool.nxt(), reads=modd_keys)
                dve(["effB1", "g1b"], ["effB1"], lambda e: e.scalar_tensor_tensor(
                    out=effB1, in0=effB1, scalar=1.0, in1=g1b, op0=ALU.add, op1=ALU.mult))
                def p1_front(j):
                    b = j % 2
                    xk = ("xt", b)
                    P.dma(SP, [], [xk], lambda e, b=b, j=j: e.dma_start(
                        out=xt[b], in_=xe[row0 + j * 128:row0 + (j + 1) * 128, :]), xsem[b])
                    ssa = small[:, j:j + 1]
                    rsa = small[:, 16 + j:17 + j]
                    act_fn(junk, xt[b], AF.Square, [xk], [("ss", j)], accum=ssa)
                    rms_rstd(ssa, rsa, ("ss", j), ("rs", j))
                    dve([xk, ("rs", j), "effB1"], [xk], lambda e, b=b, rsa=rsa: e.scalar_tensor_tensor(
                        out=xt[b], in0=xt[b], scalar=rsa, in1=effB1, op0=ALU.mult, op1=ALU.mult))
                    dve([xk, "shiftB1"], [("h1tok", b)], lambda e, b=b: e.tensor_tensor(out=h1tok[b], in0=xt[b], in1=shiftB1, op=ALU.add))

                def p1_back(j):
                    b = j % 2
                    for hb_ in range(2):
                        q = (j % 2) * 2 + hb_
                        tbv = PS[q // 2][:, (q % 2) * 512:(q % 2) * 512 + 512].bitcast(BF16)

                        def emit(e, b=b, hb_=hb_, tbv=tbv):
                            ins = None
                            for k in range(8):
                                dc = hb_ * 8 + k
                                ins = e.transpose(tbv[:, k * 128:(k + 1) * 128], h1tok[b][:, dc * 128:(dc + 1) * 128], identB)
                            return ins
                        pe_group([("h1tok", b), "identB"], [pkey(q)], emit)
                        if j < 4:
                            dst = h1halo[:, hb_ * 8:(hb_ + 1) * 8, j * 128:(j + 1) * 128]
                            dk = ("h1halo", j)
                        else:
                            dst = h1own[:, hb_ * 8:(hb_ + 1) * 8, (j - 4) * 128:(j - 3) * 128]
                            dk = ("h1own", j - 4)
                        act_fn(dst, tbv.rearrange("p (k t) -> p k t", k=8), AF.Copy, [pkey(q)], [dk])

                p1_front(0)
                for j in range(12):
                    if j + 1 < 12:
                        p1_front(j + 1)
                    p1_back(j)
                if debug is not None and debug["stage"] == "h1":
                    dbg_dump([(h1own.rearrange("p a b -> p (a b)").bitcast(F32), 8192, [("h1own", i) for i in range(8)]),
                              (h1halo.rearrange("p a b -> p (a b)").bitcast(F32), 4096, [("h1halo", i) for i in range(4)]),
                              (modT, 96, ["modT"]), (effT, 32, ["effT"])])
                    break

                def h1_rhs(dc, t0, n):
                    if t0 < 512:
                        assert t0 + n <= 512
                        return h1halo[:, dc, t0:t0 + n]
                    return h1own[:, dc, t0 - 512:t0 - 512 + n]

                def h1_keys(t0, n):
                    ks = []
                    for t in range(t0 // 128, (t0 + n) // 128):
                        ks.append(("h1halo", t) if t < 4 else ("h1own", t - 4))
                    return ks

                P.fence()
                o = MIX0
                qT = sb(o, [4, 1024], BF16); o += 8192
                kT = sb(o, [4, 1536], BF16); o += 12288
                Vt = sb(o, [12, 8 * 65], BF16); o += 12480
                o = (o + 63) // 64 * 64
                biasS = sb(o, [8, 384]); o += 12288
                Pb = [sb(o + i * 1280, [640], BF16) for i in range(3)]; o += 3840
                ybt = sb(o, [512], BF16); o += 1024
                rden = sb(o, [8]); o += 32
                assert o <= PH_END
                for hh in range(2):
                    P.dma(SP, [], ["biasS"], lambda e, hh=hh: e.dma_start(
                        out=biasS.rearrange("p a b -> p (a b)"), in_=biasT[:, hh * 3072:(hh + 1) * 3072]), cpool.nxt())
                    wk, wkk = w_get(pl["k"][hh])
                    for tt in range(3):
                        for cc in range(4):
                            bnk = (tt * 4 + cc) % 8
                            def emit(e, tt=tt, cc=cc, bnk=bnk):
                                return mm_acc(e, pbank(bnk), [(wk[:, dc, cc * 128:(cc + 1) * 128], h1_rhs(dc, tt * 512, 512)) for dc in range(16)])
                            pe_group([wkk] + h1_keys(tt * 512, 512), [pkey(bnk)], emit)
                            act_fn(kT[:, cc, tt * 512:(tt + 1) * 512], pbank(bnk), AF.Copy, [pkey(bnk)], [("kT", tt)])
                    w_rel(pl["k"][hh])
                    wv, wvk = w_get(pl["vb"][hh])
                    for j in range(12):
                        bnk = j % 8
                        def emit(e, j=j, bnk=bnk):
                            return mm_acc(e, pbank(bnk), [(h1_rhs(dc, j * 128, 128), wv[:, dc, :]) for dc in range(16)])
                        pe_group([wvk] + h1_keys(j * 128, 128), [pkey(bnk)], emit)
                        vdst = Vt[:, j, :].rearrange("p (h d) -> p h d", h=8)
                        sc = fl if j < 4 else 1.0
                        act_fn(vdst[:, :, 0:64], pbank(bnk).rearrange("p (h d) -> p h d", h=8), AF.Copy,
                               [pkey(bnk), "flags"], [("Vt", j)], scale=sc)
                        act_fn(vdst[:, :, 64:65], ones8.rearrange("p (h d) -> p h d", d=1), AF.Copy,
                               ["ones8", "flags"], [("Vt", j)], scale=sc)
                    w_rel(pl["vb"][hh])
                    wq, wqk = w_get(pl["q"][hh])
                    for tt in range(2):
                        for cc in range(4):
                            bnk = (tt * 4 + cc) % 8
                            def emit(e, tt=tt, cc=cc, bnk=bnk):
                                return mm_acc(e, pbank(bnk), [(wq[:, dc, cc * 128:(cc + 1) * 128], h1_rhs(dc, 512 + tt * 512, 512)) for dc in range(16)])
                            pe_group([wqk] + h1_keys(512 + tt * 512, 512), [pkey(bnk)], emit)
                            act_fn(qT[:, cc, tt * 512:(tt + 1) * 512], pbank(bnk), AF.Copy, [pkey(bnk)], [("qT", tt)], scale=0.125)
                    w_rel(pl["q"][hh])
                    NSET = 3
                    SK = 2
                    jobs = [(i, hl) for i in range(8) for hl in range(8)]

                    def st_S(n):
                        i, hl = jobs[n]
                        j = i + 4
                        h = hh * 8 + hl
                        cc = hl // 2
                        pb0 = (hl % 2) * 64
                        st = n % NSET
                        s0, s1 = 2 * st, 2 * st + 1

                        def emit(e):
                            ins = None
                            qv = qT[pb0:pb0 + 64, cc, i * 128:(i + 1) * 128]
                            for dl in range(5):
                                kt = j - dl
                                kv = kT[pb0:pb0 + 64, cc, kt * 128:(kt + 1) * 128]
                                if dl < 3:
                                    outp = pbank(s0, 128, dl * 128)
                                else:
                                    outp = pbank(s1, 128, (dl - 3) * 128)
                                if dl == 4:
                                    e.matmul(outp, kv, qv, start=True, stop=False)
                                    ins = e.matmul(outp, urow, vrow, start=False, stop=True)
                                else:
                                    ins = e.matmul(outp, kv, qv, start=True, stop=True)
                            return ins
                        pe_group([("qT", i // 4), "uvrow"] + [("kT", (j - dl) // 4) for dl in range(5)], [pkey(s0), pkey(s1)], emit)
                        dve([pkey(s0), "biasS"], [pkey(s0)], lambda e: e.tensor_tensor(
                            out=pbank(s0, 384), in0=pbank(s0, 384), in1=biasS[:, hl, :], op=ALU.add))
                        act_fn(Pb[st][:, 0:384], pbank(s0, 384), AF.Exp, [pkey(s0)], [("Pb", st)])
                        act_fn(Pb[st][:, 384:640], pbank(s1, 256), AF.Exp, [pkey(s1), "tabc"], [("Pb", st)],
                               bias=tabc[:, h:h + 1])

                    def st_PV(n):
                        i, hl = jobs[n]
                        j = i + 4
                        st = n % NSET
                        ob = 6 + hl // 4

                        def emit(e):
                            pairs = []
                            for dl in range(5):
                                kt = j - dl
                                pairs.append((Pb[st][:, dl * 128:(dl + 1) * 128], Vt[:, kt, hl * 65:(hl + 1) * 65]))
                            return mm_acc(e, pbank(ob, 65, (hl % 4) * 65), pairs)
                        pe_group([("Pb", st)] + [("Vt", j - dl) for dl in range(5)], [pkey(ob)], emit)

                    def st_norm(i):
                        for ob in (6, 7):
                            ov = pbank(ob, 260).rearrange("p (h d) -> p h d", h=4)
                            dve([pkey(ob)], ["rden"], lambda e, ob=ob, ov=ov: e.reciprocal(
                                out=rden[:, (ob - 6) * 4:(ob - 5) * 4], in_=ov[:, :, 64]))
                            for k in range(4):
                                hl = (ob - 6) * 4 + k
                                dve([pkey(ob), "rden"], ["ybt"], lambda e, ov=ov, k=k, hl=hl: e.tensor_scalar(
                                    out=ybt[:, hl * 64:(hl + 1) * 64], in0=ov[:, k, 0:64], scalar1=rden[:, hl:hl + 1],
                                    scalar2=None, op0=ALU.mult))

                    def st_T(i):
                        tb = PS[0][:, 0:512].bitcast(BF16)

                        def emit(e):
                            ins = None
                            for k in range(4):
                                ins = e.transpose(tb[:, k * 128:(k + 1) * 128], ybt[:, k * 128:(k + 1) * 128], identB)
                            return ins
                        pe_group(["ybt", "identB"], [pkey(0)], emit)
                        act_fn(ybT[:, hh * 4:(hh + 1) * 4, i * 128:(i + 1) * 128],
                               tb[:, 0:512].rearrange("p (k t) -> p k t", k=4), AF.Copy, [pkey(0)], [("ybT", i)])

                    pendT = []
                    for n in range(len(jobs) + SK + 2):
                        if n < len(jobs):
                            st_S(n)
                        m = n - SK
                        if 0 <= m < len(jobs):
                            st_PV(m)
                            if jobs[m][1] == 7:
                                st_norm(jobs[m][0])
                                pendT.append((jobs[m][0], n + 2))
                        for (ti, due) in pendT:
                            if due == n:
                                st_T(ti)
                if debug is not None and debug["stage"] == "attn":
                    dbg_dump([(ybT.rearrange("p a b -> p (a b)").bitcast(F32), 4096, [("ybT", i) for i in range(8)])])
                    break

                P.fence()
                o = GM0
                yaT = sb(o, [8, 1024], BF16); o += 16384
                uT = sb(o, [8, 1024], BF16); o += 16384
                vn = [sb(o + i * 2048, [1024], BF16) for i in range(2)]; o += 4096
                vg = [sb(o + i * 4096, [1024]) for i in range(2)]; o += 8192
                lnGb = sb(o, [1024]); o += 4096
                lnBb = sb(o, [1024]); o += 4096
                bSb = sb(o, [1024]); o += 4096
                t1 = sb(o, [1024]); o += 4096
                bst = sb(o, [12]); o += 48
                assert o <= PH_END
                gkeys = [("kT", t) for t in range(3)] + [("qT", t) for t in range(2)] + [("Vt", j) for j in range(12)] + \
                    ["biasS", "ybt", "rden"] + [("Pb", i) for i in range(2)] + [("tmpS", i) for i in range(2)]
                sp_load(lnGb, lnG[0].partition_broadcast(128), ["lnGb"], cpool.nxt())
                sp_load(lnBb, lnB[0].partition_broadcast(128), ["lnBb"], cpool.nxt())
                sp_load(bSb, bS[0].partition_broadcast(128), ["bSb"], cpool.nxt())
                for uu in range(2):
                    wu, wuk = w_get(pl["u"][uu])
                    for tt in range(2):
                        for cc in range(4):
                            bnk = (tt * 4 + cc) % 8
                            def emit(e, tt=tt, cc=cc, bnk=bnk, wu=wu):
                                return mm_acc(e, pbank(bnk), [(wu[:, dc, cc * 128:(cc + 1) * 128], h1_rhs(dc, 512 + tt * 512, 512)) for dc in range(16)])
                            pe_group([wuk] + h1_keys(512 + tt * 512, 512), [pkey(bnk)], emit)
                            act_fn(uT[:, uu * 4 + cc, tt * 512:(tt + 1) * 512], pbank(bnk), AF.Gelu,
                                   [pkey(bnk)], [("uT", tt)])
                    w_rel(pl["u"][uu])
                wv0, wv0k = w_get(pl["v"][0])
                wv1, wv1k = w_get(pl["v"][1])
                wvs = [(wv0, wv0k), (wv1, wv1k)]
                def gm_front(i):
                    b = i % 2
                    for vu in range(2):
                        bnk = b * 4 + vu
                        def emit(e, i=i, vu=vu, bnk=bnk):
                            return mm_acc(e, pbank(bnk), [(h1_rhs(dc, 512 + i * 128, 128), wvs[vu][0][:, dc, :]) for dc in range(16)])
                        pe_group([wvs[vu][1]] + h1_keys(512 + i * 128, 128), [pkey(bnk)], emit)
                        act_fn(vg[b][:, vu * 512:(vu + 1) * 512], pbank(bnk), AF.Gelu, [pkey(bnk)], [("vg", b)])
                    st6 = small[:, 64 + b * 12:64 + b * 12 + 12]
                    mv = small[:, 96 + b * 4:96 + b * 4 + 2]
                    rs_ = small[:, 98 + b * 4:99 + b * 4]
                    for vu in range(2):
                        dve([("vg", b)], [("st6", b)], lambda e, b=b, vu=vu, st6=st6: e.bn_stats(
                            out=st6[:, vu * 6:(vu + 1) * 6], in_=vg[b][:, vu * 512:(vu + 1) * 512]))
                    dve([("st6", b)], [("mv", b)], lambda e, st6=st6, mv=mv: e.bn_aggr(out=mv, in_=st6))
                    act_fn(rs_, mv[:, 1:2], AF.Sqrt, [("mv", b)], [("rsv", b)], bias=EPS)
                    dve([("rsv", b)], [("rsv", b)], lambda e, rs_=rs_: e.reciprocal(out=rs_, in_=rs_))
                    dve([("vg", b), ("mv", b), ("rsv", b)], [("vg", b)], lambda e, b=b, mv=mv, rs_=rs_: e.tensor_scalar(
                        out=vg[b], in0=vg[b], scalar1=mv[:, 0:1], scalar2=rs_, op0=ALU.subtract, op1=ALU.mult))
                    dve([("vg", b), "lnGb"], [("vg", b)], lambda e, b=b: e.tensor_tensor(out=vg[b], in0=vg[b], in1=lnGb, op=ALU.mult))
                    dve([("vg", b), "lnBb"], [("vn", b)], lambda e, b=b: e.tensor_tensor(out=vn[b], in0=vg[b], in1=lnBb, op=ALU.add))

                def gm_back(i):
                    b = i % 2
                    for gh in range(2):
                        bnk = b * 4 + 2 + gh
                        def emit(e, b=b, gh=gh, bnk=bnk):
                            ins = None
                            for g4 in range(4):
                                g = gh * 4 + g4
                                ins = e.matmul(pbank(bnk, 128, g4 * 128), vn[b][:, g * 128:(g + 1) * 128], WsT[:, g, :], start=True, stop=True)
                            return ins
                        pe_group([("vn", b), "WsT"], [pkey(bnk)], emit)
                        dve([pkey(bnk), "bSb"], ["t1"], lambda e, gh=gh, bnk=bnk: e.tensor_tensor(
                            out=t1[:, gh * 512:(gh + 1) * 512], in0=pbank(bnk), in1=bSb[:, gh * 512:(gh + 1) * 512], op=ALU.add))
                        dve(["t1", ("uT", i // 4)], [("yaT", i)], lambda e, gh=gh, i=i: e.tensor_tensor(
                            out=yaT[:, gh * 4:(gh + 1) * 4, i * 128:(i + 1) * 128],
                            in0=t1[:, gh * 512:(gh + 1) * 512].rearrange("p (g t) -> p g t", g=4),
                            in1=uT[:, gh * 4:(gh + 1) * 4, i * 128:(i + 1) * 128], op=ALU.mult))
                gm_front(0)
                for i in range(8):
                    if i + 1 < 8:
                        gm_front(i + 1)
                    gm_back(i)
                w_rel(pl["v"][0])
                w_rel(pl["v"][1])
                if debug is not None and debug["stage"] == "gmlp":
                    dbg_dump([(yaT.rearrange("p a b -> p (a b)").bitcast(F32), 4096, [("yaT", i) for i in range(8)])])
                    break

                P.fence()
                o = GM0 + 16384
                mergedT = sb(o, [16, 1024], BF16); o += 32768
                sg = [sb(o + i * 2048, [512]) for i in range(4)]; o += 8192
                assert o <= PH_END
                mkeys = [("uT", t) for t in range(2)] + [("vg", i) for i in range(2)] + [("vn", i) for i in range(2)] + ["t1", "lnGb", "lnBb", "bSb"]
                for q in range(4):
                    wga, wgak = w_get(pl["gA"][q])
                    wgb, wgbk = w_get(pl["gB"][q])
                    wa, wak = w_get(pl["brA"][q // 2])
                    wb, wbk = w_get(pl["brB"][q // 2])
                    for tt in range(2):
                        for c4 in range(4):
                            dcc = q * 4 + c4
                            st = (tt * 4 + c4) % 2
                            bA, bB, bYa, bYb = st * 4, st * 4 + 1, st * 4 + 2, st * 4 + 3
                            ts_ = slice(tt * 512, (tt + 1) * 512)

                            def emitA(e, c4=c4, ts_=ts_, bA=bA, wga=wga):
                                return mm_acc(e, pbank(bA), [(wga[:, dc, c4 * 128:(c4 + 1) * 128], h1own[:, dc, ts_]) for dc in range(16)])
                            pe_group([wgak] + [("h1own", t) for t in range(tt * 4, tt * 4 + 4)], [pkey(bA)], emitA)

                            def emitB(e, c4=c4, ts_=ts_, bB=bB, wgb=wgb):
                                return mm_acc(e, pbank(bB), [(wgb[:, dc, c4 * 128:(c4 + 1) * 128], h1own[:, dc, ts_]) for dc in range(16)])
                            pe_group([wgbk] + [("h1own", t) for t in range(tt * 4, tt * 4 + 4)], [pkey(bB)], emitB)
                            col = (q % 2) * 512 + c4 * 128

                            def emitYa(e, col=col, ts_=ts_, bYa=bYa, wa=wa):
                                return mm_acc(e, pbank(bYa), [(wa[:, kc, col:col + 128], yaT[:, kc, ts_]) for kc in range(8)])
                            pe_group([wak] + [("yaT", t) for t in range(tt * 4, tt * 4 + 4)], [pkey(bYa)], emitYa)

                            def emitYb(e, col=col, ts_=ts_, bYb=bYb, wb=wb):
                                return mm_acc(e, pbank(bYb), [(wb[:, kc, col:col + 128], ybT[:, kc, ts_]) for kc in range(8)])
                            pe_group([wbk] + [("ybT", t) for t in range(tt * 4, tt * 4 + 4)], [pkey(bYb)], emitYb)
                            sga, sgb = sg[st * 2], sg[st * 2 + 1]
                            act_fn(sga, pbank(bA), AF.Sigmoid, [pkey(bA)], [("sg", st * 2)])
                            act_fn(sgb, pbank(bB), AF.Sigmoid, [pkey(bB)], [("sg", st * 2 + 1)])
                            dve([("sg", st * 2), pkey(bYa)], [("sg", st * 2)], lambda e, sga=sga, bYa=bYa: e.tensor_tensor(
                                out=sga, in0=sga, in1=pbank(bYa), op=ALU.mult))
                            dve([("sg", st * 2 + 1), pkey(bYb)], [("sg", st * 2 + 1)], lambda e, sgb=sgb, bYb=bYb: e.tensor_tensor(
                                out=sgb, in0=sgb, in1=pbank(bYb), op=ALU.mult))
                            dve([("sg", st * 2), ("sg", st * 2 + 1)], [("mergedT", tt)], lambda e, sga=sga, sgb=sgb, dcc=dcc, ts_=ts_: e.tensor_tensor(
                                out=mergedT[:, dcc, ts_], in0=sga, in1=sgb, op=ALU.add))
                    w_rel(pl["gA"][q])
                    w_rel(pl["gB"][q])
                    if q % 2 == 1:
                        w_rel(pl["brA"][q // 2])
                        w_rel(pl["brB"][q // 2])
                if debug is not None and debug["stage"] == "merge":
                    dbg_dump([(mergedT.rearrange("p a b -> p (a b)").bitcast(F32), 8192, [("mergedT", i) for i in range(2)])])
                    break

                P.fence()
                w2effb = sb(PH0, [2048])
                shift2b = sb(PH0 + 8192, [2048])
                h2row = [sb(PH0 + 16384 + i * 4096, [2048], BF16) for i in range(2)]
                g2b = sb(PH0 + 24576, [2048])
                gate1b = sb(PH0 + 32768, [2048])
                h2f = sb(PH0 + 40960, [16, 128])
                xt6 = [sb(PH0 + 49152 + i * 8192, [2048]) for i in range(2)]
                assert PH0 + 65536 == GM0 + 16384
                xn6 = sb(PH0 + 98304, [2048])
                junk6 = sb(PH0 + 106496, [2048], BF16)
                tmpm = sb(PH0 + 110592, [512])
                dead = [("h1own", t) for t in range(8)] + [("h1halo", t) for t in range(4)] + [("ybT", t) for t in range(8)] + \
                    [("yaT", t) for t in range(8)] + [("sg", i) for i in range(4)]
                sp_load(gate1b, modd[0, 2 * D:3 * D].partition_broadcast(128), ["gate1b"], cpool.nxt(), reads=modd_keys)
                sp_load(g2b, g2row[0].partition_broadcast(128), ["g2b"], cpool.nxt())
                sp_load(w2effb, modd[0, 4 * D:5 * D].partition_broadcast(128), ["w2effb"], cpool.nxt(), reads=modd_keys)
                sp_load(shift2b, modd[0, 3 * D:4 * D].partition_broadcast(128), ["shift2b"], cpool.nxt(), reads=modd_keys)
                dve(["w2effb", "g2b"], ["w2effb"], lambda e: e.scalar_tensor_tensor(
                    out=w2effb, in0=w2effb, scalar=1.0, in1=g2b, op0=ALU.add, op1=ALU.mult))
                wos = [w_get(u) for u in pl["wo"]]

                def p6_mm_pe(i):
                    b = i % 2
                    P.dma(SP, [], [("xt6", b)], lambda e, b=b, i=i: e.dma_start(
                        out=xt6[b], in_=xe[row0 + HALO + i * 128:row0 + HALO + (i + 1) * 128, :]), xsem[b])
                    for cu in range(4):
                        def emit(e, i=i, cu=cu):
                            return mm_acc(e, pbank(cu), [(mergedT[:, kc, i * 128:(i + 1) * 128], wos[cu][0][:, kc, :]) for kc in range(16)])
                        pe_group([wos[cu][1], ("mergedT", i // 4)], [pkey(cu)], emit)

                def p6_mm_dve(i):
                    b = i % 2
                    for cu in range(4):
                        dve([pkey(cu), "gate1b"], ["tmpm"], lambda e, cu=cu: e.tensor_tensor(
                            out=tmpm, in0=pbank(cu), in1=gate1b[:, cu * 512:(cu + 1) * 512], op=ALU.mult))
                        dve(["tmpm", ("xt6", b)], [("xt6", b)], lambda e, cu=cu, b=b: e.tensor_tensor(
                            out=xt6[b][:, cu * 512:(cu + 1) * 512], in0=xt6[b][:, cu * 512:(cu + 1) * 512], in1=tmpm, op=ALU.add))

                def p6_front(i):
                    b = i % 2
                    xk = ("xt6", b)
                    ssa = small[:, 32 + i:33 + i]
                    rsa = small[:, 48 + i:49 + i]
                    act_fn(junk6, xt6[b], AF.Square, [xk], [("ss2", i)], accum=ssa)
                    rms_rstd(ssa, rsa, ("ss2", i), ("rs2", i))
                    P.dma(SP, [xk], [("x1s", p * 8 + i)], lambda e, b=b, i=i: e.dma_start(
                        out=x1s[p * TP + i * 128:p * TP + (i + 1) * 128, :], in_=xt6[b]), osem[b])
                    dve([xk, ("rs2", i), "w2effb"], ["xn6"], lambda e, b=b, rsa=rsa: e.scalar_tensor_tensor(
                        out=xn6, in0=xt6[b], scalar=rsa, in1=w2effb, op0=ALU.mult, op1=ALU.mult))
                    dve(["xn6", "shift2b"], ["xn6"], lambda e: e.tensor_tensor(out=xn6, in0=xn6, in1=shift2b, op=ALU.add))
                    hb = i % 2
                    act_fn(h2row[hb], xn6, AF.Copy, ["xn6"], [("h2row", hb)])
                    P.dma(SP, [("h2row", hb)], [("h2d", p * 8 + i)], lambda e, hb=hb, i=i: e.dma_start(
                        out=h2d[p * TP + i * 128:p * TP + (i + 1) * 128, :], in_=h2row[hb]), hsem[hb])
                def p6_back(i):
                    b = i % 2
                    for q in range(4):
                        def emit(e, q=q):
                            ins = None
                            for k in range(4):
                                dc = q * 4 + k
                                ins = e.transpose(pbank(4 + q, 128, k * 128), xn6[:, dc * 128:(dc + 1) * 128], identF)
                            return ins
                        pe_group(["xn6", "identF"], [pkey(4 + q)], emit)
                        act_fn(h2f[:, q * 4:(q + 1) * 4, :], pbank(4 + q).rearrange("p (k t) -> p k t", k=4), AF.Copy,
                               [pkey(4 + q)], [("h2f", q)])
                    LB = 0

                    def emit(e):
                        return mm_acc(e, pbank(LB, 36), [(h2f[:, dc, :], wr[:, dc, :]) for dc in range(16)])
                    pe_group([("h2f", q) for q in range(4)] + ["wr"], [pkey(LB)], emit)
                    lg = rt[:, 0:36]
                    dve([pkey(LB)], ["rt"], lambda e: e.tensor_copy(out=lg, in_=pbank(LB, 36)))
                    gl = rt[:, 0:4]
                    el = rt[:, 4:36]
                    gmax = rt[:, 40:41]
                    ngmax = rt[:, 41:42]
                    gsum = rt[:, 42:43]
                    gw = rt[:, 43:44]
                    goh = rt[:, 44:48]
                    gex = rt[:, 48:52]
                    esel = rt[:, 56:64]
                    etmp = rt[:, 64:96]
                    m1 = rt[:, 96:97]
                    m2 = rt[:, 97:98]
                    oh1 = rt[:, 104:112]
                    oh2 = rt[:, 112:120]
                    msk = rt[:, 120:128]
                    dd = rt[:, 128:129]
                    w1_ = rt[:, 129:130]
                    w2_ = rt[:, 130:131]
                    ew = rt[:, 136:144]
                    gwo = rt[:, 144:148]
                    R = ["rt"]
                    dve(R, R, lambda e: e.tensor_reduce(out=gmax, in_=gl, axis=AX.X, op=ALU.max))
                    dve(R, R, lambda e: e.tensor_scalar(out=ngmax, in0=gmax, scalar1=-1.0, scalar2=None, op0=ALU.mult))
                    act_fn(gex, gl, AF.Exp, R, R, bias=ngmax, accum=gsum)
                    dve(R, R, lambda e: e.reciprocal(out=gw, in_=gsum))
                    dve(R, R, lambda e: e.tensor_scalar(out=goh, in0=gl, scalar1=gmax, scalar2=None, op0=ALU.is_equal))
                    dve(R, R, lambda e: e.tensor_scalar(out=esel, in0=el[:, 0:8], scalar1=goh[:, 0:1], scalar2=None, op0=ALU.mult))
                    for g in range(1, 4):
                        dve(R, R, lambda e, g=g: e.scalar_tensor_tensor(
                            out=esel, in0=el[:, g * 8:(g + 1) * 8], scalar=goh[:, g:g + 1], in1=esel, op0=ALU.mult, op1=ALU.add))
                    dve(R, R, lambda e: e.tensor_reduce(out=m1, in_=esel, axis=AX.X, op=ALU.max))
                    dve(R, R, lambda e: e.tensor_scalar(out=oh1, in0=esel, scalar1=m1, scalar2=None, op0=ALU.is_equal))
                    dve(R, R, lambda e: e.scalar_tensor_tensor(out=msk, in0=oh1, scalar=-1e30, in1=esel, op0=ALU.mult, op1=ALU.add))
                    dve(R, R, lambda e: e.tensor_reduce(out=m2, in_=msk, axis=AX.X, op=ALU.max))
                    dve(R, R, lambda e: e.tensor_scalar(out=oh2, in0=msk, scalar1=m2, scalar2=None, op0=ALU.is_equal))
                    dve(R, R, lambda e: e.tensor_tensor(out=dd, in0=m2, in1=m1, op=ALU.subtract))
                    act_fn(dd, dd, AF.Exp, R, R)
                    dve(R, R, lambda e: e.tensor_scalar(out=w1_, in0=dd, scalar1=1.0, scalar2=None, op0=ALU.add))
                    dve(R, R, lambda e: e.reciprocal(out=w1_, in_=w1_))
                    dve(R, R, lambda e: e.tensor_tensor(out=w2_, in0=dd, in1=w1_, op=ALU.mult))
                    dve(R, R, lambda e: e.tensor_scalar(out=ew, in0=oh1, scalar1=w1_, scalar2=None, op0=ALU.mult))
                    dve(R, R, lambda e: e.scalar_tensor_tensor(out=ew, in0=oh2, scalar=w2_, in1=ew, op0=ALU.mult, op1=ALU.add))
                    dve(R, R, lambda e: e.tensor_scalar(out=gwo, in0=goh, scalar1=gw, scalar2=None, op0=ALU.mult))
                    ti = p * 8 + i
                    for g in range(4):
                        dve(R, [("sel", ti)], lambda e, g=g, ti=ti: e.tensor_scalar(
                            out=sel0[:, ti, g * 8:(g + 1) * 8], in0=oh1, scalar1=goh[:, g:g + 1], scalar2=None, op0=ALU.mult))
                        dve(R, [("sel", ti)], lambda e, g=g, ti=ti: e.tensor_scalar(
                            out=sel1[:, ti, g * 8:(g + 1) * 8], in0=oh2, scalar1=goh[:, g:g + 1], scalar2=None, op0=ALU.mult))
                    dve(R, [("wkk", ti)], lambda e, ti=ti: e.tensor_tensor(out=wsl[:, ti, 0:1], in0=w1_, in1=gw, op=ALU.mult))
                    dve(R, [("wkk", ti)], lambda e, ti=ti: e.tensor_tensor(out=wsl[:, ti, 1:2], in0=w2_, in1=gw, op=ALU.mult))

                p6_mm_pe(0)
                p6_mm_dve(0)
                for i in range(8):
                    if i + 1 < 8:
                        p6_mm_pe(i + 1)
                    p6_front(i)
                    if i + 1 < 8:
                        p6_mm_dve(i + 1)
                    p6_back(i)
                for u in pl["wo"]:
                    w_rel(u)
                if debug is not None and debug["stage"] == "p6":
                    break


            if debug is None or debug["stage"] in ("meta", "moe"):
                P.nofloor.discard("pool")
                P.fence()
                o = PH0 + (MSLOT - NSLOT) * SLOT_B
                selA = sb(o, [16, 32]); o += 2048
                cum = sb(o, [16, 32]); o += 2048
                pos = sb(o, [16, 32]); o += 2048
                tmpd = sb(o, [16, 32]); o += 2048
                destf = sb(o, [32]); o += 128
                desti = sb(o, [32], I32); o += 128
                cntf = sb(o, [32]); o += 128
                cnti = sb(o, [32], I32); o += 128
                fillt = sb(o, [2048], I32); o += 8192
                Utri = sb(o, [128]); o += 512
                OnesM = sb(o, [128]); o += 512
                META_END = o
                sp_load(Utri, Utri_d, ["Utri"], cpool.nxt())
                sp_load(OnesM, Ones_d, ["OnesM"], cpool.nxt())
                selk = [("sel", t) for t in range(16)]
                dve(selk, ["selA"], lambda e: e.tensor_tensor(out=selA, in0=sel0, in1=sel1, op=ALU.add))
                dve([], ["cum"], lambda e: e.memset(cum[:, 0, :], 0.0))
                for t in range(1, 16):
                    dve(["cum", "selA"], ["cum"], lambda e, t=t: e.tensor_tensor(
                        out=cum[:, t, :], in0=cum[:, t - 1, :], in1=selA[:, t - 1, :], op=ALU.add))
                def emit(e):
                    ins = None
                    for t in range(16):
                        e.matmul(pbank(0, 32, t * 32), OnesM, cum[:, t, :], start=True, stop=False)
                        ins = e.matmul(pbank(0, 32, t * 32), Utri, selA[:, t, :], start=False, stop=True)
                    return ins
                pe_group(["cum", "selA", "OnesM", "Utri"], [pkey(0)], emit)
                dve([pkey(0)], ["pos"], lambda e: e.tensor_copy(out=pos, in_=pbank(0).rearrange("p (t x) -> p t x", t=16)))
                dve(["cum", "selA"], ["tmpd"], lambda e: e.tensor_tensor(out=tmpd[:, 0, :], in0=cum[:, 15, :], in1=selA[:, 15, :], op=ALU.add))
                pe_group(["tmpd", "OnesM"], [pkey(1)], lambda e: e.matmul(pbank(1, 32), OnesM, tmpd[:, 0, :], start=True, stop=True))
                dve([pkey(1)], ["cntf"], lambda e: e.tensor_copy(out=cntf, in_=pbank(1, 32)))
                dve(["cntf"], ["cnti"], lambda e: e.tensor_copy(out=cnti, in_=cntf))
                P.dma(SP, ["cnti"], ["cntd"], lambda e: e.dma_start(out=cntd, in_=cnti[0:1, :]), cpool.nxt())
                dve(["pos", "ecap"], ["pos"], lambda e: e.tensor_tensor(
                    out=pos, in0=pos, in1=ecap.unsqueeze(1).to_broadcast([128, 16, 32]), op=ALU.add))
                for k, selx in enumerate((sel0, sel1)):
                    dve(["pos"] + selk, ["tmpd"], lambda e, selx=selx: e.tensor_tensor(out=tmpd, in0=pos, in1=selx, op=ALU.mult))
                    dve(["tmpd"], ["destf"], lambda e, k=k: e.tensor_reduce(
                        out=destf.rearrange("p (t k) -> p t k", k=2)[:, :, k], in_=tmpd, axis=AX.X, op=ALU.add))
                dve(["destf"], ["desti"], lambda e: e.tensor_copy(out=desti, in_=destf))
                sp_load(fillt, fillv_d, ["fillt"], cpool.nxt())
                P.dma(SP, ["fillt"], ["table"], lambda e: e.dma_start(
                    out=table.rearrange("(p r) c -> p (r c)", p=128), in_=fillt), cpool.nxt())
                for t in range(16):
                    for k in range(2):
                        P.dma(POOL, ["desti", "vals", "table"], [("tabw", t, k)], lambda e, t=t, k=k: e.indirect_dma_start(
                            out=table, out_offset=bass.IndirectOffsetOnAxis(ap=desti[:, t * 2 + k:t * 2 + k + 1], axis=0),
                            in_=vals[:, (t * 2 + k) * 4:(t * 2 + k) * 4 + 4], in_offset=None,
                            oob_is_err=False), tpool.nxt())
                tabkeys = [("tabw", t, k) for t in range(16) for k in range(2)]
                if debug is not None and debug["stage"] == "meta":
                    dbg_dump([(destf, 32, ["destf"]), (cntf, 32, ["cntf"]), (wsl.rearrange("p a b -> p (a b)"), 32, [("wkk", t) for t in range(16)]),
                              (sel0.rearrange("p a b -> p (a b)"), 512, selk), (sel1.rearrange("p a b -> p (a b)"), 512, selk)])

            if debug is None or debug["stage"] in ("moe",):
                P.fence()
                o = PH0 + (MSLOT - NSLOT) * SLOT_B
                NXG = 5
                idxt = [sb(o + i * 16, [4], I32) for i in range(NXG)]; o += 128
                xg = [sb(o + i * 4096, [2048], BF16) for i in range(NXG)]; o += NXG * 4096
                xgT = sb(o, [16, 128], BF16); o += 4096
                sAm = sb(o, [512]); o += 2048
                actm = sb(o, [512], BF16); o += 1024
                actTm = sb(o, [4, 128], BF16); o += 1024
                NY = 4
                Ysb = [sb(o + i * 8192, [2048]) for i in range(NY)]; o += NY * 8192
                MOE_END = o
                assert o <= PH_END

                class MW:
                    units = []
                    released = []
                    nloaded = 0

                def m_view(slot, a, b):
                    return sb(RING0 + slot * SLOT_B, [a, b], BF16)

                def m_pump():
                    while MW.nloaded < len(MW.units):
                        u = MW.nloaded
                        if u >= MSLOT and not MW.released[u - MSLOT]:
                            break
                        src, a, b = MW.units[u]
                        slot = u % MSLOT
                        dst = m_view(slot, a, b)
                        P.dma(POOL, [], [("mring", slot)], lambda e, dst=dst, src=src: e.dma_start(out=dst, in_=src), mr_sem[slot])
                        MW.nloaded += 1

                def m_get(u):
                    m_pump()
                    assert u < MW.nloaded
                    src, a, b = MW.units[u]
                    return m_view(u % MSLOT, a, b), ("mring", u % MSLOT)

                def m_rel(u):
                    MW.released[u] = True

                for e_ in range(NE):
                    wpat = "(p c) n -> p c n" if CONTIG else "(c p) n -> p c n"
                    for src, a, b in ((w1[e_].rearrange(wpat, p=128), 16, 512),
                                      (w3[e_].rearrange(wpat, p=128), 16, 512),
                                      (w2[e_].rearrange("(c p) n -> p c n", p=128), 4, 2048)):
                        MW.units.append((src, a, b))
                        MW.released.append(False)

                regs = {}
                if target is not None:
                    regs[target] = handle.alloc_register("cnt_" + target)
                tcount = [0]

                def gather_ops(ei, t, xb):
                    row0 = ei * CAP + t * 128
                    P.dma(SP, tabkeys + ["table"], [("idxt", xb)], lambda e: e.dma_start(out=idxt[xb], in_=table[row0:row0 + 128, :]), ixsem[xb])
                    P.dma(POOL, [("idxt", xb)] + [("h2d", i) for i in range(16)], [("xg", xb)], lambda e: e.indirect_dma_start(
                        out=xg[xb], out_offset=None, in_=h2d, in_offset=bass.IndirectOffsetOnAxis(ap=idxt[xb][:, 0:1], axis=0),
                        oob_is_err=False), gasem[xb])

                NSTAT = int(os.environ.get('K_NSTAT', '2'))

                def tile_ops(ei, t, wa, wak, wb, wbk, wc, wck):
                    n = tcount[0]
                    tcount[0] += 1
                    b = n % NY
                    if t < NSTAT:
                        xb = (ei % 2) * NSTAT + t
                    else:
                        xb = NXG - 1
                        gather_ops(ei, t, xb)
                    tb0 = PS[0][:, 0:512].bitcast(BF16)
                    tb1 = PS[0][:, 512:1024].bitcast(BF16)
                    for hb_, tb in enumerate((tb0, tb1)):
                        def emit(e, hb_=hb_, tb=tb):
                            ins = None
                            for k in range(8):
                                dc = hb_ * 8 + k
                                src_cols = xg[xb][:, dc:2048:16] if CONTIG else xg[xb][:, dc * 128:(dc + 1) * 128]
                                ins = e.transpose(tb[:, k * 128:(k + 1) * 128], src_cols, identB)
                            return ins
                        pe_group([("xg", xb), "identB"], [pkey(hb_)], emit)
                    act_fn(xgT[:, 0:8, :], tb0.rearrange("p (k t) -> p k t", k=8), AF.Copy, [pkey(0)], [("xgT", 0)])
                    dve([pkey(1)], [("xgT", 1)], lambda e: e.tensor_copy(out=xgT[:, 8:16, :], in_=tb1.rearrange("p (k t) -> p k t", k=8)))
                    pe_group([wak, ("xgT", 0), ("xgT", 1)], [pkey(2)], lambda e: mm_acc(e, pbank(2), [(xgT[:, dc, :], wa[:, dc, :]) for dc in range(16)]))
                    pe_group([wbk, ("xgT", 0), ("xgT", 1)], [pkey(3)], lambda e: mm_acc(e, pbank(3), [(xgT[:, dc, :], wb[:, dc, :]) for dc in range(16)]))
                    act_fn(sAm, pbank(2), AF.Silu, [pkey(2)], ["sAm"])
                    dve(["sAm", pkey(3)], ["actm"], lambda e: e.tensor_tensor(out=actm, in0=sAm, in1=pbank(3), op=ALU.mult))

                    def emit(e):
                        ins = None
                        for k in range(4):
                            ins = e.transpose(tb0[:, k * 128:(k + 1) * 128], actm[:, k * 128:(k + 1) * 128], identB)
                        return ins
                    pe_group(["actm", "identB"], [pkey(0)], emit)
                    act_fn(actTm, tb0[:, 0:512].rearrange("p (k t) -> p k t", k=4), AF.Copy, [pkey(0)], ["actTm"])
                    for nb in range(4):
                        pe_group([wck, "actTm"], [pkey(4 + nb)], lambda e, nb=nb: mm_acc(
                            e, pbank(4 + nb), [(actTm[:, fc, :], wc[:, fc, nb * 512:(nb + 1) * 512]) for fc in range(4)]))
                    act_fn(Ysb[b][:, 0:1024], PS[2][:, :], AF.Copy, [pkey(4), pkey(5)], [("Ysb", b, 0)])
                    dve([pkey(6), pkey(7)], [("Ysb", b, 1)], lambda e: e.tensor_copy(out=Ysb[b][:, 1024:2048], in_=PS[3][:, :]))
                    for hf in range(2):
                        P.dma(POOL, [("Ysb", b, hf), ("idxt", xb)], [("Ybuf", n, hf)], lambda e, hf=hf: e.indirect_dma_start(
                            out=Ybuf4, out_offset=bass.IndirectOffsetOnAxis(ap=idxt[xb][:, 1 + hf:2 + hf], axis=0),
                            in_=Ysb[b][:, hf * 1024:(hf + 1) * 1024], in_offset=None,
                            oob_is_err=False), scsem[b * 2 + hf])

                MAXT = TOKC // 128

                GRP = 4

                def tiles_from(ei, t, tend, rv, ws):
                    if t >= tend:
                        return
                    with P.guard(lambda h: rv > t * 128):
                        tile_ops(ei, t, *ws)
                        tiles_from(ei, t + 1, tend, rv, ws)

                for t_ in range(NSTAT):
                    gather_ops(0, t_, t_)
                for ei in range(NE):
                    if ei + 1 < NE:
                        for t_ in range(NSTAT):
                            gather_ops(ei + 1, t_, ((ei + 1) % 2) * NSTAT + t_)
                    wa, wak = m_get(3 * ei)
                    wb, wbk = m_get(3 * ei + 1)
                    wc, wck = m_get(3 * ei + 2)
                    rv = None
                    for eng in P.engines:
                        r = P.raw(eng, ["cntd"], lambda h, ei=ei: h.reg_load(regs[target], cntd[0:1, ei:ei + 1]))
                    if target is not None:
                        rv = handle.snap(regs[target])
                    for g0 in range(0, MAXT, GRP):
                        tiles_from(ei, g0, g0 + GRP, rv, (wa, wak, wb, wbk, wc, wck))
                    m_rel(3 * ei)
                    m_rel(3 * ei + 1)
                    m_rel(3 * ei + 2)
                    m_pump()

                P.fence()
                o = RING0
                gate2b = sb(o, [2048]); o += 8192
                fgb = sb(o, [2048]); o += 8192
                NF = 3
                xt8 = [sb(o + i * 8192, [2048]) for i in range(NF)]; o += NF * 8192
                y0t = [sb(o + i * 8192, [2048]) for i in range(NF)]; o += NF * 8192
                y1t = [sb(o + i * 8192, [2048]) for i in range(NF)]; o += NF * 8192
                junk8 = sb(o, [2048], BF16); o += 4096
                assert o <= PH_END
                ybkeys = [("Ybuf", n, hf) for n in range(tcount[0]) for hf in range(2)]
                sp_load(gate2b, modd[0, 5 * D:6 * D].partition_broadcast(128), ["gate2b"], cpool.nxt(), reads=modd_keys)
                sp_load(fgb, fg[0].partition_broadcast(128), ["fgb"], cpool.nxt())

                def fin_loads(i):
                    b = i % NF
                    P.dma(SP, [("x1s", i)], [("xt8", b)], lambda e: e.dma_start(
                        out=xt8[b], in_=x1s[i * 128:(i + 1) * 128, :]), fsem[b])
                    P.dma(SP, ybkeys, [("y0t", b)], lambda e: e.dma_start(
                        out=y0t[b], in_=Ybuf[i * 128:(i + 1) * 128, :]), fsem[3 + b])
                    P.dma(SP, ybkeys, [("y1t", b)], lambda e: e.dma_start(
                        out=y1t[b], in_=Ybuf[TOKC + i * 128:TOKC + (i + 1) * 128, :]), fsem[6 + b])

                fin_loads(0)
                fin_loads(1)
                for i in range(16):
                    b = i % NF
                    xk = ("xt8", b)
                    if i + 2 < 16:
                        fin_loads(i + 2)
                    act_fn(y0t[b], y0t[b], AF.Copy, [("y0t", b), ("wkk", i)], [("y0t", b)], scale=wsl[:, i, 0:1])
                    dve([("y0t", b), ("y1t", b), ("wkk", i)], [("y0t", b)], lambda e, b=b, i=i: e.scalar_tensor_tensor(
                        out=y0t[b], in0=y1t[b], scalar=wsl[:, i, 1:2], in1=y0t[b], op0=ALU.mult, op1=ALU.add))
                    dve([("y0t", b), "gate2b"], [("y0t", b)], lambda e, b=b: e.tensor_tensor(out=y0t[b], in0=y0t[b], in1=gate2b, op=ALU.mult))
                    dve([("y0t", b), xk], [xk], lambda e, b=b: e.tensor_tensor(out=xt8[b], in0=xt8[b], in1=y0t[b], op=ALU.add))
                    ssa = small[:, 128 + i:129 + i]
                    rsa = small[:, 144 + i:145 + i]
                    act_fn(junk8, xt8[b], AF.Square, [xk], [("ss8", i)], accum=ssa)
                    rms_rstd(ssa, rsa, ("ss8", i), ("rs8", i))
                    dve([xk, ("rs8", i), "fgb"], [xk], lambda e, b=b, rsa=rsa: e.scalar_tensor_tensor(
                        out=xt8[b], in0=xt8[b], scalar=rsa, in1=fgb, op0=ALU.mult, op1=ALU.mult))
                    P.dma(SP, [xk], [("y", i)], lambda e, b=b, i=i: e.dma_start(
                        out=y[i * 128:(i + 1) * 128, :], in_=xt8[b]), fsem[9 + b])

            outkeys = [("y", i) for i in range(16)] if debug is None else [k for k in P.state.keys() if isinstance(k, tuple) and k[0] in ("dbg", "y")]
            P.final_wait(SP, outkeys)

        with nc.Block() as block:
            @block.tensor
            def _(e):
                program("pe", e)

            @block.scalar
            def _(e):
                program("act", e)

            @block.vector
            def _(e):
                program("dve", e)

            @block.gpsimd
            def _(e):
                program("pool", e)

            @block.sync
            def _(e):
                program("sp", e)
    return nc


_NC_CACHE = {}


def _vals_const():
    v = np.zeros((128, 16, 2, 4), np.int32)
    p = np.arange(128)[:, None]
    t = np.arange(16)[None, :]
    tok = t * 128 + p
    for k in range(2):
        v[:, :, k, 0] = tok
        v[:, :, k, 1] = 2 * (k * TOKC + tok)
        v[:, :, k, 2] = 2 * (k * TOKC + tok) + 1
    return np.ascontiguousarray(v.reshape(128, 128))


def _fill_const():
    r = np.arange(512)
    row = np.zeros((512, 4), np.int32)
    row[:, 1] = 4 * TOKC + (r % 128)
    row[:, 2] = 4 * TOKC + 128 + (r % 128)
    return np.ascontiguousarray(np.broadcast_to(row.reshape(1, 2048), (128, 2048)))


def _host_inputs(inputs):
    f = np.float32
    x = np.asarray(inputs["x"], f)
    c = np.asarray(inputs["c"], f)
    L = 0
    tab = np.asarray(inputs["rel_bias"], f)[L]
    ext = np.concatenate([tab, np.repeat(tab[:, -1:], 400, axis=1)], axis=1)
    k = np.arange(128)[:, None]
    q = np.arange(128)[None, :]
    bt = np.empty((128, 16, 3, 128), f)
    for dl in range(3):
        idx = 256 + dl * 128 + q - k
        bt[:, :, dl, :] = np.transpose(ext[:, idx], (1, 0, 2))
    bt[64:, :, 0, :64] = NEG
    wr = np.concatenate([np.asarray(inputs["w_group"], f)[L]] +
                        [np.asarray(inputs["w_expert"], f)[L, g] for g in range(4)], axis=1)
    shared = {
        "ident": np.eye(128, dtype=f),
        "ada_w": np.ascontiguousarray(np.asarray(inputs["ada_w"], f)[L]),
        "ada_b": np.ascontiguousarray(np.asarray(inputs["ada_b"], f)[L][None, :]),
        "g1T": np.ascontiguousarray(np.asarray(inputs["norm1_g"], f)[L].reshape(16, 128).T),
        "g2T": np.ascontiguousarray(np.asarray(inputs["norm2_g"], f)[L].reshape(16, 128).T),
        "lnG": np.ascontiguousarray(np.asarray(inputs["gmlp_ln_g"], f)[L][None, :]),
        "lnB": np.ascontiguousarray(np.asarray(inputs["gmlp_ln_b"], f)[L][None, :]),
        "bS": np.ascontiguousarray(np.asarray(inputs["gmlp_b_s"], f)[L].reshape(1, 1024)),
        "wS": np.ascontiguousarray(np.asarray(inputs["gmlp_w_s"], f)[L]),
        "biasT": np.ascontiguousarray(bt.reshape(128, 16 * 384)),
        "tabc": np.ascontiguousarray(tab[:, 512][None, :]),
        "w_in": np.ascontiguousarray(np.asarray(inputs["w_in"], f)[L]),
        "wbrA": np.ascontiguousarray(np.asarray(inputs["w_branch_a"], f)[L]),
        "wbrB": np.ascontiguousarray(np.asarray(inputs["w_branch_b"], f)[L]),
        "wout": np.ascontiguousarray(np.asarray(inputs["w_out"], f)[L]),
        "wr": np.ascontiguousarray(wr),
        "w1": np.ascontiguousarray(np.asarray(inputs["w1"], f)[L].reshape(NE, D, 512)),
        "w3": np.ascontiguousarray(np.asarray(inputs["w3"], f)[L].reshape(NE, D, 512)),
        "w2": np.ascontiguousarray(np.asarray(inputs["w2"], f)[L].reshape(NE, 512, D)),
        "fg": np.ascontiguousarray(np.asarray(inputs["final_g"], f)[None, :]),
        "g2row": np.ascontiguousarray(np.asarray(inputs["norm2_g"], f)[L][None, :]),
        "g1row": np.ascontiguousarray(np.asarray(inputs["norm1_g"], f)[L][None, :]),
        "Utri": np.triu(np.ones((128, 128), f), 1),
        "OnesM": np.ones((128, 128), f),
        "ecap": np.ascontiguousarray(np.broadcast_to((np.arange(NE) * CAP).astype(f)[None, :], (128, NE))),
        "vals": _vals_const(),
        "fillv": _fill_const(),
    }
    in_maps = []
    for core in range(8):
        b = core // 4
        s0 = (core % 4) * TOKC
        xe = np.zeros((TOKC + HALO, D), f)
        if s0 == 0:
            xe[HALO:] = x[b, 0:TOKC]
        else:
            xe[:] = x[b, s0 - HALO:s0 + TOKC]
        flags = np.ones((128, 2), f)
        if s0 == 0:
            flags[:, 0] = 0.0
        m = dict(shared)
        m["xe"] = xe
        m["cT"] = np.ascontiguousarray(c[b].reshape(16, 128).T)
        m["flags"] = flags
        in_maps.append(m)
    return in_maps


def kernel(**inputs):
    if "nc" not in _NC_CACHE:
        _NC_CACHE["nc"] = build()
    nc = _NC_CACHE["nc"]
    in_maps = _host_inputs(inputs)
    res = run_bass_kernel_spmd(nc, in_maps, core_ids=list(range(8)))
    out = np.empty((2, 8192, D), np.float32)
    for core in range(8):
        b = core // 4
        s0 = (core % 4) * TOKC
        out[b, s0:s0 + TOKC] = res.results[core]["y"]
    return out
```

```python
import contextlib
import os
import numpy as np
import concourse.bass as bass
import concourse.mybir as mybir
from concourse.bass_utils import run_bass_kernel_spmd

F32 = mybir.dt.float32
BF16 = mybir.dt.bfloat16
I32 = mybir.dt.int32
AF = mybir.ActivationFunctionType
ALU = mybir.AluOpType
AX = mybir.AxisListType

D = 2048
TOKC = 2048
TP = 1024
HALO = 512
NPASS = 2
EPS = 1e-6
NSLOT = 5
SLOT_B = 16384
NEG = -30000.0
EVAC_SPLIT = os.environ.get('K_EVAC', '0') == '1'
ADA_DVE = os.environ.get('K_ADA', '1') == '1'
PREFETCH = os.environ.get('K_PRE', '1') == '1'
CONTIG = os.environ.get('K_CONTIG', '1') == '1'
NE = 32
CAP = 2048
MSLOT = 8
BIGIDX = 1 << 24


class Sem:
    def __init__(self, h, shared=False):
        self.h = h
        self.n = 0
        self.shared = shared


class Eng:
    def __init__(self, name, sem, selfwait=True):
        self.name = name
        self.sem = sem
        self.prog = []
        self.waited = {}
        self.selfwait = selfwait


class Prog:
    def __init__(self, target, handle):
        self.state = {}
        self.floor = {}
        self.nofloor = set()
        self.target = target
        self.handle = handle
        self.inc_log = []
        self.alias = {}
        self.engines = []

    def fence(self):
        for k, st in self.state.items():
            if isinstance(k, tuple) and k[0] in ("ring", "mring"):
                continue
            evs = ([st[0]] if st[0] else []) + list(st[1].items())
            for s, v in evs:
                if s.shared:
                    v = s.n
                if self.floor.get(s, 0) < v:
                    self.floor[s] = v

    def _expand(self, keys):
        out = []
        for k in keys:
            out.extend(self.alias.get(k, (k,)) if isinstance(k, tuple) else (k,))
        return out

    def _deps(self, reads, writes, eng=None):
        reads = self._expand(reads)
        writes = self._expand(writes)
        w = {}

        def add(ev):
            if ev is None:
                return
            s, v = ev
            if s.shared:
                v = s.n
            if w.get(s, 0) < v:
                w[s] = v

        if eng is not None and eng.name not in self.nofloor:
            for s, v in self.floor.items():
                add((s, v))

        for k in reads:
            st = self.state.get(k)
            if st:
                add(st[0])
        for k in writes:
            st = self.state.get(k)
            if st:
                add(st[0])
                for s, v in st[1].items():
                    add((s, v))
        return w

    def _commit(self, reads, writes, ev):
        reads = self._expand(reads)
        writes = self._expand(writes)
        s, v = ev
        for k in reads:
            st = self.state.setdefault(k, [None, {}])
            if st[1].get(s, 0) < v:
                st[1][s] = v
        for k in writes:
            self.state[k] = [ev, {}]

    def op(self, eng, reads, writes, emit):
        w = self._deps(reads, writes, eng)
        wl = []
        for s, v in w.items():
            if s is eng.sem and not eng.selfwait:
                continue
            if eng.waited.get(s, 0) < v:
                eng.waited[s] = v
                wl.append((s, v))
        self.inc_log.append((eng.name, eng.sem, 1))
        eng.sem.n += 1
        ev = (eng.sem, eng.sem.n)
        sem = eng.sem

        if eng.name == self.target:
            e = self.handle
            for s, v in wl:
                e.wait_ge(s.h, v)
            emit(e).then_inc(sem.h, 1)
        self._commit(reads, writes, ev)

    def dma(self, eng, reads, writes, emit, dsem):
        w = self._deps(reads, writes, eng)
        if dsem.n > 0 and w.get(dsem, 0) < dsem.n:
            w[dsem] = dsem.n
        wl = []
        for s, v in w.items():
            if eng.waited.get(s, 0) < v:
                eng.waited[s] = v
                wl.append((s, v))
        self.inc_log.append((eng.name, dsem, 16))
        dsem.n += 16
        ev = (dsem, dsem.n)

        if eng.name == self.target:
            e = self.handle
            for s, v in wl:
                e.wait_ge(s.h, v)
            emit(e).then_inc(dsem.h, 16)
        self._commit(reads, writes, ev)

    def raw(self, eng, reads, emit):
        w = self._deps(reads, [], eng)
        wl = []
        for s, v in w.items():
            if s is eng.sem and not eng.selfwait:
                continue
            if eng.waited.get(s, 0) < v:
                eng.waited[s] = v
                wl.append((s, v))
        if eng.name == self.target:
            for s, v in wl:
                self.handle.wait_ge(s.h, v)
            return emit(self.handle)
        return None

    @contextlib.contextmanager
    def guard(self, cond_fn):
        snap_waited = {e.name: dict(e.waited) for e in self.engines}
        log0 = len(self.inc_log)
        before = {}
        ctx = None
        if self.target is not None:
            ctx = self.handle.If(cond_fn(self.handle))
            ctx.__enter__()
        sems_seen = {}
        n_at_entry = lambda sem: sems_seen.setdefault(id(sem), sem.n)
        entry_vals = {}
        class _Peek:
            pass
        all_sems = set()
        for (_, sem, _) in self.inc_log:
            all_sems.add(sem)
        entry_vals = {sem: sem.n for sem in all_sems}
        try:
            yield
        finally:
            if ctx is not None:
                ctx.__exit__(None, None, None)
                comp = {}
                order = []
                for (en, sem, amt) in self.inc_log[log0:]:
                    if en == self.target:
                        if sem not in comp:
                            comp[sem] = 0
                            order.append(sem)
                        comp[sem] += amt
                ectx = self.handle.Else()
                ectx.__enter__()
                for sem in order:
                    v0 = entry_vals.get(sem, 0)
                    if v0 > 0:
                        self.handle.wait_ge(sem.h, v0)
                    self.handle.sem_inc(sem.h, comp[sem])
                ectx.__exit__(None, None, None)
            for e in self.engines:
                e.waited = snap_waited[e.name]

    def final_wait(self, eng, keys):
        w = self._deps(keys, keys)
        wl = list(w.items())

        if eng.name == self.target:
            for s, v in wl:
                self.handle.wait_ge(s.h, v)


def build(debug=None):
    nc = bass.Bass("TRN2", target_bir_lowering=False)

    def din(name, shape):
        return nc.dram_tensor(name, list(shape), F32, kind="ExternalInput").ap()

    xe = din("xe", [TOKC + HALO, D])
    cT = din("cT", [128, 16])
    flags_d = din("flags", [128, 2])
    ident_d = din("ident", [128, 128])
    ada_w = din("ada_w", [D, 6 * D])
    ada_b = din("ada_b", [1, 6 * D])
    g1T_d = din("g1T", [128, 16])
    g2T_d = din("g2T", [128, 16])
    lnG = din("lnG", [1, 1024])
    lnB = din("lnB", [1, 1024])
    bS = din("bS", [1, 1024])
    wS = din("wS", [8, 128, 128])
    biasT = din("biasT", [128, 16 * 384])
    tabc_d = din("tabc", [1, 16])
    w_in = din("w_in", [D, 9216])
    wbrA = din("wbrA", [1024, D])
    wbrB = din("wbrB", [1024, D])
    wout = din("wout", [D, D])
    wr_d = din("wr", [D, 36])
    w1 = din("w1", [NE, D, 512])
    w3 = din("w3", [NE, D, 512])
    w2 = din("w2", [NE, 512, D])
    fg = din("fg", [1, D])
    g2row = din("g2row", [1, D])
    g1row = din("g1row", [1, D])
    Utri_d = din("Utri", [128, 128])
    Ones_d = din("OnesM", [128, 128])
    ecap_d = din("ecap", [128, 32])
    vals_d = nc.dram_tensor("vals", [128, 128], I32, kind="ExternalInput").ap()
    fillv_d = nc.dram_tensor("fillv", [128, 2048], I32, kind="ExternalInput").ap()
    h2d = nc.dram_tensor("h2d", [TOKC, D], BF16, kind="Internal").ap()
    table = nc.dram_tensor("table", [NE * CAP, 4], I32, kind="Internal").ap()
    cntd = nc.dram_tensor("cntd", [1, NE], I32, kind="Internal").ap()
    Ybuf4 = nc.dram_tensor("Ybuf", [4 * TOKC + 256, D // 2], F32, kind="Internal").ap()
    Ybuf = Ybuf4.rearrange("(r h) c -> r (h c)", h=2)
    y = nc.dram_tensor("y", [TOKC, D], F32, kind="ExternalOutput").ap()
    x1s = nc.dram_tensor("x1s", [TOKC, D], F32, kind="Internal").ap()
    modd = nc.dram_tensor("modd", [1, 6 * D], F32, kind="Internal").ap()
    dbg_out = None
    if debug is not None:
        dbg_out = nc.dram_tensor("dbg", [128, debug["words"]], F32, kind="ExternalOutput").ap()

    es = contextlib.ExitStack()
    with es:
        es.enter_context(nc.allow_low_precision("bf16 matmul operands by design"))
        ARENA_W = 53200
        arena = es.enter_context(nc.sbuf_tensor("arena", [128, ARENA_W], F32))
        PS = [es.enter_context(nc.psum_tensor(f"ps{i}", [128, 1024], F32)) for i in range(4)]

        SEMH = {}
        for _n in (["s_pe", "s_act", "s_dve", "s_pool", "s_sp", "s_const", "s_misc"] + [f"s_ring{i}" for i in range(NSLOT)] + [f"s_ringb{i}" for i in range(NSLOT)] + [f"s_adb{i}" for i in range(3)] +
                   [f"s_x{i}" for i in range(2)] + [f"s_o{i}" for i in range(2)] + [f"s_ad{i}" for i in range(3)] +
                   [f"s_mr{i}" for i in range(MSLOT)] + [f"s_ix{i}" for i in range(5)] + [f"s_ga{i}" for i in range(5)] +
                   [f"s_sc{i}" for i in range(8)] + [f"s_h{i}" for i in range(2)] + [f"s_y{i}" for i in range(4)] + ["s_tab"] +
                   [f"s_cp{i}" for i in range(8)] + [f"s_f{i}" for i in range(12)] + [f"s_tp{i}" for i in range(4)] + [f"s_ab{i}" for i in range(2)] + [f"s_md{i}" for i in range(2)]):
            SEMH[_n] = es.enter_context(nc.semaphore(_n))

        def program(target, handle):
            def newsem(name):
                return Sem(SEMH[name])

            PE = Eng("pe", newsem("s_pe"), selfwait=False)
            ACT = Eng("act", newsem("s_act"))
            DVE = Eng("dve", newsem("s_dve"))
            POOL = Eng("pool", newsem("s_pool"))
            SP = Eng("sp", newsem("s_sp"))
            ring_sem = [newsem(f"s_ring{i}") for i in range(NSLOT)]
            ring_semb = [newsem(f"s_ringb{i}") for i in range(NSLOT)]
            adsemb = [newsem(f"s_adb{i}") for i in range(3)]
            xsem = [newsem(f"s_x{i}") for i in range(2)]
            osem = [newsem(f"s_o{i}") for i in range(2)]
            csem = newsem("s_const")
            csem.shared = True
            msem = newsem("s_misc")
            msem.shared = True
            adsem = [newsem(f"s_ad{i}") for i in range(3)]
            mr_sem = [newsem(f"s_mr{i}") for i in range(MSLOT)]
            ixsem = [newsem(f"s_ix{i}") for i in range(5)]
            gasem = [newsem(f"s_ga{i}") for i in range(5)]
            scsem = [newsem(f"s_sc{i}") for i in range(8)]

            class SemPool:
                def __init__(self, sems):
                    self.sems = sems
                    self.i = 0

                def nxt(self):
                    sm = self.sems[self.i % len(self.sems)]
                    self.i += 1
                    return sm

            cpool = SemPool([newsem(f"s_cp{i}") for i in range(8)])
            tpool = SemPool([newsem(f"s_tp{i}") for i in range(4)])
            absem = [newsem(f"s_ab{i}") for i in range(2)]
            fsem = [newsem(f"s_f{i}") for i in range(12)]
            mdsem = [newsem(f"s_md{i}") for i in range(2)]
            hsem = [newsem(f"s_h{i}") for i in range(2)]
            ysem = [newsem(f"s_y{i}") for i in range(4)]
            tabsem = newsem("s_tab")
            tabsem.shared = True
            P = Prog(target, handle)
            P.nofloor.add("pool")
            P.engines = [PE, ACT, DVE, POOL, SP]

            def sb(off, shape, dt=F32, parts=(0, 128)):
                esz = 4 if dt in (F32, I32) else 2
                n = int(np.prod(shape))
                nbytes = n * esz
                assert off % 4 == 0 and nbytes % 4 == 0
                assert off + nbytes <= ARENA_W * 4, (off, nbytes)
                ap = arena[parts[0]:parts[1], off // 4:(off + nbytes) // 4]
                if dt != F32:
                    ap = ap.bitcast(dt)
                if len(shape) == 2:
                    ap = ap.rearrange("p (a b) -> p a b", a=shape[0], b=shape[1])
                elif len(shape) == 3:
                    ap = ap.rearrange("p (a b c) -> p a b c", a=shape[0], b=shape[1], c=shape[2])
                return ap

            def pbank(b, n=512, off=0):
                return PS[b // 2][:, (b % 2) * 512 + off:(b % 2) * 512 + off + n]

            def pkey(b):
                return ("ps", b)

            C0 = 0
            o = C0
            identF = sb(o, [128]); o += 512
            identB = sb(o, [128], BF16); o += 256
            modT = sb(o, [96]); o += 384
            effT = sb(o, [32]); o += 128
            g1T = sb(o, [16]); o += 64
            g2T = sb(o, [16]); o += 64
            tabc = sb(o, [16]); o += 64
            flags = sb(o, [2]); o += 8
            ones8 = sb(o, [8]); o += 32
            wr = sb(o, [16, 36]); o += 2304
            WsT = sb(o, [8, 128], BF16); o += 2048
            small = sb(o, [256]); o += 1024
            rt = sb(o, [256]); o += 1024
            urow = sb(o, [128], BF16, parts=(0, 1)); o += 256
            vrow = sb(o, [128], BF16, parts=(0, 1)); o += 256
            sel0 = sb(o, [16, 32]); o += 2048
            sel1 = sb(o, [16, 32]); o += 2048
            wsl = sb(o, [16, 2]); o += 128
            ecap = sb(o, [32]); o += 128
            vals = sb(o, [128], I32); o += 512
            CONST_END = o
            RING0 = (CONST_END + 63) // 64 * 64
            PH0 = RING0 + NSLOT * SLOT_B
            PH_END = ARENA_W * 4

            def ring_view(slot, a, b):
                return sb(RING0 + slot * SLOT_B, [a, b], BF16)

            class WS:
                units = []
                released = []
                nloaded = 0

            def w_add(src, a, b):
                WS.units.append((src, a, b))
                WS.released.append(False)
                return len(WS.units) - 1

            def w_pump():
                while WS.nloaded < len(WS.units):
                    u = WS.nloaded
                    if u >= NSLOT and not WS.released[u - NSLOT]:
                        break
                    src, a, b = WS.units[u]
                    slot = u % NSLOT
                    dst = ring_view(slot, a, b)
                    P.alias[("ring", slot)] = (("ring", slot, 0), ("ring", slot, 1))
                    h_ = a // 2
                    P.dma(POOL, [], [("ring", slot, 0)],
                          lambda e, dst=dst, src=src: e.dma_start(out=dst[:, 0:h_, :], in_=src[:, 0:h_, :]),
                          ring_sem[slot])
                    P.dma(POOL, [], [("ring", slot, 1)],
                          lambda e, dst=dst, src=src: e.dma_start(out=dst[:, h_:a, :], in_=src[:, h_:a, :]),
                          ring_semb[slot])
                    WS.nloaded += 1

            def w_get(u):
                w_pump()
                assert u < WS.nloaded, ("weight unit not loadable", u, WS.nloaded)
                src, a, b = WS.units[u]
                slot = u % NSLOT
                return ring_view(slot, a, b), ("ring", slot)

            def w_rel(u):
                WS.released[u] = True
                w_pump()

            def win_blk(c0):
                return w_in[:, c0:c0 + 512].rearrange("(c p) n -> p c n", p=128)

            plan = []
            for p in range(NPASS):
                pl = {}
                pl["k"] = [None, None]
                pl["vb"] = [None, None]
                pl["q"] = [None, None]
                for hh in range(2):
                    pl["k"][hh] = w_add(win_blk(3072 + hh * 512), 16, 512)
                    pl["vb"][hh] = w_add(win_blk(4096 + hh * 512), 16, 512)
                    pl["q"][hh] = w_add(win_blk(2048 + hh * 512), 16, 512)
                pl["u"] = [w_add(win_blk(0 + i * 512), 16, 512) for i in range(2)]
                pl["v"] = [w_add(win_blk(1024 + i * 512), 16, 512) for i in range(2)]
                pl["gA"] = [None] * 4
                pl["gB"] = [None] * 4
                pl["brA"] = [None] * 2
                pl["brB"] = [None] * 2
                for q in range(4):
                    pl["gA"][q] = w_add(win_blk(5120 + q * 512), 16, 512)
                    pl["gB"][q] = w_add(win_blk(7168 + q * 512), 16, 512)
                    if q % 2 == 0:
                        hf = q // 2
                        pl["brA"][hf] = w_add(wbrA[:, hf * 1024:(hf + 1) * 1024].rearrange("(c p) n -> p c n", p=128), 8, 1024)
                        pl["brB"][hf] = w_add(wbrB[:, hf * 1024:(hf + 1) * 1024].rearrange("(c p) n -> p c n", p=128), 8, 1024)
                pl["wo"] = [w_add(wout[:, cu * 512:(cu + 1) * 512].rearrange("(c p) n -> p c n", p=128), 16, 512) for cu in range(4)]
                plan.append(pl)

            def act_fn(out, in_, func, reads, writes, scale=1.0, bias=0.0, accum=None):
                def emit(e):
                    kw = {}
                    if accum is not None:
                        kw["accum_out"] = accum
                    return e.activation(out=out, in_=in_, func=func, bias=bias, scale=scale, **kw)
                P.op(ACT, reads, writes, emit)

            def dve(reads, writes, emit):
                P.op(DVE, reads, writes, emit)

            def pe_group(reads, writes, emit):
                P.op(PE, reads, writes, emit)

            def mm_acc(e, out, pairs):
                n = len(pairs)
                ins = None
                for i, (l, r) in enumerate(pairs):
                    ins = e.matmul(out, l, r, start=(i == 0), stop=(i == n - 1))
                return ins

            def sp_load(dst, src, writes, sem, reads=()):
                P.dma(SP, list(reads), list(writes), lambda e: e.dma_start(out=dst, in_=src), sem)

            sp_load(identF, ident_d, ["identF"], cpool.nxt())
            sp_load(flags, flags_d, ["flags"], cpool.nxt())
            sp_load(g1T, g1T_d, ["g1T"], cpool.nxt())
            sp_load(g2T, g2T_d, ["g2T"], cpool.nxt())
            sp_load(tabc, tabc_d[0].partition_broadcast(128), ["tabc"], cpool.nxt())
            sp_load(wr, wr_d.rearrange("(c p) n -> p c n", p=128), ["wr"], cpool.nxt())
            sp_load(ecap, ecap_d, ["ecap"], cpool.nxt())
            sp_load(vals, vals_d, ["vals"], cpool.nxt())
            scT = sb(PH0, [16])
            cTs = sb(PH0 + 64, [16])
            one11 = sb(PH0 + 128, [1])
            sp_load(cTs, cT, ["cTs"], cpool.nxt())
            dve(["identF"], ["identB"], lambda e: e.tensor_copy(out=identB, in_=identF))
            dve([], ["ones8"], lambda e: e.memset(ones8, 1.0))
            dve([], ["uvrow"], lambda e: e.memset(urow[:, 0:64], NEG))
            dve([], ["uvrow"], lambda e: e.memset(urow[:, 64:128], 0.0))
            dve([], ["uvrow"], lambda e: e.memset(vrow[:, 0:64], 0.0))
            dve([], ["uvrow"], lambda e: e.memset(vrow[:, 64:128], 1.0))
            dve([], ["one11"], lambda e: e.memset(one11, 1.0))
            act_fn(scT, cTs, AF.Silu, ["cTs"], ["scT"])

            wsf = sb(PH0 + 1024, [8, 128])
            P.dma(SP, [], ["wsf"], lambda e: e.dma_start(out=wsf, in_=wS.rearrange("g t s -> t g s")), cpool.nxt())
            dve(["wsf"], ["wsf"], lambda e: e.memset(wsf[0:64, :, 64:128], 0.0))
            for hb in range(2):
                def emit(e, hb=hb):
                    ins = None
                    for g in range(4):
                        gg = hb * 4 + g
                        ins = e.transpose(pbank(hb, 128, g * 128), wsf[:, gg, :], identF)
                    return ins
                pe_group(["wsf", "identF"], [pkey(hb)], emit)
                dve([pkey(hb)], ["WsT"], lambda e, hb=hb: e.tensor_copy(
                    out=WsT[:, hb * 4:(hb + 1) * 4, :], in_=pbank(hb).rearrange("p (g t) -> p g t", g=4)))

            AD0 = PH0 + 5120
            NAD = 3
            adbuf = [sb(AD0 + i * 32768, [16, 512]) for i in range(NAD)]
            abrow = sb(AD0 + NAD * 32768, [512], parts=(0, 1))
            abrow2 = sb(AD0 + NAD * 32768 + 2048, [512], parts=(0, 1))
            mrow = [sb(AD0 + NAD * 32768 + 4096 + i * 2048, [512], parts=(0, 1)) for i in range(2)]
            accb = [sb(AD0 + NAD * 32768 + 8192 + i * 2048, [512]) for i in range(2)]
            NB = 24
            MTB = 7
            def ada_load(blk):
                bi = blk % NAD
                P.alias[("adbuf", bi)] = (("adbuf", bi, 0), ("adbuf", bi, 1))
                srcv = ada_w[:, blk * 512:(blk + 1) * 512].rearrange("(c p) n -> p c n", p=128)
                P.dma(SP, [], [("adbuf", bi, 0)], lambda e: e.dma_start(out=adbuf[bi][:, 0:8, :], in_=srcv[:, 0:8, :]), adsem[bi])
                P.dma(SP, [], [("adbuf", bi, 1)], lambda e: e.dma_start(out=adbuf[bi][:, 8:16, :], in_=srcv[:, 8:16, :]), adsemb[bi])

            for blk in range(min(NAD - 1, NB)):
                ada_load(blk)
            for blk in range(NB):
                bi = blk % NAD
                if blk + NAD - 1 < NB:
                    ada_load(blk + NAD - 1)
                ab = abrow if blk % 2 == 0 else abrow2
                abk = ("abrow", blk % 2)
                P.dma(SP, [], [abk], lambda e, ab=ab, blk=blk: e.dma_start(out=ab, in_=ada_b[:, blk * 512:(blk + 1) * 512]), absem[blk % 2])
                pb_ = blk % 2

                ac = accb[blk % 2]
                ack = ("accb", blk % 2)
                NDV = 10
                pe_group([("adbuf", bi), "scT"], [pkey(pb_)], lambda e, bi=bi, pb_=pb_: [
                    e.matmul(pbank(pb_)[0:1, :], scT[:, dc:dc + 1], adbuf[bi][:, dc, :], start=(dc == NDV), stop=False)
                    for dc in range(NDV, 16)][-1])
                dve([("adbuf", bi), "scT"], [ack], lambda e, bi=bi, ac=ac: e.tensor_scalar(
                    out=ac, in0=adbuf[bi][:, 0, :], scalar1=scT[:, 0:1], scalar2=None, op0=ALU.mult))
                for dc in range(1, NDV):
                    dve([("adbuf", bi), "scT", ack], [ack], lambda e, bi=bi, ac=ac, dc=dc: e.scalar_tensor_tensor(
                        out=ac, in0=adbuf[bi][:, dc, :], scalar=scT[:, dc:dc + 1], in1=ac, op0=ALU.mult, op1=ALU.add))
                pe_group([ack, "ones8"], [pkey(pb_)], lambda e, ac=ac, pb_=pb_: e.matmul(
                    pbank(pb_)[0:1, :], ones8[:, 0:1], ac, start=False, stop=True))
                mr = mrow[blk % 2]
                mrk = ("mrow", blk % 2)
                dve([pkey(pb_), abk], [mrk], lambda e, mr=mr, ab=ab, pb_=pb_: e.tensor_tensor(
                    out=mr, in0=pbank(pb_)[0:1, :], in1=ab, op=ALU.add))
                P.dma(SP, [mrk], [("modd", blk)], lambda e, mr=mr, blk=blk: e.dma_start(
                    out=modd[:, blk * 512:(blk + 1) * 512], in_=mr), mdsem[blk % 2])

                def emit2(e, mr=mr, blk=blk):
                    ins = None
                    for k in range(4):
                        ins = e.matmul(pbank(MTB)[:, blk * 4 + k:blk * 4 + k + 1], mr[0:1, k * 128:(k + 1) * 128], one11[0:1, 0:1],
                                       start=True, stop=True)
                    return ins
                pe_group([mrk, "one11"], [("mtb", blk)], emit2)
            dve([("mtb", b) for b in range(NB)], ["modT"], lambda e: e.tensor_copy(out=modT, in_=pbank(MTB)[:, 0:96]))
            dve(["modT", "g1T"], ["effT"], lambda e: e.scalar_tensor_tensor(
                out=effT[:, 0:16], in0=modT[:, 16:32], scalar=1.0, in1=g1T, op0=ALU.add, op1=ALU.mult))
            dve(["modT", "g2T"], ["effT"], lambda e: e.scalar_tensor_tensor(
                out=effT[:, 16:32], in0=modT[:, 64:80], scalar=1.0, in1=g2T, op0=ALU.add, op1=ALU.mult))
            shift1 = modT[:, 0:16]
            shift2 = modT[:, 48:64]
            setup_keys = ["modT", "effT", "identF", "identB", "flags", "tabc", "wr", "WsT", "ones8"]
            setup_scratch = ["scT", "cTs", "one11", "wsf"] + [("adbuf", i) for i in range(3)] + \
                [("abrow", i) for i in range(2)] + [("mrow", i) for i in range(2)]
            modd_keys = [("modd", b) for b in range(NB)]

            dbg_done = [False]

            def dbg_dump(items):
                off = 0
                for ap, nw, keys in items:
                    P.dma(SP, list(keys), [("dbg", off)], lambda e, ap=ap, off=off, nw=nw: e.dma_start(
                        out=dbg_out[:, off:off + nw], in_=ap), msem)
                    off += nw
                dbg_done[0] = True

            def rms_rstd(ss_ap, rstd_ap, key_ss, key_rstd):
                act_fn(rstd_ap, ss_ap, AF.Sqrt, [key_ss], [key_rstd], scale=1.0 / D, bias=EPS)
                dve([key_rstd], [key_rstd], lambda e: e.reciprocal(out=rstd_ap, in_=rstd_ap))

            for p in range(NPASS if debug is None else debug.get("npass", NPASS)):
                pl = plan[p]
                row0 = p * TP
                fl = flags[:, p:p + 1]
                o = PH0
                h1own = sb(o, [16, 1024], BF16); o += 32768
                ybT = sb(o, [8, 1024], BF16); o += 16384
                GM0 = o
                h1halo = sb(o, [16, 512], BF16); o += 16384
                MIX0 = o
                xt = [sb(MIX0 + i * 8192, [2048]) for i in range(2)]
                junk = sb(MIX0 + 16384, [2048], BF16)
                P.fence()
                effB1 = sb(MIX0 + 20480, [2048])
                shiftB1 = sb(MIX0 + 28672, [2048])
                g1b = sb(PH0 + 32768, [2048])
                h1tok = [sb(MIX0 + 36864 + i * 4096, [2048], BF16) for i in range(2)]
                sp_load(g1b, g1row[0].partition_broadcast(128), ["g1b"], cpool.nxt())
                sp_load(effB1, modd[0, D:2 * D].partition_broadcast(128), ["effB1"], cpool.nxt(), reads=modd_keys)
                sp_load(shiftB1, modd[0, 0:D].partition_broadcast(128), ["shiftB1"], cpool.nxt(), reads=modd_keys)
                dve(["effB1", "g1b"], ["effB1"], lambda e: e.scalar_tensor_tensor(
                    out=effB1, in0=effB1, scalar=1.0, in1=g1b, op0=ALU.add, op1=ALU.mult))
                def p1_front(j):
                    b = j % 2
                    xk = ("xt", b)
                    P.dma(SP, [], [xk], lambda e, b=b, j=j: e.dma_start(
                        out=xt[b], in_=xe[row0 + j * 128:row0 + (j + 1) * 128, :]), xsem[b])
                    ssa = small[:, j:j + 1]
                    rsa = small[:, 16 + j:17 + j]
                    act_fn(junk, xt[b], AF.Square, [xk], [("ss", j)], accum=ssa)
                    rms_rstd(ssa, rsa, ("ss", j), ("rs", j))
                    dve([xk, ("rs", j), "effB1"], [xk], lambda e, b=b, rsa=rsa: e.scalar_tensor_tensor(
                        out=xt[b], in0=xt[b], scalar=rsa, in1=effB1, op0=ALU.mult, op1=ALU.mult))
                    dve([xk, "shiftB1"], [("h1tok", b)], lambda e, b=b: e.tensor_tensor(out=h1tok[b], in0=xt[b], in1=shiftB1, op=ALU.add))

                def p1_back(j):
                    b = j % 2
                    for hb_ in range(2):
                        q = (j % 2) * 2 + hb_
                        tbv = PS[q // 2][:, (q % 2) * 512:(q % 2) * 512 + 512].bitcast(BF16)

                        def emit(e, b=b, hb_=hb_, tbv=tbv):
                            ins = None
                            for k in range(8):
                                dc = hb_ * 8 + k
                                ins = e.transpose(tbv[:, k * 128:(k + 1) * 128], h1tok[b][:, dc * 128:(dc + 1) * 128], identB)
                            return ins
                        pe_group([("h1tok", b), "identB"], [pkey(q)], emit)
                        if j < 4:
                            dst = h1halo[:, hb_ * 8:(hb_ + 1) * 8, j * 128:(j + 1) * 128]
                            dk = ("h1halo", j)
                        else:
                            dst = h1own[:, hb_ * 8:(hb_ + 1) * 8, (j - 4) * 128:(j - 3) * 128]
                            dk = ("h1own", j - 4)
                        act_fn(dst, tbv.rearrange("p (k t) -> p k t", k=8), AF.Copy, [pkey(q)], [dk])

                p1_front(0)
                for j in range(12):
                    if j + 1 < 12:
                        p1_front(j + 1)
                    p1_back(j)
                if debug is not None and debug["stage"] == "h1":
                    dbg_dump([(h1own.rearrange("p a b -> p (a b)").bitcast(F32), 8192, [("h1own", i) for i in range(8)]),
                              (h1halo.rearrange("p a b -> p (a b)").bitcast(F32), 4096, [("h1halo", i) for i in range(4)]),
                              (modT, 96, ["modT"]), (effT, 32, ["effT"])])
                    break

                def h1_rhs(dc, t0, n):
                    if t0 < 512:
                        assert t0 + n <= 512
                        return h1halo[:, dc, t0:t0 + n]
                    return h1own[:, dc, t0 - 512:t0 - 512 + n]

                def h1_keys(t0, n):
                    ks = []
                    for t in range(t0 // 128, (t0 + n) // 128):
                        ks.append(("h1halo", t) if t < 4 else ("h1own", t - 4))
                    return ks

                P.fence()
                o = MIX0
                qT = sb(o, [4, 1024], BF16); o += 8192
                kT = sb(o, [4, 1536], BF16); o += 12288
                Vt = sb(o, [12, 8 * 65], BF16); o += 12480
                o = (o + 63) // 64 * 64
                biasS = sb(o, [8, 384]); o += 12288
                Pb = [sb(o + i * 1280, [640], BF16) for i in range(3)]; o += 3840
                ybt = sb(o, [512], BF16); o += 1024
                rden = sb(o, [8]); o += 32
                assert o <= PH_END
                for hh in range(2):
                    P.dma(SP, [], ["biasS"], lambda e, hh=hh: e.dma_start(
                        out=biasS.rearrange("p a b -> p (a b)"), in_=biasT[:, hh * 3072:(hh + 1) * 3072]), cpool.nxt())
                    wk, wkk = w_get(pl["k"][hh])
                    for tt in range(3):
                        for cc in range(4):
                            bnk = (tt * 4 + cc) % 8
                            def emit(e, tt=tt, cc=cc, bnk=bnk):
                                return mm_acc(e, pbank(bnk), [(wk[:, dc, cc * 128:(cc + 1) * 128], h1_rhs(dc, tt * 512, 512)) for dc in range(16)])
                            pe_group([wkk] + h1_keys(tt * 512, 512), [pkey(bnk)], emit)
                            act_fn(kT[:, cc, tt * 512:(tt + 1) * 512], pbank(bnk), AF.Copy, [pkey(bnk)], [("kT", tt)])
                    w_rel(pl["k"][hh])
                    wv, wvk = w_get(pl["vb"][hh])
                    for j in range(12):
                        bnk = j % 8
                        def emit(e, j=j, bnk=bnk):
                            return mm_acc(e, pbank(bnk), [(h1_rhs(dc, j * 128, 128), wv[:, dc, :]) for dc in range(16)])
                        pe_group([wvk] + h1_keys(j * 128, 128), [pkey(bnk)], emit)
                        vdst = Vt[:, j, :].rearrange("p (h d) -> p h d", h=8)
                        sc = fl if j < 4 else 1.0
                        act_fn(vdst[:, :, 0:64], pbank(bnk).rearrange("p (h d) -> p h d", h=8), AF.Copy,
                               [pkey(bnk), "flags"], [("Vt", j)], scale=sc)
                        act_fn(vdst[:, :, 64:65], ones8.rearrange("p (h d) -> p h d", d=1), AF.Copy,
                               ["ones8", "flags"], [("Vt", j)], scale=sc)
                    w_rel(pl["vb"][hh])
                    wq, wqk = w_get(pl["q"][hh])
                    for tt in range(2):
                        for cc in range(4):
                            bnk = (tt * 4 + cc) % 8
                            def emit(e, tt=tt, cc=cc, bnk=bnk):
                                return mm_acc(e, pbank(bnk), [(wq[:, dc, cc * 128:(cc + 1) * 128], h1_rhs(dc, 512 + tt * 512, 512)) for dc in range(16)])
                            pe_group([wqk] + h1_keys(512 + tt * 512, 512), [pkey(bnk)], emit)
                            act_fn(qT[:, cc, tt * 512:(tt + 1) * 512], pbank(bnk), AF.Copy, [pkey(bnk)], [("qT", tt)], scale=0.125)
                    w_rel(pl["q"][hh])
                    NSET = 3
                    SK = 2
                    jobs = [(i, hl) for i in range(8) for hl in range(8)]

                    def st_S(n):
                        i, hl = jobs[n]
                        j = i + 4
                        h = hh * 8 + hl
                        cc = hl // 2
                        pb0 = (hl % 2) * 64
                        st = n % NSET
                        s0, s1 = 2 * st, 2 * st + 1

                        def emit(e):
                            ins = None
                            qv = qT[pb0:pb0 + 64, cc, i * 128:(i + 1) * 128]
                            for dl in range(5):
                                kt = j - dl
                                kv = kT[pb0:pb0 + 64, cc, kt * 128:(kt + 1) * 128]
                                if dl < 3:
                                    outp = pbank(s0, 128, dl * 128)
                                else:
                                    outp = pbank(s1, 128, (dl - 3) * 128)
                                if dl == 4:
                                    e.matmul(outp, kv, qv, start=True, stop=False)
                                    ins = e.matmul(outp, urow, vrow, start=False, stop=True)
                                else:
                                    ins = e.matmul(outp, kv, qv, start=True, stop=True)
                            return ins
                        pe_group([("qT", i // 4), "uvrow"] + [("kT", (j - dl) // 4) for dl in range(5)], [pkey(s0), pkey(s1)], emit)
                        dve([pkey(s0), "biasS"], [pkey(s0)], lambda e: e.tensor_tensor(
                            out=pbank(s0, 384), in0=pbank(s0, 384), in1=biasS[:, hl, :], op=ALU.add))
                        act_fn(Pb[st][:, 0:384], pbank(s0, 384), AF.Exp, [pkey(s0)], [("Pb", st)])
                        act_fn(Pb[st][:, 384:640], pbank(s1, 256), AF.Exp, [pkey(s1), "tabc"], [("Pb", st)],
                               bias=tabc[:, h:h + 1])

                    def st_PV(n):
                        i, hl = jobs[n]
                        j = i + 4
                        st = n % NSET
                        ob = 6 + hl // 4

                        def emit(e):
                            pairs = []
                            for dl in range(5):
                                kt = j - dl
                                pairs.append((Pb[st][:, dl * 128:(dl + 1) * 128], Vt[:, kt, hl * 65:(hl + 1) * 65]))
                            return mm_acc(e, pbank(ob, 65, (hl % 4) * 65), pairs)
                        pe_group([("Pb", st)] + [("Vt", j - dl) for dl in range(5)], [pkey(ob)], emit)

                    def st_norm(i):
                        for ob in (6, 7):
                            ov = pbank(ob, 260).rearrange("p (h d) -> p h d", h=4)
                            dve([pkey(ob)], ["rden"], lambda e, ob=ob, ov=ov: e.reciprocal(
                                out=rden[:, (ob - 6) * 4:(ob - 5) * 4], in_=ov[:, :, 64]))
                            for k in range(4):
                                hl = (ob - 6) * 4 + k
                                dve([pkey(ob), "rden"], ["ybt"], lambda e, ov=ov, k=k, hl=hl: e.tensor_scalar(
                                    out=ybt[:, hl * 64:(hl + 1) * 64], in0=ov[:, k, 0:64], scalar1=rden[:, hl:hl + 1],
                                    scalar2=None, op0=ALU.mult))

                    def st_T(i):
                        tb = PS[0][:, 0:512].bitcast(BF16)

                        def emit(e):
                            ins = None
                            for k in range(4):
                                ins = e.transpose(tb[:, k * 128:(k + 1) * 128], ybt[:, k * 128:(k + 1) * 128], identB)
                            return ins
                        pe_group(["ybt", "identB"], [pkey(0)], emit)
                        act_fn(ybT[:, hh * 4:(hh + 1) * 4, i * 128:(i + 1) * 128],
                               tb[:, 0:512].rearrange("p (k t) -> p k t", k=4), AF.Copy, [pkey(0)], [("ybT", i)])

                    pendT = []
                    for n in range(len(jobs) + SK + 2):
                        if n < len(jobs):
                            st_S(n)
                        m = n - SK
                        if 0 <= m < len(jobs):
                            st_PV(m)
                            if jobs[m][1] == 7:
                                st_norm(jobs[m][0])
                                pendT.append((jobs[m][0], n + 2))
                        for (ti, due) in pendT:
                            if due == n:
                                st_T(ti)
                if debug is not None and debug["stage"] == "attn":
                    dbg_dump([(ybT.rearrange("p a b -> p (a b)").bitcast(F32), 4096, [("ybT", i) for i in range(8)])])
                    break

                P.fence()
                o = GM0
                yaT = sb(o, [8, 1024], BF16); o += 16384
                uT = sb(o, [8, 1024], BF16); o += 16384
                vn = [sb(o + i * 2048, [1024], BF16) for i in range(2)]; o += 4096
                vg = [sb(o + i * 4096, [1024]) for i in range(2)]; o += 8192
                lnGb = sb(o, [1024]); o += 4096
                lnBb = sb(o, [1024]); o += 4096
                bSb = sb(o, [1024]); o += 4096
                t1 = sb(o, [1024]); o += 4096
                bst = sb(o, [12]); o += 48
                assert o <= PH_END
                gkeys = [("kT", t) for t in range(3)] + [("qT", t) for t in range(2)] + [("Vt", j) for j in range(12)] + \
                    ["biasS", "ybt", "rden"] + [("Pb", i) for i in range(2)] + [("tmpS", i) for i in range(2)]
                sp_load(lnGb, lnG[0].partition_broadcast(128), ["lnGb"], cpool.nxt())
                sp_load(lnBb, lnB[0].partition_broadcast(128), ["lnBb"], cpool.nxt())
                sp_load(bSb, bS[0].partition_broadcast(128), ["bSb"], cpool.nxt())
                for uu in range(2):
                    wu, wuk = w_get(pl["u"][uu])
                    for tt in range(2):
                        for cc in range(4):
                            bnk = (tt * 4 + cc) % 8
                            def emit(e, tt=tt, cc=cc, bnk=bnk, wu=wu):
                                return mm_acc(e, pbank(bnk), [(wu[:, dc, cc * 128:(cc + 1) * 128], h1_rhs(dc, 512 + tt * 512, 512)) for dc in range(16)])
                            pe_group([wuk] + h1_keys(512 + tt * 512, 512), [pkey(bnk)], emit)
                            act_fn(uT[:, uu * 4 + cc, tt * 512:(tt + 1) * 512], pbank(bnk), AF.Gelu,
                                   [pkey(bnk)], [("uT", tt)])
                    w_rel(pl["u"][uu])
                wv0, wv0k = w_get(pl["v"][0])
                wv1, wv1k = w_get(pl["v"][1])
                wvs = [(wv0, wv0k), (wv1, wv1k)]
                def gm_front(i):
                    b = i % 2
                    for vu in range(2):
                        bnk = b * 4 + vu
                        def emit(e, i=i, vu=vu, bnk=bnk):
                            return mm_acc(e, pbank(bnk), [(h1_rhs(dc, 512 + i * 128, 128), wvs[vu][0][:, dc, :]) for dc in range(16)])
                        pe_group([wvs[vu][1]] + h1_keys(512 + i * 128, 128), [pkey(bnk)], emit)
                        act_fn(vg[b][:, vu * 512:(vu + 1) * 512], pbank(bnk), AF.Gelu, [pkey(bnk)], [("vg", b)])
                    st6 = small[:, 64 + b * 12:64 + b * 12 + 12]
                    mv = small[:, 96 + b * 4:96 + b * 4 + 2]
                    rs_ = small[:, 98 + b * 4:99 + b * 4]
                    for vu in range(2):
                        dve([("vg", b)], [("st6", b)], lambda e, b=b, vu=vu, st6=st6: e.bn_stats(
                            out=st6[:, vu * 6:(vu + 1) * 6], in_=vg[b][:, vu * 512:(vu + 1) * 512]))
                    dve([("st6", b)], [("mv", b)], lambda e, st6=st6, mv=mv: e.bn_aggr(out=mv, in_=st6))
                    act_fn(rs_, mv[:, 1:2], AF.Sqrt, [("mv", b)], [("rsv", b)], bias=EPS)
                    dve([("rsv", b)], [("rsv", b)], lambda e, rs_=rs_: e.reciprocal(out=rs_, in_=rs_))
                    dve([("vg", b), ("mv", b), ("rsv", b)], [("vg", b)], lambda e, b=b, mv=mv, rs_=rs_: e.tensor_scalar(
                        out=vg[b], in0=vg[b], scalar1=mv[:, 0:1], scalar2=rs_, op0=ALU.subtract, op1=ALU.mult))
                    dve([("vg", b), "lnGb"], [("vg", b)], lambda e, b=b: e.tensor_tensor(out=vg[b], in0=vg[b], in1=lnGb, op=ALU.mult))
                    dve([("vg", b), "lnBb"], [("vn", b)], lambda e, b=b: e.tensor_tensor(out=vn[b], in0=vg[b], in1=lnBb, op=ALU.add))

                def gm_back(i):
                    b = i % 2
                    for gh in range(2):
                        bnk = b * 4 + 2 + gh
                        def emit(e, b=b, gh=gh, bnk=bnk):
                            ins = None
                            for g4 in range(4):
                                g = gh * 4 + g4
                                ins = e.matmul(pbank(bnk, 128, g4 * 128), vn[b][:, g * 128:(g + 1) * 128], WsT[:, g, :], start=True, stop=True)
                            return ins
                        pe_group([("vn", b), "WsT"], [pkey(bnk)], emit)
                        dve([pkey(bnk), "bSb"], ["t1"], lambda e, gh=gh, bnk=bnk: e.tensor_tensor(
                            out=t1[:, gh * 512:(gh + 1) * 512], in0=pbank(bnk), in1=bSb[:, gh * 512:(gh + 1) * 512], op=ALU.add))
                        dve(["t1", ("uT", i // 4)], [("yaT", i)], lambda e, gh=gh, i=i: e.tensor_tensor(
                            out=yaT[:, gh * 4:(gh + 1) * 4, i * 128:(i + 1) * 128],
                            in0=t1[:, gh * 512:(gh + 1) * 512].rearrange("p (g t) -> p g t", g=4),
                            in1=uT[:, gh * 4:(gh + 1) * 4, i * 128:(i + 1) * 128], op=ALU.mult))
                gm_front(0)
                for i in range(8):
                    if i + 1 < 8:
                        gm_front(i + 1)
                    gm_back(i)
                w_rel(pl["v"][0])
                w_rel(pl["v"][1])
                if debug is not None and debug["stage"] == "gmlp":
                    dbg_dump([(yaT.rearrange("p a b -> p (a b)").bitcast(F32), 4096, [("yaT", i) for i in range(8)])])
                    break

                P.fence()
                o = GM0 + 16384
                mergedT = sb(o, [16, 1024], BF16); o += 32768
                sg = [sb(o + i * 2048, [512]) for i in range(4)]; o += 8192
                assert o <= PH_END
                mkeys = [("uT", t) for t in range(2)] + [("vg", i) for i in range(2)] + [("vn", i) for i in range(2)] + ["t1", "lnGb", "lnBb", "bSb"]
                for q in range(4):
                    wga, wgak = w_get(pl["gA"][q])
                    wgb, wgbk = w_get(pl["gB"][q])
                    wa, wak = w_get(pl["brA"][q // 2])
                    wb, wbk = w_get(pl["brB"][q // 2])
                    for tt in range(2):
                        for c4 in range(4):
                            dcc = q * 4 + c4
                            st = (tt * 4 + c4) % 2
                            bA, bB, bYa, bYb = st * 4, st * 4 + 1, st * 4 + 2, st * 4 + 3
                            ts_ = slice(tt * 512, (tt + 1) * 512)

                            def emitA(e, c4=c4, ts_=ts_, bA=bA, wga=wga):
                                return mm_acc(e, pbank(bA), [(wga[:, dc, c4 * 128:(c4 + 1) * 128], h1own[:, dc, ts_]) for dc in range(16)])
                            pe_group([wgak] + [("h1own", t) for t in range(tt * 4, tt * 4 + 4)], [pkey(bA)], emitA)

                            def emitB(e, c4=c4, ts_=ts_, bB=bB, wgb=wgb):
                                return mm_acc(e, pbank(bB), [(wgb[:, dc, c4 * 128:(c4 + 1) * 128], h1own[:, dc, ts_]) for dc in range(16)])
                            pe_group([wgbk] + [("h1own", t) for t in range(tt * 4, tt * 4 + 4)], [pkey(bB)], emitB)
                            col = (q % 2) * 512 + c4 * 128

                            def emitYa(e, col=col, ts_=ts_, bYa=bYa, wa=wa):
                                return mm_acc(e, pbank(bYa), [(wa[:, kc, col:col + 128], yaT[:, kc, ts_]) for kc in range(8)])
                            pe_group([wak] + [("yaT", t) for t in range(tt * 4, tt * 4 + 4)], [pkey(bYa)], emitYa)

                            def emitYb(e, col=col, ts_=ts_, bYb=bYb, wb=wb):
                                return mm_acc(e, pbank(bYb), [(wb[:, kc, col:col + 128], ybT[:, kc, ts_]) for kc in range(8)])
                            pe_group([wbk] + [("ybT", t) for t in range(tt * 4, tt * 4 + 4)], [pkey(bYb)], emitYb)
                            sga, sgb = sg[st * 2], sg[st * 2 + 1]
                            act_fn(sga, pbank(bA), AF.Sigmoid, [pkey(bA)], [("sg", st * 2)])
                            act_fn(sgb, pbank(bB), AF.Sigmoid, [pkey(bB)], [("sg", st * 2 + 1)])
                            dve([("sg", st * 2), pkey(bYa)], [("sg", st * 2)], lambda e, sga=sga, bYa=bYa: e.tensor_tensor(
                                out=sga, in0=sga, in1=pbank(bYa), op=ALU.mult))
                            dve([("sg", st * 2 + 1), pkey(bYb)], [("sg", st * 2 + 1)], lambda e, sgb=sgb, bYb=bYb: e.tensor_tensor(
                                out=sgb, in0=sgb, in1=pbank(bYb), op=ALU.mult))
                            dve([("sg", st * 2), ("sg", st * 2 + 1)], [("mergedT", tt)], lambda e, sga=sga, sgb=sgb, dcc=dcc, ts_=ts_: e.tensor_tensor(
                                out=mergedT[:, dcc, ts_], in0=sga, in1=sgb, op=ALU.add))
                    w_rel(pl["gA"][q])
                    w_rel(pl["gB"][q])
                    if q % 2 == 1:
                        w_rel(pl["brA"][q // 2])
                        w_rel(pl["brB"][q // 2])
                if debug is not None and debug["stage"] == "merge":
                    dbg_dump([(mergedT.rearrange("p a b -> p (a b)").bitcast(F32), 8192, [("mergedT", i) for i in range(2)])])
                    break

                P.fence()
                w2effb = sb(PH0, [2048])
                shift2b = sb(PH0 + 8192, [2048])
                h2row = [sb(PH0 + 16384 + i * 4096, [2048], BF16) for i in range(2)]
                g2b = sb(PH0 + 24576, [2048])
                gate1b = sb(PH0 + 32768, [2048])
                h2f = sb(PH0 + 40960, [16, 128])
                xt6 = [sb(PH0 + 49152 + i * 8192, [2048]) for i in range(2)]
                assert PH0 + 65536 == GM0 + 16384
                xn6 = sb(PH0 + 98304, [2048])
                junk6 = sb(PH0 + 106496, [2048], BF16)
                tmpm = sb(PH0 + 110592, [512])
                dead = [("h1own", t) for t in range(8)] + [("h1halo", t) for t in range(4)] + [("ybT", t) for t in range(8)] + \
                    [("yaT", t) for t in range(8)] + [("sg", i) for i in range(4)]
                sp_load(gate1b, modd[0, 2 * D:3 * D].partition_broadcast(128), ["gate1b"], cpool.nxt(), reads=modd_keys)
                sp_load(g2b, g2row[0].partition_broadcast(128), ["g2b"], cpool.nxt())
                sp_load(w2effb, modd[0, 4 * D:5 * D].partition_broadcast(128), ["w2effb"], cpool.nxt(), reads=modd_keys)
                sp_load(shift2b, modd[0, 3 * D:4 * D].partition_broadcast(128), ["shift2b"], cpool.nxt(), reads=modd_keys)
                dve(["w2effb", "g2b"], ["w2effb"], lambda e: e.scalar_tensor_tensor(
                    out=w2effb, in0=w2effb, scalar=1.0, in1=g2b, op0=ALU.add, op1=ALU.mult))
                wos = [w_get(u) for u in pl["wo"]]

                def p6_mm_pe(i):
                    b = i % 2
                    P.dma(SP, [], [("xt6", b)], lambda e, b=b, i=i: e.dma_start(
                        out=xt6[b], in_=xe[row0 + HALO + i * 128:row0 + HALO + (i + 1) * 128, :]), xsem[b])
                    for cu in range(4):
                        def emit(e, i=i, cu=cu):
                            return mm_acc(e, pbank(cu), [(mergedT[:, kc, i * 128:(i + 1) * 128], wos[cu][0][:, kc, :]) for kc in range(16)])
                        pe_group([wos[cu][1], ("mergedT", i // 4)], [pkey(cu)], emit)

                def p6_mm_dve(i):
                    b = i % 2
                    for cu in range(4):
                        dve([pkey(cu), "gate1b"], ["tmpm"], lambda e, cu=cu: e.tensor_tensor(
                            out=tmpm, in0=pbank(cu), in1=gate1b[:, cu * 512:(cu + 1) * 512], op=ALU.mult))
                        dve(["tmpm", ("xt6", b)], [("xt6", b)], lambda e, cu=cu, b=b: e.tensor_tensor(
                            out=xt6[b][:, cu * 512:(cu + 1) * 512], in0=xt6[b][:, cu * 512:(cu + 1) * 512], in1=tmpm, op=ALU.add))

                def p6_front(i):
                    b = i % 2
                    xk = ("xt6", b)
                    ssa = small[:, 32 + i:33 + i]
                    rsa = small[:, 48 + i:49 + i]
                    act_fn(junk6, xt6[b], AF.Square, [xk], [("ss2", i)], accum=ssa)
                    rms_rstd(ssa, rsa, ("ss2", i), ("rs2", i))
                    P.dma(SP, [xk], [("x1s", p * 8 + i)], lambda e, b=b, i=i: e.dma_start(
                        out=x1s[p * TP + i * 128:p * TP + (i + 1) * 128, :], in_=xt6[b]), osem[b])
                    dve([xk, ("rs2", i), "w2effb"], ["xn6"], lambda e, b=b, rsa=rsa: e.scalar_tensor_tensor(
                        out=xn6, in0=xt6[b], scalar=rsa, in1=w2effb, op0=ALU.mult, op1=ALU.mult))
                    dve(["xn6", "shift2b"], ["xn6"], lambda e: e.tensor_tensor(out=xn6, in0=xn6, in1=shift2b, op=ALU.add))
                    hb = i % 2
                    act_fn(h2row[hb], xn6, AF.Copy, ["xn6"], [("h2row", hb)])
                    P.dma(SP, [("h2row", hb)], [("h2d", p * 8 + i)], lambda e, hb=hb, i=i: e.dma_start(
                        out=h2d[p * TP + i * 128:p * TP + (i + 1) * 128, :], in_=h2row[hb]), hsem[hb])
                def p6_back(i):
                    b = i % 2
                    for q in range(4):
                        def emit(e, q=q):
                            ins = None
                            for k in range(4):
                                dc = q * 4 + k
                                ins = e.transpose(pbank(4 + q, 128, k * 128), xn6[:, dc * 128:(dc + 1) * 128], identF)
                            return ins
                        pe_group(["xn6", "identF"], [pkey(4 + q)], emit)
                        act_fn(h2f[:, q * 4:(q + 1) * 4, :], pbank(4 + q).rearrange("p (k t) -> p k t", k=4), AF.Copy,
                               [pkey(4 + q)], [("h2f", q)])
                    LB = 0

                    def emit(e):
                        return mm_acc(e, pbank(LB, 36), [(h2f[:, dc, :], wr[:, dc, :]) for dc in range(16)])
                    pe_group([("h2f", q) for q in range(4)] + ["wr"], [pkey(LB)], emit)
                    lg = rt[:, 0:36]
                    dve([pkey(LB)], ["rt"], lambda e: e.tensor_copy(out=lg, in_=pbank(LB, 36)))
                    gl = rt[:, 0:4]
                    el = rt[:, 4:36]
                    gmax = rt[:, 40:41]
                    ngmax = rt[:, 41:42]
                    gsum = rt[:, 42:43]
                    gw = rt[:, 43:44]
                    goh = rt[:, 44:48]
                    gex = rt[:, 48:52]
                    esel = rt[:, 56:64]
                    etmp = rt[:, 64:96]
                    m1 = rt[:, 96:97]
                    m2 = rt[:, 97:98]
                    oh1 = rt[:, 104:112]
                    oh2 = rt[:, 112:120]
                    msk = rt[:, 120:128]
                    dd = rt[:, 128:129]
                    w1_ = rt[:, 129:130]
                    w2_ = rt[:, 130:131]
                    ew = rt[:, 136:144]
                    gwo = rt[:, 144:148]
                    R = ["rt"]
                    dve(R, R, lambda e: e.tensor_reduce(out=gmax, in_=gl, axis=AX.X, op=ALU.max))
                    dve(R, R, lambda e: e.tensor_scalar(out=ngmax, in0=gmax, scalar1=-1.0, scalar2=None, op0=ALU.mult))
                    act_fn(gex, gl, AF.Exp, R, R, bias=ngmax, accum=gsum)
                    dve(R, R, lambda e: e.reciprocal(out=gw, in_=gsum))
                    dve(R, R, lambda e: e.tensor_scalar(out=goh, in0=gl, scalar1=gmax, scalar2=None, op0=ALU.is_equal))
                    dve(R, R, lambda e: e.tensor_scalar(out=esel, in0=el[:, 0:8], scalar1=goh[:, 0:1], scalar2=None, op0=ALU.mult))
                    for g in range(1, 4):
                        dve(R, R, lambda e, g=g: e.scalar_tensor_tensor(
                            out=esel, in0=el[:, g * 8:(g + 1) * 8], scalar=goh[:, g:g + 1], in1=esel, op0=ALU.mult, op1=ALU.add))
                    dve(R, R, lambda e: e.tensor_reduce(out=m1, in_=esel, axis=AX.X, op=ALU.max))
                    dve(R, R, lambda e: e.tensor_scalar(out=oh1, in0=esel, scalar1=m1, scalar2=None, op0=ALU.is_equal))
                    dve(R, R, lambda e: e.scalar_tensor_tensor(out=msk, in0=oh1, scalar=-1e30, in1=esel, op0=ALU.mult, op1=ALU.add))
                    dve(R, R, lambda e: e.tensor_reduce(out=m2, in_=msk, axis=AX.X, op=ALU.max))
                    dve(R, R, lambda e: e.tensor_scalar(out=oh2, in0=msk, scalar1=m2, scalar2=None, op0=ALU.is_equal))
                    dve(R, R, lambda e: e.tensor_tensor(out=dd, in0=m2, in1=m1, op=ALU.subtract))
                    act_fn(dd, dd, AF.Exp, R, R)
                    dve(R, R, lambda e: e.tensor_scalar(out=w1_, in0=dd, scalar1=1.0, scalar2=None, op0=ALU.add))
                    dve(R, R, lambda e: e.reciprocal(out=w1_, in_=w1_))
                    dve(R, R, lambda e: e.tensor_tensor(out=w2_, in0=dd, in1=w1_, op=ALU.mult))
                    dve(R, R, lambda e: e.tensor_scalar(out=ew, in0=oh1, scalar1=w1_, scalar2=None, op0=ALU.mult))
                    dve(R, R, lambda e: e.scalar_tensor_tensor(out=ew, in0=oh2, scalar=w2_, in1=ew, op0=ALU.mult, op1=ALU.add))
                    dve(R, R, lambda e: e.tensor_scalar(out=gwo, in0=goh, scalar1=gw, scalar2=None, op0=ALU.mult))
                    ti = p * 8 + i
                    for g in range(4):
                        dve(R, [("sel", ti)], lambda e, g=g, ti=ti: e.tensor_scalar(
                            out=sel0[:, ti, g * 8:(g + 1) * 8], in0=oh1, scalar1=goh[:, g:g + 1], scalar2=None, op0=ALU.mult))
                        dve(R, [("sel", ti)], lambda e, g=g, ti=ti: e.tensor_scalar(
                            out=sel1[:, ti, g * 8:(g + 1) * 8], in0=oh2, scalar1=goh[:, g:g + 1], scalar2=None, op0=ALU.mult))
                    dve(R, [("wkk", ti)], lambda e, ti=ti: e.tensor_tensor(out=wsl[:, ti, 0:1], in0=w1_, in1=gw, op=ALU.mult))
                    dve(R, [("wkk", ti)], lambda e, ti=ti: e.tensor_tensor(out=wsl[:, ti, 1:2], in0=w2_, in1=gw, op=ALU.mult))

                p6_mm_pe(0)
                p6_mm_dve(0)
                for i in range(8):
                    if i + 1 < 8:
                        p6_mm_pe(i + 1)
                    p6_front(i)
                    if i + 1 < 8:
                        p6_mm_dve(i + 1)
                    p6_back(i)
                for u in pl["wo"]:
                    w_rel(u)
                if debug is not None and debug["stage"] == "p6":
                    break


            class MW:
                units = []
                released = []
                nloaded = 0

            def m_view(slot, a, b):
                return sb(RING0 + slot * SLOT_B, [a, b], BF16)

            def m_pump(limit=None):
                while MW.nloaded < len(MW.units):
                    u = MW.nloaded
                    if limit is not None and u >= limit:
                        break
                    if u >= MSLOT and not MW.released[u - MSLOT]:
                        break
                    src, a, b = MW.units[u]
                    slot = u % MSLOT
                    dst = m_view(slot, a, b)
                    wk_ = [("mring", slot)]
                    if u < NSLOT:
                        wk_.append(("ring", slot))
                    P.dma(POOL, [], wk_, lambda e, dst=dst, src=src: e.dma_start(out=dst, in_=src), mr_sem[slot])
                    MW.nloaded += 1

            def m_get(u):
                m_pump()
                assert u < MW.nloaded
                src, a, b = MW.units[u]
                return m_view(u % MSLOT, a, b), ("mring", u % MSLOT)

            def m_rel(u):
                MW.released[u] = True

            for e_ in range(NE):
                wpat = "(p c) n -> p c n" if CONTIG else "(c p) n -> p c n"
                for src, a, b in ((w1[e_].rearrange(wpat, p=128), 16, 512),
                                  (w3[e_].rearrange(wpat, p=128), 16, 512),
                                  (w2[e_].rearrange("(c p) n -> p c n", p=128), 4, 2048)):
                    MW.units.append((src, a, b))
                    MW.released.append(False)

            if debug is None or debug["stage"] in ("meta", "moe"):
                m_pump(limit=NSLOT)

            if debug is None or debug["stage"] in ("meta", "moe"):
                P.nofloor.discard("pool")
                P.fence()
                o = PH0 + (MSLOT - NSLOT) * SLOT_B
                selA = sb(o, [16, 32]); o += 2048
                cum = sb(o, [16, 32]); o += 2048
                pos = sb(o, [16, 32]); o += 2048
                tmpd = sb(o, [16, 32]); o += 2048
                destf = sb(o, [32]); o += 128
                desti = sb(o, [32], I32); o += 128
                cntf = sb(o, [32]); o += 128
                cnti = sb(o, [32], I32); o += 128
                fillt = sb(o, [2048], I32); o += 8192
                Utri = sb(o, [128]); o += 512
                OnesM = sb(o, [128]); o += 512
                META_END = o
                sp_load(Utri, Utri_d, ["Utri"], cpool.nxt())
                sp_load(OnesM, Ones_d, ["OnesM"], cpool.nxt())
                selk = [("sel", t) for t in range(16)]
                dve(selk, ["selA"], lambda e: e.tensor_tensor(out=selA, in0=sel0, in1=sel1, op=ALU.add))
                dve([], ["cum"], lambda e: e.memset(cum[:, 0, :], 0.0))
                for t in range(1, 16):
                    dve(["cum", "selA"], ["cum"], lambda e, t=t: e.tensor_tensor(
                        out=cum[:, t, :], in0=cum[:, t - 1, :], in1=selA[:, t - 1, :], op=ALU.add))
                def emit(e):
                    ins = None
                    for t in range(16):
                        e.matmul(pbank(0, 32, t * 32), OnesM, cum[:, t, :], start=True, stop=False)
                        ins = e.matmul(pbank(0, 32, t * 32), Utri, selA[:, t, :], start=False, stop=True)
                    return ins
                pe_group(["cum", "selA", "OnesM", "Utri"], [pkey(0)], emit)
                dve([pkey(0)], ["pos"], lambda e: e.tensor_copy(out=pos, in_=pbank(0).rearrange("p (t x) -> p t x", t=16)))
                dve(["cum", "selA"], ["tmpd"], lambda e: e.tensor_tensor(out=tmpd[:, 0, :], in0=cum[:, 15, :], in1=selA[:, 15, :], op=ALU.add))
                pe_group(["tmpd", "OnesM"], [pkey(1)], lambda e: e.matmul(pbank(1, 32), OnesM, tmpd[:, 0, :], start=True, stop=True))
                dve([pkey(1)], ["cntf"], lambda e: e.tensor_copy(out=cntf, in_=pbank(1, 32)))
                dve(["cntf"], ["cnti"], lambda e: e.tensor_copy(out=cnti, in_=cntf))
                P.dma(SP, ["cnti"], ["cntd"], lambda e: e.dma_start(out=cntd, in_=cnti[0:1, :]), cpool.nxt())
                dve(["pos", "ecap"], ["pos"], lambda e: e.tensor_tensor(
                    out=pos, in0=pos, in1=ecap.unsqueeze(1).to_broadcast([128, 16, 32]), op=ALU.add))
                for k, selx in enumerate((sel0, sel1)):
                    dve(["pos"] + selk, ["tmpd"], lambda e, selx=selx: e.tensor_tensor(out=tmpd, in0=pos, in1=selx, op=ALU.mult))
                    dve(["tmpd"], ["destf"], lambda e, k=k: e.tensor_reduce(
                        out=destf.rearrange("p (t k) -> p t k", k=2)[:, :, k], in_=tmpd, axis=AX.X, op=ALU.add))
                dve(["destf"], ["desti"], lambda e: e.tensor_copy(out=desti, in_=destf))
                sp_load(fillt, fillv_d, ["fillt"], cpool.nxt())
                P.dma(SP, ["fillt"], ["table"], lambda e: e.dma_start(
                    out=table.rearrange("(p r) c -> p (r c)", p=128), in_=fillt), cpool.nxt())
                for t in range(16):
                    for k in range(2):
                        P.dma(POOL, ["desti", "vals", "table"], [("tabw", t, k)], lambda e, t=t, k=k: e.indirect_dma_start(
                            out=table, out_offset=bass.IndirectOffsetOnAxis(ap=desti[:, t * 2 + k:t * 2 + k + 1], axis=0),
                            in_=vals[:, (t * 2 + k) * 4:(t * 2 + k) * 4 + 4], in_offset=None,
                            oob_is_err=False), tpool.nxt())
                tabkeys = [("tabw", t, k) for t in range(16) for k in range(2)]
                if debug is not None and debug["stage"] == "meta":
                    dbg_dump([(destf, 32, ["destf"]), (cntf, 32, ["cntf"]), (wsl.rearrange("p a b -> p (a b)"), 32, [("wkk", t) for t in range(16)]),
                              (sel0.rearrange("p a b -> p (a b)"), 512, selk), (sel1.rearrange("p a b -> p (a b)"), 512, selk)])

            if debug is None or debug["stage"] in ("moe",):
                P.fence()
                o = PH0 + (MSLOT - NSLOT) * SLOT_B
                NXG = 5
                idxt = [sb(o + i * 16, [4], I32) for i in range(NXG)]; o += 128
                xg = [sb(o + i * 4096, [2048], BF16) for i in range(NXG)]; o += NXG * 4096
                xgT = sb(o, [16, 128], BF16); o += 4096
                sAm = sb(o, [512]); o += 2048
                actm = sb(o, [512], BF16); o += 1024
                actTm = sb(o, [4, 128], BF16); o += 1024
                NY = 4
                Ysb = [sb(o + i * 8192, [2048]) for i in range(NY)]; o += NY * 8192
                MOE_END = o
                assert o <= PH_END

                regs = {}
                if target is not None:
                    regs[target] = handle.alloc_register("cnt_" + target)
                tcount = [0]

                def gather_ops(ei, t, xb):
                    row0 = ei * CAP + t * 128
                    P.dma(SP, tabkeys + ["table"], [("idxt", xb)], lambda e: e.dma_start(out=idxt[xb], in_=table[row0:row0 + 128, :]), ixsem[xb])
                    P.dma(POOL, [("idxt", xb)] + [("h2d", i) for i in range(16)], [("xg", xb)], lambda e: e.indirect_dma_start(
                        out=xg[xb], out_offset=None, in_=h2d, in_offset=bass.IndirectOffsetOnAxis(ap=idxt[xb][:, 0:1], axis=0),
                        oob_is_err=False), gasem[xb])

                NSTAT = int(os.environ.get('K_NSTAT', '2'))

                def tile_ops(ei, t, wa, wak, wb, wbk, wc, wck):
                    n = tcount[0]
                    tcount[0] += 1
                    b = n % NY
                    if t < NSTAT:
                        xb = (ei % 2) * NSTAT + t
                    else:
                        xb = NXG - 1
                        gather_ops(ei, t, xb)
                    tb0 = PS[0][:, 0:512].bitcast(BF16)
                    tb1 = PS[0][:, 512:1024].bitcast(BF16)
                    for hb_, tb in enumerate((tb0, tb1)):
                        def emit(e, hb_=hb_, tb=tb):
                            ins = None
                            for k in range(8):
                                dc = hb_ * 8 + k
                                src_cols = xg[xb][:, dc:2048:16] if CONTIG else xg[xb][:, dc * 128:(dc + 1) * 128]
                                ins = e.transpose(tb[:, k * 128:(k + 1) * 128], src_cols, identB)
                            return ins
                        pe_group([("xg", xb), "identB"], [pkey(hb_)], emit)
                    act_fn(xgT[:, 0:8, :], tb0.rearrange("p (k t) -> p k t", k=8), AF.Copy, [pkey(0)], [("xgT", 0)])
                    dve([pkey(1)], [("xgT", 1)], lambda e: e.tensor_copy(out=xgT[:, 8:16, :], in_=tb1.rearrange("p (k t) -> p k t", k=8)))
                    pe_group([wak, ("xgT", 0), ("xgT", 1)], [pkey(2)], lambda e: mm_acc(e, pbank(2), [(xgT[:, dc, :], wa[:, dc, :]) for dc in range(16)]))
                    pe_group([wbk, ("xgT", 0), ("xgT", 1)], [pkey(3)], lambda e: mm_acc(e, pbank(3), [(xgT[:, dc, :], wb[:, dc, :]) for dc in range(16)]))
                    act_fn(sAm, pbank(2), AF.Silu, [pkey(2)], ["sAm"])
                    dve(["sAm", pkey(3)], ["actm"], lambda e: e.tensor_tensor(out=actm, in0=sAm, in1=pbank(3), op=ALU.mult))

                    def emit(e):
                        ins = None
                        for k in range(4):
                            ins = e.transpose(tb0[:, k * 128:(k + 1) * 128], actm[:, k * 128:(k + 1) * 128], identB)
                        return ins
                    pe_group(["actm", "identB"], [pkey(0)], emit)
                    act_fn(actTm, tb0[:, 0:512].rearrange("p (k t) -> p k t", k=4), AF.Copy, [pkey(0)], ["actTm"])
                    for nb in range(4):
                        pe_group([wck, "actTm"], [pkey(4 + nb)], lambda e, nb=nb: mm_acc(
                            e, pbank(4 + nb), [(actTm[:, fc, :], wc[:, fc, nb * 512:(nb + 1) * 512]) for fc in range(4)]))
                    act_fn(Ysb[b][:, 0:1024], PS[2][:, :], AF.Copy, [pkey(4), pkey(5)], [("Ysb", b, 0)])
                    dve([pkey(6), pkey(7)], [("Ysb", b, 1)], lambda e: e.tensor_copy(out=Ysb[b][:, 1024:2048], in_=PS[3][:, :]))
                    for hf in range(2):
                        P.dma(POOL, [("Ysb", b, hf), ("idxt", xb)], [("Ybuf", n, hf)], lambda e, hf=hf: e.indirect_dma_start(
                            out=Ybuf4, out_offset=bass.IndirectOffsetOnAxis(ap=idxt[xb][:, 1 + hf:2 + hf], axis=0),
                            in_=Ysb[b][:, hf * 1024:(hf + 1) * 1024], in_offset=None,
                            oob_is_err=False), scsem[b * 2 + hf])

                MAXT = TOKC // 128

                GRP = 4

                def tiles_from(ei, t, tend, rv, ws):
                    if t >= tend:
                        return
                    with P.guard(lambda h: rv > t * 128):
                        tile_ops(ei, t, *ws)
                        tiles_from(ei, t + 1, tend, rv, ws)

                for t_ in range(NSTAT):
                    gather_ops(0, t_, t_)
                for ei in range(NE):
                    if ei + 1 < NE:
                        for t_ in range(NSTAT):
                            gather_ops(ei + 1, t_, ((ei + 1) % 2) * NSTAT + t_)
                    wa, wak = m_get(3 * ei)
                    wb, wbk = m_get(3 * ei + 1)
                    wc, wck = m_get(3 * ei + 2)
                    rv = None
                    for eng in P.engines:
                        r = P.raw(eng, ["cntd"], lambda h, ei=ei: h.reg_load(regs[target], cntd[0:1, ei:ei + 1]))
                    if target is not None:
                        rv = handle.snap(regs[target])
                    for g0 in range(0, MAXT, GRP):
                        tiles_from(ei, g0, g0 + GRP, rv, (wa, wak, wb, wbk, wc, wck))
                    m_rel(3 * ei)
                    m_rel(3 * ei + 1)
                    m_rel(3 * ei + 2)
                    m_pump()

                P.fence()
                o = RING0
                gate2b = sb(o, [2048]); o += 8192
                fgb = sb(o, [2048]); o += 8192
                NF = 3
                xt8 = [sb(o + i * 8192, [2048]) for i in range(NF)]; o += NF * 8192
                y0t = [sb(o + i * 8192, [2048]) for i in range(NF)]; o += NF * 8192
                y1t = [sb(o + i * 8192, [2048]) for i in range(NF)]; o += NF * 8192
                junk8 = sb(o, [2048], BF16); o += 4096
                assert o <= PH_END
                ybkeys = [("Ybuf", n, hf) for n in range(tcount[0]) for hf in range(2)]
                sp_load(gate2b, modd[0, 5 * D:6 * D].partition_broadcast(128), ["gate2b"], cpool.nxt(), reads=modd_keys)
                sp_load(fgb, fg[0].partition_broadcast(128), ["fgb"], cpool.nxt())

                def fin_loads(i):
                    b = i % NF
                    P.dma(SP, [("x1s", i)], [("xt8", b)], lambda e: e.dma_start(
                        out=xt8[b], in_=x1s[i * 128:(i + 1) * 128, :]), fsem[b])
                    P.dma(SP, ybkeys, [("y0t", b)], lambda e: e.dma_start(
                        out=y0t[b], in_=Ybuf[i * 128:(i + 1) * 128, :]), fsem[3 + b])
                    P.dma(SP, ybkeys, [("y1t", b)], lambda e: e.dma_start(
                        out=y1t[b], in_=Ybuf[TOKC + i * 128:TOKC + (i + 1) * 128, :]), fsem[6 + b])

                fin_loads(0)
                fin_loads(1)
                for i in range(16):
                    b = i % NF
                    xk = ("xt8", b)
                    if i + 2 < 16:
                        fin_loads(i + 2)
                    act_fn(y0t[b], y0t[b], AF.Copy, [("y0t", b), ("wkk", i)], [("y0t", b)], scale=wsl[:, i, 0:1])
                    dve([("y0t", b), ("y1t", b), ("wkk", i)], [("y0t", b)], lambda e, b=b, i=i: e.scalar_tensor_tensor(
                        out=y0t[b], in0=y1t[b], scalar=wsl[:, i, 1:2], in1=y0t[b], op0=ALU.mult, op1=ALU.add))
                    dve([("y0t", b), "gate2b"], [("y0t", b)], lambda e, b=b: e.tensor_tensor(out=y0t[b], in0=y0t[b], in1=gate2b, op=ALU.mult))
                    dve([("y0t", b), xk], [xk], lambda e, b=b: e.tensor_tensor(out=xt8[b], in0=xt8[b], in1=y0t[b], op=ALU.add))
                    ssa = small[:, 128 + i:129 + i]
                    rsa = small[:, 144 + i:145 + i]
                    act_fn(junk8, xt8[b], AF.Square, [xk], [("ss8", i)], accum=ssa)
                    rms_rstd(ssa, rsa, ("ss8", i), ("rs8", i))
                    dve([xk, ("rs8", i), "fgb"], [xk], lambda e, b=b, rsa=rsa: e.scalar_tensor_tensor(
                        out=xt8[b], in0=xt8[b], scalar=rsa, in1=fgb, op0=ALU.mult, op1=ALU.mult))
                    P.dma(SP, [xk], [("y", i)], lambda e, b=b, i=i: e.dma_start(
                        out=y[i * 128:(i + 1) * 128, :], in_=xt8[b]), fsem[9 + b])

            outkeys = [("y", i) for i in range(16)] if debug is None else [k for k in P.state.keys() if isinstance(k, tuple) and k[0] in ("dbg", "y")]
            P.final_wait(SP, outkeys)

        with nc.Block() as block:
            @block.tensor
            def _(e):
                program("pe", e)

            @block.scalar
            def _(e):
                program("act", e)

            @block.vector
            def _(e):
                program("dve", e)

            @block.gpsimd
            def _(e):
                program("pool", e)

            @block.sync
            def _(e):
                program("sp", e)
    return nc


_NC_CACHE = {}


def _vals_const():
    v = np.zeros((128, 16, 2, 4), np.int32)
    p = np.arange(128)[:, None]
    t = np.arange(16)[None, :]
    tok = t * 128 + p
    for k in range(2):
        v[:, :, k, 0] = tok
        v[:, :, k, 1] = 2 * (k * TOKC + tok)
        v[:, :, k, 2] = 2 * (k * TOKC + tok) + 1
    return np.ascontiguousarray(v.reshape(128, 128))


def _fill_const():
    r = np.arange(512)
    row = np.zeros((512, 4), np.int32)
    row[:, 1] = 4 * TOKC + (r % 128)
    row[:, 2] = 4 * TOKC + 128 + (r % 128)
    return np.ascontiguousarray(np.broadcast_to(row.reshape(1, 2048), (128, 2048)))


def _host_inputs(inputs):
    f = np.float32
    x = np.asarray(inputs["x"], f)
    c = np.asarray(inputs["c"], f)
    L = 0
    tab = np.asarray(inputs["rel_bias"], f)[L]
    ext = np.concatenate([tab, np.repeat(tab[:, -1:], 400, axis=1)], axis=1)
    k = np.arange(128)[:, None]
    q = np.arange(128)[None, :]
    bt = np.empty((128, 16, 3, 128), f)
    for dl in range(3):
        idx = 256 + dl * 128 + q - k
        bt[:, :, dl, :] = np.transpose(ext[:, idx], (1, 0, 2))
    bt[64:, :, 0, :64] = NEG
    wr = np.concatenate([np.asarray(inputs["w_group"], f)[L]] +
                        [np.asarray(inputs["w_expert"], f)[L, g] for g in range(4)], axis=1)
    shared = {
        "ident": np.eye(128, dtype=f),
        "ada_w": np.ascontiguousarray(np.asarray(inputs["ada_w"], f)[L]),
        "ada_b": np.ascontiguousarray(np.asarray(inputs["ada_b"], f)[L][None, :]),
        "g1T": np.ascontiguousarray(np.asarray(inputs["norm1_g"], f)[L].reshape(16, 128).T),
        "g2T": np.ascontiguousarray(np.asarray(inputs["norm2_g"], f)[L].reshape(16, 128).T),
        "lnG": np.ascontiguousarray(np.asarray(inputs["gmlp_ln_g"], f)[L][None, :]),
        "lnB": np.ascontiguousarray(np.asarray(inputs["gmlp_ln_b"], f)[L][None, :]),
        "bS": np.ascontiguousarray(np.asarray(inputs["gmlp_b_s"], f)[L].reshape(1, 1024)),
        "wS": np.ascontiguousarray(np.asarray(inputs["gmlp_w_s"], f)[L]),
        "biasT": np.ascontiguousarray(bt.reshape(128, 16 * 384)),
        "tabc": np.ascontiguousarray(tab[:, 512][None, :]),
        "w_in": np.ascontiguousarray(np.asarray(inputs["w_in"], f)[L]),
        "wbrA": np.ascontiguousarray(np.asarray(inputs["w_branch_a"], f)[L]),
        "wbrB": np.ascontiguousarray(np.asarray(inputs["w_branch_b"], f)[L]),
        "wout": np.ascontiguousarray(np.asarray(inputs["w_out"], f)[L]),
        "wr": np.ascontiguousarray(wr),
        "w1": np.ascontiguousarray(np.asarray(inputs["w1"], f)[L].reshape(NE, D, 512)),
        "w3": np.ascontiguousarray(np.asarray(inputs["w3"], f)[L].reshape(NE, D, 512)),
        "w2": np.ascontiguousarray(np.asarray(inputs["w2"], f)[L].reshape(NE, 512, D)),
        "fg": np.ascontiguousarray(np.asarray(inputs["final_g"], f)[None, :]),
        "g2row": np.ascontiguousarray(np.asarray(inputs["norm2_g"], f)[L][None, :]),
        "g1row": np.ascontiguousarray(np.asarray(inputs["norm1_g"], f)[L][None, :]),
        "Utri": np.triu(np.ones((128, 128), f), 1),
        "OnesM": np.ones((128, 128), f),
        "ecap": np.ascontiguousarray(np.broadcast_to((np.arange(NE) * CAP).astype(f)[None, :], (128, NE))),
        "vals": _vals_const(),
        "fillv": _fill_const(),
    }
    in_maps = []
    for core in range(8):
        b = core // 4
        s0 = (core % 4) * TOKC
        xe = np.zeros((TOKC + HALO, D), f)
        if s0 == 0:
            xe[HALO:] = x[b, 0:TOKC]
        else:
            xe[:] = x[b, s0 - HALO:s0 + TOKC]
        flags = np.ones((128, 2), f)
        if s0 == 0:
            flags[:, 0] = 0.0
        m = dict(shared)
        m["xe"] = xe
        m["cT"] = np.ascontiguousarray(c[b].reshape(16, 128).T)
        m["flags"] = flags
        in_maps.append(m)
    return in_maps


def kernel(**inputs):
    if "nc" not in _NC_CACHE:
        _NC_CACHE["nc"] = build()
    nc = _NC_CACHE["nc"]
    in_maps = _host_inputs(inputs)
    res = run_bass_kernel_spmd(nc, in_maps, core_ids=list(range(8)))
    out = np.empty((2, 8192, D), np.float32)
    for core in range(8):
        b = core // 4
        s0 = (core % 4) * TOKC
        out[b, s0:s0 + TOKC] = res.results[core]["y"]
    return out
```

```python
import contextlib
import os
import numpy as np
import concourse.bass as bass
import concourse.mybir as mybir
from concourse.bass_utils import run_bass_kernel_spmd

F32 = mybir.dt.float32
BF16 = mybir.dt.bfloat16
I32 = mybir.dt.int32
AF = mybir.ActivationFunctionType
ALU = mybir.AluOpType
AX = mybir.AxisListType

D = 2048
TOKC = 2048
TP = 1024
HALO = 512
NPASS = 2
EPS = 1e-6
NSLOT = 5
SLOT_B = 16384
NEG = -30000.0
EVAC_SPLIT = os.environ.get('K_EVAC', '0') == '1'
ADA_DVE = os.environ.get('K_ADA', '1') == '1'
PREFETCH = os.environ.get('K_PRE', '1') == '1'
CONTIG = os.environ.get('K_CONTIG', '1') == '1'
NE = 32
CAP = 2048
MSLOT = 8
BIGIDX = 1 << 24


class Sem:
    def __init__(self, h, shared=False):
        self.h = h
        self.n = 0
        self.shared = shared


class Eng:
    def __init__(self, name, sem, selfwait=True):
        self.name = name
        self.sem = sem
        self.prog = []
        self.waited = {}
        self.selfwait = selfwait


class Prog:
    def __init__(self, target, handle):
        self.state = {}
        self.floor = {}
        self.nofloor = set()
        self.target = target
        self.handle = handle
        self.inc_log = []
        self.alias = {}
        self.engines = []

    def fence(self):
        for k, st in self.state.items():
            if isinstance(k, tuple) and k[0] in ("ring", "mring"):
                continue
            evs = ([st[0]] if st[0] else []) + list(st[1].items())
            for s, v in evs:
                if s.shared:
                    v = s.n
                if self.floor.get(s, 0) < v:
                    self.floor[s] = v

    def _expand(self, keys):
        out = []
        for k in keys:
            out.extend(self.alias.get(k, (k,)) if isinstance(k, tuple) else (k,))
        return out

    def _deps(self, reads, writes, eng=None):
        reads = self._expand(reads)
        writes = self._expand(writes)
        w = {}

        def add(ev):
            if ev is None:
                return
            s, v = ev
            if s.shared:
                v = s.n
            if w.get(s, 0) < v:
                w[s] = v

        if eng is not None and eng.name not in self.nofloor:
            for s, v in self.floor.items():
                add((s, v))

        for k in reads:
            st = self.state.get(k)
            if st:
                add(st[0])
        for k in writes:
            st = self.state.get(k)
            if st:
                add(st[0])
                for s, v in st[1].items():
                    add((s, v))
        return w

    def _commit(self, reads, writes, ev):
        reads = self._expand(reads)
        writes = self._expand(writes)
        s, v = ev
        for k in reads:
            st = self.state.setdefault(k, [None, {}])
            if st[1].get(s, 0) < v:
                st[1][s] = v
        for k in writes:
            self.state[k] = [ev, {}]

    def op(self, eng, reads, writes, emit):
        w = self._deps(reads, writes, eng)
        wl = []
        for s, v in w.items():
            if s is eng.sem and not eng.selfwait:
                continue
            if eng.waited.get(s, 0) < v:
                eng.waited[s] = v
                wl.append((s, v))
        self.inc_log.append((eng.name, eng.sem, 1))
        eng.sem.n += 1
        ev = (eng.sem, eng.sem.n)
        sem = eng.sem

        if eng.name == self.target:
            e = self.handle
            for s, v in wl:
                e.wait_ge(s.h, v)
            emit(e).then_inc(sem.h, 1)
        self._commit(reads, writes, ev)

    def dma(self, eng, reads, writes, emit, dsem):
        w = self._deps(reads, writes, eng)
        if dsem.n > 0 and w.get(dsem, 0) < dsem.n:
            w[dsem] = dsem.n
        wl = []
        for s, v in w.items():
            if eng.waited.get(s, 0) < v:
                eng.waited[s] = v
                wl.append((s, v))
        self.inc_log.append((eng.name, dsem, 16))
        dsem.n += 16
        ev = (dsem, dsem.n)

        if eng.name == self.target:
            e = self.handle
            for s, v in wl:
                e.wait_ge(s.h, v)
            emit(e).then_inc(dsem.h, 16)
        self._commit(reads, writes, ev)

    def raw(self, eng, reads, emit):
        w = self._deps(reads, [], eng)
        wl = []
        for s, v in w.items():
            if s is eng.sem and not eng.selfwait:
                continue
            if eng.waited.get(s, 0) < v:
                eng.waited[s] = v
                wl.append((s, v))
        if eng.name == self.target:
            for s, v in wl:
                self.handle.wait_ge(s.h, v)
            return emit(self.handle)
        return None

    @contextlib.contextmanager
    def guard(self, cond_fn):
        snap_waited = {e.name: dict(e.waited) for e in self.engines}
        log0 = len(self.inc_log)
        before = {}
        ctx = None
        if self.target is not None:
            ctx = self.handle.If(cond_fn(self.handle))
            ctx.__enter__()
        sems_seen = {}
        n_at_entry = lambda sem: sems_seen.setdefault(id(sem), sem.n)
        entry_vals = {}
        class _Peek:
            pass
        all_sems = set()
        for (_, sem, _) in self.inc_log:
            all_sems.add(sem)
        entry_vals = {sem: sem.n for sem in all_sems}
        try:
            yield
        finally:
            if ctx is not None:
                ctx.__exit__(None, None, None)
                comp = {}
                order = []
                for (en, sem, amt) in self.inc_log[log0:]:
                    if en == self.target:
                        if sem not in comp:
                            comp[sem] = 0
                            order.append(sem)
                        comp[sem] += amt
                ectx = self.handle.Else()
                ectx.__enter__()
                for sem in order:
                    v0 = entry_vals.get(sem, 0)
                    if v0 > 0:
                        self.handle.wait_ge(sem.h, v0)
                    self.handle.sem_inc(sem.h, comp[sem])
                ectx.__exit__(None, None, None)
            for e in self.engines:
                e.waited = snap_waited[e.name]

    def final_wait(self, eng, keys):
        w = self._deps(keys, keys)
        wl = list(w.items())

        if eng.name == self.target:
            for s, v in wl:
                self.handle.wait_ge(s.h, v)


def build(debug=None):
    nc = bass.Bass("TRN2", target_bir_lowering=False)

    def din(name, shape):
        return nc.dram_tensor(name, list(shape), F32, kind="ExternalInput").ap()

    xe = din("xe", [TOKC + HALO, D])
    cT = din("cT", [128, 16])
    flags_d = din("flags", [128, 2])
    ident_d = din("ident", [128, 128])
    ada_w = din("ada_w", [D, 6 * D])
    ada_b = din("ada_b", [1, 6 * D])
    g1T_d = din("g1T", [128, 16])
    g2T_d = din("g2T", [128, 16])
    lnG = din("lnG", [1, 1024])
    lnB = din("lnB", [1, 1024])
    bS = din("bS", [1, 1024])
    wS = din("wS", [8, 128, 128])
    biasT = din("biasT", [128, 16 * 384])
    tabc_d = din("tabc", [1, 16])
    w_in = din("w_in", [D, 9216])
    wbrA = din("wbrA", [1024, D])
    wbrB = din("wbrB", [1024, D])
    wout = din("wout", [D, D])
    wr_d = din("wr", [D, 36])
    w1 = din("w1", [NE, D, 512])
    w3 = din("w3", [NE, D, 512])
    w2 = din("w2", [NE, 512, D])
    fg = din("fg", [1, D])
    g2row = din("g2row", [1, D])
    g1row = din("g1row", [1, D])
    Utri_d = din("Utri", [128, 128])
    Ones_d = din("OnesM", [128, 128])
    ecap_d = din("ecap", [128, 32])
    vals_d = nc.dram_tensor("vals", [128, 128], I32, kind="ExternalInput").ap()
    fillv_d = nc.dram_tensor("fillv", [128, 2048], I32, kind="ExternalInput").ap()
    h2d = nc.dram_tensor("h2d", [TOKC, D], BF16, kind="Internal").ap()
    table = nc.dram_tensor("table", [NE * CAP, 4], I32, kind="Internal").ap()
    cntd = nc.dram_tensor("cntd", [1, NE], I32, kind="Internal").ap()
    Ybuf4 = nc.dram_tensor("Ybuf", [4 * TOKC + 256, D // 2], F32, kind="Internal").ap()
    Ybuf = Ybuf4.rearrange("(r h) c -> r (h c)", h=2)
    y = nc.dram_tensor("y", [TOKC, D], F32, kind="ExternalOutput").ap()
    x1s = nc.dram_tensor("x1s", [TOKC, D], F32, kind="Internal").ap()
    modd = nc.dram_tensor("modd", [1, 6 * D], F32, kind="Internal").ap()
    dbg_out = None
    if debug is not None:
        dbg_out = nc.dram_tensor("dbg", [128, debug["words"]], F32, kind="ExternalOutput").ap()

    es = contextlib.ExitStack()
    with es:
        es.enter_context(nc.allow_low_precision("bf16 matmul operands by design"))
        ARENA_W = 53200
        arena = es.enter_context(nc.sbuf_tensor("arena", [128, ARENA_W], F32))
        PS = [es.enter_context(nc.psum_tensor(f"ps{i}", [128, 1024], F32)) for i in range(4)]

        SEMH = {}
        for _n in (["s_pe", "s_act", "s_dve", "s_pool", "s_sp", "s_const", "s_misc"] + [f"s_ring{i}" for i in range(NSLOT)] + [f"s_ringb{i}" for i in range(NSLOT)] + [f"s_adb{i}" for i in range(3)] +
                   [f"s_x{i}" for i in range(2)] + [f"s_o{i}" for i in range(2)] + [f"s_ad{i}" for i in range(3)] +
                   [f"s_mr{i}" for i in range(MSLOT)] + [f"s_ix{i}" for i in range(5)] + [f"s_ga{i}" for i in range(5)] +
                   [f"s_sc{i}" for i in range(8)] + [f"s_h{i}" for i in range(2)] + [f"s_y{i}" for i in range(4)] + ["s_tab"] +
                   [f"s_cp{i}" for i in range(8)] + [f"s_f{i}" for i in range(12)] + [f"s_tp{i}" for i in range(4)] + [f"s_ab{i}" for i in range(2)] + [f"s_md{i}" for i in range(2)]):
            SEMH[_n] = es.enter_context(nc.semaphore(_n))

        def program(target, handle):
            def newsem(name):
                return Sem(SEMH[name])

            PE = Eng("pe", newsem("s_pe"), selfwait=False)
            ACT = Eng("act", newsem("s_act"))
            DVE = Eng("dve", newsem("s_dve"))
            POOL = Eng("pool", newsem("s_pool"))
            SP = Eng("sp", newsem("s_sp"))
            ring_sem = [newsem(f"s_ring{i}") for i in range(NSLOT)]
            ring_semb = [newsem(f"s_ringb{i}") for i in range(NSLOT)]
            adsemb = [newsem(f"s_adb{i}") for i in range(3)]
            xsem = [newsem(f"s_x{i}") for i in range(2)]
            osem = [newsem(f"s_o{i}") for i in range(2)]
            csem = newsem("s_const")
            csem.shared = True
            msem = newsem("s_misc")
            msem.shared = True
            adsem = [newsem(f"s_ad{i}") for i in range(3)]
            mr_sem = [newsem(f"s_mr{i}") for i in range(MSLOT)]
            ixsem = [newsem(f"s_ix{i}") for i in range(5)]
            gasem = [newsem(f"s_ga{i}") for i in range(5)]
            scsem = [newsem(f"s_sc{i}") for i in range(8)]

            class SemPool:
                def __init__(self, sems):
                    self.sems = sems
                    self.i = 0

                def nxt(self):
                    sm = self.sems[self.i % len(self.sems)]
                    self.i += 1
                    return sm

            cpool = SemPool([newsem(f"s_cp{i}") for i in range(8)])
            tpool = SemPool([newsem(f"s_tp{i}") for i in range(4)])
            absem = [newsem(f"s_ab{i}") for i in range(2)]
            fsem = [newsem(f"s_f{i}") for i in range(12)]
            mdsem = [newsem(f"s_md{i}") for i in range(2)]
            hsem = [newsem(f"s_h{i}") for i in range(2)]
            ysem = [newsem(f"s_y{i}") for i in range(4)]
            tabsem = newsem("s_tab")
            tabsem.shared = True
            P = Prog(target, handle)
            P.nofloor.add("pool")
            P.engines = [PE, ACT, DVE, POOL, SP]

            def sb(off, shape, dt=F32, parts=(0, 128)):
                esz = 4 if dt in (F32, I32) else 2
                n = int(np.prod(shape))
                nbytes = n * esz
                assert off % 4 == 0 and nbytes % 4 == 0
                assert off + nbytes <= ARENA_W * 4, (off, nbytes)
                ap = arena[parts[0]:parts[1], off // 4:(off + nbytes) // 4]
                if dt != F32:
                    ap = ap.bitcast(dt)
                if len(shape) == 2:
                    ap = ap.rearrange("p (a b) -> p a b", a=shape[0], b=shape[1])
                elif len(shape) == 3:
                    ap = ap.rearrange("p (a b c) -> p a b c", a=shape[0], b=shape[1], c=shape[2])
                return ap

            def pbank(b, n=512, off=0):
                return PS[b // 2][:, (b % 2) * 512 + off:(b % 2) * 512 + off + n]

            def pkey(b):
                return ("ps", b)

            C0 = 0
            o = C0
            identF = sb(o, [128]); o += 512
            identB = sb(o, [128], BF16); o += 256
            modT = sb(o, [96]); o += 384
            effT = sb(o, [32]); o += 128
            g1T = sb(o, [16]); o += 64
            g2T = sb(o, [16]); o += 64
            tabc = sb(o, [16]); o += 64
            flags = sb(o, [2]); o += 8
            ones8 = sb(o, [8]); o += 32
            wr = sb(o, [16, 36]); o += 2304
            WsT = sb(o, [8, 128], BF16); o += 2048
            small = sb(o, [256]); o += 1024
            rt = sb(o, [256]); o += 1024
            urow = sb(o, [128], BF16, parts=(0, 1)); o += 256
            vrow = sb(o, [128], BF16, parts=(0, 1)); o += 256
            sel0 = sb(o, [16, 32]); o += 2048
            sel1 = sb(o, [16, 32]); o += 2048
            wsl = sb(o, [16, 2]); o += 128
            ecap = sb(o, [32]); o += 128
            vals = sb(o, [128], I32); o += 512
            CONST_END = o
            RING0 = (CONST_END + 63) // 64 * 64
            PH0 = RING0 + NSLOT * SLOT_B
            PH_END = ARENA_W * 4

            def ring_view(slot, a, b):
                return sb(RING0 + slot * SLOT_B, [a, b], BF16)

            class WS:
                units = []
                released = []
                nloaded = 0

            def w_add(src, a, b):
                WS.units.append((src, a, b))
                WS.released.append(False)
                return len(WS.units) - 1

            def w_pump():
                while WS.nloaded < len(WS.units):
                    u = WS.nloaded
                    if u >= NSLOT and not WS.released[u - NSLOT]:
                        break
                    src, a, b = WS.units[u]
                    slot = u % NSLOT
                    dst = ring_view(slot, a, b)
                    P.alias[("ring", slot)] = (("ring", slot, 0), ("ring", slot, 1))
                    h_ = a // 2
                    P.dma(POOL, ["modT"], [("ring", slot, 0)],
                          lambda e, dst=dst, src=src: e.dma_start(out=dst[:, 0:h_, :], in_=src[:, 0:h_, :]),
                          ring_sem[slot])
                    P.dma(POOL, ["modT"], [("ring", slot, 1)],
                          lambda e, dst=dst, src=src: e.dma_start(out=dst[:, h_:a, :], in_=src[:, h_:a, :]),
                          ring_semb[slot])
                    WS.nloaded += 1

            def w_get(u):
                w_pump()
                assert u < WS.nloaded, ("weight unit not loadable", u, WS.nloaded)
                src, a, b = WS.units[u]
                slot = u % NSLOT
                return ring_view(slot, a, b), ("ring", slot)

            def w_rel(u):
                WS.released[u] = True
                w_pump()

            def win_blk(c0):
                return w_in[:, c0:c0 + 512].rearrange("(c p) n -> p c n", p=128)

            plan = []
            for p in range(NPASS):
                pl = {}
                pl["k"] = [None, None]
                pl["vb"] = [None, None]
                pl["q"] = [None, None]
                for hh in range(2):
                    pl["k"][hh] = w_add(win_blk(3072 + hh * 512), 16, 512)
                    pl["vb"][hh] = w_add(win_blk(4096 + hh * 512), 16, 512)
                    pl["q"][hh] = w_add(win_blk(2048 + hh * 512), 16, 512)
                pl["u"] = [w_add(win_blk(0 + i * 512), 16, 512) for i in range(2)]
                pl["v"] = [w_add(win_blk(1024 + i * 512), 16, 512) for i in range(2)]
                pl["gA"] = [None] * 4
                pl["gB"] = [None] * 4
                pl["brA"] = [None] * 2
                pl["brB"] = [None] * 2
                for q in range(4):
                    pl["gA"][q] = w_add(win_blk(5120 + q * 512), 16, 512)
                    pl["gB"][q] = w_add(win_blk(7168 + q * 512), 16, 512)
                    if q % 2 == 0:
                        hf = q // 2
                        pl["brA"][hf] = w_add(wbrA[:, hf * 1024:(hf + 1) * 1024].rearrange("(c p) n -> p c n", p=128), 8, 1024)
                        pl["brB"][hf] = w_add(wbrB[:, hf * 1024:(hf + 1) * 1024].rearrange("(c p) n -> p c n", p=128), 8, 1024)
                pl["wo"] = [w_add(wout[:, cu * 512:(cu + 1) * 512].rearrange("(c p) n -> p c n", p=128), 16, 512) for cu in range(4)]
                plan.append(pl)

            def act_fn(out, in_, func, reads, writes, scale=1.0, bias=0.0, accum=None):
                def emit(e):
                    kw = {}
                    if accum is not None:
                        kw["accum_out"] = accum
                    return e.activation(out=out, in_=in_, func=func, bias=bias, scale=scale, **kw)
                P.op(ACT, reads, writes, emit)

            def dve(reads, writes, emit):
                P.op(DVE, reads, writes, emit)

            def pe_group(reads, writes, emit):
                P.op(PE, reads, writes, emit)

            def mm_acc(e, out, pairs):
                n = len(pairs)
                ins = None
                for i, (l, r) in enumerate(pairs):
                    ins = e.matmul(out, l, r, start=(i == 0), stop=(i == n - 1))
                return ins

            def sp_load(dst, src, writes, sem, reads=()):
                P.dma(SP, list(reads), list(writes), lambda e: e.dma_start(out=dst, in_=src), sem)

            sp_load(identF, ident_d, ["identF"], cpool.nxt())
            sp_load(flags, flags_d, ["flags"], cpool.nxt())
            sp_load(g1T, g1T_d, ["g1T"], cpool.nxt())
            sp_load(g2T, g2T_d, ["g2T"], cpool.nxt())
            sp_load(tabc, tabc_d[0].partition_broadcast(128), ["tabc"], cpool.nxt())
            sp_load(wr, wr_d.rearrange("(c p) n -> p c n", p=128), ["wr"], cpool.nxt())
            sp_load(ecap, ecap_d, ["ecap"], cpool.nxt())
            sp_load(vals, vals_d, ["vals"], cpool.nxt())
            scT = sb(PH0, [16])
            cTs = sb(PH0 + 64, [16])
            one11 = sb(PH0 + 128, [1])
            sp_load(cTs, cT, ["cTs"], cpool.nxt())
            dve(["identF"], ["identB"], lambda e: e.tensor_copy(out=identB, in_=identF))
            dve([], ["ones8"], lambda e: e.memset(ones8, 1.0))
            dve([], ["uvrow"], lambda e: e.memset(urow[:, 0:64], NEG))
            dve([], ["uvrow"], lambda e: e.memset(urow[:, 64:128], 0.0))
            dve([], ["uvrow"], lambda e: e.memset(vrow[:, 0:64], 0.0))
            dve([], ["uvrow"], lambda e: e.memset(vrow[:, 64:128], 1.0))
            dve([], ["one11"], lambda e: e.memset(one11, 1.0))
            act_fn(scT, cTs, AF.Silu, ["cTs"], ["scT"])

            wsf = sb(PH0 + 1024, [8, 128])
            P.dma(SP, [], ["wsf"], lambda e: e.dma_start(out=wsf, in_=wS.rearrange("g t s -> t g s")), cpool.nxt())
            dve(["wsf"], ["wsf"], lambda e: e.memset(wsf[0:64, :, 64:128], 0.0))
            for hb in range(2):
                def emit(e, hb=hb):
                    ins = None
                    for g in range(4):
                        gg = hb * 4 + g
                        ins = e.transpose(pbank(hb, 128, g * 128), wsf[:, gg, :], identF)
                    return ins
                pe_group(["wsf", "identF"], [pkey(hb)], emit)
                dve([pkey(hb)], ["WsT"], lambda e, hb=hb: e.tensor_copy(
                    out=WsT[:, hb * 4:(hb + 1) * 4, :], in_=pbank(hb).rearrange("p (g t) -> p g t", g=4)))

            AD0 = PH0 + 5120
            NAD = 3
            adbuf = [sb(AD0 + i * 32768, [16, 512]) for i in range(NAD)]
            abrow = sb(AD0 + NAD * 32768, [512], parts=(0, 1))
            abrow2 = sb(AD0 + NAD * 32768 + 2048, [512], parts=(0, 1))
            mrow = [sb(AD0 + NAD * 32768 + 4096 + i * 2048, [512], parts=(0, 1)) for i in range(2)]
            accb = [sb(AD0 + NAD * 32768 + 8192 + i * 2048, [512]) for i in range(2)]
            NB = 24
            MTB = 7
            def ada_load(blk):
                bi = blk % NAD
                P.alias[("adbuf", bi)] = (("adbuf", bi, 0), ("adbuf", bi, 1))
                srcv = ada_w[:, blk * 512:(blk + 1) * 512].rearrange("(c p) n -> p c n", p=128)
                P.dma(SP, [], [("adbuf", bi, 0)], lambda e: e.dma_start(out=adbuf[bi][:, 0:8, :], in_=srcv[:, 0:8, :]), adsem[bi])
                P.dma(SP, [], [("adbuf", bi, 1)], lambda e: e.dma_start(out=adbuf[bi][:, 8:16, :], in_=srcv[:, 8:16, :]), adsemb[bi])

            for blk in range(min(NAD - 1, NB)):
                ada_load(blk)
            for blk in range(NB):
                bi = blk % NAD
                if blk + NAD - 1 < NB:
                    ada_load(blk + NAD - 1)
                ab = abrow if blk % 2 == 0 else abrow2
                abk = ("abrow", blk % 2)
                P.dma(SP, [], [abk], lambda e, ab=ab, blk=blk: e.dma_start(out=ab, in_=ada_b[:, blk * 512:(blk + 1) * 512]), absem[blk % 2])
                pb_ = blk % 2

                ac = accb[blk % 2]
                ack = ("accb", blk % 2)
                NDV = 10
                pe_group([("adbuf", bi), "scT"], [pkey(pb_)], lambda e, bi=bi, pb_=pb_: [
                    e.matmul(pbank(pb_)[0:1, :], scT[:, dc:dc + 1], adbuf[bi][:, dc, :], start=(dc == NDV), stop=False)
                    for dc in range(NDV, 16)][-1])
                dve([("adbuf", bi), "scT"], [ack], lambda e, bi=bi, ac=ac: e.tensor_scalar(
                    out=ac, in0=adbuf[bi][:, 0, :], scalar1=scT[:, 0:1], scalar2=None, op0=ALU.mult))
                for dc in range(1, NDV):
                    dve([("adbuf", bi), "scT", ack], [ack], lambda e, bi=bi, ac=ac, dc=dc: e.scalar_tensor_tensor(
                        out=ac, in0=adbuf[bi][:, dc, :], scalar=scT[:, dc:dc + 1], in1=ac, op0=ALU.mult, op1=ALU.add))
                pe_group([ack, "ones8"], [pkey(pb_)], lambda e, ac=ac, pb_=pb_: e.matmul(
                    pbank(pb_)[0:1, :], ones8[:, 0:1], ac, start=False, stop=True))
                mr = mrow[blk % 2]
                mrk = ("mrow", blk % 2)
                dve([pkey(pb_), abk], [mrk], lambda e, mr=mr, ab=ab, pb_=pb_: e.tensor_tensor(
                    out=mr, in0=pbank(pb_)[0:1, :], in1=ab, op=ALU.add))
                P.dma(SP, [mrk], [("modd", blk)], lambda e, mr=mr, blk=blk: e.dma_start(
                    out=modd[:, blk * 512:(blk + 1) * 512], in_=mr), mdsem[blk % 2])

                def emit2(e, mr=mr, blk=blk):
                    ins = None
                    for k in range(4):
                        ins = e.matmul(pbank(MTB)[:, blk * 4 + k:blk * 4 + k + 1], mr[0:1, k * 128:(k + 1) * 128], one11[0:1, 0:1],
                                       start=True, stop=True)
                    return ins
                pe_group([mrk, "one11"], [("mtb", blk)], emit2)
            dve([("mtb", b) for b in range(NB)], ["modT"], lambda e: e.tensor_copy(out=modT, in_=pbank(MTB)[:, 0:96]))
            dve(["modT", "g1T"], ["effT"], lambda e: e.scalar_tensor_tensor(
                out=effT[:, 0:16], in0=modT[:, 16:32], scalar=1.0, in1=g1T, op0=ALU.add, op1=ALU.mult))
            dve(["modT", "g2T"], ["effT"], lambda e: e.scalar_tensor_tensor(
                out=effT[:, 16:32], in0=modT[:, 64:80], scalar=1.0, in1=g2T, op0=ALU.add, op1=ALU.mult))
            shift1 = modT[:, 0:16]
            shift2 = modT[:, 48:64]
            setup_keys = ["modT", "effT", "identF", "identB", "flags", "tabc", "wr", "WsT", "ones8"]
            setup_scratch = ["scT", "cTs", "one11", "wsf"] + [("adbuf", i) for i in range(3)] + \
                [("abrow", i) for i in range(2)] + [("mrow", i) for i in range(2)]
            modd_keys = [("modd", b) for b in range(NB)]

            dbg_done = [False]

            def dbg_dump(items):
                off = 0
                for ap, nw, keys in items:
                    P.dma(SP, list(keys), [("dbg", off)], lambda e, ap=ap, off=off, nw=nw: e.dma_start(
                        out=dbg_out[:, off:off + nw], in_=ap), msem)
                    off += nw
                dbg_done[0] = True

            def rms_rstd(ss_ap, rstd_ap, key_ss, key_rstd):
                act_fn(rstd_ap, ss_ap, AF.Sqrt, [key_ss], [key_rstd], scale=1.0 / D, bias=EPS)
                dve([key_rstd], [key_rstd], lambda e: e.reciprocal(out=rstd_ap, in_=rstd_ap))

            for p in range(NPASS if debug is None else debug.get("npass", NPASS)):
                pl = plan[p]
                row0 = p * TP
                fl = flags[:, p:p + 1]
                o = PH0
                h1own = sb(o, [16, 1024], BF16); o += 32768
                ybT = sb(o, [8, 1024], BF16); o += 16384
                GM0 = o
                h1halo = sb(o, [16, 512], BF16); o += 16384
                MIX0 = o
                xt = [sb(MIX0 + i * 8192, [2048]) for i in range(2)]
                junk = sb(MIX0 + 16384, [2048], BF16)
                P.fence()
                effB1 = sb(MIX0 + 20480, [2048])
                shiftB1 = sb(MIX0 + 28672, [2048])
                g1b = sb(PH0 + 32768, [2048])
                h1tok = [sb(MIX0 + 36864 + i * 4096, [2048], BF16) for i in range(2)]
                sp_load(g1b, g1row[0].partition_broadcast(128), ["g1b"], cpool.nxt())
                sp_load(effB1, modd[0, D:2 * D].partition_broadcast(128), ["effB1"], cpool.nxt(), reads=modd_keys)
                sp_load(shiftB1, modd[0, 0:D].partition_broadcast(128), ["shiftB1"], cpool.nxt(), reads=modd_keys)
                dve(["effB1", "g1b"], ["effB1"], lambda e: e.scalar_tensor_tensor(
                    out=effB1, in0=effB1, scalar=1.0, in1=g1b, op0=ALU.add, op1=ALU.mult))
                def p1_front(j):
                    b = j % 2
                    xk = ("xt", b)
                    P.dma(SP, [], [xk], lambda e, b=b, j=j: e.dma_start(
                        out=xt[b], in_=xe[row0 + j * 128:row0 + (j + 1) * 128, :]), xsem[b])
                    ssa = small[:, j:j + 1]
                    rsa = small[:, 16 + j:17 + j]
                    act_fn(junk, xt[b], AF.Square, [xk], [("ss", j)], accum=ssa)
                    rms_rstd(ssa, rsa, ("ss", j), ("rs", j))
                    dve([xk, ("rs", j), "effB1"], [xk], lambda e, b=b, rsa=rsa: e.scalar_tensor_tensor(
                        out=xt[b], in0=xt[b], scalar=rsa, in1=effB1, op0=ALU.mult, op1=ALU.mult))
                    dve([xk, "shiftB1"], [("h1tok", b)], lambda e, b=b: e.tensor_tensor(out=h1tok[b], in0=xt[b], in1=shiftB1, op=ALU.add))

                def p1_back(j):
                    b = j % 2
                    for hb_ in range(2):
                        q = (j % 2) * 2 + hb_
                        tbv = PS[q // 2][:, (q % 2) * 512:(q % 2) * 512 + 512].bitcast(BF16)

                        def emit(e, b=b, hb_=hb_, tbv=tbv):
                            ins = None
                            for k in range(8):
                                dc = hb_ * 8 + k
                                ins = e.transpose(tbv[:, k * 128:(k + 1) * 128], h1tok[b][:, dc * 128:(dc + 1) * 128], identB)
                            return ins
                        pe_group([("h1tok", b), "identB"], [pkey(q)], emit)
                        if j < 4:
                            dst = h1halo[:, hb_ * 8:(hb_ + 1) * 8, j * 128:(j + 1) * 128]
                            dk = ("h1halo", j)
                        else:
                            dst = h1own[:, hb_ * 8:(hb_ + 1) * 8, (j - 4) * 128:(j - 3) * 128]
                            dk = ("h1own", j - 4)
                        act_fn(dst, tbv.rearrange("p (k t) -> p k t", k=8), AF.Copy, [pkey(q)], [dk])

                p1_front(0)
                for j in range(12):
                    if j + 1 < 12:
                        p1_front(j + 1)
                    p1_back(j)
                if debug is not None and debug["stage"] == "h1":
                    dbg_dump([(h1own.rearrange("p a b -> p (a b)").bitcast(F32), 8192, [("h1own", i) for i in range(8)]),
                              (h1halo.rearrange("p a b -> p (a b)").bitcast(F32), 4096, [("h1halo", i) for i in range(4)]),
                              (modT, 96, ["modT"]), (effT, 32, ["effT"])])
                    break

                def h1_rhs(dc, t0, n):
                    if t0 < 512:
                        assert t0 + n <= 512
                        return h1halo[:, dc, t0:t0 + n]
                    return h1own[:, dc, t0 - 512:t0 - 512 + n]

                def h1_keys(t0, n):
                    ks = []
                    for t in range(t0 // 128, (t0 + n) // 128):
                        ks.append(("h1halo", t) if t < 4 else ("h1own", t - 4))
                    return ks

                P.fence()
                o = MIX0
                qT = sb(o, [4, 1024], BF16); o += 8192
                kT = sb(o, [4, 1536], BF16); o += 12288
                Vt = sb(o, [12, 8 * 65], BF16); o += 12480
                o = (o + 63) // 64 * 64
                biasS = sb(o, [8, 384]); o += 12288
                Pb = [sb(o + i * 1280, [640], BF16) for i in range(3)]; o += 3840
                ybt = sb(o, [512], BF16); o += 1024
                rden = sb(o, [8]); o += 32
                assert o <= PH_END
                for hh in range(2):
                    P.dma(SP, [], ["biasS"], lambda e, hh=hh: e.dma_start(
                        out=biasS.rearrange("p a b -> p (a b)"), in_=biasT[:, hh * 3072:(hh + 1) * 3072]), cpool.nxt())
                    wk, wkk = w_get(pl["k"][hh])
                    for tt in range(3):
                        for cc in range(4):
                            bnk = (tt * 4 + cc) % 8
                            def emit(e, tt=tt, cc=cc, bnk=bnk):
                                return mm_acc(e, pbank(bnk), [(wk[:, dc, cc * 128:(cc + 1) * 128], h1_rhs(dc, tt * 512, 512)) for dc in range(16)])
                            pe_group([wkk] + h1_keys(tt * 512, 512), [pkey(bnk)], emit)
                            act_fn(kT[:, cc, tt * 512:(tt + 1) * 512], pbank(bnk), AF.Copy, [pkey(bnk)], [("kT", tt)])
                    w_rel(pl["k"][hh])
                    wv, wvk = w_get(pl["vb"][hh])
                    for j in range(12):
                        bnk = j % 8
                        def emit(e, j=j, bnk=bnk):
                            return mm_acc(e, pbank(bnk), [(h1_rhs(dc, j * 128, 128), wv[:, dc, :]) for dc in range(16)])
                        pe_group([wvk] + h1_keys(j * 128, 128), [pkey(bnk)], emit)
                        vdst = Vt[:, j, :].rearrange("p (h d) -> p h d", h=8)
                        sc = fl if j < 4 else 1.0
                        act_fn(vdst[:, :, 0:64], pbank(bnk).rearrange("p (h d) -> p h d", h=8), AF.Copy,
                               [pkey(bnk), "flags"], [("Vt", j)], scale=sc)
                        act_fn(vdst[:, :, 64:65], ones8.rearrange("p (h d) -> p h d", d=1), AF.Copy,
                               ["ones8", "flags"], [("Vt", j)], scale=sc)
                    w_rel(pl["vb"][hh])
                    wq, wqk = w_get(pl["q"][hh])
                    for tt in range(2):
                        for cc in range(4):
                            bnk = (tt * 4 + cc) % 8
                            def emit(e, tt=tt, cc=cc, bnk=bnk):
                                return mm_acc(e, pbank(bnk), [(wq[:, dc, cc * 128:(cc + 1) * 128], h1_rhs(dc, 512 + tt * 512, 512)) for dc in range(16)])
                            pe_group([wqk] + h1_keys(512 + tt * 512, 512), [pkey(bnk)], emit)
                            act_fn(qT[:, cc, tt * 512:(tt + 1) * 512], pbank(bnk), AF.Copy, [pkey(bnk)], [("qT", tt)], scale=0.125)
                    w_rel(pl["q"][hh])
                    NSET = 3
                    SK = 2
                    jobs = [(i, hl) for i in range(8) for hl in range(8)]

                    def st_S(n):
                        i, hl = jobs[n]
                        j = i + 4
                        h = hh * 8 + hl
                        cc = hl // 2
                        pb0 = (hl % 2) * 64
                        st = n % NSET
                        s0, s1 = 2 * st, 2 * st + 1

                        def emit(e):
                            ins = None
                            qv = qT[pb0:pb0 + 64, cc, i * 128:(i + 1) * 128]
                            for dl in range(5):
                                kt = j - dl
                                kv = kT[pb0:pb0 + 64, cc, kt * 128:(kt + 1) * 128]
                                if dl < 3:
                                    outp = pbank(s0, 128, dl * 128)
                                else:
                                    outp = pbank(s1, 128, (dl - 3) * 128)
                                if dl == 4:
                                    e.matmul(outp, kv, qv, start=True, stop=False)
                                    ins = e.matmul(outp, urow, vrow, start=False, stop=True)
                                else:
                                    ins = e.matmul(outp, kv, qv, start=True, stop=True)
                            return ins
                        pe_group([("qT", i // 4), "uvrow"] + [("kT", (j - dl) // 4) for dl in range(5)], [pkey(s0), pkey(s1)], emit)
                        dve([pkey(s0), "biasS"], [pkey(s0)], lambda e: e.tensor_tensor(
                            out=pbank(s0, 384), in0=pbank(s0, 384), in1=biasS[:, hl, :], op=ALU.add))
                        act_fn(Pb[st][:, 0:384], pbank(s0, 384), AF.Exp, [pkey(s0)], [("Pb", st)])
                        act_fn(Pb[st][:, 384:640], pbank(s1, 256), AF.Exp, [pkey(s1), "tabc"], [("Pb", st)],
                               bias=tabc[:, h:h + 1])

                    def st_PV(n):
                        i, hl = jobs[n]
                        j = i + 4
                        st = n % NSET
                        ob = 6 + hl // 4

                        def emit(e):
                            pairs = []
                            for dl in range(5):
                                kt = j - dl
                                pairs.append((Pb[st][:, dl * 128:(dl + 1) * 128], Vt[:, kt, hl * 65:(hl + 1) * 65]))
                            return mm_acc(e, pbank(ob, 65, (hl % 4) * 65), pairs)
                        pe_group([("Pb", st)] + [("Vt", j - dl) for dl in range(5)], [pkey(ob)], emit)

                    def st_norm(i):
                        for ob in (6, 7):
                            ov = pbank(ob, 260).rearrange("p (h d) -> p h d", h=4)
                            dve([pkey(ob)], ["rden"], lambda e, ob=ob, ov=ov: e.reciprocal(
                                out=rden[:, (ob - 6) * 4:(ob - 5) * 4], in_=ov[:, :, 64]))
                            for k in range(4):
                                hl = (ob - 6) * 4 + k
                                dve([pkey(ob), "rden"], ["ybt"], lambda e, ov=ov, k=k, hl=hl: e.tensor_scalar(
                                    out=ybt[:, hl * 64:(hl + 1) * 64], in0=ov[:, k, 0:64], scalar1=rden[:, hl:hl + 1],
                                    scalar2=None, op0=ALU.mult))

                    def st_T(i):
                        tb = PS[0][:, 0:512].bitcast(BF16)

                        def emit(e):
                            ins = None
                            for k in range(4):
                                ins = e.transpose(tb[:, k * 128:(k + 1) * 128], ybt[:, k * 128:(k + 1) * 128], identB)
                            return ins
                        pe_group(["ybt", "identB"], [pkey(0)], emit)
                        act_fn(ybT[:, hh * 4:(hh + 1) * 4, i * 128:(i + 1) * 128],
                               tb[:, 0:512].rearrange("p (k t) -> p k t", k=4), AF.Copy, [pkey(0)], [("ybT", i)])

                    pendT = []
                    for n in range(len(jobs) + SK + 2):
                        if n < len(jobs):
                            st_S(n)
                        m = n - SK
                        if 0 <= m < len(jobs):
                            st_PV(m)
                            if jobs[m][1] == 7:
                                st_norm(jobs[m][0])
                                pendT.append((jobs[m][0], n + 2))
                        for (ti, due) in pendT:
                            if due == n:
                                st_T(ti)
                if debug is not None and debug["stage"] == "attn":
                    dbg_dump([(ybT.rearrange("p a b -> p (a b)").bitcast(F32), 4096, [("ybT", i) for i in range(8)])])
                    break

                P.fence()
                o = GM0
                yaT = sb(o, [8, 1024], BF16); o += 16384
                uT = sb(o, [8, 1024], BF16); o += 16384
                vn = [sb(o + i * 2048, [1024], BF16) for i in range(2)]; o += 4096
                vg = [sb(o + i * 4096, [1024]) for i in range(2)]; o += 8192
                lnGb = sb(o, [1024]); o += 4096
                lnBb = sb(o, [1024]); o += 4096
                bSb = sb(o, [1024]); o += 4096
                t1 = sb(o, [1024]); o += 4096
                bst = sb(o, [12]); o += 48
                assert o <= PH_END
                gkeys = [("kT", t) for t in range(3)] + [("qT", t) for t in range(2)] + [("Vt", j) for j in range(12)] + \
                    ["biasS", "ybt", "rden"] + [("Pb", i) for i in range(2)] + [("tmpS", i) for i in range(2)]
                sp_load(lnGb, lnG[0].partition_broadcast(128), ["lnGb"], cpool.nxt())
                sp_load(lnBb, lnB[0].partition_broadcast(128), ["lnBb"], cpool.nxt())
                sp_load(bSb, bS[0].partition_broadcast(128), ["bSb"], cpool.nxt())
                for uu in range(2):
                    wu, wuk = w_get(pl["u"][uu])
                    for tt in range(2):
                        for cc in range(4):
                            bnk = (tt * 4 + cc) % 8
                            def emit(e, tt=tt, cc=cc, bnk=bnk, wu=wu):
                                return mm_acc(e, pbank(bnk), [(wu[:, dc, cc * 128:(cc + 1) * 128], h1_rhs(dc, 512 + tt * 512, 512)) for dc in range(16)])
                            pe_group([wuk] + h1_keys(512 + tt * 512, 512), [pkey(bnk)], emit)
                            act_fn(uT[:, uu * 4 + cc, tt * 512:(tt + 1) * 512], pbank(bnk), AF.Gelu,
                                   [pkey(bnk)], [("uT", tt)])
                    w_rel(pl["u"][uu])
                wv0, wv0k = w_get(pl["v"][0])
                wv1, wv1k = w_get(pl["v"][1])
                wvs = [(wv0, wv0k), (wv1, wv1k)]
                def gm_front(i):
                    b = i % 2
                    for vu in range(2):
                        bnk = b * 4 + vu
                        def emit(e, i=i, vu=vu, bnk=bnk):
                            return mm_acc(e, pbank(bnk), [(h1_rhs(dc, 512 + i * 128, 128), wvs[vu][0][:, dc, :]) for dc in range(16)])
                        pe_group([wvs[vu][1]] + h1_keys(512 + i * 128, 128), [pkey(bnk)], emit)
                        act_fn(vg[b][:, vu * 512:(vu + 1) * 512], pbank(bnk), AF.Gelu, [pkey(bnk)], [("vg", b)])
                    st6 = small[:, 64 + b * 12:64 + b * 12 + 12]
                    mv = small[:, 96 + b * 4:96 + b * 4 + 2]
                    rs_ = small[:, 98 + b * 4:99 + b * 4]
                    for vu in range(2):
                        dve([("vg", b)], [("st6", b)], lambda e, b=b, vu=vu, st6=st6: e.bn_stats(
                            out=st6[:, vu * 6:(vu + 1) * 6], in_=vg[b][:, vu * 512:(vu + 1) * 512]))
                    dve([("st6", b)], [("mv", b)], lambda e, st6=st6, mv=mv: e.bn_aggr(out=mv, in_=st6))
                    act_fn(rs_, mv[:, 1:2], AF.Sqrt, [("mv", b)], [("rsv", b)], bias=EPS)
                    dve([("rsv", b)], [("rsv", b)], lambda e, rs_=rs_: e.reciprocal(out=rs_, in_=rs_))
                    dve([("vg", b), ("mv", b), ("rsv", b)], [("vg", b)], lambda e, b=b, mv=mv, rs_=rs_: e.tensor_scalar(
                        out=vg[b], in0=vg[b], scalar1=mv[:, 0:1], scalar2=rs_, op0=ALU.subtract, op1=ALU.mult))
                    dve([("vg", b), "lnGb"], [("vg", b)], lambda e, b=b: e.tensor_tensor(out=vg[b], in0=vg[b], in1=lnGb, op=ALU.mult))
                    dve([("vg", b), "lnBb"], [("vn", b)], lambda e, b=b: e.tensor_tensor(out=vn[b], in0=vg[b], in1=lnBb, op=ALU.add))

                def gm_back(i):
                    b = i % 2
                    for gh in range(2):
                        bnk = b * 4 + 2 + gh
                        def emit(e, b=b, gh=gh, bnk=bnk):
                            ins = None
                            for g4 in range(4):
                                g = gh * 4 + g4
                                ins = e.matmul(pbank(bnk, 128, g4 * 128), vn[b][:, g * 128:(g + 1) * 128], WsT[:, g, :], start=True, stop=True)
                            return ins
                        pe_group([("vn", b), "WsT"], [pkey(bnk)], emit)
                        dve([pkey(bnk), "bSb"], ["t1"], lambda e, gh=gh, bnk=bnk: e.tensor_tensor(
                            out=t1[:, gh * 512:(gh + 1) * 512], in0=pbank(bnk), in1=bSb[:, gh * 512:(gh + 1) * 512], op=ALU.add))
                        dve(["t1", ("uT", i // 4)], [("yaT", i)], lambda e, gh=gh, i=i: e.tensor_tensor(
                            out=yaT[:, gh * 4:(gh + 1) * 4, i * 128:(i + 1) * 128],
                            in0=t1[:, gh * 512:(gh + 1) * 512].rearrange("p (g t) -> p g t", g=4),
                            in1=uT[:, gh * 4:(gh + 1) * 4, i * 128:(i + 1) * 128], op=ALU.mult))
                gm_front(0)
                for i in range(8):
                    if i + 1 < 8:
                        gm_front(i + 1)
                    gm_back(i)
                w_rel(pl["v"][0])
                w_rel(pl["v"][1])
                if debug is not None and debug["stage"] == "gmlp":
                    dbg_dump([(yaT.rearrange("p a b -> p (a b)").bitcast(F32), 4096, [("yaT", i) for i in range(8)])])
                    break

                P.fence()
                o = GM0 + 16384
                mergedT = sb(o, [16, 1024], BF16); o += 32768
                sg = [sb(o + i * 2048, [512]) for i in range(4)]; o += 8192
                assert o <= PH_END
                mkeys = [("uT", t) for t in range(2)] + [("vg", i) for i in range(2)] + [("vn", i) for i in range(2)] + ["t1", "lnGb", "lnBb", "bSb"]
                for q in range(4):
                    wga, wgak = w_get(pl["gA"][q])
                    wgb, wgbk = w_get(pl["gB"][q])
                    wa, wak = w_get(pl["brA"][q // 2])
                    wb, wbk = w_get(pl["brB"][q // 2])
                    for tt in range(2):
                        for c4 in range(4):
                            dcc = q * 4 + c4
                            st = (tt * 4 + c4) % 2
                            bA, bB, bYa, bYb = st * 4, st * 4 + 1, st * 4 + 2, st * 4 + 3
                            ts_ = slice(tt * 512, (tt + 1) * 512)

                            def emitA(e, c4=c4, ts_=ts_, bA=bA, wga=wga):
                                return mm_acc(e, pbank(bA), [(wga[:, dc, c4 * 128:(c4 + 1) * 128], h1own[:, dc, ts_]) for dc in range(16)])
                            pe_group([wgak] + [("h1own", t) for t in range(tt * 4, tt * 4 + 4)], [pkey(bA)], emitA)

                            def emitB(e, c4=c4, ts_=ts_, bB=bB, wgb=wgb):
                                return mm_acc(e, pbank(bB), [(wgb[:, dc, c4 * 128:(c4 + 1) * 128], h1own[:, dc, ts_]) for dc in range(16)])
                            pe_group([wgbk] + [("h1own", t) for t in range(tt * 4, tt * 4 + 4)], [pkey(bB)], emitB)
                            col = (q % 2) * 512 + c4 * 128

                            def emitYa(e, col=col, ts_=ts_, bYa=bYa, wa=wa):
                                return mm_acc(e, pbank(bYa), [(wa[:, kc, col:col + 128], yaT[:, kc, ts_]) for kc in range(8)])
                            pe_group([wak] + [("yaT", t) for t in range(tt * 4, tt * 4 + 4)], [pkey(bYa)], emitYa)

                            def emitYb(e, col=col, ts_=ts_, bYb=bYb, wb=wb):
                                return mm_acc(e, pbank(bYb), [(wb[:, kc, col:col + 128], ybT[:, kc, ts_]) for kc in range(8)])
                            pe_group([wbk] + [("ybT", t) for t in range(tt * 4, tt * 4 + 4)], [pkey(bYb)], emitYb)
                            sga, sgb = sg[st * 2], sg[st * 2 + 1]
                            act_fn(sga, pbank(bA), AF.Sigmoid, [pkey(bA)], [("sg", st * 2)])
                            act_fn(sgb, pbank(bB), AF.Sigmoid, [pkey(bB)], [("sg", st * 2 + 1)])
                            dve([("sg", st * 2), pkey(bYa)], [("sg", st * 2)], lambda e, sga=sga, bYa=bYa: e.tensor_tensor(
                                out=sga, in0=sga, in1=pbank(bYa), op=ALU.mult))
                            dve([("sg", st * 2 + 1), pkey(bYb)], [("sg", st * 2 + 1)], lambda e, sgb=sgb, bYb=bYb: e.tensor_tensor(
                                out=sgb, in0=sgb, in1=pbank(bYb), op=ALU.mult))
                            dve([("sg", st * 2), ("sg", st * 2 + 1)], [("mergedT", tt)], lambda e, sga=sga, sgb=sgb, dcc=dcc, ts_=ts_: e.tensor_tensor(
                                out=mergedT[:, dcc, ts_], in0=sga, in1=sgb, op=ALU.add))
                    w_rel(pl["gA"][q])
                    w_rel(pl["gB"][q])
                    if q % 2 == 1:
                        w_rel(pl["brA"][q // 2])
                        w_rel(pl["brB"][q // 2])
                if debug is not None and debug["stage"] == "merge":
                    dbg_dump([(mergedT.rearrange("p a b -> p (a b)").bitcast(F32), 8192, [("mergedT", i) for i in range(2)])])
                    break

                P.fence()
                w2effb = sb(PH0, [2048])
                shift2b = sb(PH0 + 8192, [2048])
                h2row = [sb(PH0 + 16384 + i * 4096, [2048], BF16) for i in range(2)]
                g2b = sb(PH0 + 24576, [2048])
                gate1b = sb(PH0 + 32768, [2048])
                h2f = sb(PH0 + 40960, [16, 128])
                xt6 = [sb(PH0 + 49152 + i * 8192, [2048]) for i in range(2)]
                assert PH0 + 65536 == GM0 + 16384
                xn6 = sb(PH0 + 98304, [2048])
                junk6 = sb(PH0 + 106496, [2048], BF16)
                tmpm = sb(PH0 + 110592, [512])
                dead = [("h1own", t) for t in range(8)] + [("h1halo", t) for t in range(4)] + [("ybT", t) for t in range(8)] + \
                    [("yaT", t) for t in range(8)] + [("sg", i) for i in range(4)]
                sp_load(gate1b, modd[0, 2 * D:3 * D].partition_broadcast(128), ["gate1b"], cpool.nxt(), reads=modd_keys)
                sp_load(g2b, g2row[0].partition_broadcast(128), ["g2b"], cpool.nxt())
                sp_load(w2effb, modd[0, 4 * D:5 * D].partition_broadcast(128), ["w2effb"], cpool.nxt(), reads=modd_keys)
                sp_load(shift2b, modd[0, 3 * D:4 * D].partition_broadcast(128), ["shift2b"], cpool.nxt(), reads=modd_keys)
                dve(["w2effb", "g2b"], ["w2effb"], lambda e: e.scalar_tensor_tensor(
                    out=w2effb, in0=w2effb, scalar=1.0, in1=g2b, op0=ALU.add, op1=ALU.mult))
                wos = [w_get(u) for u in pl["wo"]]

                def p6_mm_pe(i):
                    b = i % 2
                    P.dma(SP, [], [("xt6", b)], lambda e, b=b, i=i: e.dma_start(
                        out=xt6[b], in_=xe[row0 + HALO + i * 128:row0 + HALO + (i + 1) * 128, :]), xsem[b])
                    for cu in range(4):
                        def emit(e, i=i, cu=cu):
                            return mm_acc(e, pbank(cu), [(mergedT[:, kc, i * 128:(i + 1) * 128], wos[cu][0][:, kc, :]) for kc in range(16)])
                        pe_group([wos[cu][1], ("mergedT", i // 4)], [pkey(cu)], emit)

                def p6_mm_dve(i):
                    b = i % 2
                    for cu in range(4):
                        dve([pkey(cu), "gate1b"], ["tmpm"], lambda e, cu=cu: e.tensor_tensor(
                            out=tmpm, in0=pbank(cu), in1=gate1b[:, cu * 512:(cu + 1) * 512], op=ALU.mult))
                        dve(["tmpm", ("xt6", b)], [("xt6", b)], lambda e, cu=cu, b=b: e.tensor_tensor(
                            out=xt6[b][:, cu * 512:(cu + 1) * 512], in0=xt6[b][:, cu * 512:(cu + 1) * 512], in1=tmpm, op=ALU.add))

                def p6_front(i):
                    b = i % 2
                    xk = ("xt6", b)
                    ssa = small[:, 32 + i:33 + i]
                    rsa = small[:, 48 + i:49 + i]
                    act_fn(junk6, xt6[b], AF.Square, [xk], [("ss2", i)], accum=ssa)
                    rms_rstd(ssa, rsa, ("ss2", i), ("rs2", i))
                    P.dma(SP, [xk], [("x1s", p * 8 + i)], lambda e, b=b, i=i: e.dma_start(
                        out=x1s[p * TP + i * 128:p * TP + (i + 1) * 128, :], in_=xt6[b]), osem[b])
                    dve([xk, ("rs2", i), "w2effb"], ["xn6"], lambda e, b=b, rsa=rsa: e.scalar_tensor_tensor(
                        out=xn6, in0=xt6[b], scalar=rsa, in1=w2effb, op0=ALU.mult, op1=ALU.mult))
                    dve(["xn6", "shift2b"], ["xn6"], lambda e: e.tensor_tensor(out=xn6, in0=xn6, in1=shift2b, op=ALU.add))
                    hb = i % 2
                    act_fn(h2row[hb], xn6, AF.Copy, ["xn6"], [("h2row", hb)])
                    P.dma(SP, [("h2row", hb)], [("h2d", p * 8 + i)], lambda e, hb=hb, i=i: e.dma_start(
                        out=h2d[p * TP + i * 128:p * TP + (i + 1) * 128, :], in_=h2row[hb]), hsem[hb])
                def p6_back(i):
                    b = i % 2
                    for q in range(4):
                        def emit(e, q=q):
                            ins = None
                            for k in range(4):
                                dc = q * 4 + k
                                ins = e.transpose(pbank(4 + q, 128, k * 128), xn6[:, dc * 128:(dc + 1) * 128], identF)
                            return ins
                        pe_group(["xn6", "identF"], [pkey(4 + q)], emit)
                        act_fn(h2f[:, q * 4:(q + 1) * 4, :], pbank(4 + q).rearrange("p (k t) -> p k t", k=4), AF.Copy,
                               [pkey(4 + q)], [("h2f", q)])
                    LB = 0

                    def emit(e):
                        return mm_acc(e, pbank(LB, 36), [(h2f[:, dc, :], wr[:, dc, :]) for dc in range(16)])
                    pe_group([("h2f", q) for q in range(4)] + ["wr"], [pkey(LB)], emit)
                    lg = rt[:, 0:36]
                    dve([pkey(LB)], ["rt"], lambda e: e.tensor_copy(out=lg, in_=pbank(LB, 36)))
                    gl = rt[:, 0:4]
                    el = rt[:, 4:36]
                    gmax = rt[:, 40:41]
                    ngmax = rt[:, 41:42]
                    gsum = rt[:, 42:43]
                    gw = rt[:, 43:44]
                    goh = rt[:, 44:48]
                    gex = rt[:, 48:52]
                    esel = rt[:, 56:64]
                    etmp = rt[:, 64:96]
                    m1 = rt[:, 96:97]
                    m2 = rt[:, 97:98]
                    oh1 = rt[:, 104:112]
                    oh2 = rt[:, 112:120]
                    msk = rt[:, 120:128]
                    dd = rt[:, 128:129]
                    w1_ = rt[:, 129:130]
                    w2_ = rt[:, 130:131]
                    ew = rt[:, 136:144]
                    gwo = rt[:, 144:148]
                    R = ["rt"]
                    dve(R, R, lambda e: e.tensor_reduce(out=gmax, in_=gl, axis=AX.X, op=ALU.max))
                    dve(R, R, lambda e: e.tensor_scalar(out=ngmax, in0=gmax, scalar1=-1.0, scalar2=None, op0=ALU.mult))
                    act_fn(gex, gl, AF.Exp, R, R, bias=ngmax, accum=gsum)
                    dve(R, R, lambda e: e.reciprocal(out=gw, in_=gsum))
                    dve(R, R, lambda e: e.tensor_scalar(out=goh, in0=gl, scalar1=gmax, scalar2=None, op0=ALU.is_equal))
                    dve(R, R, lambda e: e.tensor_scalar(out=esel, in0=el[:, 0:8], scalar1=goh[:, 0:1], scalar2=None, op0=ALU.mult))
                    for g in range(1, 4):
                        dve(R, R, lambda e, g=g: e.scalar_tensor_tensor(
                            out=esel, in0=el[:, g * 8:(g + 1) * 8], scalar=goh[:, g:g + 1], in1=esel, op0=ALU.mult, op1=ALU.add))
                    dve(R, R, lambda e: e.tensor_reduce(out=m1, in_=esel, axis=AX.X, op=ALU.max))
                    dve(R, R, lambda e: e.tensor_scalar(out=oh1, in0=esel, scalar1=m1, scalar2=None, op0=ALU.is_equal))
                    dve(R, R, lambda e: e.scalar_tensor_tensor(out=msk, in0=oh1, scalar=-1e30, in1=esel, op0=ALU.mult, op1=ALU.add))
                    dve(R, R, lambda e: e.tensor_reduce(out=m2, in_=msk, axis=AX.X, op=ALU.max))
                    dve(R, R, lambda e: e.tensor_scalar(out=oh2, in0=msk, scalar1=m2, scalar2=None, op0=ALU.is_equal))
                    dve(R, R, lambda e: e.tensor_tensor(out=dd, in0=m2, in1=m1, op=ALU.subtract))
                    act_fn(dd, dd, AF.Exp, R, R)
                    dve(R, R, lambda e: e.tensor_scalar(out=w1_, in0=dd, scalar1=1.0, scalar2=None, op0=ALU.add))
                    dve(R, R, lambda e: e.reciprocal(out=w1_, in_=w1_))
                    dve(R, R, lambda e: e.tensor_tensor(out=w2_, in0=dd, in1=w1_, op=ALU.mult))
                    dve(R, R, lambda e: e.tensor_scalar(out=ew, in0=oh1, scalar1=w1_, scalar2=None, op0=ALU.mult))
                    dve(R, R, lambda e: e.scalar_tensor_tensor(out=ew, in0=oh2, scalar=w2_, in1=ew, op0=ALU.mult, op1=ALU.add))
                    dve(R, R, lambda e: e.tensor_scalar(out=gwo, in0=goh, scalar1=gw, scalar2=None, op0=ALU.mult))
                    ti = p * 8 + i
                    for g in range(4):
                        dve(R, [("sel", ti)], lambda e, g=g, ti=ti: e.tensor_scalar(
                            out=sel0[:, ti, g * 8:(g + 1) * 8], in0=oh1, scalar1=goh[:, g:g + 1], scalar2=None, op0=ALU.mult))
                        dve(R, [("sel", ti)], lambda e, g=g, ti=ti: e.tensor_scalar(
                            out=sel1[:, ti, g * 8:(g + 1) * 8], in0=oh2, scalar1=goh[:, g:g + 1], scalar2=None, op0=ALU.mult))
                    dve(R, [("wkk", ti)], lambda e, ti=ti: e.tensor_tensor(out=wsl[:, ti, 0:1], in0=w1_, in1=gw, op=ALU.mult))
                    dve(R, [("wkk", ti)], lambda e, ti=ti: e.tensor_tensor(out=wsl[:, ti, 1:2], in0=w2_, in1=gw, op=ALU.mult))

                p6_mm_pe(0)
                p6_mm_dve(0)
                for i in range(8):
                    if i + 1 < 8:
                        p6_mm_pe(i + 1)
                    p6_front(i)
                    if i + 1 < 8:
                        p6_mm_dve(i + 1)
                    p6_back(i)
                for u in pl["wo"]:
                    w_rel(u)
                if debug is not None and debug["stage"] == "p6":
                    break


            class MW:
                units = []
                released = []
                nloaded = 0

            def m_view(slot, a, b):
                return sb(RING0 + slot * SLOT_B, [a, b], BF16)

            def m_pump(limit=None):
                while MW.nloaded < len(MW.units):
                    u = MW.nloaded
                    if limit is not None and u >= limit:
                        break
                    if u >= MSLOT and not MW.released[u - MSLOT]:
                        break
                    src, a, b = MW.units[u]
                    slot = u % MSLOT
                    dst = m_view(slot, a, b)
                    wk_ = [("mring", slot)]
                    if u < NSLOT:
                        wk_.append(("ring", slot))
                    P.dma(POOL, [], wk_, lambda e, dst=dst, src=src: e.dma_start(out=dst, in_=src), mr_sem[slot])
                    MW.nloaded += 1

            def m_get(u):
                m_pump()
                assert u < MW.nloaded
                src, a, b = MW.units[u]
                return m_view(u % MSLOT, a, b), ("mring", u % MSLOT)

            def m_rel(u):
                MW.released[u] = True

            for e_ in range(NE):
                wpat = "(p c) n -> p c n" if CONTIG else "(c p) n -> p c n"
                for src, a, b in ((w1[e_].rearrange(wpat, p=128), 16, 512),
                                  (w3[e_].rearrange(wpat, p=128), 16, 512),
                                  (w2[e_].rearrange("(c p) n -> p c n", p=128), 4, 2048)):
                    MW.units.append((src, a, b))
                    MW.released.append(False)

            if debug is None or debug["stage"] in ("meta", "moe"):
                m_pump(limit=NSLOT)

            if debug is None or debug["stage"] in ("meta", "moe"):
                P.nofloor.discard("pool")
                P.fence()
                o = PH0 + (MSLOT - NSLOT) * SLOT_B
                selA = sb(o, [16, 32]); o += 2048
                cum = sb(o, [16, 32]); o += 2048
                pos = sb(o, [16, 32]); o += 2048
                tmpd = sb(o, [16, 32]); o += 2048
                destf = sb(o, [32]); o += 128
                desti = sb(o, [32], I32); o += 128
                cntf = sb(o, [32]); o += 128
                cnti = sb(o, [32], I32); o += 128
                fillt = sb(o, [2048], I32); o += 8192
                Utri = sb(o, [128]); o += 512
                OnesM = sb(o, [128]); o += 512
                META_END = o
                sp_load(Utri, Utri_d, ["Utri"], cpool.nxt())
                sp_load(OnesM, Ones_d, ["OnesM"], cpool.nxt())
                selk = [("sel", t) for t in range(16)]
                dve(selk, ["selA"], lambda e: e.tensor_tensor(out=selA, in0=sel0, in1=sel1, op=ALU.add))
                dve([], ["cum"], lambda e: e.memset(cum[:, 0, :], 0.0))
                for t in range(1, 16):
                    dve(["cum", "selA"], ["cum"], lambda e, t=t: e.tensor_tensor(
                        out=cum[:, t, :], in0=cum[:, t - 1, :], in1=selA[:, t - 1, :], op=ALU.add))
                def emit(e):
                    ins = None
                    for t in range(16):
                        e.matmul(pbank(0, 32, t * 32), OnesM, cum[:, t, :], start=True, stop=False)
                        ins = e.matmul(pbank(0, 32, t * 32), Utri, selA[:, t, :], start=False, stop=True)
                    return ins
                pe_group(["cum", "selA", "OnesM", "Utri"], [pkey(0)], emit)
                dve([pkey(0)], ["pos"], lambda e: e.tensor_copy(out=pos, in_=pbank(0).rearrange("p (t x) -> p t x", t=16)))
                dve(["cum", "selA"], ["tmpd"], lambda e: e.tensor_tensor(out=tmpd[:, 0, :], in0=cum[:, 15, :], in1=selA[:, 15, :], op=ALU.add))
                pe_group(["tmpd", "OnesM"], [pkey(1)], lambda e: e.matmul(pbank(1, 32), OnesM, tmpd[:, 0, :], start=True, stop=True))
                dve([pkey(1)], ["cntf"], lambda e: e.tensor_copy(out=cntf, in_=pbank(1, 32)))
                dve(["cntf"], ["cnti"], lambda e: e.tensor_copy(out=cnti, in_=cntf))
                P.dma(SP, ["cnti"], ["cntd"], lambda e: e.dma_start(out=cntd, in_=cnti[0:1, :]), cpool.nxt())
                dve(["pos", "ecap"], ["pos"], lambda e: e.tensor_tensor(
                    out=pos, in0=pos, in1=ecap.unsqueeze(1).to_broadcast([128, 16, 32]), op=ALU.add))
                for k, selx in enumerate((sel0, sel1)):
                    dve(["pos"] + selk, ["tmpd"], lambda e, selx=selx: e.tensor_tensor(out=tmpd, in0=pos, in1=selx, op=ALU.mult))
                    dve(["tmpd"], ["destf"], lambda e, k=k: e.tensor_reduce(
                        out=destf.rearrange("p (t k) -> p t k", k=2)[:, :, k], in_=tmpd, axis=AX.X, op=ALU.add))
                dve(["destf"], ["desti"], lambda e: e.tensor_copy(out=desti, in_=destf))
                sp_load(fillt, fillv_d, ["fillt"], cpool.nxt())
                P.dma(SP, ["fillt"], ["table"], lambda e: e.dma_start(
                    out=table.rearrange("(p r) c -> p (r c)", p=128), in_=fillt), cpool.nxt())
                for t in range(16):
                    for k in range(2):
                        P.dma(POOL, ["desti", "vals", "table"], [("tabw", t, k)], lambda e, t=t, k=k: e.indirect_dma_start(
                            out=table, out_offset=bass.IndirectOffsetOnAxis(ap=desti[:, t * 2 + k:t * 2 + k + 1], axis=0),
                            in_=vals[:, (t * 2 + k) * 4:(t * 2 + k) * 4 + 4], in_offset=None,
                            oob_is_err=False), tpool.nxt())
                tabkeys = [("tabw", t, k) for t in range(16) for k in range(2)]
                if debug is not None and debug["stage"] == "meta":
                    dbg_dump([(destf, 32, ["destf"]), (cntf, 32, ["cntf"]), (wsl.rearrange("p a b -> p (a b)"), 32, [("wkk", t) for t in range(16)]),
                              (sel0.rearrange("p a b -> p (a b)"), 512, selk), (sel1.rearrange("p a b -> p (a b)"), 512, selk)])

            if debug is None or debug["stage"] in ("moe",):
                P.fence()
                o = PH0 + (MSLOT - NSLOT) * SLOT_B
                NXG = 5
                idxt = [sb(o + i * 16, [4], I32) for i in range(NXG)]; o += 128
                xg = [sb(o + i * 4096, [2048], BF16) for i in range(NXG)]; o += NXG * 4096
                xgT = sb(o, [16, 128], BF16); o += 4096
                sAm = sb(o, [512]); o += 2048
                actm = sb(o, [512], BF16); o += 1024
                actTm = sb(o, [4, 128], BF16); o += 1024
                NY = 4
                Ysb = [sb(o + i * 8192, [2048]) for i in range(NY)]; o += NY * 8192
                MOE_END = o
                assert o <= PH_END

                regs = {}
                if target is not None:
                    regs[target] = handle.alloc_register("cnt_" + target)
                tcount = [0]

                def gather_ops(ei, t, xb):
                    row0 = ei * CAP + t * 128
                    P.dma(SP, tabkeys + ["table"], [("idxt", xb)], lambda e: e.dma_start(out=idxt[xb], in_=table[row0:row0 + 128, :]), ixsem[xb])
                    P.dma(POOL, [("idxt", xb)] + [("h2d", i) for i in range(16)], [("xg", xb)], lambda e: e.indirect_dma_start(
                        out=xg[xb], out_offset=None, in_=h2d, in_offset=bass.IndirectOffsetOnAxis(ap=idxt[xb][:, 0:1], axis=0),
                        oob_is_err=False), gasem[xb])

                NSTAT = int(os.environ.get('K_NSTAT', '2'))

                def tile_ops(ei, t, wa, wak, wb, wbk, wc, wck):
                    n = tcount[0]
                    tcount[0] += 1
                    b = n % NY
                    if t < NSTAT:
                        xb = (ei % 2) * NSTAT + t
                    else:
                        xb = NXG - 1
                        gather_ops(ei, t, xb)
                    tb0 = PS[0][:, 0:512].bitcast(BF16)
                    tb1 = PS[0][:, 512:1024].bitcast(BF16)
                    for hb_, tb in enumerate((tb0, tb1)):
                        def emit(e, hb_=hb_, tb=tb):
                            ins = None
                            for k in range(8):
                                dc = hb_ * 8 + k
                                src_cols = xg[xb][:, dc:2048:16] if CONTIG else xg[xb][:, dc * 128:(dc + 1) * 128]
                                ins = e.transpose(tb[:, k * 128:(k + 1) * 128], src_cols, identB)
                            return ins
                        pe_group([("xg", xb), "identB"], [pkey(hb_)], emit)
                    act_fn(xgT[:, 0:8, :], tb0.rearrange("p (k t) -> p k t", k=8), AF.Copy, [pkey(0)], [("xgT", 0)])
                    dve([pkey(1)], [("xgT", 1)], lambda e: e.tensor_copy(out=xgT[:, 8:16, :], in_=tb1.rearrange("p (k t) -> p k t", k=8)))
                    pe_group([wak, ("xgT", 0), ("xgT", 1)], [pkey(2)], lambda e: mm_acc(e, pbank(2), [(xgT[:, dc, :], wa[:, dc, :]) for dc in range(16)]))
                    pe_group([wbk, ("xgT", 0), ("xgT", 1)], [pkey(3)], lambda e: mm_acc(e, pbank(3), [(xgT[:, dc, :], wb[:, dc, :]) for dc in range(16)]))
                    act_fn(sAm, pbank(2), AF.Silu, [pkey(2)], ["sAm"])
                    dve(["sAm", pkey(3)], ["actm"], lambda e: e.tensor_tensor(out=actm, in0=sAm, in1=pbank(3), op=ALU.mult))

                    def emit(e):
                        ins = None
                        for k in range(4):
                            ins = e.transpose(tb0[:, k * 128:(k + 1) * 128], actm[:, k * 128:(k + 1) * 128], identB)
                        return ins
                    pe_group(["actm", "identB"], [pkey(0)], emit)
                    act_fn(actTm, tb0[:, 0:512].rearrange("p (k t) -> p k t", k=4), AF.Copy, [pkey(0)], ["actTm"])
                    for nb in range(4):
                        pe_group([wck, "actTm"], [pkey(4 + nb)], lambda e, nb=nb: mm_acc(
                            e, pbank(4 + nb), [(actTm[:, fc, :], wc[:, fc, nb * 512:(nb + 1) * 512]) for fc in range(4)]))
                    act_fn(Ysb[b][:, 0:1024], PS[2][:, :], AF.Copy, [pkey(4), pkey(5)], [("Ysb", b, 0)])
                    dve([pkey(6), pkey(7)], [("Ysb", b, 1)], lambda e: e.tensor_copy(out=Ysb[b][:, 1024:2048], in_=PS[3][:, :]))
                    for hf in range(2):
                        P.dma(POOL, [("Ysb", b, hf), ("idxt", xb)], [("Ybuf", n, hf)], lambda e, hf=hf: e.indirect_dma_start(
                            out=Ybuf4, out_offset=bass.IndirectOffsetOnAxis(ap=idxt[xb][:, 1 + hf:2 + hf], axis=0),
                            in_=Ysb[b][:, hf * 1024:(hf + 1) * 1024], in_offset=None,
                            oob_is_err=False), scsem[b * 2 + hf])

                MAXT = TOKC // 128

                GRP = 4

                def tiles_from(ei, t, tend, rv, ws):
                    if t >= tend:
                        return
                    with P.guard(lambda h: rv > t * 128):
                        tile_ops(ei, t, *ws)
                        tiles_from(ei, t + 1, tend, rv, ws)

                for t_ in range(NSTAT):
                    gather_ops(0, t_, t_)
                for ei in range(NE):
                    if ei + 1 < NE:
                        for t_ in range(NSTAT):
                            gather_ops(ei + 1, t_, ((ei + 1) % 2) * NSTAT + t_)
                    wa, wak = m_get(3 * ei)
                    wb, wbk = m_get(3 * ei + 1)
                    wc, wck = m_get(3 * ei + 2)
                    rv = None
                    for eng in P.engines:
                        r = P.raw(eng, ["cntd"], lambda h, ei=ei: h.reg_load(regs[target], cntd[0:1, ei:ei + 1]))
                    if target is not None:
                        rv = handle.snap(regs[target])
                    for g0 in range(0, MAXT, GRP):
                        tiles_from(ei, g0, g0 + GRP, rv, (wa, wak, wb, wbk, wc, wck))
                    m_rel(3 * ei)
                    m_rel(3 * ei + 1)
                    m_rel(3 * ei + 2)
                    m_pump()

                P.fence()
                o = RING0
                gate2b = sb(o, [2048]); o += 8192
                fgb = sb(o, [2048]); o += 8192
                NF = 3
                xt8 = [sb(o + i * 8192, [2048]) for i in range(NF)]; o += NF * 8192
                y0t = [sb(o + i * 8192, [2048]) for i in range(NF)]; o += NF * 8192
                y1t = [sb(o + i * 8192, [2048]) for i in range(NF)]; o += NF * 8192
                junk8 = sb(o, [2048], BF16); o += 4096
                assert o <= PH_END
                ybkeys = [("Ybuf", n, hf) for n in range(tcount[0]) for hf in range(2)]
                sp_load(gate2b, modd[0, 5 * D:6 * D].partition_broadcast(128), ["gate2b"], cpool.nxt(), reads=modd_keys)
                sp_load(fgb, fg[0].partition_broadcast(128), ["fgb"], cpool.nxt())

                def fin_loads(i):
                    b = i % NF
                    P.dma(SP, [("x1s", i)], [("xt8", b)], lambda e: e.dma_start(
                        out=xt8[b], in_=x1s[i * 128:(i + 1) * 128, :]), fsem[b])
                    P.dma(SP, ybkeys, [("y0t", b)], lambda e: e.dma_start(
                        out=y0t[b], in_=Ybuf[i * 128:(i + 1) * 128, :]), fsem[3 + b])
                    P.dma(SP, ybkeys, [("y1t", b)], lambda e: e.dma_start(
                        out=y1t[b], in_=Ybuf[TOKC + i * 128:TOKC + (i + 1) * 128, :]), fsem[6 + b])

                fin_loads(0)
                fin_loads(1)
                for i in range(16):
                    b = i % NF
                    xk = ("xt8", b)
                    if i + 2 < 16:
                        fin_loads(i + 2)
                    act_fn(y0t[b], y0t[b], AF.Copy, [("y0t", b), ("wkk", i)], [("y0t", b)], scale=wsl[:, i, 0:1])
                    dve([("y0t", b), ("y1t", b), ("wkk", i)], [("y0t", b)], lambda e, b=b, i=i: e.scalar_tensor_tensor(
                        out=y0t[b], in0=y1t[b], scalar=wsl[:, i, 1:2], in1=y0t[b], op0=ALU.mult, op1=ALU.add))
                    dve([("y0t", b), "gate2b"], [("y0t", b)], lambda e, b=b: e.tensor_tensor(out=y0t[b], in0=y0t[b], in1=gate2b, op=ALU.mult))
                    dve([("y0t", b), xk], [xk], lambda e, b=b: e.tensor_tensor(out=xt8[b], in0=xt8[b], in1=y0t[b], op=ALU.add))
                    ssa = small[:, 128 + i:129 + i]
                    rsa = small[:, 144 + i:145 + i]
                    act_fn(junk8, xt8[b], AF.Square, [xk], [("ss8", i)], accum=ssa)
                    rms_rstd(ssa, rsa, ("ss8", i), ("rs8", i))
                    dve([xk, ("rs8", i), "fgb"], [xk], lambda e, b=b, rsa=rsa: e.scalar_tensor_tensor(
                        out=xt8[b], in0=xt8[b], scalar=rsa, in1=fgb, op0=ALU.mult, op1=ALU.mult))
                    P.dma(SP, [xk], [("y", i)], lambda e, b=b, i=i: e.dma_start(
                        out=y[i * 128:(i + 1) * 128, :], in_=xt8[b]), fsem[9 + b])

            outkeys = [("y", i) for i in range(16)] if debug is None else [k for k in P.state.keys() if isinstance(k, tuple) and k[0] in ("dbg", "y")]
            P.final_wait(SP, outkeys)

        with nc.Block() as block:
            @block.tensor
            def _(e):
                program("pe", e)

            @block.scalar
            def _(e):
                program("act", e)

            @block.vector
            def _(e):
                program("dve", e)

            @block.gpsimd
            def _(e):
                program("pool", e)

            @block.sync
            def _(e):
                program("sp", e)
    return nc


_NC_CACHE = {}


def _vals_const():
    v = np.zeros((128, 16, 2, 4), np.int32)
    p = np.arange(128)[:, None]
    t = np.arange(16)[None, :]
    tok = t * 128 + p
    for k in range(2):
        v[:, :, k, 0] = tok
        v[:, :, k, 1] = 2 * (k * TOKC + tok)
        v[:, :, k, 2] = 2 * (k * TOKC + tok) + 1
    return np.ascontiguousarray(v.reshape(128, 128))


def _fill_const():
    r = np.arange(512)
    row = np.zeros((512, 4), np.int32)
    row[:, 1] = 4 * TOKC + (r % 128)
    row[:, 2] = 4 * TOKC + 128 + (r % 128)
    return np.ascontiguousarray(np.broadcast_to(row.reshape(1, 2048), (128, 2048)))


def _host_inputs(inputs):
    f = np.float32
    x = np.asarray(inputs["x"], f)
    c = np.asarray(inputs["c"], f)
    L = 0
    tab = np.asarray(inputs["rel_bias"], f)[L]
    ext = np.concatenate([tab, np.repeat(tab[:, -1:], 400, axis=1)], axis=1)
    k = np.arange(128)[:, None]
    q = np.arange(128)[None, :]
    bt = np.empty((128, 16, 3, 128), f)
    for dl in range(3):
        idx = 256 + dl * 128 + q - k
        bt[:, :, dl, :] = np.transpose(ext[:, idx], (1, 0, 2))
    bt[64:, :, 0, :64] = NEG
    wr = np.concatenate([np.asarray(inputs["w_group"], f)[L]] +
                        [np.asarray(inputs["w_expert"], f)[L, g] for g in range(4)], axis=1)
    shared = {
        "ident": np.eye(128, dtype=f),
        "ada_w": np.ascontiguousarray(np.asarray(inputs["ada_w"], f)[L]),
        "ada_b": np.ascontiguousarray(np.asarray(inputs["ada_b"], f)[L][None, :]),
        "g1T": np.ascontiguousarray(np.asarray(inputs["norm1_g"], f)[L].reshape(16, 128).T),
        "g2T": np.ascontiguousarray(np.asarray(inputs["norm2_g"], f)[L].reshape(16, 128).T),
        "lnG": np.ascontiguousarray(np.asarray(inputs["gmlp_ln_g"], f)[L][None, :]),
        "lnB": np.ascontiguousarray(np.asarray(inputs["gmlp_ln_b"], f)[L][None, :]),
        "bS": np.ascontiguousarray(np.asarray(inputs["gmlp_b_s"], f)[L].reshape(1, 1024)),
        "wS": np.ascontiguousarray(np.asarray(inputs["gmlp_w_s"], f)[L]),
        "biasT": np.ascontiguousarray(bt.reshape(128, 16 * 384)),
        "tabc": np.ascontiguousarray(tab[:, 512][None, :]),
        "w_in": np.ascontiguousarray(np.asarray(inputs["w_in"], f)[L]),
        "wbrA": np.ascontiguousarray(np.asarray(inputs["w_branch_a"], f)[L]),
        "wbrB": np.ascontiguousarray(np.asarray(inputs["w_branch_b"], f)[L]),
        "wout": np.ascontiguousarray(np.asarray(inputs["w_out"], f)[L]),
        "wr": np.ascontiguousarray(wr),
        "w1": np.ascontiguousarray(np.asarray(inputs["w1"], f)[L].reshape(NE, D, 512)),
        "w3": np.ascontiguousarray(np.asarray(inputs["w3"], f)[L].reshape(NE, D, 512)),
        "w2": np.ascontiguousarray(np.asarray(inputs["w2"], f)[L].reshape(NE, 512, D)),
        "fg": np.ascontiguousarray(np.asarray(inputs["final_g"], f)[None, :]),
        "g2row": np.ascontiguousarray(np.asarray(inputs["norm2_g"], f)[L][None, :]),
        "g1row": np.ascontiguousarray(np.asarray(inputs["norm1_g"], f)[L][None, :]),
        "Utri": np.triu(np.ones((128, 128), f), 1),
        "OnesM": np.ones((128, 128), f),
        "ecap": np.ascontiguousarray(np.broadcast_to((np.arange(NE) * CAP).astype(f)[None, :], (128, NE))),
        "vals": _vals_const(),
        "fillv": _fill_const(),
    }
    in_maps = []
    for core in range(8):
        b = core // 4
        s0 = (core % 4) * TOKC
        xe = np.zeros((TOKC + HALO, D), f)
        if s0 == 0:
            xe[HALO:] = x[b, 0:TOKC]
        else:
            xe[:] = x[b, s0 - HALO:s0 + TOKC]
        flags = np.ones((128, 2), f)
        if s0 == 0:
            flags[:, 0] = 0.0
        m = dict(shared)
        m["xe"] = xe
        m["cT"] = np.ascontiguousarray(c[b].reshape(16, 128).T)
        m["flags"] = flags
        in_maps.append(m)
    return in_maps


def kernel(**inputs):
    if "nc" not in _NC_CACHE:
        _NC_CACHE["nc"] = build()
    nc = _NC_CACHE["nc"]
    in_maps = _host_inputs(inputs)
    res = run_bass_kernel_spmd(nc, in_maps, core_ids=list(range(8)))
    out = np.empty((2, 8192, D), np.float32)
    for core in range(8):
        b = core // 4
        s0 = (core % 4) * TOKC
        out[b, s0:s0 + TOKC] = res.results[core]["y"]
    return out
```

```python
import contextlib
import os
import numpy as np
import concourse.bass as bass
import concourse.mybir as mybir
from concourse.bass_utils import run_bass_kernel_spmd

F32 = mybir.dt.float32
BF16 = mybir.dt.bfloat16
I32 = mybir.dt.int32
AF = mybir.ActivationFunctionType
ALU = mybir.AluOpType
AX = mybir.AxisListType

D = 2048
TOKC = 2048
TP = 1024
HALO = 512
NPASS = 2
EPS = 1e-6
NSLOT = 5
SLOT_B = 16384
NEG = -30000.0
EVAC_SPLIT = os.environ.get('K_EVAC', '0') == '1'
ADA_DVE = os.environ.get('K_ADA', '1') == '1'
PREFETCH = os.environ.get('K_PRE', '1') == '1'
CONTIG = os.environ.get('K_CONTIG', '1') == '1'
NE = 32
CAP = 2048
MSLOT = 8
BIGIDX = 1 << 24


class Sem:
    def __init__(self, h, shared=False):
        self.h = h
        self.n = 0
        self.shared = shared


class Eng:
    def __init__(self, name, sem, selfwait=True):
        self.name = name
        self.sem = sem
        self.prog = []
        self.waited = {}
        self.selfwait = selfwait


class Prog:
    def __init__(self, target, handle):
        self.state = {}
        self.floor = {}
        self.nofloor = set()
        self.target = target
        self.handle = handle
        self.inc_log = []
        self.alias = {}
        self.engines = []

    def fence(self):
        for k, st in self.state.items():
            if isinstance(k, tuple) and k[0] in ("ring", "mring"):
                continue
            evs = ([st[0]] if st[0] else []) + list(st[1].items())
            for s, v in evs:
                if s.shared:
                    v = s.n
                if self.floor.get(s, 0) < v:
                    self.floor[s] = v

    def _expand(self, keys):
        out = []
        for k in keys:
            out.extend(self.alias.get(k, (k,)) if isinstance(k, tuple) else (k,))
        return out

    def _deps(self, reads, writes, eng=None):
        reads = self._expand(reads)
        writes = self._expand(writes)
        w = {}

        def add(ev):
            if ev is None:
                return
            s, v = ev
            if s.shared:
                v = s.n
            if w.get(s, 0) < v:
                w[s] = v

        if eng is not None and eng.name not in self.nofloor:
            for s, v in self.floor.items():
                add((s, v))

        for k in reads:
            st = self.state.get(k)
            if st:
                add(st[0])
        for k in writes:
            st = self.state.get(k)
            if st:
                add(st[0])
                for s, v in st[1].items():
                    add((s, v))
        return w

    def _commit(self, reads, writes, ev):
        reads = self._expand(reads)
        writes = self._expand(writes)
        s, v = ev
        for k in reads:
            st = self.state.setdefault(k, [None, {}])
            if st[1].get(s, 0) < v:
                st[1][s] = v
        for k in writes:
            self.state[k] = [ev, {}]

    def op(self, eng, reads, writes, emit):
        w = self._deps(reads, writes, eng)
        wl = []
        for s, v in w.items():
            if s is eng.sem and not eng.selfwait:
                continue
            if eng.waited.get(s, 0) < v:
                eng.waited[s] = v
                wl.append((s, v))
        self.inc_log.append((eng.name, eng.sem, 1))
        eng.sem.n += 1
        ev = (eng.sem, eng.sem.n)
        sem = eng.sem

        if eng.name == self.target:
            e = self.handle
            for s, v in wl:
                e.wait_ge(s.h, v)
            emit(e).then_inc(sem.h, 1)
        self._commit(reads, writes, ev)

    def dma(self, eng, reads, writes, emit, dsem):
        w = self._deps(reads, writes, eng)
        if dsem.n > 0 and w.get(dsem, 0) < dsem.n:
            w[dsem] = dsem.n
        wl = []
        for s, v in w.items():
            if eng.waited.get(s, 0) < v:
                eng.waited[s] = v
                wl.append((s, v))
        self.inc_log.append((eng.name, dsem, 16))
        dsem.n += 16
        ev = (dsem, dsem.n)

        if eng.name == self.target:
            e = self.handle
            for s, v in wl:
                e.wait_ge(s.h, v)
            emit(e).then_inc(dsem.h, 16)
        self._commit(reads, writes, ev)

    def raw(self, eng, reads, emit):
        w = self._deps(reads, [], eng)
        wl = []
        for s, v in w.items():
            if s is eng.sem and not eng.selfwait:
                continue
            if eng.waited.get(s, 0) < v:
                eng.waited[s] = v
                wl.append((s, v))
        if eng.name == self.target:
            for s, v in wl:
                self.handle.wait_ge(s.h, v)
            return emit(self.handle)
        return None

    @contextlib.contextmanager
    def guard(self, cond_fn):
        snap_waited = {e.name: dict(e.waited) for e in self.engines}
        log0 = len(self.inc_log)
        before = {}
        ctx = None
        if self.target is not None:
            ctx = self.handle.If(cond_fn(self.handle))
            ctx.__enter__()
        sems_seen = {}
        n_at_entry = lambda sem: sems_seen.setdefault(id(sem), sem.n)
        entry_vals = {}
        class _Peek:
            pass
        all_sems = set()
        for (_, sem, _) in self.inc_log:
            all_sems.add(sem)
        entry_vals = {sem: sem.n for sem in all_sems}
        try:
            yield
        finally:
            if ctx is not None:
                ctx.__exit__(None, None, None)
                comp = {}
                order = []
                for (en, sem, amt) in self.inc_log[log0:]:
                    if en == self.target:
                        if sem not in comp:
                            comp[sem] = 0
                            order.append(sem)
                        comp[sem] += amt
                ectx = self.handle.Else()
                ectx.__enter__()
                for sem in order:
                    v0 = entry_vals.get(sem, 0)
                    if v0 > 0:
                        self.handle.wait_ge(sem.h, v0)
                    self.handle.sem_inc(sem.h, comp[sem])
                ectx.__exit__(None, None, None)
            for e in self.engines:
                e.waited = snap_waited[e.name]

    def final_wait(self, eng, keys):
        w = self._deps(keys, keys)
        wl = list(w.items())

        if eng.name == self.target:
            for s, v in wl:
                self.handle.wait_ge(s.h, v)


def build(debug=None):
    nc = bass.Bass("TRN2", target_bir_lowering=False)

    def din(name, shape):
        return nc.dram_tensor(name, list(shape), F32, kind="ExternalInput").ap()

    xe = din("xe", [TOKC + HALO, D])
    cT = din("cT", [128, 16])
    flags_d = din("flags", [128, 2])
    ident_d = din("ident", [128, 128])
    ada_w = din("ada_w", [D, 6 * D])
    ada_b = din("ada_b", [1, 6 * D])
    g1T_d = din("g1T", [128, 16])
    g2T_d = din("g2T", [128, 16])
    lnG = din("lnG", [1, 1024])
    lnB = din("lnB", [1, 1024])
    bS = din("bS", [1, 1024])
    wS = din("wS", [8, 128, 128])
    biasT = din("biasT", [128, 16 * 384])
    tabc_d = din("tabc", [1, 16])
    w_in = din("w_in", [D, 9216])
    wbrA = din("wbrA", [1024, D])
    wbrB = din("wbrB", [1024, D])
    wout = din("wout", [D, D])
    wr_d = din("wr", [D, 36])
    w1 = din("w1", [NE, D, 512])
    w3 = din("w3", [NE, D, 512])
    w2 = din("w2", [NE, 512, D])
    fg = din("fg", [1, D])
    g2row = din("g2row", [1, D])
    g1row = din("g1row", [1, D])
    Utri_d = din("Utri", [128, 128])
    Ones_d = din("OnesM", [128, 128])
    ecap_d = din("ecap", [128, 32])
    vals_d = nc.dram_tensor("vals", [128, 128], I32, kind="ExternalInput").ap()
    fillv_d = nc.dram_tensor("fillv", [128, 2048], I32, kind="ExternalInput").ap()
    h2d = nc.dram_tensor("h2d", [TOKC, D], BF16, kind="Internal").ap()
    table = nc.dram_tensor("table", [NE * CAP, 4], I32, kind="Internal").ap()
    cntd = nc.dram_tensor("cntd", [1, NE], I32, kind="Internal").ap()
    Ybuf4 = nc.dram_tensor("Ybuf", [4 * TOKC + 256, D // 2], F32, kind="Internal").ap()
    Ybuf = Ybuf4.rearrange("(r h) c -> r (h c)", h=2)
    y = nc.dram_tensor("y", [TOKC, D], F32, kind="ExternalOutput").ap()
    x1s = nc.dram_tensor("x1s", [TOKC, D], F32, kind="Internal").ap()
    modd = nc.dram_tensor("modd", [1, 6 * D], F32, kind="Internal").ap()
    dbg_out = None
    if debug is not None:
        dbg_out = nc.dram_tensor("dbg", [128, debug["words"]], F32, kind="ExternalOutput").ap()

    es = contextlib.ExitStack()
    with es:
        es.enter_context(nc.allow_low_precision("bf16 matmul operands by design"))
        ARENA_W = 53200
        arena = es.enter_context(nc.sbuf_tensor("arena", [128, ARENA_W], F32))
        PS = [es.enter_context(nc.psum_tensor(f"ps{i}", [128, 1024], F32)) for i in range(4)]

        SEMH = {}
        for _n in (["s_pe", "s_act", "s_dve", "s_pool", "s_sp", "s_const", "s_misc"] + [f"s_ring{i}" for i in range(NSLOT)] + [f"s_ringb{i}" for i in range(NSLOT)] + [f"s_adb{i}" for i in range(3)] +
                   [f"s_x{i}" for i in range(2)] + [f"s_o{i}" for i in range(2)] + [f"s_ad{i}" for i in range(3)] +
                   [f"s_mr{i}" for i in range(MSLOT)] + [f"s_ix{i}" for i in range(5)] + [f"s_ga{i}" for i in range(5)] +
                   [f"s_sc{i}" for i in range(8)] + [f"s_h{i}" for i in range(2)] + [f"s_y{i}" for i in range(4)] + ["s_tab"] +
                   [f"s_cp{i}" for i in range(8)] + [f"s_f{i}" for i in range(12)] + [f"s_tp{i}" for i in range(4)] + [f"s_ab{i}" for i in range(2)] + [f"s_md{i}" for i in range(2)]):
            SEMH[_n] = es.enter_context(nc.semaphore(_n))

        def program(target, handle):
            def newsem(name):
                return Sem(SEMH[name])

            PE = Eng("pe", newsem("s_pe"), selfwait=False)
            ACT = Eng("act", newsem("s_act"))
            DVE = Eng("dve", newsem("s_dve"))
            POOL = Eng("pool", newsem("s_pool"))
            SP = Eng("sp", newsem("s_sp"))
            ring_sem = [newsem(f"s_ring{i}") for i in range(NSLOT)]
            ring_semb = [newsem(f"s_ringb{i}") for i in range(NSLOT)]
            adsemb = [newsem(f"s_adb{i}") for i in range(3)]
            xsem = [newsem(f"s_x{i}") for i in range(2)]
            osem = [newsem(f"s_o{i}") for i in range(2)]
            csem = newsem("s_const")
            csem.shared = True
            msem = newsem("s_misc")
            msem.shared = True
            adsem = [newsem(f"s_ad{i}") for i in range(3)]
            mr_sem = [newsem(f"s_mr{i}") for i in range(MSLOT)]
            ixsem = [newsem(f"s_ix{i}") for i in range(5)]
            gasem = [newsem(f"s_ga{i}") for i in range(5)]
            scsem = [newsem(f"s_sc{i}") for i in range(8)]

            class SemPool:
                def __init__(self, sems):
                    self.sems = sems
                    self.i = 0

                def nxt(self):
                    sm = self.sems[self.i % len(self.sems)]
                    self.i += 1
                    return sm

            cpool = SemPool([newsem(f"s_cp{i}") for i in range(8)])
            tpool = SemPool([newsem(f"s_tp{i}") for i in range(4)])
            absem = [newsem(f"s_ab{i}") for i in range(2)]
            fsem = [newsem(f"s_f{i}") for i in range(12)]
            mdsem = [newsem(f"s_md{i}") for i in range(2)]
            hsem = [newsem(f"s_h{i}") for i in range(2)]
            ysem = [newsem(f"s_y{i}") for i in range(4)]
            tabsem = newsem("s_tab")
            tabsem.shared = True
            P = Prog(target, handle)
            P.nofloor.add("pool")
            P.engines = [PE, ACT, DVE, POOL, SP]

            def sb(off, shape, dt=F32, parts=(0, 128)):
                esz = 4 if dt in (F32, I32) else 2
                n = int(np.prod(shape))
                nbytes = n * esz
                assert off % 4 == 0 and nbytes % 4 == 0
                assert off + nbytes <= ARENA_W * 4, (off, nbytes)
                ap = arena[parts[0]:parts[1], off // 4:(off + nbytes) // 4]
                if dt != F32:
                    ap = ap.bitcast(dt)
                if len(shape) == 2:
                    ap = ap.rearrange("p (a b) -> p a b", a=shape[0], b=shape[1])
                elif len(shape) == 3:
                    ap = ap.rearrange("p (a b c) -> p a b c", a=shape[0], b=shape[1], c=shape[2])
                return ap

            def pbank(b, n=512, off=0):
                return PS[b // 2][:, (b % 2) * 512 + off:(b % 2) * 512 + off + n]

            def pkey(b):
                return ("ps", b)

            C0 = 0
            o = C0
            identF = sb(o, [128]); o += 512
            identB = sb(o, [128], BF16); o += 256
            modT = sb(o, [96]); o += 384
            effT = sb(o, [32]); o += 128
            g1T = sb(o, [16]); o += 64
            g2T = sb(o, [16]); o += 64
            tabc = sb(o, [16]); o += 64
            flags = sb(o, [2]); o += 8
            ones8 = sb(o, [8]); o += 32
            wr = sb(o, [16, 36]); o += 2304
            WsT = sb(o, [8, 128], BF16); o += 2048
            small = sb(o, [256]); o += 1024
            rt = sb(o, [256]); o += 1024
            urow = sb(o, [128], BF16, parts=(0, 1)); o += 256
            vrow = sb(o, [128], BF16, parts=(0, 1)); o += 256
            sel0 = sb(o, [16, 32]); o += 2048
            sel1 = sb(o, [16, 32]); o += 2048
            wsl = sb(o, [16, 2]); o += 128
            ecap = sb(o, [32]); o += 128
            vals = sb(o, [128], I32); o += 512
            CONST_END = o
            RING0 = (CONST_END + 63) // 64 * 64
            PH0 = RING0 + NSLOT * SLOT_B
            PH_END = ARENA_W * 4

            def ring_view(slot, a, b):
                return sb(RING0 + slot * SLOT_B, [a, b], BF16)

            class WS:
                units = []
                released = []
                nloaded = 0

            def w_add(src, a, b):
                WS.units.append((src, a, b))
                WS.released.append(False)
                return len(WS.units) - 1

            def w_pump():
                while WS.nloaded < len(WS.units):
                    u = WS.nloaded
                    if u >= NSLOT and not WS.released[u - NSLOT]:
                        break
                    src, a, b = WS.units[u]
                    slot = u % NSLOT
                    dst = ring_view(slot, a, b)
                    P.alias[("ring", slot)] = (("ring", slot, 0), ("ring", slot, 1))
                    h_ = a // 2
                    P.dma(POOL, [], [("ring", slot, 0)],
                          lambda e, dst=dst, src=src: e.dma_start(out=dst[:, 0:h_, :], in_=src[:, 0:h_, :]),
                          ring_sem[slot])
                    P.dma(POOL, [], [("ring", slot, 1)],
                          lambda e, dst=dst, src=src: e.dma_start(out=dst[:, h_:a, :], in_=src[:, h_:a, :]),
                          ring_semb[slot])
                    WS.nloaded += 1

            def w_get(u):
                w_pump()
                assert u < WS.nloaded, ("weight unit not loadable", u, WS.nloaded)
                src, a, b = WS.units[u]
                slot = u % NSLOT
                return ring_view(slot, a, b), ("ring", slot)

            def w_rel(u):
                WS.released[u] = True
                w_pump()

            def win_blk(c0):
                return w_in[:, c0:c0 + 512].rearrange("(c p) n -> p c n", p=128)

            plan = []
            for p in range(NPASS):
                pl = {}
                pl["k"] = [None, None]
                pl["vb"] = [None, None]
                pl["q"] = [None, None]
                for hh in range(2):
                    pl["k"][hh] = w_add(win_blk(3072 + hh * 512), 16, 512)
                    pl["vb"][hh] = w_add(win_blk(4096 + hh * 512), 16, 512)
                    pl["q"][hh] = w_add(win_blk(2048 + hh * 512), 16, 512)
                pl["u"] = [w_add(win_blk(0 + i * 512), 16, 512) for i in range(2)]
                pl["v"] = [w_add(win_blk(1024 + i * 512), 16, 512) for i in range(2)]
                pl["gA"] = [None] * 4
                pl["gB"] = [None] * 4
                pl["brA"] = [None] * 2
                pl["brB"] = [None] * 2
                for q in range(4):
                    pl["gA"][q] = w_add(win_blk(5120 + q * 512), 16, 512)
                    pl["gB"][q] = w_add(win_blk(7168 + q * 512), 16, 512)
                    if q % 2 == 0:
                        hf = q // 2
                        pl["brA"][hf] = w_add(wbrA[:, hf * 1024:(hf + 1) * 1024].rearrange("(c p) n -> p c n", p=128), 8, 1024)
                        pl["brB"][hf] = w_add(wbrB[:, hf * 1024:(hf + 1) * 1024].rearrange("(c p) n -> p c n", p=128), 8, 1024)
                pl["wo"] = [w_add(wout[:, cu * 512:(cu + 1) * 512].rearrange("(c p) n -> p c n", p=128), 16, 512) for cu in range(4)]
                plan.append(pl)

            def act_fn(out, in_, func, reads, writes, scale=1.0, bias=0.0, accum=None):
                def emit(e):
                    kw = {}
                    if accum is not None:
                        kw["accum_out"] = accum
                    return e.activation(out=out, in_=in_, func=func, bias=bias, scale=scale, **kw)
                P.op(ACT, reads, writes, emit)

            def dve(reads, writes, emit):
                P.op(DVE, reads, writes, emit)

            def pe_group(reads, writes, emit):
                P.op(PE, reads, writes, emit)

            def mm_acc(e, out, pairs):
                n = len(pairs)
                ins = None
                for i, (l, r) in enumerate(pairs):
                    ins = e.matmul(out, l, r, start=(i == 0), stop=(i == n - 1))
                return ins

            def sp_load(dst, src, writes, sem, reads=()):
                P.dma(SP, list(reads), list(writes), lambda e: e.dma_start(out=dst, in_=src), sem)

            sp_load(identF, ident_d, ["identF"], cpool.nxt())
            sp_load(flags, flags_d, ["flags"], cpool.nxt())
            sp_load(g1T, g1T_d, ["g1T"], cpool.nxt())
            sp_load(g2T, g2T_d, ["g2T"], cpool.nxt())
            sp_load(tabc, tabc_d[0].partition_broadcast(128), ["tabc"], cpool.nxt())
            sp_load(wr, wr_d.rearrange("(c p) n -> p c n", p=128), ["wr"], cpool.nxt())
            sp_load(ecap, ecap_d, ["ecap"], cpool.nxt())
            sp_load(vals, vals_d, ["vals"], cpool.nxt())
            scT = sb(PH0, [16])
            cTs = sb(PH0 + 64, [16])
            one11 = sb(PH0 + 128, [1])
            sp_load(cTs, cT, ["cTs"], cpool.nxt())
            dve(["identF"], ["identB"], lambda e: e.tensor_copy(out=identB, in_=identF))
            dve([], ["ones8"], lambda e: e.memset(ones8, 1.0))
            dve([], ["uvrow"], lambda e: e.memset(urow[:, 0:64], NEG))
            dve([], ["uvrow"], lambda e: e.memset(urow[:, 64:128], 0.0))
            dve([], ["uvrow"], lambda e: e.memset(vrow[:, 0:64], 0.0))
            dve([], ["uvrow"], lambda e: e.memset(vrow[:, 64:128], 1.0))
            dve([], ["one11"], lambda e: e.memset(one11, 1.0))
            act_fn(scT, cTs, AF.Silu, ["cTs"], ["scT"])

            wsf = sb(PH0 + 1024, [8, 128])
            P.dma(SP, [], ["wsf"], lambda e: e.dma_start(out=wsf, in_=wS.rearrange("g t s -> t g s")), cpool.nxt())
            dve(["wsf"], ["wsf"], lambda e: e.memset(wsf[0:64, :, 64:128], 0.0))
            for hb in range(2):
                def emit(e, hb=hb):
                    ins = None
                    for g in range(4):
                        gg = hb * 4 + g
                        ins = e.transpose(pbank(hb, 128, g * 128), wsf[:, gg, :], identF)
                    return ins
                pe_group(["wsf", "identF"], [pkey(hb)], emit)
                dve([pkey(hb)], ["WsT"], lambda e, hb=hb: e.tensor_copy(
                    out=WsT[:, hb * 4:(hb + 1) * 4, :], in_=pbank(hb).rearrange("p (g t) -> p g t", g=4)))

            AD0 = PH0 + 5120
            NAD = 3
            adbuf = [sb(AD0 + i * 32768, [16, 512]) for i in range(NAD)]
            abrow = sb(AD0 + NAD * 32768, [512], parts=(0, 1))
            abrow2 = sb(AD0 + NAD * 32768 + 2048, [512], parts=(0, 1))
            mrow = [sb(AD0 + NAD * 32768 + 4096 + i * 2048, [512], parts=(0, 1)) for i in range(2)]
            accb = [sb(AD0 + NAD * 32768 + 8192 + i * 2048, [512]) for i in range(2)]
            NB = 24
            MTB = 7
            def ada_load(blk):
                bi = blk % NAD
                P.alias[("adbuf", bi)] = (("adbuf", bi, 0), ("adbuf", bi, 1))
                srcv = ada_w[:, blk * 512:(blk + 1) * 512].rearrange("(c p) n -> p c n", p=128)
                P.dma(SP, [], [("adbuf", bi, 0)], lambda e: e.dma_start(out=adbuf[bi][:, 0:8, :], in_=srcv[:, 0:8, :]), adsem[bi])
                P.dma(SP, [], [("adbuf", bi, 1)], lambda e: e.dma_start(out=adbuf[bi][:, 8:16, :], in_=srcv[:, 8:16, :]), adsemb[bi])

            for blk in range(min(NAD - 1, NB)):
                ada_load(blk)
            NDV = 10

            def ada_front(blk):
                bi = blk % NAD
                if blk + NAD - 1 < NB:
                    ada_load(blk + NAD - 1)
                ab = abrow if blk % 2 == 0 else abrow2
                abk = ("abrow", blk % 2)
                P.dma(SP, [], [abk], lambda e: e.dma_start(out=ab, in_=ada_b[:, blk * 512:(blk + 1) * 512]), absem[blk % 2])
                pb_ = blk % 2
                ac = accb[blk % 2]
                ack = ("accb", blk % 2)
                pe_group([("adbuf", bi), "scT"], [pkey(pb_)], lambda e: [
                    e.matmul(pbank(pb_)[0:1, :], scT[:, dc:dc + 1], adbuf[bi][:, dc, :], start=(dc == NDV), stop=False)
                    for dc in range(NDV, 16)][-1])
                dve([("adbuf", bi), "scT"], [ack], lambda e: e.tensor_scalar(
                    out=ac, in0=adbuf[bi][:, 0, :], scalar1=scT[:, 0:1], scalar2=None, op0=ALU.mult))
                for dc in range(1, NDV):
                    dve([("adbuf", bi), "scT", ack], [ack], lambda e, dc=dc: e.scalar_tensor_tensor(
                        out=ac, in0=adbuf[bi][:, dc, :], scalar=scT[:, dc:dc + 1], in1=ac, op0=ALU.mult, op1=ALU.add))

            def ada_back(blk):
                ab = abrow if blk % 2 == 0 else abrow2
                abk = ("abrow", blk % 2)
                pb_ = blk % 2
                ac = accb[blk % 2]
                ack = ("accb", blk % 2)
                pe_group([ack, "ones8"], [pkey(pb_)], lambda e: e.matmul(
                    pbank(pb_)[0:1, :], ones8[:, 0:1], ac, start=False, stop=True))
                mr = mrow[blk % 2]
                mrk = ("mrow", blk % 2)
                dve([pkey(pb_), abk], [mrk], lambda e: e.tensor_tensor(
                    out=mr, in0=pbank(pb_)[0:1, :], in1=ab, op=ALU.add))
                P.dma(SP, [mrk], [("modd", blk)], lambda e: e.dma_start(
                    out=modd[:, blk * 512:(blk + 1) * 512], in_=mr), mdsem[blk % 2])

                def emit2(e):
                    ins = None
                    for k in range(4):
                        ins = e.matmul(pbank(MTB)[:, blk * 4 + k:blk * 4 + k + 1], mr[0:1, k * 128:(k + 1) * 128], one11[0:1, 0:1],
                                       start=True, stop=True)
                    return ins
                pe_group([mrk, "one11"], [("mtb", blk)], emit2)

            ada_front(0)
            for blk in range(NB):
                if blk + 1 < NB:
                    ada_front(blk + 1)
                ada_back(blk)
            dve([("mtb", b) for b in range(NB)], ["modT"], lambda e: e.tensor_copy(out=modT, in_=pbank(MTB)[:, 0:96]))
            dve(["modT", "g1T"], ["effT"], lambda e: e.scalar_tensor_tensor(
                out=effT[:, 0:16], in0=modT[:, 16:32], scalar=1.0, in1=g1T, op0=ALU.add, op1=ALU.mult))
            dve(["modT", "g2T"], ["effT"], lambda e: e.scalar_tensor_tensor(
                out=effT[:, 16:32], in0=modT[:, 64:80], scalar=1.0, in1=g2T, op0=ALU.add, op1=ALU.mult))
            shift1 = modT[:, 0:16]
            shift2 = modT[:, 48:64]
            setup_keys = ["modT", "effT", "identF", "identB", "flags", "tabc", "wr", "WsT", "ones8"]
            setup_scratch = ["scT", "cTs", "one11", "wsf"] + [("adbuf", i) for i in range(3)] + \
                [("abrow", i) for i in range(2)] + [("mrow", i) for i in range(2)]
            modd_keys = [("modd", b) for b in range(NB)]

            dbg_done = [False]

            def dbg_dump(items):
                off = 0
                for ap, nw, keys in items:
                    P.dma(SP, list(keys), [("dbg", off)], lambda e, ap=ap, off=off, nw=nw: e.dma_start(
                        out=dbg_out[:, off:off + nw], in_=ap), msem)
                    off += nw
                dbg_done[0] = True

            def rms_rstd(ss_ap, rstd_ap, key_ss, key_rstd):
                act_fn(rstd_ap, ss_ap, AF.Sqrt, [key_ss], [key_rstd], scale=1.0 / D, bias=EPS)
                dve([key_rstd], [key_rstd], lambda e: e.reciprocal(out=rstd_ap, in_=rstd_ap))

            for p in range(NPASS if debug is None else debug.get("npass", NPASS)):
                pl = plan[p]
                row0 = p * TP
                fl = flags[:, p:p + 1]
                o = PH0
                h1own = sb(o, [16, 1024], BF16); o += 32768
                ybT = sb(o, [8, 1024], BF16); o += 16384
                GM0 = o
                h1halo = sb(o, [16, 512], BF16); o += 16384
                MIX0 = o
                xt = [sb(MIX0 + i * 8192, [2048]) for i in range(2)]
                junk = sb(MIX0 + 16384, [2048], BF16)
                P.fence()
                effB1 = sb(MIX0 + 20480, [2048])
                shiftB1 = sb(MIX0 + 28672, [2048])
                g1b = sb(PH0 + 32768, [2048])
                h1tok = [sb(MIX0 + 36864 + i * 4096, [2048], BF16) for i in range(2)]
                sp_load(g1b, g1row[0].partition_broadcast(128), ["g1b"], cpool.nxt())
                sp_load(effB1, modd[0, D:2 * D].partition_broadcast(128), ["effB1"], cpool.nxt(), reads=modd_keys)
                sp_load(shiftB1, modd[0, 0:D].partition_broadcast(128), ["shiftB1"], cpool.nxt(), reads=modd_keys)
                dve(["effB1", "g1b"], ["effB1"], lambda e: e.scalar_tensor_tensor(
                    out=effB1, in0=effB1, scalar=1.0, in1=g1b, op0=ALU.add, op1=ALU.mult))
                def p1_front(j):
                    b = j % 2
                    xk = ("xt", b)
                    P.dma(SP, [], [xk], lambda e, b=b, j=j: e.dma_start(
                        out=xt[b], in_=xe[row0 + j * 128:row0 + (j + 1) * 128, :]), xsem[b])
                    ssa = small[:, j:j + 1]
                    rsa = small[:, 16 + j:17 + j]
                    act_fn(junk, xt[b], AF.Square, [xk], [("ss", j)], accum=ssa)
                    rms_rstd(ssa, rsa, ("ss", j), ("rs", j))
                    dve([xk, ("rs", j), "effB1"], [xk], lambda e, b=b, rsa=rsa: e.scalar_tensor_tensor(
                        out=xt[b], in0=xt[b], scalar=rsa, in1=effB1, op0=ALU.mult, op1=ALU.mult))
                    dve([xk, "shiftB1"], [("h1tok", b)], lambda e, b=b: e.tensor_tensor(out=h1tok[b], in0=xt[b], in1=shiftB1, op=ALU.add))

                def p1_back(j):
                    b = j % 2
                    for hb_ in range(2):
                        q = (j % 2) * 2 + hb_
                        tbv = PS[q // 2][:, (q % 2) * 512:(q % 2) * 512 + 512].bitcast(BF16)

                        def emit(e, b=b, hb_=hb_, tbv=tbv):
                            ins = None
                            for k in range(8):
                                dc = hb_ * 8 + k
                                ins = e.transpose(tbv[:, k * 128:(k + 1) * 128], h1tok[b][:, dc * 128:(dc + 1) * 128], identB)
                            return ins
                        pe_group([("h1tok", b), "identB"], [pkey(q)], emit)
                        if j < 4:
                            dst = h1halo[:, hb_ * 8:(hb_ + 1) * 8, j * 128:(j + 1) * 128]
                            dk = ("h1halo", j)
                        else:
                            dst = h1own[:, hb_ * 8:(hb_ + 1) * 8, (j - 4) * 128:(j - 3) * 128]
                            dk = ("h1own", j - 4)
                        act_fn(dst, tbv.rearrange("p (k t) -> p k t", k=8), AF.Copy, [pkey(q)], [dk])

                p1_front(0)
                for j in range(12):
                    if j + 1 < 12:
                        p1_front(j + 1)
                    p1_back(j)
                if debug is not None and debug["stage"] == "h1":
                    dbg_dump([(h1own.rearrange("p a b -> p (a b)").bitcast(F32), 8192, [("h1own", i) for i in range(8)]),
                              (h1halo.rearrange("p a b -> p (a b)").bitcast(F32), 4096, [("h1halo", i) for i in range(4)]),
                              (modT, 96, ["modT"]), (effT, 32, ["effT"])])
                    break

                def h1_rhs(dc, t0, n):
                    if t0 < 512:
                        assert t0 + n <= 512
                        return h1halo[:, dc, t0:t0 + n]
                    return h1own[:, dc, t0 - 512:t0 - 512 + n]

                def h1_keys(t0, n):
                    ks = []
                    for t in range(t0 // 128, (t0 + n) // 128):
                        ks.append(("h1halo", t) if t < 4 else ("h1own", t - 4))
                    return ks

                P.fence()
                o = MIX0
                qT = sb(o, [4, 1024], BF16); o += 8192
                kT = sb(o, [4, 1536], BF16); o += 12288
                Vt = sb(o, [12, 8 * 65], BF16); o += 12480
                o = (o + 63) // 64 * 64
                biasS = sb(o, [8, 384]); o += 12288
                Pb = [sb(o + i * 1280, [640], BF16) for i in range(3)]; o += 3840
                ybt = sb(o, [512], BF16); o += 1024
                rden = sb(o, [8]); o += 32
                assert o <= PH_END
                for hh in range(2):
                    P.dma(SP, [], ["biasS"], lambda e, hh=hh: e.dma_start(
                        out=biasS.rearrange("p a b -> p (a b)"), in_=biasT[:, hh * 3072:(hh + 1) * 3072]), cpool.nxt())
                    wk, wkk = w_get(pl["k"][hh])
                    for tt in range(3):
                        for cc in range(4):
                            bnk = (tt * 4 + cc) % 8
                            def emit(e, tt=tt, cc=cc, bnk=bnk):
                                return mm_acc(e, pbank(bnk), [(wk[:, dc, cc * 128:(cc + 1) * 128], h1_rhs(dc, tt * 512, 512)) for dc in range(16)])
                            pe_group([wkk] + h1_keys(tt * 512, 512), [pkey(bnk)], emit)
                            act_fn(kT[:, cc, tt * 512:(tt + 1) * 512], pbank(bnk), AF.Copy, [pkey(bnk)], [("kT", tt)])
                    w_rel(pl["k"][hh])
                    wv, wvk = w_get(pl["vb"][hh])
                    for j in range(12):
                        bnk = j % 8
                        def emit(e, j=j, bnk=bnk):
                            return mm_acc(e, pbank(bnk), [(h1_rhs(dc, j * 128, 128), wv[:, dc, :]) for dc in range(16)])
                        pe_group([wvk] + h1_keys(j * 128, 128), [pkey(bnk)], emit)
                        vdst = Vt[:, j, :].rearrange("p (h d) -> p h d", h=8)
                        sc = fl if j < 4 else 1.0
                        act_fn(vdst[:, :, 0:64], pbank(bnk).rearrange("p (h d) -> p h d", h=8), AF.Copy,
                               [pkey(bnk), "flags"], [("Vt", j)], scale=sc)
                        act_fn(vdst[:, :, 64:65], ones8.rearrange("p (h d) -> p h d", d=1), AF.Copy,
                               ["ones8", "flags"], [("Vt", j)], scale=sc)
                    w_rel(pl["vb"][hh])
                    wq, wqk = w_get(pl["q"][hh])
                    for tt in range(2):
                        for cc in range(4):
                            bnk = (tt * 4 + cc) % 8
                            def emit(e, tt=tt, cc=cc, bnk=bnk):
                                return mm_acc(e, pbank(bnk), [(wq[:, dc, cc * 128:(cc + 1) * 128], h1_rhs(dc, 512 + tt * 512, 512)) for dc in range(16)])
                            pe_group([wqk] + h1_keys(512 + tt * 512, 512), [pkey(bnk)], emit)
                            act_fn(qT[:, cc, tt * 512:(tt + 1) * 512], pbank(bnk), AF.Copy, [pkey(bnk)], [("qT", tt)], scale=0.125)
                    w_rel(pl["q"][hh])
                    NSET = 3
                    SK = 2
                    jobs = [(i, hl) for i in range(8) for hl in range(8)]

                    def st_S(n):
                        i, hl = jobs[n]
                        j = i + 4
                        h = hh * 8 + hl
                        cc = hl // 2
                        pb0 = (hl % 2) * 64
                        st = n % NSET
                        s0, s1 = 2 * st, 2 * st + 1

                        def emit(e):
                            ins = None
                            qv = qT[pb0:pb0 + 64, cc, i * 128:(i + 1) * 128]
                            for dl in range(5):
                                kt = j - dl
                                kv = kT[pb0:pb0 + 64, cc, kt * 128:(kt + 1) * 128]
                                if dl < 3:
                                    outp = pbank(s0, 128, dl * 128)
                                else:
                                    outp = pbank(s1, 128, (dl - 3) * 128)
                                if dl == 4:
                                    e.matmul(outp, kv, qv, start=True, stop=False)
                                    ins = e.matmul(outp, urow, vrow, start=False, stop=True)
                                else:
                                    ins = e.matmul(outp, kv, qv, start=True, stop=True)
                            return ins
                        pe_group([("qT", i // 4), "uvrow"] + [("kT", (j - dl) // 4) for dl in range(5)], [pkey(s0), pkey(s1)], emit)
                        dve([pkey(s0), "biasS"], [pkey(s0)], lambda e: e.tensor_tensor(
                            out=pbank(s0, 384), in0=pbank(s0, 384), in1=biasS[:, hl, :], op=ALU.add))
                        act_fn(Pb[st][:, 0:384], pbank(s0, 384), AF.Exp, [pkey(s0)], [("Pb", st)])
                        act_fn(Pb[st][:, 384:640], pbank(s1, 256), AF.Exp, [pkey(s1), "tabc"], [("Pb", st)],
                               bias=tabc[:, h:h + 1])

                    def st_PV(n):
                        i, hl = jobs[n]
                        j = i + 4
                        st = n % NSET
                        ob = 6 + hl // 4

                        def emit(e):
                            pairs = []
                            for dl in range(5):
                                kt = j - dl
                                pairs.append((Pb[st][:, dl * 128:(dl + 1) * 128], Vt[:, kt, hl * 65:(hl + 1) * 65]))
                            return mm_acc(e, pbank(ob, 65, (hl % 4) * 65), pairs)
                        pe_group([("Pb", st)] + [("Vt", j - dl) for dl in range(5)], [pkey(ob)], emit)

                    def st_norm(i):
                        for ob in (6, 7):
                            ov = pbank(ob, 260).rearrange("p (h d) -> p h d", h=4)
                            dve([pkey(ob)], ["rden"], lambda e, ob=ob, ov=ov: e.reciprocal(
                                out=rden[:, (ob - 6) * 4:(ob - 5) * 4], in_=ov[:, :, 64]))
                            for k in range(4):
                                hl = (ob - 6) * 4 + k
                                dve([pkey(ob), "rden"], ["ybt"], lambda e, ov=ov, k=k, hl=hl: e.tensor_scalar(
                                    out=ybt[:, hl * 64:(hl + 1) * 64], in0=ov[:, k, 0:64], scalar1=rden[:, hl:hl + 1],
                                    scalar2=None, op0=ALU.mult))

                    def st_T(i):
                        tb = PS[0][:, 0:512].bitcast(BF16)

                        def emit(e):
                            ins = None
                            for k in range(4):
                                ins = e.transpose(tb[:, k * 128:(k + 1) * 128], ybt[:, k * 128:(k + 1) * 128], identB)
                            return ins
                        pe_group(["ybt", "identB"], [pkey(0)], emit)
                        act_fn(ybT[:, hh * 4:(hh + 1) * 4, i * 128:(i + 1) * 128],
                               tb[:, 0:512].rearrange("p (k t) -> p k t", k=4), AF.Copy, [pkey(0)], [("ybT", i)])

                    pendT = []
                    for n in range(len(jobs) + SK + 2):
                        if n < len(jobs):
                            st_S(n)
                        m = n - SK
                        if 0 <= m < len(jobs):
                            st_PV(m)
                            if jobs[m][1] == 7:
                                st_norm(jobs[m][0])
                                pendT.append((jobs[m][0], n + 2))
                        for (ti, due) in pendT:
                            if due == n:
                                st_T(ti)
                if debug is not None and debug["stage"] == "attn":
                    dbg_dump([(ybT.rearrange("p a b -> p (a b)").bitcast(F32), 4096, [("ybT", i) for i in range(8)])])
                    break

                P.fence()
                o = GM0
                yaT = sb(o, [8, 1024], BF16); o += 16384
                uT = sb(o, [8, 1024], BF16); o += 16384
                vn = [sb(o + i * 2048, [1024], BF16) for i in range(2)]; o += 4096
                vg = [sb(o + i * 4096, [1024]) for i in range(2)]; o += 8192
                lnGb = sb(o, [1024]); o += 4096
                lnBb = sb(o, [1024]); o += 4096
                bSb = sb(o, [1024]); o += 4096
                t1 = sb(o, [1024]); o += 4096
                bst = sb(o, [12]); o += 48
                assert o <= PH_END
                gkeys = [("kT", t) for t in range(3)] + [("qT", t) for t in range(2)] + [("Vt", j) for j in range(12)] + \
                    ["biasS", "ybt", "rden"] + [("Pb", i) for i in range(2)] + [("tmpS", i) for i in range(2)]
                sp_load(lnGb, lnG[0].partition_broadcast(128), ["lnGb"], cpool.nxt())
                sp_load(lnBb, lnB[0].partition_broadcast(128), ["lnBb"], cpool.nxt())
                sp_load(bSb, bS[0].partition_broadcast(128), ["bSb"], cpool.nxt())
                for uu in range(2):
                    wu, wuk = w_get(pl["u"][uu])
                    for tt in range(2):
                        for cc in range(4):
                            bnk = (tt * 4 + cc) % 8
                            def emit(e, tt=tt, cc=cc, bnk=bnk, wu=wu):
                                return mm_acc(e, pbank(bnk), [(wu[:, dc, cc * 128:(cc + 1) * 128], h1_rhs(dc, 512 + tt * 512, 512)) for dc in range(16)])
                            pe_group([wuk] + h1_keys(512 + tt * 512, 512), [pkey(bnk)], emit)
                            act_fn(uT[:, uu * 4 + cc, tt * 512:(tt + 1) * 512], pbank(bnk), AF.Gelu,
                                   [pkey(bnk)], [("uT", tt)])
                    w_rel(pl["u"][uu])
                wv0, wv0k = w_get(pl["v"][0])
                wv1, wv1k = w_get(pl["v"][1])
                wvs = [(wv0, wv0k), (wv1, wv1k)]
                def gm_front(i):
                    b = i % 2
                    for vu in range(2):
                        bnk = b * 4 + vu
                        def emit(e, i=i, vu=vu, bnk=bnk):
                            return mm_acc(e, pbank(bnk), [(h1_rhs(dc, 512 + i * 128, 128), wvs[vu][0][:, dc, :]) for dc in range(16)])
                        pe_group([wvs[vu][1]] + h1_keys(512 + i * 128, 128), [pkey(bnk)], emit)
                        act_fn(vg[b][:, vu * 512:(vu + 1) * 512], pbank(bnk), AF.Gelu, [pkey(bnk)], [("vg", b)])
                    st6 = small[:, 64 + b * 12:64 + b * 12 + 12]
                    mv = small[:, 96 + b * 4:96 + b * 4 + 2]
                    rs_ = small[:, 98 + b * 4:99 + b * 4]
                    for vu in range(2):
                        dve([("vg", b)], [("st6", b)], lambda e, b=b, vu=vu, st6=st6: e.bn_stats(
                            out=st6[:, vu * 6:(vu + 1) * 6], in_=vg[b][:, vu * 512:(vu + 1) * 512]))
                    dve([("st6", b)], [("mv", b)], lambda e, st6=st6, mv=mv: e.bn_aggr(out=mv, in_=st6))
                    act_fn(rs_, mv[:, 1:2], AF.Sqrt, [("mv", b)], [("rsv", b)], bias=EPS)
                    dve([("rsv", b)], [("rsv", b)], lambda e, rs_=rs_: e.reciprocal(out=rs_, in_=rs_))
                    dve([("vg", b), ("mv", b), ("rsv", b)], [("vg", b)], lambda e, b=b, mv=mv, rs_=rs_: e.tensor_scalar(
                        out=vg[b], in0=vg[b], scalar1=mv[:, 0:1], scalar2=rs_, op0=ALU.subtract, op1=ALU.mult))
                    dve([("vg", b), "lnGb"], [("vg", b)], lambda e, b=b: e.tensor_tensor(out=vg[b], in0=vg[b], in1=lnGb, op=ALU.mult))
                    dve([("vg", b), "lnBb"], [("vn", b)], lambda e, b=b: e.tensor_tensor(out=vn[b], in0=vg[b], in1=lnBb, op=ALU.add))

                def gm_back(i):
                    b = i % 2
                    for gh in range(2):
                        bnk = b * 4 + 2 + gh
                        def emit(e, b=b, gh=gh, bnk=bnk):
                            ins = None
                            for g4 in range(4):
                                g = gh * 4 + g4
                                ins = e.matmul(pbank(bnk, 128, g4 * 128), vn[b][:, g * 128:(g + 1) * 128], WsT[:, g, :], start=True, stop=True)
                            return ins
                        pe_group([("vn", b), "WsT"], [pkey(bnk)], emit)
                        dve([pkey(bnk), "bSb"], ["t1"], lambda e, gh=gh, bnk=bnk: e.tensor_tensor(
                            out=t1[:, gh * 512:(gh + 1) * 512], in0=pbank(bnk), in1=bSb[:, gh * 512:(gh + 1) * 512], op=ALU.add))
                        dve(["t1", ("uT", i // 4)], [("yaT", i)], lambda e, gh=gh, i=i: e.tensor_tensor(
                            out=yaT[:, gh * 4:(gh + 1) * 4, i * 128:(i + 1) * 128],
                            in0=t1[:, gh * 512:(gh + 1) * 512].rearrange("p (g t) -> p g t", g=4),
                            in1=uT[:, gh * 4:(gh + 1) * 4, i * 128:(i + 1) * 128], op=ALU.mult))
                gm_front(0)
                for i in range(8):
                    if i + 1 < 8:
                        gm_front(i + 1)
                    gm_back(i)
                w_rel(pl["v"][0])
                w_rel(pl["v"][1])
                if debug is not None and debug["stage"] == "gmlp":
                    dbg_dump([(yaT.rearrange("p a b -> p (a b)").bitcast(F32), 4096, [("yaT", i) for i in range(8)])])
                    break

                P.fence()
                o = GM0 + 16384
                mergedT = sb(o, [16, 1024], BF16); o += 32768
                sg = [sb(o + i * 2048, [512]) for i in range(4)]; o += 8192
                assert o <= PH_END
                mkeys = [("uT", t) for t in range(2)] + [("vg", i) for i in range(2)] + [("vn", i) for i in range(2)] + ["t1", "lnGb", "lnBb", "bSb"]
                for q in range(4):
                    wga, wgak = w_get(pl["gA"][q])
                    wgb, wgbk = w_get(pl["gB"][q])
                    wa, wak = w_get(pl["brA"][q // 2])
                    wb, wbk = w_get(pl["brB"][q // 2])
                    for tt in range(2):
                        for c4 in range(4):
                            dcc = q * 4 + c4
                            st = (tt * 4 + c4) % 2
                            bA, bB, bYa, bYb = st * 4, st * 4 + 1, st * 4 + 2, st * 4 + 3
                            ts_ = slice(tt * 512, (tt + 1) * 512)

                            def emitA(e, c4=c4, ts_=ts_, bA=bA, wga=wga):
                                return mm_acc(e, pbank(bA), [(wga[:, dc, c4 * 128:(c4 + 1) * 128], h1own[:, dc, ts_]) for dc in range(16)])
                            pe_group([wgak] + [("h1own", t) for t in range(tt * 4, tt * 4 + 4)], [pkey(bA)], emitA)

                            def emitB(e, c4=c4, ts_=ts_, bB=bB, wgb=wgb):
                                return mm_acc(e, pbank(bB), [(wgb[:, dc, c4 * 128:(c4 + 1) * 128], h1own[:, dc, ts_]) for dc in range(16)])
                            pe_group([wgbk] + [("h1own", t) for t in range(tt * 4, tt * 4 + 4)], [pkey(bB)], emitB)
                            col = (q % 2) * 512 + c4 * 128

                            def emitYa(e, col=col, ts_=ts_, bYa=bYa, wa=wa):
                                return mm_acc(e, pbank(bYa), [(wa[:, kc, col:col + 128], yaT[:, kc, ts_]) for kc in range(8)])
                            pe_group([wak] + [("yaT", t) for t in range(tt * 4, tt * 4 + 4)], [pkey(bYa)], emitYa)

                            def emitYb(e, col=col, ts_=ts_, bYb=bYb, wb=wb):
                                return mm_acc(e, pbank(bYb), [(wb[:, kc, col:col + 128], ybT[:, kc, ts_]) for kc in range(8)])
                            pe_group([wbk] + [("ybT", t) for t in range(tt * 4, tt * 4 + 4)], [pkey(bYb)], emitYb)
                            sga, sgb = sg[st * 2], sg[st * 2 + 1]
                            act_fn(sga, pbank(bA), AF.Sigmoid, [pkey(bA)], [("sg", st * 2)])
                            act_fn(sgb, pbank(bB), AF.Sigmoid, [pkey(bB)], [("sg", st * 2 + 1)])
                            dve([("sg", st * 2), pkey(bYa)], [("sg", st * 2)], lambda e, sga=sga, bYa=bYa: e.tensor_tensor(
                                out=sga, in0=sga, in1=pbank(bYa), op=ALU.mult))
                            dve([("sg", st * 2 + 1), pkey(bYb)], [("sg", st * 2 + 1)], lambda e, sgb=sgb, bYb=bYb: e.tensor_tensor(
                                out=sgb, in0=sgb, in1=pbank(bYb), op=ALU.mult))
                            dve([("sg", st * 2), ("sg", st * 2 + 1)], [("mergedT", tt)], lambda e, sga=sga, sgb=sgb, dcc=dcc, ts_=ts_: e.tensor_tensor(
                                out=mergedT[:, dcc, ts_], in0=sga, in1=sgb, op=ALU.add))
                    w_rel(pl["gA"][q])
                    w_rel(pl["gB"][q])
                    if q % 2 == 1:
                        w_rel(pl["brA"][q // 2])
                        w_rel(pl["brB"][q // 2])
                if debug is not None and debug["stage"] == "merge":
                    dbg_dump([(mergedT.rearrange("p a b -> p (a b)").bitcast(F32), 8192, [("mergedT", i) for i in range(2)])])
                    break

                P.fence()
                w2effb = sb(PH0, [2048])
                shift2b = sb(PH0 + 8192, [2048])
                h2row = [sb(PH0 + 16384 + i * 4096, [2048], BF16) for i in range(2)]
                g2b = sb(PH0 + 24576, [2048])
                gate1b = sb(PH0 + 32768, [2048])
                h2f = sb(PH0 + 40960, [16, 128])
                xt6 = [sb(PH0 + 49152 + i * 8192, [2048]) for i in range(2)]
                assert PH0 + 65536 == GM0 + 16384
                xn6 = sb(PH0 + 98304, [2048])
                junk6 = sb(PH0 + 106496, [2048], BF16)
                tmpm = sb(PH0 + 110592, [512])
                dead = [("h1own", t) for t in range(8)] + [("h1halo", t) for t in range(4)] + [("ybT", t) for t in range(8)] + \
                    [("yaT", t) for t in range(8)] + [("sg", i) for i in range(4)]
                sp_load(gate1b, modd[0, 2 * D:3 * D].partition_broadcast(128), ["gate1b"], cpool.nxt(), reads=modd_keys)
                sp_load(g2b, g2row[0].partition_broadcast(128), ["g2b"], cpool.nxt())
                sp_load(w2effb, modd[0, 4 * D:5 * D].partition_broadcast(128), ["w2effb"], cpool.nxt(), reads=modd_keys)
                sp_load(shift2b, modd[0, 3 * D:4 * D].partition_broadcast(128), ["shift2b"], cpool.nxt(), reads=modd_keys)
                dve(["w2effb", "g2b"], ["w2effb"], lambda e: e.scalar_tensor_tensor(
                    out=w2effb, in0=w2effb, scalar=1.0, in1=g2b, op0=ALU.add, op1=ALU.mult))
                wos = [w_get(u) for u in pl["wo"]]

                def p6_mm_pe(i):
                    b = i % 2
                    P.dma(SP, [], [("xt6", b)], lambda e, b=b, i=i: e.dma_start(
                        out=xt6[b], in_=xe[row0 + HALO + i * 128:row0 + HALO + (i + 1) * 128, :]), xsem[b])
                    for cu in range(4):
                        def emit(e, i=i, cu=cu):
                            return mm_acc(e, pbank(cu), [(mergedT[:, kc, i * 128:(i + 1) * 128], wos[cu][0][:, kc, :]) for kc in range(16)])
                        pe_group([wos[cu][1], ("mergedT", i // 4)], [pkey(cu)], emit)

                def p6_mm_dve(i):
                    b = i % 2
                    for cu in range(4):
                        dve([pkey(cu), "gate1b"], ["tmpm"], lambda e, cu=cu: e.tensor_tensor(
                            out=tmpm, in0=pbank(cu), in1=gate1b[:, cu * 512:(cu + 1) * 512], op=ALU.mult))
                        dve(["tmpm", ("xt6", b)], [("xt6", b)], lambda e, cu=cu, b=b: e.tensor_tensor(
                            out=xt6[b][:, cu * 512:(cu + 1) * 512], in0=xt6[b][:, cu * 512:(cu + 1) * 512], in1=tmpm, op=ALU.add))

                def p6_front(i):
                    b = i % 2
                    xk = ("xt6", b)
                    ssa = small[:, 32 + i:33 + i]
                    rsa = small[:, 48 + i:49 + i]
                    act_fn(junk6, xt6[b], AF.Square, [xk], [("ss2", i)], accum=ssa)
                    rms_rstd(ssa, rsa, ("ss2", i), ("rs2", i))
                    P.dma(SP, [xk], [("x1s", p * 8 + i)], lambda e, b=b, i=i: e.dma_start(
                        out=x1s[p * TP + i * 128:p * TP + (i + 1) * 128, :], in_=xt6[b]), osem[b])
                    dve([xk, ("rs2", i), "w2effb"], ["xn6"], lambda e, b=b, rsa=rsa: e.scalar_tensor_tensor(
                        out=xn6, in0=xt6[b], scalar=rsa, in1=w2effb, op0=ALU.mult, op1=ALU.mult))
                    dve(["xn6", "shift2b"], ["xn6"], lambda e: e.tensor_tensor(out=xn6, in0=xn6, in1=shift2b, op=ALU.add))
                    hb = i % 2
                    act_fn(h2row[hb], xn6, AF.Copy, ["xn6"], [("h2row", hb)])
                    P.dma(SP, [("h2row", hb)], [("h2d", p * 8 + i)], lambda e, hb=hb, i=i: e.dma_start(
                        out=h2d[p * TP + i * 128:p * TP + (i + 1) * 128, :], in_=h2row[hb]), hsem[hb])
                def p6_back(i):
                    b = i % 2
                    for q in range(4):
                        def emit(e, q=q):
                            ins = None
                            for k in range(4):
                                dc = q * 4 + k
                                ins = e.transpose(pbank(4 + q, 128, k * 128), xn6[:, dc * 128:(dc + 1) * 128], identF)
                            return ins
                        pe_group(["xn6", "identF"], [pkey(4 + q)], emit)
                        act_fn(h2f[:, q * 4:(q + 1) * 4, :], pbank(4 + q).rearrange("p (k t) -> p k t", k=4), AF.Copy,
                               [pkey(4 + q)], [("h2f", q)])
                    LB = 0

                    def emit(e):
                        return mm_acc(e, pbank(LB, 36), [(h2f[:, dc, :], wr[:, dc, :]) for dc in range(16)])
                    pe_group([("h2f", q) for q in range(4)] + ["wr"], [pkey(LB)], emit)
                    lg = rt[:, 0:36]
                    dve([pkey(LB)], ["rt"], lambda e: e.tensor_copy(out=lg, in_=pbank(LB, 36)))
                    gl = rt[:, 0:4]
                    el = rt[:, 4:36]
                    gmax = rt[:, 40:41]
                    ngmax = rt[:, 41:42]
                    gsum = rt[:, 42:43]
                    gw = rt[:, 43:44]
                    goh = rt[:, 44:48]
                    gex = rt[:, 48:52]
                    esel = rt[:, 56:64]
                    etmp = rt[:, 64:96]
                    m1 = rt[:, 96:97]
                    m2 = rt[:, 97:98]
                    oh1 = rt[:, 104:112]
                    oh2 = rt[:, 112:120]
                    msk = rt[:, 120:128]
                    dd = rt[:, 128:129]
                    w1_ = rt[:, 129:130]
                    w2_ = rt[:, 130:131]
                    ew = rt[:, 136:144]
                    gwo = rt[:, 144:148]
                    R = ["rt"]
                    dve(R, R, lambda e: e.tensor_reduce(out=gmax, in_=gl, axis=AX.X, op=ALU.max))
                    dve(R, R, lambda e: e.tensor_scalar(out=ngmax, in0=gmax, scalar1=-1.0, scalar2=None, op0=ALU.mult))
                    act_fn(gex, gl, AF.Exp, R, R, bias=ngmax, accum=gsum)
                    dve(R, R, lambda e: e.reciprocal(out=gw, in_=gsum))
                    dve(R, R, lambda e: e.tensor_scalar(out=goh, in0=gl, scalar1=gmax, scalar2=None, op0=ALU.is_equal))
                    dve(R, R, lambda e: e.tensor_scalar(out=esel, in0=el[:, 0:8], scalar1=goh[:, 0:1], scalar2=None, op0=ALU.mult))
                    for g in range(1, 4):
                        dve(R, R, lambda e, g=g: e.scalar_tensor_tensor(
                            out=esel, in0=el[:, g * 8:(g + 1) * 8], scalar=goh[:, g:g + 1], in1=esel, op0=ALU.mult, op1=ALU.add))
                    dve(R, R, lambda e: e.tensor_reduce(out=m1, in_=esel, axis=AX.X, op=ALU.max))
                    dve(R, R, lambda e: e.tensor_scalar(out=oh1, in0=esel, scalar1=m1, scalar2=None, op0=ALU.is_equal))
                    dve(R, R, lambda e: e.scalar_tensor_tensor(out=msk, in0=oh1, scalar=-1e30, in1=esel, op0=ALU.mult, op1=ALU.add))
                    dve(R, R, lambda e: e.tensor_reduce(out=m2, in_=msk, axis=AX.X, op=ALU.max))
                    dve(R, R, lambda e: e.tensor_scalar(out=oh2, in0=msk, scalar1=m2, scalar2=None, op0=ALU.is_equal))
                    dve(R, R, lambda e: e.tensor_tensor(out=dd, in0=m2, in1=m1, op=ALU.subtract))
                    act_fn(dd, dd, AF.Exp, R, R)
                    dve(R, R, lambda e: e.tensor_scalar(out=w1_, in0=dd, scalar1=1.0, scalar2=None, op0=ALU.add))
                    dve(R, R, lambda e: e.reciprocal(out=w1_, in_=w1_))
                    dve(R, R, lambda e: e.tensor_tensor(out=w2_, in0=dd, in1=w1_, op=ALU.mult))
                    dve(R, R, lambda e: e.tensor_scalar(out=ew, in0=oh1, scalar1=w1_, scalar2=None, op0=ALU.mult))
                    dve(R, R, lambda e: e.scalar_tensor_tensor(out=ew, in0=oh2, scalar=w2_, in1=ew, op0=ALU.mult, op1=ALU.add))
                    dve(R, R, lambda e: e.tensor_scalar(out=gwo, in0=goh, scalar1=gw, scalar2=None, op0=ALU.mult))
                    ti = p * 8 + i
                    for g in range(4):
                        dve(R, [("sel", ti)], lambda e, g=g, ti=ti: e.tensor_scalar(
                            out=sel0[:, ti, g * 8:(g + 1) * 8], in0=oh1, scalar1=goh[:, g:g + 1], scalar2=None, op0=ALU.mult))
                        dve(R, [("sel", ti)], lambda e, g=g, ti=ti: e.tensor_scalar(
                            out=sel1[:, ti, g * 8:(g + 1) * 8], in0=oh2, scalar1=goh[:, g:g + 1], scalar2=None, op0=ALU.mult))
                    dve(R, [("wkk", ti)], lambda e, ti=ti: e.tensor_tensor(out=wsl[:, ti, 0:1], in0=w1_, in1=gw, op=ALU.mult))
                    dve(R, [("wkk", ti)], lambda e, ti=ti: e.tensor_tensor(out=wsl[:, ti, 1:2], in0=w2_, in1=gw, op=ALU.mult))

                p6_mm_pe(0)
                p6_mm_dve(0)
                for i in range(8):
                    if i + 1 < 8:
                        p6_mm_pe(i + 1)
                    p6_front(i)
                    if i + 1 < 8:
                        p6_mm_dve(i + 1)
                    p6_back(i)
                for u in pl["wo"]:
                    w_rel(u)
                if debug is not None and debug["stage"] == "p6":
                    break


            class MW:
                units = []
                released = []
                nloaded = 0

            def m_view(slot, a, b):
                return sb(RING0 + slot * SLOT_B, [a, b], BF16)

            def m_pump(limit=None):
                while MW.nloaded < len(MW.units):
                    u = MW.nloaded
                    if limit is not None and u >= limit:
                        break
                    if u >= MSLOT and not MW.released[u - MSLOT]:
                        break
                    src, a, b = MW.units[u]
                    slot = u % MSLOT
                    dst = m_view(slot, a, b)
                    wk_ = [("mring", slot)]
                    if u < NSLOT:
                        wk_.append(("ring", slot))
                    P.dma(POOL, [], wk_, lambda e, dst=dst, src=src: e.dma_start(out=dst, in_=src), mr_sem[slot])
                    MW.nloaded += 1

            def m_get(u):
                m_pump()
                assert u < MW.nloaded
                src, a, b = MW.units[u]
                return m_view(u % MSLOT, a, b), ("mring", u % MSLOT)

            def m_rel(u):
                MW.released[u] = True

            for e_ in range(NE):
                wpat = "(p c) n -> p c n" if CONTIG else "(c p) n -> p c n"
                for src, a, b in ((w1[e_].rearrange(wpat, p=128), 16, 512),
                                  (w3[e_].rearrange(wpat, p=128), 16, 512),
                                  (w2[e_].rearrange("(c p) n -> p c n", p=128), 4, 2048)):
                    MW.units.append((src, a, b))
                    MW.released.append(False)

            if debug is None or debug["stage"] in ("meta", "moe"):
                m_pump(limit=NSLOT)

            if debug is None or debug["stage"] in ("meta", "moe"):
                P.nofloor.discard("pool")
                P.fence()
                o = PH0 + (MSLOT - NSLOT) * SLOT_B
                selA = sb(o, [16, 32]); o += 2048
                cum = sb(o, [16, 32]); o += 2048
                pos = sb(o, [16, 32]); o += 2048
                tmpd = sb(o, [16, 32]); o += 2048
                destf = sb(o, [32]); o += 128
                desti = sb(o, [32], I32); o += 128
                cntf = sb(o, [32]); o += 128
                cnti = sb(o, [32], I32); o += 128
                fillt = sb(o, [2048], I32); o += 8192
                Utri = sb(o, [128]); o += 512
                OnesM = sb(o, [128]); o += 512
                META_END = o
                sp_load(Utri, Utri_d, ["Utri"], cpool.nxt())
                sp_load(OnesM, Ones_d, ["OnesM"], cpool.nxt())
                selk = [("sel", t) for t in range(16)]
                dve(selk, ["selA"], lambda e: e.tensor_tensor(out=selA, in0=sel0, in1=sel1, op=ALU.add))
                dve([], ["cum"], lambda e: e.memset(cum[:, 0, :], 0.0))
                for t in range(1, 16):
                    dve(["cum", "selA"], ["cum"], lambda e, t=t: e.tensor_tensor(
                        out=cum[:, t, :], in0=cum[:, t - 1, :], in1=selA[:, t - 1, :], op=ALU.add))
                def emit(e):
                    ins = None
                    for t in range(16):
                        e.matmul(pbank(0, 32, t * 32), OnesM, cum[:, t, :], start=True, stop=False)
                        ins = e.matmul(pbank(0, 32, t * 32), Utri, selA[:, t, :], start=False, stop=True)
                    return ins
                pe_group(["cum", "selA", "OnesM", "Utri"], [pkey(0)], emit)
                dve([pkey(0)], ["pos"], lambda e: e.tensor_copy(out=pos, in_=pbank(0).rearrange("p (t x) -> p t x", t=16)))
                dve(["cum", "selA"], ["tmpd"], lambda e: e.tensor_tensor(out=tmpd[:, 0, :], in0=cum[:, 15, :], in1=selA[:, 15, :], op=ALU.add))
                pe_group(["tmpd", "OnesM"], [pkey(1)], lambda e: e.matmul(pbank(1, 32), OnesM, tmpd[:, 0, :], start=True, stop=True))
                dve([pkey(1)], ["cntf"], lambda e: e.tensor_copy(out=cntf, in_=pbank(1, 32)))
                dve(["cntf"], ["cnti"], lambda e: e.tensor_copy(out=cnti, in_=cntf))
                P.dma(SP, ["cnti"], ["cntd"], lambda e: e.dma_start(out=cntd, in_=cnti[0:1, :]), cpool.nxt())
                dve(["pos", "ecap"], ["pos"], lambda e: e.tensor_tensor(
                    out=pos, in0=pos, in1=ecap.unsqueeze(1).to_broadcast([128, 16, 32]), op=ALU.add))
                for k, selx in enumerate((sel0, sel1)):
                    dve(["pos"] + selk, ["tmpd"], lambda e, selx=selx: e.tensor_tensor(out=tmpd, in0=pos, in1=selx, op=ALU.mult))
                    dve(["tmpd"], ["destf"], lambda e, k=k: e.tensor_reduce(
                        out=destf.rearrange("p (t k) -> p t k", k=2)[:, :, k], in_=tmpd, axis=AX.X, op=ALU.add))
                dve(["destf"], ["desti"], lambda e: e.tensor_copy(out=desti, in_=destf))
                sp_load(fillt, fillv_d, ["fillt"], cpool.nxt())
                P.dma(SP, ["fillt"], ["table"], lambda e: e.dma_start(
                    out=table.rearrange("(p r) c -> p (r c)", p=128), in_=fillt), cpool.nxt())
                for t in range(16):
                    for k in range(2):
                        P.dma(POOL, ["desti", "vals", "table"], [("tabw", t, k)], lambda e, t=t, k=k: e.indirect_dma_start(
                            out=table, out_offset=bass.IndirectOffsetOnAxis(ap=desti[:, t * 2 + k:t * 2 + k + 1], axis=0),
                            in_=vals[:, (t * 2 + k) * 4:(t * 2 + k) * 4 + 4], in_offset=None,
                            oob_is_err=False), tpool.nxt())
                tabkeys = [("tabw", t, k) for t in range(16) for k in range(2)]
                if debug is not None and debug["stage"] == "meta":
                    dbg_dump([(destf, 32, ["destf"]), (cntf, 32, ["cntf"]), (wsl.rearrange("p a b -> p (a b)"), 32, [("wkk", t) for t in range(16)]),
                              (sel0.rearrange("p a b -> p (a b)"), 512, selk), (sel1.rearrange("p a b -> p (a b)"), 512, selk)])

            if debug is None or debug["stage"] in ("moe",):
                P.fence()
                o = PH0 + (MSLOT - NSLOT) * SLOT_B
                NXG = 5
                idxt = [sb(o + i * 16, [4], I32) for i in range(NXG)]; o += 128
                xg = [sb(o + i * 4096, [2048], BF16) for i in range(NXG)]; o += NXG * 4096
                xgT = sb(o, [16, 128], BF16); o += 4096
                sAm = sb(o, [512]); o += 2048
                actm = sb(o, [512], BF16); o += 1024
                actTm = sb(o, [4, 128], BF16); o += 1024
                NY = 4
                Ysb = [sb(o + i * 8192, [2048]) for i in range(NY)]; o += NY * 8192
                MOE_END = o
                assert o <= PH_END

                regs = {}
                if target is not None:
                    regs[target] = handle.alloc_register("cnt_" + target)
                tcount = [0]

                def gather_ops(ei, t, xb):
                    row0 = ei * CAP + t * 128
                    P.dma(SP, tabkeys + ["table"], [("idxt", xb)], lambda e: e.dma_start(out=idxt[xb], in_=table[row0:row0 + 128, :]), ixsem[xb])
                    P.dma(POOL, [("idxt", xb)] + [("h2d", i) for i in range(16)], [("xg", xb)], lambda e: e.indirect_dma_start(
                        out=xg[xb], out_offset=None, in_=h2d, in_offset=bass.IndirectOffsetOnAxis(ap=idxt[xb][:, 0:1], axis=0),
                        oob_is_err=False), gasem[xb])

                NSTAT = int(os.environ.get('K_NSTAT', '2'))

                def tile_ops(ei, t, wa, wak, wb, wbk, wc, wck):
                    n = tcount[0]
                    tcount[0] += 1
                    b = n % NY
                    if t < NSTAT:
                        xb = (ei % 2) * NSTAT + t
                    else:
                        xb = NXG - 1
                        gather_ops(ei, t, xb)
                    tb0 = PS[0][:, 0:512].bitcast(BF16)
                    tb1 = PS[0][:, 512:1024].bitcast(BF16)
                    for hb_, tb in enumerate((tb0, tb1)):
                        def emit(e, hb_=hb_, tb=tb):
                            ins = None
                            for k in range(8):
                                dc = hb_ * 8 + k
                                src_cols = xg[xb][:, dc:2048:16] if CONTIG else xg[xb][:, dc * 128:(dc + 1) * 128]
                                ins = e.transpose(tb[:, k * 128:(k + 1) * 128], src_cols, identB)
                            return ins
                        pe_group([("xg", xb), "identB"], [pkey(hb_)], emit)
                    act_fn(xgT[:, 0:8, :], tb0.rearrange("p (k t) -> p k t", k=8), AF.Copy, [pkey(0)], [("xgT", 0)])
                    dve([pkey(1)], [("xgT", 1)], lambda e: e.tensor_copy(out=xgT[:, 8:16, :], in_=tb1.rearrange("p (k t) -> p k t", k=8)))
                    pe_group([wak, ("xgT", 0), ("xgT", 1)], [pkey(2)], lambda e: mm_acc(e, pbank(2), [(xgT[:, dc, :], wa[:, dc, :]) for dc in range(16)]))
                    pe_group([wbk, ("xgT", 0), ("xgT", 1)], [pkey(3)], lambda e: mm_acc(e, pbank(3), [(xgT[:, dc, :], wb[:, dc, :]) for dc in range(16)]))
                    act_fn(sAm, pbank(2), AF.Silu, [pkey(2)], ["sAm"])
                    dve(["sAm", pkey(3)], ["actm"], lambda e: e.tensor_tensor(out=actm, in0=sAm, in1=pbank(3), op=ALU.mult))

                    def emit(e):
                        ins = None
                        for k in range(4):
                            ins = e.transpose(tb0[:, k * 128:(k + 1) * 128], actm[:, k * 128:(k + 1) * 128], identB)
                        return ins
                    pe_group(["actm", "identB"], [pkey(0)], emit)
                    act_fn(actTm, tb0[:, 0:512].rearrange("p (k t) -> p k t", k=4), AF.Copy, [pkey(0)], ["actTm"])
                    for nb in range(4):
                        pe_group([wck, "actTm"], [pkey(4 + nb)], lambda e, nb=nb: mm_acc(
                            e, pbank(4 + nb), [(actTm[:, fc, :], wc[:, fc, nb * 512:(nb + 1) * 512]) for fc in range(4)]))
                    act_fn(Ysb[b][:, 0:1024], PS[2][:, :], AF.Copy, [pkey(4), pkey(5)], [("Ysb", b, 0)])
                    dve([pkey(6), pkey(7)], [("Ysb", b, 1)], lambda e: e.tensor_copy(out=Ysb[b][:, 1024:2048], in_=PS[3][:, :]))
                    for hf in range(2):
                        P.dma(POOL, [("Ysb", b, hf), ("idxt", xb)], [("Ybuf", n, hf)], lambda e, hf=hf: e.indirect_dma_start(
                            out=Ybuf4, out_offset=bass.IndirectOffsetOnAxis(ap=idxt[xb][:, 1 + hf:2 + hf], axis=0),
                            in_=Ysb[b][:, hf * 1024:(hf + 1) * 1024], in_offset=None,
                            oob_is_err=False), scsem[b * 2 + hf])

                MAXT = TOKC // 128

                GRP = 4

                def tiles_from(ei, t, tend, rv, ws):
                    if t >= tend:
                        return
                    with P.guard(lambda h: rv > t * 128):
                        tile_ops(ei, t, *ws)
                        tiles_from(ei, t + 1, tend, rv, ws)

                for t_ in range(NSTAT):
                    gather_ops(0, t_, t_)
                for ei in range(NE):
                    if ei + 1 < NE:
                        for t_ in range(NSTAT):
                            gather_ops(ei + 1, t_, ((ei + 1) % 2) * NSTAT + t_)
                    wa, wak = m_get(3 * ei)
                    wb, wbk = m_get(3 * ei + 1)
                    wc, wck = m_get(3 * ei + 2)
                    rv = None
                    for eng in P.engines:
                        r = P.raw(eng, ["cntd"], lambda h, ei=ei: h.reg_load(regs[target], cntd[0:1, ei:ei + 1]))
                    if target is not None:
                        rv = handle.snap(regs[target])
                    for g0 in range(0, MAXT, GRP):
                        tiles_from(ei, g0, g0 + GRP, rv, (wa, wak, wb, wbk, wc, wck))
                    m_rel(3 * ei)
                    m_rel(3 * ei + 1)
                    m_rel(3 * ei + 2)
                    m_pump()

                P.fence()
                o = RING0
                gate2b = sb(o, [2048]); o += 8192
                fgb = sb(o, [2048]); o += 8192
                NF = 3
                xt8 = [sb(o + i * 8192, [2048]) for i in range(NF)]; o += NF * 8192
                y0t = [sb(o + i * 8192, [2048]) for i in range(NF)]; o += NF * 8192
                y1t = [sb(o + i * 8192, [2048]) for i in range(NF)]; o += NF * 8192
                junk8 = sb(o, [2048], BF16); o += 4096
                assert o <= PH_END
                ybkeys = [("Ybuf", n, hf) for n in range(tcount[0]) for hf in range(2)]
                sp_load(gate2b, modd[0, 5 * D:6 * D].partition_broadcast(128), ["gate2b"], cpool.nxt(), reads=modd_keys)
                sp_load(fgb, fg[0].partition_broadcast(128), ["fgb"], cpool.nxt())

                def fin_loads(i):
                    b = i % NF
                    P.dma(SP, [("x1s", i)], [("xt8", b)], lambda e: e.dma_start(
                        out=xt8[b], in_=x1s[i * 128:(i + 1) * 128, :]), fsem[b])
                    P.dma(SP, ybkeys, [("y0t", b)], lambda e: e.dma_start(
                        out=y0t[b], in_=Ybuf[i * 128:(i + 1) * 128, :]), fsem[3 + b])
                    P.dma(SP, ybkeys, [("y1t", b)], lambda e: e.dma_start(
                        out=y1t[b], in_=Ybuf[TOKC + i * 128:TOKC + (i + 1) * 128, :]), fsem[6 + b])

                fin_loads(0)
                fin_loads(1)
                for i in range(16):
                    b = i % NF
                    xk = ("xt8", b)
                    if i + 2 < 16:
                        fin_loads(i + 2)
                    act_fn(y0t[b], y0t[b], AF.Copy, [("y0t", b), ("wkk", i)], [("y0t", b)], scale=wsl[:, i, 0:1])
                    dve([("y0t", b), ("y1t", b), ("wkk", i)], [("y0t", b)], lambda e, b=b, i=i: e.scalar_tensor_tensor(
                        out=y0t[b], in0=y1t[b], scalar=wsl[:, i, 1:2], in1=y0t[b], op0=ALU.mult, op1=ALU.add))
                    dve([("y0t", b), "gate2b"], [("y0t", b)], lambda e, b=b: e.tensor_tensor(out=y0t[b], in0=y0t[b], in1=gate2b, op=ALU.mult))
                    dve([("y0t", b), xk], [xk], lambda e, b=b: e.tensor_tensor(out=xt8[b], in0=xt8[b], in1=y0t[b], op=ALU.add))
                    ssa = small[:, 128 + i:129 + i]
                    rsa = small[:, 144 + i:145 + i]
                    act_fn(junk8, xt8[b], AF.Square, [xk], [("ss8", i)], accum=ssa)
                    rms_rstd(ssa, rsa, ("ss8", i), ("rs8", i))
                    dve([xk, ("rs8", i), "fgb"], [xk], lambda e, b=b, rsa=rsa: e.scalar_tensor_tensor(
                        out=xt8[b], in0=xt8[b], scalar=rsa, in1=fgb, op0=ALU.mult, op1=ALU.mult))
                    P.dma(SP, [xk], [("y", i)], lambda e, b=b, i=i: e.dma_start(
                        out=y[i * 128:(i + 1) * 128, :], in_=xt8[b]), fsem[9 + b])

            outkeys = [("y", i) for i in range(16)] if debug is None else [k for k in P.state.keys() if isinstance(k, tuple) and k[0] in ("dbg", "y")]
            P.final_wait(SP, outkeys)

        with nc.Block() as block:
            @block.tensor
            def _(e):
                program("pe", e)

            @block.scalar
            def _(e):
                program("act", e)

            @block.vector
            def _(e):
                program("dve", e)

            @block.gpsimd
            def _(e):
                program("pool", e)

            @block.sync
            def _(e):
                program("sp", e)
    return nc


_NC_CACHE = {}


def _vals_const():
    v = np.zeros((128, 16, 2, 4), np.int32)
    p = np.arange(128)[:, None]
    t = np.arange(16)[None, :]
    tok = t * 128 + p
    for k in range(2):
        v[:, :, k, 0] = tok
        v[:, :, k, 1] = 2 * (k * TOKC + tok)
        v[:, :, k, 2] = 2 * (k * TOKC + tok) + 1
    return np.ascontiguousarray(v.reshape(128, 128))


def _fill_const():
    r = np.arange(512)
    row = np.zeros((512, 4), np.int32)
    row[:, 1] = 4 * TOKC + (r % 128)
    row[:, 2] = 4 * TOKC + 128 + (r % 128)
    return np.ascontiguousarray(np.broadcast_to(row.reshape(1, 2048), (128, 2048)))


def _host_inputs(inputs):
    f = np.float32
    x = np.asarray(inputs["x"], f)
    c = np.asarray(inputs["c"], f)
    L = 0
    tab = np.asarray(inputs["rel_bias"], f)[L]
    ext = np.concatenate([tab, np.repeat(tab[:, -1:], 400, axis=1)], axis=1)
    k = np.arange(128)[:, None]
    q = np.arange(128)[None, :]
    bt = np.empty((128, 16, 3, 128), f)
    for dl in range(3):
        idx = 256 + dl * 128 + q - k
        bt[:, :, dl, :] = np.transpose(ext[:, idx], (1, 0, 2))
    bt[64:, :, 0, :64] = NEG
    wr = np.concatenate([np.asarray(inputs["w_group"], f)[L]] +
                        [np.asarray(inputs["w_expert"], f)[L, g] for g in range(4)], axis=1)
    shared = {
        "ident": np.eye(128, dtype=f),
        "ada_w": np.ascontiguousarray(np.asarray(inputs["ada_w"], f)[L]),
        "ada_b": np.ascontiguousarray(np.asarray(inputs["ada_b"], f)[L][None, :]),
        "g1T": np.ascontiguousarray(np.asarray(inputs["norm1_g"], f)[L].reshape(16, 128).T),
        "g2T": np.ascontiguousarray(np.asarray(inputs["norm2_g"], f)[L].reshape(16, 128).T),
        "lnG": np.ascontiguousarray(np.asarray(inputs["gmlp_ln_g"], f)[L][None, :]),
        "lnB": np.ascontiguousarray(np.asarray(inputs["gmlp_ln_b"], f)[L][None, :]),
        "bS": np.ascontiguousarray(np.asarray(inputs["gmlp_b_s"], f)[L].reshape(1, 1024)),
        "wS": np.ascontiguousarray(np.asarray(inputs["gmlp_w_s"], f)[L]),
        "biasT": np.ascontiguousarray(bt.reshape(128, 16 * 384)),
        "tabc": np.ascontiguousarray(tab[:, 512][None, :]),
        "w_in": np.ascontiguousarray(np.asarray(inputs["w_in"], f)[L]),
        "wbrA": np.ascontiguousarray(np.asarray(inputs["w_branch_a"], f)[L]),
        "wbrB": np.ascontiguousarray(np.asarray(inputs["w_branch_b"], f)[L]),
        "wout": np.ascontiguousarray(np.asarray(inputs["w_out"], f)[L]),
        "wr": np.ascontiguousarray(wr),
        "w1": np.ascontiguousarray(np.asarray(inputs["w1"], f)[L].reshape(NE, D, 512)),
        "w3": np.ascontiguousarray(np.asarray(inputs["w3"], f)[L].reshape(NE, D, 512)),
        "w2": np.ascontiguousarray(np.asarray(inputs["w2"], f)[L].reshape(NE, 512, D)),
        "fg": np.ascontiguousarray(np.asarray(inputs["final_g"], f)[None, :]),
        "g2row": np.ascontiguousarray(np.asarray(inputs["norm2_g"], f)[L][None, :]),
        "g1row": np.ascontiguousarray(np.asarray(inputs["norm1_g"], f)[L][None, :]),
        "Utri": np.triu(np.ones((128, 128), f), 1),
        "OnesM": np.ones((128, 128), f),
        "ecap": np.ascontiguousarray(np.broadcast_to((np.arange(NE) * CAP).astype(f)[None, :], (128, NE))),
        "vals": _vals_const(),
        "fillv": _fill_const(),
    }
    in_maps = []
    for core in range(8):
        b = core // 4
        s0 = (core % 4) * TOKC
        xe = np.zeros((TOKC + HALO, D), f)
        if s0 == 0:
            xe[HALO:] = x[b, 0:TOKC]
        else:
            xe[:] = x[b, s0 - HALO:s0 + TOKC]
        flags = np.ones((128, 2), f)
        if s0 == 0:
            flags[:, 0] = 0.0
        m = dict(shared)
        m["xe"] = xe
        m["cT"] = np.ascontiguousarray(c[b].reshape(16, 128).T)
        m["flags"] = flags
        in_maps.append(m)
    return in_maps


def kernel(**inputs):
    if "nc" not in _NC_CACHE:
        _NC_CACHE["nc"] = build()
    nc = _NC_CACHE["nc"]
    in_maps = _host_inputs(inputs)
    res = run_bass_kernel_spmd(nc, in_maps, core_ids=list(range(8)))
    out = np.empty((2, 8192, D), np.float32)
    for core in range(8):
        b = core // 4
        s0 = (core % 4) * TOKC
        out[b, s0:s0 + TOKC] = res.results[core]["y"]
    return out
```

```python
import contextlib
import os
import numpy as np
import concourse.bass as bass
import concourse.mybir as mybir
from concourse.bass_utils import run_bass_kernel_spmd

F32 = mybir.dt.float32
BF16 = mybir.dt.bfloat16
I32 = mybir.dt.int32
AF = mybir.ActivationFunctionType
ALU = mybir.AluOpType
AX = mybir.AxisListType

D = 2048
TOKC = 2048
TP = 1024
HALO = 512
NPASS = 2
EPS = 1e-6
NSLOT = 5
SLOT_B = 16384
NEG = -30000.0
EVAC_SPLIT = os.environ.get('K_EVAC', '0') == '1'
ADA_DVE = os.environ.get('K_ADA', '1') == '1'
PREFETCH = os.environ.get('K_PRE', '1') == '1'
CONTIG = os.environ.get('K_CONTIG', '1') == '1'
NE = 32
CAP = 2048
MSLOT = 8
BIGIDX = 1 << 24


class Sem:
    def __init__(self, h, shared=False):
        self.h = h
        self.n = 0
        self.shared = shared


class Eng:
    def __init__(self, name, sem, selfwait=True):
        self.name = name
        self.sem = sem
        self.prog = []
        self.waited = {}
        self.selfwait = selfwait


class Prog:
    def __init__(self, target, handle):
        self.state = {}
        self.floor = {}
        self.nofloor = set()
        self.target = target
        self.handle = handle
        self.inc_log = []
        self.alias = {}
        self.engines = []

    def fence(self):
        for k, st in self.state.items():
            if isinstance(k, tuple) and k[0] in ("ring", "mring"):
                continue
            evs = ([st[0]] if st[0] else []) + list(st[1].items())
            for s, v in evs:
                if s.shared:
                    v = s.n
                if self.floor.get(s, 0) < v:
                    self.floor[s] = v

    def _expand(self, keys):
        out = []
        for k in keys:
            out.extend(self.alias.get(k, (k,)) if isinstance(k, tuple) else (k,))
        return out

    def _deps(self, reads, writes, eng=None):
        reads = self._expand(reads)
        writes = self._expand(writes)
        w = {}

        def add(ev):
            if ev is None:
                return
            s, v = ev
            if s.shared:
                v = s.n
            if w.get(s, 0) < v:
                w[s] = v

        if eng is not None and eng.name not in self.nofloor:
            for s, v in self.floor.items():
                add((s, v))

        for k in reads:
            st = self.state.get(k)
            if st:
                add(st[0])
        for k in writes:
            st = self.state.get(k)
            if st:
                add(st[0])
                for s, v in st[1].items():
                    add((s, v))
        return w

    def _commit(self, reads, writes, ev):
        reads = self._expand(reads)
        writes = self._expand(writes)
        s, v = ev
        for k in reads:
            st = self.state.setdefault(k, [None, {}])
            if st[1].get(s, 0) < v:
                st[1][s] = v
        for k in writes:
            self.state[k] = [ev, {}]

    def op(self, eng, reads, writes, emit):
        w = self._deps(reads, writes, eng)
        wl = []
        for s, v in w.items():
            if s is eng.sem and not eng.selfwait:
                continue
            if eng.waited.get(s, 0) < v:
                eng.waited[s] = v
                wl.append((s, v))
        self.inc_log.append((eng.name, eng.sem, 1))
        eng.sem.n += 1
        ev = (eng.sem, eng.sem.n)
        sem = eng.sem

        if eng.name == self.target:
            e = self.handle
            for s, v in wl:
                e.wait_ge(s.h, v)
            emit(e).then_inc(sem.h, 1)
        self._commit(reads, writes, ev)

    def dma(self, eng, reads, writes, emit, dsem):
        w = self._deps(reads, writes, eng)
        if dsem.n > 0 and w.get(dsem, 0) < dsem.n:
            w[dsem] = dsem.n
        wl = []
        for s, v in w.items():
            if eng.waited.get(s, 0) < v:
                eng.waited[s] = v
                wl.append((s, v))
        self.inc_log.append((eng.name, dsem, 16))
        dsem.n += 16
        ev = (dsem, dsem.n)

        if eng.name == self.target:
            e = self.handle
            for s, v in wl:
                e.wait_ge(s.h, v)
            emit(e).then_inc(dsem.h, 16)
        self._commit(reads, writes, ev)

    def raw(self, eng, reads, emit):
        w = self._deps(reads, [], eng)
        wl = []
        for s, v in w.items():
            if s is eng.sem and not eng.selfwait:
                continue
            if eng.waited.get(s, 0) < v:
                eng.waited[s] = v
                wl.append((s, v))
        if eng.name == self.target:
            for s, v in wl:
                self.handle.wait_ge(s.h, v)
            return emit(self.handle)
        return None

    @contextlib.contextmanager
    def guard(self, cond_fn):
        snap_waited = {e.name: dict(e.waited) for e in self.engines}
        log0 = len(self.inc_log)
        before = {}
        ctx = None
        if self.target is not None:
            ctx = self.handle.If(cond_fn(self.handle))
            ctx.__enter__()
        sems_seen = {}
        n_at_entry = lambda sem: sems_seen.setdefault(id(sem), sem.n)
        entry_vals = {}
        class _Peek:
            pass
        all_sems = set()
        for (_, sem, _) in self.inc_log:
            all_sems.add(sem)
        entry_vals = {sem: sem.n for sem in all_sems}
        try:
            yield
        finally:
            if ctx is not None:
                ctx.__exit__(None, None, None)
                comp = {}
                order = []
                for (en, sem, amt) in self.inc_log[log0:]:
                    if en == self.target:
                        if sem not in comp:
                            comp[sem] = 0
                            order.append(sem)
                        comp[sem] += amt
                ectx = self.handle.Else()
                ectx.__enter__()
                for sem in order:
                    v0 = entry_vals.get(sem, 0)
                    if v0 > 0:
                        self.handle.wait_ge(sem.h, v0)
                    self.handle.sem_inc(sem.h, comp[sem])
                ectx.__exit__(None, None, None)
            for e in self.engines:
                e.waited = snap_waited[e.name]

    def final_wait(self, eng, keys):
        w = self._deps(keys, keys)
        wl = list(w.items())

        if eng.name == self.target:
            for s, v in wl:
                self.handle.wait_ge(s.h, v)


def build(debug=None):
    nc = bass.Bass("TRN2", target_bir_lowering=False)

    def din(name, shape):
        return nc.dram_tensor(name, list(shape), F32, kind="ExternalInput").ap()

    xe = din("xe", [TOKC + HALO, D])
    cT = din("cT", [128, 16])
    flags_d = din("flags", [128, 2])
    ident_d = din("ident", [128, 128])
    ada_w = din("ada_w", [D, 6 * D])
    ada_b = din("ada_b", [1, 6 * D])
    g1T_d = din("g1T", [128, 16])
    g2T_d = din("g2T", [128, 16])
    lnG = din("lnG", [1, 1024])
    lnB = din("lnB", [1, 1024])
    bS = din("bS", [1, 1024])
    wS = din("wS", [8, 128, 128])
    biasT = din("biasT", [128, 16 * 384])
    tabc_d = din("tabc", [1, 16])
    w_in = din("w_in", [D, 9216])
    wbrA = din("wbrA", [1024, D])
    wbrB = din("wbrB", [1024, D])
    wout = din("wout", [D, D])
    wr_d = din("wr", [D, 36])
    w1 = din("w1", [NE, D, 512])
    w3 = din("w3", [NE, D, 512])
    w2 = din("w2", [NE, 512, D])
    fg = din("fg", [1, D])
    g2row = din("g2row", [1, D])
    g1row = din("g1row", [1, D])
    Utri_d = din("Utri", [128, 128])
    Ones_d = din("OnesM", [128, 128])
    ecap_d = din("ecap", [128, 32])
    vals_d = nc.dram_tensor("vals", [128, 128], I32, kind="ExternalInput").ap()
    fillv_d = nc.dram_tensor("fillv", [128, 2048], I32, kind="ExternalInput").ap()
    h2d = nc.dram_tensor("h2d", [TOKC, D], BF16, kind="Internal").ap()
    table = nc.dram_tensor("table", [NE * CAP, 4], I32, kind="Internal").ap()
    cntd = nc.dram_tensor("cntd", [1, NE], I32, kind="Internal").ap()
    Ybuf4 = nc.dram_tensor("Ybuf", [4 * TOKC + 256, D // 2], F32, kind="Internal").ap()
    Ybuf = Ybuf4.rearrange("(r h) c -> r (h c)", h=2)
    y = nc.dram_tensor("y", [TOKC, D], F32, kind="ExternalOutput").ap()
    x1s = nc.dram_tensor("x1s", [TOKC, D], F32, kind="Internal").ap()
    kTs = nc.dram_tensor("kTs", [2, 128, 4 * 512], BF16, kind="Internal").ap()
    Vts = nc.dram_tensor("Vts", [2, 128, 4 * 520], BF16, kind="Internal").ap()
    modd = nc.dram_tensor("modd", [1, 6 * D], F32, kind="Internal").ap()
    dbg_out = None
    if debug is not None:
        dbg_out = nc.dram_tensor("dbg", [128, debug["words"]], F32, kind="ExternalOutput").ap()

    es = contextlib.ExitStack()
    with es:
        es.enter_context(nc.allow_low_precision("bf16 matmul operands by design"))
        ARENA_W = 53200
        arena = es.enter_context(nc.sbuf_tensor("arena", [128, ARENA_W], F32))
        PS = [es.enter_context(nc.psum_tensor(f"ps{i}", [128, 1024], F32)) for i in range(4)]

        SEMH = {}
        for _n in (["s_pe", "s_act", "s_dve", "s_pool", "s_sp", "s_const", "s_misc"] + [f"s_ring{i}" for i in range(NSLOT)] + [f"s_ringb{i}" for i in range(NSLOT)] + [f"s_adb{i}" for i in range(3)] +
                   [f"s_x{i}" for i in range(2)] + [f"s_o{i}" for i in range(2)] + [f"s_ad{i}" for i in range(3)] +
                   [f"s_mr{i}" for i in range(MSLOT)] + [f"s_ix{i}" for i in range(5)] + [f"s_ga{i}" for i in range(5)] +
                   [f"s_sc{i}" for i in range(8)] + [f"s_h{i}" for i in range(2)] + [f"s_y{i}" for i in range(4)] + ["s_tab"] +
                   [f"s_cp{i}" for i in range(8)] + [f"s_f{i}" for i in range(12)] + [f"s_tp{i}" for i in range(4)] + [f"s_ab{i}" for i in range(2)] + [f"s_md{i}" for i in range(2)]):
            SEMH[_n] = es.enter_context(nc.semaphore(_n))

        def program(target, handle):
            def newsem(name):
                return Sem(SEMH[name])

            PE = Eng("pe", newsem("s_pe"), selfwait=False)
            ACT = Eng("act", newsem("s_act"))
            DVE = Eng("dve", newsem("s_dve"))
            POOL = Eng("pool", newsem("s_pool"))
            SP = Eng("sp", newsem("s_sp"))
            ring_sem = [newsem(f"s_ring{i}") for i in range(NSLOT)]
            ring_semb = [newsem(f"s_ringb{i}") for i in range(NSLOT)]
            adsemb = [newsem(f"s_adb{i}") for i in range(3)]
            xsem = [newsem(f"s_x{i}") for i in range(2)]
            osem = [newsem(f"s_o{i}") for i in range(2)]
            csem = newsem("s_const")
            csem.shared = True
            msem = newsem("s_misc")
            msem.shared = True
            adsem = [newsem(f"s_ad{i}") for i in range(3)]
            mr_sem = [newsem(f"s_mr{i}") for i in range(MSLOT)]
            ixsem = [newsem(f"s_ix{i}") for i in range(5)]
            gasem = [newsem(f"s_ga{i}") for i in range(5)]
            scsem = [newsem(f"s_sc{i}") for i in range(8)]

            class SemPool:
                def __init__(self, sems):
                    self.sems = sems
                    self.i = 0

                def nxt(self):
                    sm = self.sems[self.i % len(self.sems)]
                    self.i += 1
                    return sm

            cpool = SemPool([newsem(f"s_cp{i}") for i in range(8)])
            tpool = SemPool([newsem(f"s_tp{i}") for i in range(4)])
            absem = [newsem(f"s_ab{i}") for i in range(2)]
            fsem = [newsem(f"s_f{i}") for i in range(12)]
            mdsem = [newsem(f"s_md{i}") for i in range(2)]
            hsem = [newsem(f"s_h{i}") for i in range(2)]
            ysem = [newsem(f"s_y{i}") for i in range(4)]
            tabsem = newsem("s_tab")
            tabsem.shared = True
            P = Prog(target, handle)
            P.nofloor.add("pool")
            P.engines = [PE, ACT, DVE, POOL, SP]

            def sb(off, shape, dt=F32, parts=(0, 128)):
                esz = 4 if dt in (F32, I32) else 2
                n = int(np.prod(shape))
                nbytes = n * esz
                assert off % 4 == 0 and nbytes % 4 == 0
                assert off + nbytes <= ARENA_W * 4, (off, nbytes)
                ap = arena[parts[0]:parts[1], off // 4:(off + nbytes) // 4]
                if dt != F32:
                    ap = ap.bitcast(dt)
                if len(shape) == 2:
                    ap = ap.rearrange("p (a b) -> p a b", a=shape[0], b=shape[1])
                elif len(shape) == 3:
                    ap = ap.rearrange("p (a b c) -> p a b c", a=shape[0], b=shape[1], c=shape[2])
                return ap

            def pbank(b, n=512, off=0):
                return PS[b // 2][:, (b % 2) * 512 + off:(b % 2) * 512 + off + n]

            def pkey(b):
                return ("ps", b)

            C0 = 0
            o = C0
            identF = sb(o, [128]); o += 512
            identB = sb(o, [128], BF16); o += 256
            modT = sb(o, [96]); o += 384
            effT = sb(o, [32]); o += 128
            g1T = sb(o, [16]); o += 64
            g2T = sb(o, [16]); o += 64
            tabc = sb(o, [16]); o += 64
            flags = sb(o, [2]); o += 8
            ones8 = sb(o, [8]); o += 32
            wr = sb(o, [16, 36]); o += 2304
            WsT = sb(o, [8, 128], BF16); o += 2048
            small = sb(o, [256]); o += 1024
            rt = sb(o, [256]); o += 1024
            urow = sb(o, [128], BF16, parts=(0, 1)); o += 256
            vrow = sb(o, [128], BF16, parts=(0, 1)); o += 256
            sel0 = sb(o, [16, 32]); o += 2048
            sel1 = sb(o, [16, 32]); o += 2048
            wsl = sb(o, [16, 2]); o += 128
            ecap = sb(o, [32]); o += 128
            vals = sb(o, [128], I32); o += 512
            CONST_END = o
            RING0 = (CONST_END + 63) // 64 * 64
            PH0 = RING0 + NSLOT * SLOT_B
            PH_END = ARENA_W * 4

            def ring_view(slot, a, b):
                return sb(RING0 + slot * SLOT_B, [a, b], BF16)

            class WS:
                units = []
                released = []
                nloaded = 0

            def w_add(src, a, b):
                WS.units.append((src, a, b))
                WS.released.append(False)
                return len(WS.units) - 1

            def w_pump():
                while WS.nloaded < len(WS.units):
                    u = WS.nloaded
                    if u >= NSLOT and not WS.released[u - NSLOT]:
                        break
                    src, a, b = WS.units[u]
                    slot = u % NSLOT
                    dst = ring_view(slot, a, b)
                    P.alias[("ring", slot)] = (("ring", slot, 0), ("ring", slot, 1))
                    h_ = a // 2
                    P.dma(POOL, [], [("ring", slot, 0)],
                          lambda e, dst=dst, src=src: e.dma_start(out=dst[:, 0:h_, :], in_=src[:, 0:h_, :]),
                          ring_sem[slot])
                    P.dma(POOL, [], [("ring", slot, 1)],
                          lambda e, dst=dst, src=src: e.dma_start(out=dst[:, h_:a, :], in_=src[:, h_:a, :]),
                          ring_semb[slot])
                    WS.nloaded += 1

            def w_get(u):
                w_pump()
                assert u < WS.nloaded, ("weight unit not loadable", u, WS.nloaded)
                src, a, b = WS.units[u]
                slot = u % NSLOT
                return ring_view(slot, a, b), ("ring", slot)

            def w_rel(u):
                WS.released[u] = True
                w_pump()

            def win_blk(c0):
                return w_in[:, c0:c0 + 512].rearrange("(c p) n -> p c n", p=128)

            plan = []
            for p in range(NPASS):
                pl = {}
                pl["k"] = [None, None]
                pl["vb"] = [None, None]
                pl["q"] = [None, None]
                for hh in range(2):
                    pl["k"][hh] = w_add(win_blk(3072 + hh * 512), 16, 512)
                    pl["vb"][hh] = w_add(win_blk(4096 + hh * 512), 16, 512)
                    pl["q"][hh] = w_add(win_blk(2048 + hh * 512), 16, 512)
                pl["u"] = [w_add(win_blk(0 + i * 512), 16, 512) for i in range(2)]
                pl["v"] = [w_add(win_blk(1024 + i * 512), 16, 512) for i in range(2)]
                pl["gA"] = [None] * 4
                pl["gB"] = [None] * 4
                pl["brA"] = [None] * 2
                pl["brB"] = [None] * 2
                for q in range(4):
                    pl["gA"][q] = w_add(win_blk(5120 + q * 512), 16, 512)
                    pl["gB"][q] = w_add(win_blk(7168 + q * 512), 16, 512)
                    if q % 2 == 0:
                        hf = q // 2
                        pl["brA"][hf] = w_add(wbrA[:, hf * 1024:(hf + 1) * 1024].rearrange("(c p) n -> p c n", p=128), 8, 1024)
                        pl["brB"][hf] = w_add(wbrB[:, hf * 1024:(hf + 1) * 1024].rearrange("(c p) n -> p c n", p=128), 8, 1024)
                pl["wo"] = [w_add(wout[:, cu * 512:(cu + 1) * 512].rearrange("(c p) n -> p c n", p=128), 16, 512) for cu in range(4)]
                plan.append(pl)

            def act_fn(out, in_, func, reads, writes, scale=1.0, bias=0.0, accum=None):
                def emit(e):
                    kw = {}
                    if accum is not None:
                        kw["accum_out"] = accum
                    return e.activation(out=out, in_=in_, func=func, bias=bias, scale=scale, **kw)
                P.op(ACT, reads, writes, emit)

            def dve(reads, writes, emit):
                P.op(DVE, reads, writes, emit)

            def pe_group(reads, writes, emit):
                P.op(PE, reads, writes, emit)

            def mm_acc(e, out, pairs):
                n = len(pairs)
                ins = None
                for i, (l, r) in enumerate(pairs):
                    ins = e.matmul(out, l, r, start=(i == 0), stop=(i == n - 1))
                return ins

            def sp_load(dst, src, writes, sem, reads=()):
                P.dma(SP, list(reads), list(writes), lambda e: e.dma_start(out=dst, in_=src), sem)

            sp_load(identF, ident_d, ["identF"], cpool.nxt())
            sp_load(flags, flags_d, ["flags"], cpool.nxt())
            sp_load(g1T, g1T_d, ["g1T"], cpool.nxt())
            sp_load(g2T, g2T_d, ["g2T"], cpool.nxt())
            sp_load(tabc, tabc_d[0].partition_broadcast(128), ["tabc"], cpool.nxt())
            sp_load(wr, wr_d.rearrange("(c p) n -> p c n", p=128), ["wr"], cpool.nxt())
            sp_load(ecap, ecap_d, ["ecap"], cpool.nxt())
            sp_load(vals, vals_d, ["vals"], cpool.nxt())
            scT = sb(PH0, [16])
            cTs = sb(PH0 + 64, [16])
            one11 = sb(PH0 + 128, [1])
            sp_load(cTs, cT, ["cTs"], cpool.nxt())
            dve(["identF"], ["identB"], lambda e: e.tensor_copy(out=identB, in_=identF))
            dve([], ["ones8"], lambda e: e.memset(ones8, 1.0))
            dve([], ["uvrow"], lambda e: e.memset(urow[:, 0:64], NEG))
            dve([], ["uvrow"], lambda e: e.memset(urow[:, 64:128], 0.0))
            dve([], ["uvrow"], lambda e: e.memset(vrow[:, 0:64], 0.0))
            dve([], ["uvrow"], lambda e: e.memset(vrow[:, 64:128], 1.0))
            dve([], ["one11"], lambda e: e.memset(one11, 1.0))
            act_fn(scT, cTs, AF.Silu, ["cTs"], ["scT"])

            wsf = sb(PH0 + 1024, [8, 128])
            P.dma(SP, [], ["wsf"], lambda e: e.dma_start(out=wsf, in_=wS.rearrange("g t s -> t g s")), cpool.nxt())
            dve(["wsf"], ["wsf"], lambda e: e.memset(wsf[0:64, :, 64:128], 0.0))
            for hb in range(2):
                def emit(e, hb=hb):
                    ins = None
                    for g in range(4):
                        gg = hb * 4 + g
                        ins = e.transpose(pbank(hb, 128, g * 128), wsf[:, gg, :], identF)
                    return ins
                pe_group(["wsf", "identF"], [pkey(hb)], emit)
                dve([pkey(hb)], ["WsT"], lambda e, hb=hb: e.tensor_copy(
                    out=WsT[:, hb * 4:(hb + 1) * 4, :], in_=pbank(hb).rearrange("p (g t) -> p g t", g=4)))

            AD0 = PH0 + 5120
            NAD = 3
            adbuf = [sb(AD0 + i * 32768, [16, 512]) for i in range(NAD)]
            abrow = sb(AD0 + NAD * 32768, [512], parts=(0, 1))
            abrow2 = sb(AD0 + NAD * 32768 + 2048, [512], parts=(0, 1))
            mrow = [sb(AD0 + NAD * 32768 + 4096 + i * 2048, [512], parts=(0, 1)) for i in range(2)]
            accb = [sb(AD0 + NAD * 32768 + 8192 + i * 2048, [512]) for i in range(2)]
            NB = 24
            MTB = 7
            def ada_load(blk):
                bi = blk % NAD
                P.alias[("adbuf", bi)] = (("adbuf", bi, 0), ("adbuf", bi, 1))
                srcv = ada_w[:, blk * 512:(blk + 1) * 512].rearrange("(c p) n -> p c n", p=128)
                P.dma(SP, [], [("adbuf", bi, 0)], lambda e: e.dma_start(out=adbuf[bi][:, 0:8, :], in_=srcv[:, 0:8, :]), adsem[bi])
                P.dma(SP, [], [("adbuf", bi, 1)], lambda e: e.dma_start(out=adbuf[bi][:, 8:16, :], in_=srcv[:, 8:16, :]), adsemb[bi])

            for blk in range(min(NAD - 1, NB)):
                ada_load(blk)
            for blk in range(NB):
                bi = blk % NAD
                if blk + NAD - 1 < NB:
                    ada_load(blk + NAD - 1)
                ab = abrow if blk % 2 == 0 else abrow2
                abk = ("abrow", blk % 2)
                P.dma(SP, [], [abk], lambda e, ab=ab, blk=blk: e.dma_start(out=ab, in_=ada_b[:, blk * 512:(blk + 1) * 512]), absem[blk % 2])
                pb_ = blk % 2

                ac = accb[blk % 2]
                ack = ("accb", blk % 2)
                NDV = 10
                pe_group([("adbuf", bi), "scT"], [pkey(pb_)], lambda e, bi=bi, pb_=pb_: [
                    e.matmul(pbank(pb_)[0:1, :], scT[:, dc:dc + 1], adbuf[bi][:, dc, :], start=(dc == NDV), stop=False)
                    for dc in range(NDV, 16)][-1])
                dve([("adbuf", bi), "scT"], [ack], lambda e, bi=bi, ac=ac: e.tensor_scalar(
                    out=ac, in0=adbuf[bi][:, 0, :], scalar1=scT[:, 0:1], scalar2=None, op0=ALU.mult))
                for dc in range(1, NDV):
                    dve([("adbuf", bi), "scT", ack], [ack], lambda e, bi=bi, ac=ac, dc=dc: e.scalar_tensor_tensor(
                        out=ac, in0=adbuf[bi][:, dc, :], scalar=scT[:, dc:dc + 1], in1=ac, op0=ALU.mult, op1=ALU.add))
                pe_group([ack, "ones8"], [pkey(pb_)], lambda e, ac=ac, pb_=pb_: e.matmul(
                    pbank(pb_)[0:1, :], ones8[:, 0:1], ac, start=False, stop=True))
                mr = mrow[blk % 2]
                mrk = ("mrow", blk % 2)
                dve([pkey(pb_), abk], [mrk], lambda e, mr=mr, ab=ab, pb_=pb_: e.tensor_tensor(
                    out=mr, in0=pbank(pb_)[0:1, :], in1=ab, op=ALU.add))
                P.dma(SP, [mrk], [("modd", blk)], lambda e, mr=mr, blk=blk: e.dma_start(
                    out=modd[:, blk * 512:(blk + 1) * 512], in_=mr), mdsem[blk % 2])

                def emit2(e, mr=mr, blk=blk):
                    ins = None
                    for k in range(4):
                        ins = e.matmul(pbank(MTB)[:, blk * 4 + k:blk * 4 + k + 1], mr[0:1, k * 128:(k + 1) * 128], one11[0:1, 0:1],
                                       start=True, stop=True)
                    return ins
                pe_group([mrk, "one11"], [("mtb", blk)], emit2)
            dve([("mtb", b) for b in range(NB)], ["modT"], lambda e: e.tensor_copy(out=modT, in_=pbank(MTB)[:, 0:96]))
            dve(["modT", "g1T"], ["effT"], lambda e: e.scalar_tensor_tensor(
                out=effT[:, 0:16], in0=modT[:, 16:32], scalar=1.0, in1=g1T, op0=ALU.add, op1=ALU.mult))
            dve(["modT", "g2T"], ["effT"], lambda e: e.scalar_tensor_tensor(
                out=effT[:, 16:32], in0=modT[:, 64:80], scalar=1.0, in1=g2T, op0=ALU.add, op1=ALU.mult))
            shift1 = modT[:, 0:16]
            shift2 = modT[:, 48:64]
            setup_keys = ["modT", "effT", "identF", "identB", "flags", "tabc", "wr", "WsT", "ones8"]
            setup_scratch = ["scT", "cTs", "one11", "wsf"] + [("adbuf", i) for i in range(3)] + \
                [("abrow", i) for i in range(2)] + [("mrow", i) for i in range(2)]
            modd_keys = [("modd", b) for b in range(NB)]

            dbg_done = [False]

            def dbg_dump(items):
                off = 0
                for ap, nw, keys in items:
                    P.dma(SP, list(keys), [("dbg", off)], lambda e, ap=ap, off=off, nw=nw: e.dma_start(
                        out=dbg_out[:, off:off + nw], in_=ap), msem)
                    off += nw
                dbg_done[0] = True

            def rms_rstd(ss_ap, rstd_ap, key_ss, key_rstd):
                act_fn(rstd_ap, ss_ap, AF.Sqrt, [key_ss], [key_rstd], scale=1.0 / D, bias=EPS)
                dve([key_rstd], [key_rstd], lambda e: e.reciprocal(out=rstd_ap, in_=rstd_ap))

            for p in range(NPASS if debug is None else debug.get("npass", NPASS)):
                pl = plan[p]
                row0 = p * TP
                fl = flags[:, p:p + 1]
                o = PH0
                h1own = sb(o, [16, 1024], BF16); o += 32768
                ybT = sb(o, [8, 1024], BF16); o += 16384
                GM0 = o
                h1halo = sb(o, [16, 512], BF16); o += 16384
                MIX0 = o
                xt = [sb(MIX0 + i * 8192, [2048]) for i in range(2)]
                junk = sb(MIX0 + 16384, [2048], BF16)
                P.fence()
                effB1 = sb(MIX0 + 20480, [2048])
                shiftB1 = sb(MIX0 + 28672, [2048])
                g1b = sb(PH0 + 32768, [2048])
                h1tok = [sb(MIX0 + 36864 + i * 4096, [2048], BF16) for i in range(2)]
                sp_load(g1b, g1row[0].partition_broadcast(128), ["g1b"], cpool.nxt())
                sp_load(effB1, modd[0, D:2 * D].partition_broadcast(128), ["effB1"], cpool.nxt(), reads=modd_keys)
                sp_load(shiftB1, modd[0, 0:D].partition_broadcast(128), ["shiftB1"], cpool.nxt(), reads=modd_keys)
                dve(["effB1", "g1b"], ["effB1"], lambda e: e.scalar_tensor_tensor(
                    out=effB1, in0=effB1, scalar=1.0, in1=g1b, op0=ALU.add, op1=ALU.mult))
                def p1_front(j):
                    b = j % 2
                    xk = ("xt", b)
                    P.dma(SP, [], [xk], lambda e, b=b, j=j: e.dma_start(
                        out=xt[b], in_=xe[row0 + j * 128:row0 + (j + 1) * 128, :]), xsem[b])
                    ssa = small[:, j:j + 1]
                    rsa = small[:, 16 + j:17 + j]
                    act_fn(junk, xt[b], AF.Square, [xk], [("ss", j)], accum=ssa)
                    rms_rstd(ssa, rsa, ("ss", j), ("rs", j))
                    dve([xk, ("rs", j), "effB1"], [xk], lambda e, b=b, rsa=rsa: e.scalar_tensor_tensor(
                        out=xt[b], in0=xt[b], scalar=rsa, in1=effB1, op0=ALU.mult, op1=ALU.mult))
                    dve([xk, "shiftB1"], [("h1tok", b)], lambda e, b=b: e.tensor_tensor(out=h1tok[b], in0=xt[b], in1=shiftB1, op=ALU.add))

                def p1_back(j):
                    b = j % 2
                    for hb_ in range(2):
                        q = (j % 2) * 2 + hb_
                        tbv = PS[q // 2][:, (q % 2) * 512:(q % 2) * 512 + 512].bitcast(BF16)

                        def emit(e, b=b, hb_=hb_, tbv=tbv):
                            ins = None
                            for k in range(8):
                                dc = hb_ * 8 + k
                                ins = e.transpose(tbv[:, k * 128:(k + 1) * 128], h1tok[b][:, dc * 128:(dc + 1) * 128], identB)
                            return ins
                        pe_group([("h1tok", b), "identB"], [pkey(q)], emit)
                        if j < 4:
                            dst = h1halo[:, hb_ * 8:(hb_ + 1) * 8, j * 128:(j + 1) * 128]
                            dk = ("h1halo", j)
                        else:
                            dst = h1own[:, hb_ * 8:(hb_ + 1) * 8, (j - 4) * 128:(j - 3) * 128]
                            dk = ("h1own", j - 4)
                        act_fn(dst, tbv.rearrange("p (k t) -> p k t", k=8), AF.Copy, [pkey(q)], [dk])

                J0 = 0 if p == 0 else 4
                p1_front(J0)
                for j in range(J0, 12):
                    if j + 1 < 12:
                        p1_front(j + 1)
                    p1_back(j)
                if debug is not None and debug["stage"] == "h1":
                    dbg_dump([(h1own.rearrange("p a b -> p (a b)").bitcast(F32), 8192, [("h1own", i) for i in range(8)]),
                              (h1halo.rearrange("p a b -> p (a b)").bitcast(F32), 4096, [("h1halo", i) for i in range(4)]),
                              (modT, 96, ["modT"]), (effT, 32, ["effT"])])
                    break

                def h1_rhs(dc, t0, n):
                    if t0 < 512:
                        assert t0 + n <= 512
                        return h1halo[:, dc, t0:t0 + n]
                    return h1own[:, dc, t0 - 512:t0 - 512 + n]

                def h1_keys(t0, n):
                    ks = []
                    for t in range(t0 // 128, (t0 + n) // 128):
                        ks.append(("h1halo", t) if t < 4 else ("h1own", t - 4))
                    return ks

                P.fence()
                o = MIX0
                qT = sb(o, [4, 1024], BF16); o += 8192
                kT = sb(o, [4, 1536], BF16); o += 12288
                Vt = sb(o, [12, 8 * 65], BF16); o += 12480
                o = (o + 63) // 64 * 64
                biasS = sb(o, [8, 384]); o += 12288
                Pb = [sb(o + i * 1280, [640], BF16) for i in range(3)]; o += 3840
                ybt = sb(o, [512], BF16); o += 1024
                rden = sb(o, [8]); o += 32
                assert o <= PH_END
                for hh in range(2):
                    P.dma(SP, [], ["biasS"], lambda e, hh=hh: e.dma_start(
                        out=biasS.rearrange("p a b -> p (a b)"), in_=biasT[:, hh * 3072:(hh + 1) * 3072]), cpool.nxt())
                    wk, wkk = w_get(pl["k"][hh])
                    if p == 1:
                        P.dma(SP, [("kTs", hh)], [("kT", 0)], lambda e, hh=hh: e.dma_start(
                            out=kT[:, :, 0:512], in_=kTs[hh].rearrange("p (c t) -> p c t", c=4)), cpool.nxt())
                    for tt in range(3):
                        if p == 1 and tt == 0:
                            continue
                        for cc in range(4):
                            bnk = (tt * 4 + cc) % 8
                            def emit(e, tt=tt, cc=cc, bnk=bnk):
                                return mm_acc(e, pbank(bnk), [(wk[:, dc, cc * 128:(cc + 1) * 128], h1_rhs(dc, tt * 512, 512)) for dc in range(16)])
                            pe_group([wkk] + h1_keys(tt * 512, 512), [pkey(bnk)], emit)
                            act_fn(kT[:, cc, tt * 512:(tt + 1) * 512], pbank(bnk), AF.Copy, [pkey(bnk)], [("kT", tt)])
                    if p == 0:
                        P.dma(SP, [("kT", 2)], [("kTs", hh)], lambda e, hh=hh: e.dma_start(
                            out=kTs[hh].rearrange("p (c t) -> p c t", c=4), in_=kT[:, :, 1024:1536]), cpool.nxt())
                    w_rel(pl["k"][hh])
                    wv, wvk = w_get(pl["vb"][hh])
                    if p == 1:
                        P.dma(SP, [("Vts", hh)], [("Vt", j_) for j_ in range(4)], lambda e, hh=hh: e.dma_start(
                            out=Vt[:, 0:4, :], in_=Vts[hh].rearrange("p (j c) -> p j c", j=4)), cpool.nxt())
                    for j in range(12):
                        if p == 1 and j < 4:
                            continue
                        bnk = j % 8
                        def emit(e, j=j, bnk=bnk):
                            return mm_acc(e, pbank(bnk), [(h1_rhs(dc, j * 128, 128), wv[:, dc, :]) for dc in range(16)])
                        pe_group([wvk] + h1_keys(j * 128, 128), [pkey(bnk)], emit)
                        vdst = Vt[:, j, :].rearrange("p (h d) -> p h d", h=8)
                        sc = fl if j < 4 else 1.0
                        act_fn(vdst[:, :, 0:64], pbank(bnk).rearrange("p (h d) -> p h d", h=8), AF.Copy,
                               [pkey(bnk), "flags"], [("Vt", j)], scale=sc)
                        act_fn(vdst[:, :, 64:65], ones8.rearrange("p (h d) -> p h d", d=1), AF.Copy,
                               ["ones8", "flags"], [("Vt", j)], scale=sc)
                    if p == 0:
                        P.dma(SP, [("Vt", j_) for j_ in range(8, 12)], [("Vts", hh)], lambda e, hh=hh: e.dma_start(
                            out=Vts[hh].rearrange("p (j c) -> p j c", j=4), in_=Vt[:, 8:12, :]), cpool.nxt())
                    w_rel(pl["vb"][hh])
                    wq, wqk = w_get(pl["q"][hh])
                    for tt in range(2):
                        for cc in range(4):
                            bnk = (tt * 4 + cc) % 8
                            def emit(e, tt=tt, cc=cc, bnk=bnk):
                                return mm_acc(e, pbank(bnk), [(wq[:, dc, cc * 128:(cc + 1) * 128], h1_rhs(dc, 512 + tt * 512, 512)) for dc in range(16)])
                            pe_group([wqk] + h1_keys(512 + tt * 512, 512), [pkey(bnk)], emit)
                            act_fn(qT[:, cc, tt * 512:(tt + 1) * 512], pbank(bnk), AF.Copy, [pkey(bnk)], [("qT", tt)], scale=0.125)
                    w_rel(pl["q"][hh])
                    NSET = 3
                    SK = 2
                    jobs = [(i, hl) for i in range(8) for hl in range(8)]

                    def st_S(n):
                        i, hl = jobs[n]
                        j = i + 4
                        h = hh * 8 + hl
                        cc = hl // 2
                        pb0 = (hl % 2) * 64
                        st = n % NSET
                        s0, s1 = 2 * st, 2 * st + 1

                        def emit(e):
                            ins = None
                            qv = qT[pb0:pb0 + 64, cc, i * 128:(i + 1) * 128]
                            for dl in range(5):
                                kt = j - dl
                                kv = kT[pb0:pb0 + 64, cc, kt * 128:(kt + 1) * 128]
                                if dl < 3:
                                    outp = pbank(s0, 128, dl * 128)
                                else:
                                    outp = pbank(s1, 128, (dl - 3) * 128)
                                if dl == 4:
                                    e.matmul(outp, kv, qv, start=True, stop=False)
                                    ins = e.matmul(outp, urow, vrow, start=False, stop=True)
                                else:
                                    ins = e.matmul(outp, kv, qv, start=True, stop=True)
                            return ins
                        pe_group([("qT", i // 4), "uvrow"] + [("kT", (j - dl) // 4) for dl in range(5)], [pkey(s0), pkey(s1)], emit)
                        dve([pkey(s0), "biasS"], [pkey(s0)], lambda e: e.tensor_tensor(
                            out=pbank(s0, 384), in0=pbank(s0, 384), in1=biasS[:, hl, :], op=ALU.add))
                        act_fn(Pb[st][:, 0:384], pbank(s0, 384), AF.Exp, [pkey(s0)], [("Pb", st)])
                        act_fn(Pb[st][:, 384:640], pbank(s1, 256), AF.Exp, [pkey(s1), "tabc"], [("Pb", st)],
                               bias=tabc[:, h:h + 1])

                    def st_PV(n):
                        i, hl = jobs[n]
                        j = i + 4
                        st = n % NSET
                        ob = 6 + hl // 4

                        def emit(e):
                            pairs = []
                            for dl in range(5):
                                kt = j - dl
                                pairs.append((Pb[st][:, dl * 128:(dl + 1) * 128], Vt[:, kt, hl * 65:(hl + 1) * 65]))
                            return mm_acc(e, pbank(ob, 65, (hl % 4) * 65), pairs)
                        pe_group([("Pb", st)] + [("Vt", j - dl) for dl in range(5)], [pkey(ob)], emit)

                    def st_norm(i):
                        for ob in (6, 7):
                            ov = pbank(ob, 260).rearrange("p (h d) -> p h d", h=4)
                            dve([pkey(ob)], ["rden"], lambda e, ob=ob, ov=ov: e.reciprocal(
                                out=rden[:, (ob - 6) * 4:(ob - 5) * 4], in_=ov[:, :, 64]))
                            for k in range(4):
                                hl = (ob - 6) * 4 + k
                                dve([pkey(ob), "rden"], ["ybt"], lambda e, ov=ov, k=k, hl=hl: e.tensor_scalar(
                                    out=ybt[:, hl * 64:(hl + 1) * 64], in0=ov[:, k, 0:64], scalar1=rden[:, hl:hl + 1],
                                    scalar2=None, op0=ALU.mult))

                    def st_T(i):
                        tb = PS[0][:, 0:512].bitcast(BF16)

                        def emit(e):
                            ins = None
                            for k in range(4):
                                ins = e.transpose(tb[:, k * 128:(k + 1) * 128], ybt[:, k * 128:(k + 1) * 128], identB)
                            return ins
                        pe_group(["ybt", "identB"], [pkey(0)], emit)
                        act_fn(ybT[:, hh * 4:(hh + 1) * 4, i * 128:(i + 1) * 128],
                               tb[:, 0:512].rearrange("p (k t) -> p k t", k=4), AF.Copy, [pkey(0)], [("ybT", i)])

                    pendT = []
                    for n in range(len(jobs) + SK + 2):
                        if n < len(jobs):
                            st_S(n)
                        m = n - SK
                        if 0 <= m < len(jobs):
                            st_PV(m)
                            if jobs[m][1] == 7:
                                st_norm(jobs[m][0])
                                pendT.append((jobs[m][0], n + 2))
                        for (ti, due) in pendT:
                            if due == n:
                                st_T(ti)
                if debug is not None and debug["stage"] == "attn":
                    dbg_dump([(ybT.rearrange("p a b -> p (a b)").bitcast(F32), 4096, [("ybT", i) for i in range(8)])])
                    break

                P.fence()
                o = GM0
                yaT = sb(o, [8, 1024], BF16); o += 16384
                uT = sb(o, [8, 1024], BF16); o += 16384
                vn = [sb(o + i * 2048, [1024], BF16) for i in range(2)]; o += 4096
                vg = [sb(o + i * 4096, [1024]) for i in range(2)]; o += 8192
                lnGb = sb(o, [1024]); o += 4096
                lnBb = sb(o, [1024]); o += 4096
                bSb = sb(o, [1024]); o += 4096
                t1 = sb(o, [1024]); o += 4096
                bst = sb(o, [12]); o += 48
                assert o <= PH_END
                gkeys = [("kT", t) for t in range(3)] + [("qT", t) for t in range(2)] + [("Vt", j) for j in range(12)] + \
                    ["biasS", "ybt", "rden"] + [("Pb", i) for i in range(2)] + [("tmpS", i) for i in range(2)]
                sp_load(lnGb, lnG[0].partition_broadcast(128), ["lnGb"], cpool.nxt())
                sp_load(lnBb, lnB[0].partition_broadcast(128), ["lnBb"], cpool.nxt())
                sp_load(bSb, bS[0].partition_broadcast(128), ["bSb"], cpool.nxt())
                for uu in range(2):
                    wu, wuk = w_get(pl["u"][uu])
                    for tt in range(2):
                        for cc in range(4):
                            bnk = (tt * 4 + cc) % 8
                            def emit(e, tt=tt, cc=cc, bnk=bnk, wu=wu):
                                return mm_acc(e, pbank(bnk), [(wu[:, dc, cc * 128:(cc + 1) * 128], h1_rhs(dc, 512 + tt * 512, 512)) for dc in range(16)])
                            pe_group([wuk] + h1_keys(512 + tt * 512, 512), [pkey(bnk)], emit)
                            act_fn(uT[:, uu * 4 + cc, tt * 512:(tt + 1) * 512], pbank(bnk), AF.Gelu,
                                   [pkey(bnk)], [("uT", tt)])
                    w_rel(pl["u"][uu])
                wv0, wv0k = w_get(pl["v"][0])
                wv1, wv1k = w_get(pl["v"][1])
                wvs = [(wv0, wv0k), (wv1, wv1k)]
                def gm_front(i):
                    b = i % 2
                    for vu in range(2):
                        bnk = b * 4 + vu
                        def emit(e, i=i, vu=vu, bnk=bnk):
                            return mm_acc(e, pbank(bnk), [(h1_rhs(dc, 512 + i * 128, 128), wvs[vu][0][:, dc, :]) for dc in range(16)])
                        pe_group([wvs[vu][1]] + h1_keys(512 + i * 128, 128), [pkey(bnk)], emit)
                        act_fn(vg[b][:, vu * 512:(vu + 1) * 512], pbank(bnk), AF.Gelu, [pkey(bnk)], [("vg", b)])
                    st6 = small[:, 64 + b * 12:64 + b * 12 + 12]
                    mv = small[:, 96 + b * 4:96 + b * 4 + 2]
                    rs_ = small[:, 98 + b * 4:99 + b * 4]
                    for vu in range(2):
                        dve([("vg", b)], [("st6", b)], lambda e, b=b, vu=vu, st6=st6: e.bn_stats(
                            out=st6[:, vu * 6:(vu + 1) * 6], in_=vg[b][:, vu * 512:(vu + 1) * 512]))
                    dve([("st6", b)], [("mv", b)], lambda e, st6=st6, mv=mv: e.bn_aggr(out=mv, in_=st6))
                    act_fn(rs_, mv[:, 1:2], AF.Sqrt, [("mv", b)], [("rsv", b)], bias=EPS)
                    dve([("rsv", b)], [("rsv", b)], lambda e, rs_=rs_: e.reciprocal(out=rs_, in_=rs_))
                    dve([("vg", b), ("mv", b), ("rsv", b)], [("vg", b)], lambda e, b=b, mv=mv, rs_=rs_: e.tensor_scalar(
                        out=vg[b], in0=vg[b], scalar1=mv[:, 0:1], scalar2=rs_, op0=ALU.subtract, op1=ALU.mult))
                    dve([("vg", b), "lnGb"], [("vg", b)], lambda e, b=b: e.tensor_tensor(out=vg[b], in0=vg[b], in1=lnGb, op=ALU.mult))
                    dve([("vg", b), "lnBb"], [("vn", b)], lambda e, b=b: e.tensor_tensor(out=vn[b], in0=vg[b], in1=lnBb, op=ALU.add))

                def gm_back(i):
                    b = i % 2
                    for gh in range(2):
                        bnk = b * 4 + 2 + gh
                        def emit(e, b=b, gh=gh, bnk=bnk):
                            ins = None
                            for g4 in range(4):
                                g = gh * 4 + g4
                                ins = e.matmul(pbank(bnk, 128, g4 * 128), vn[b][:, g * 128:(g + 1) * 128], WsT[:, g, :], start=True, stop=True)
                            return ins
                        pe_group([("vn", b), "WsT"], [pkey(bnk)], emit)
                        dve([pkey(bnk), "bSb"], ["t1"], lambda e, gh=gh, bnk=bnk: e.tensor_tensor(
                            out=t1[:, gh * 512:(gh + 1) * 512], in0=pbank(bnk), in1=bSb[:, gh * 512:(gh + 1) * 512], op=ALU.add))
                        dve(["t1", ("uT", i // 4)], [("yaT", i)], lambda e, gh=gh, i=i: e.tensor_tensor(
                            out=yaT[:, gh * 4:(gh + 1) * 4, i * 128:(i + 1) * 128],
                            in0=t1[:, gh * 512:(gh + 1) * 512].rearrange("p (g t) -> p g t", g=4),
                            in1=uT[:, gh * 4:(gh + 1) * 4, i * 128:(i + 1) * 128], op=ALU.mult))
                gm_front(0)
                for i in range(8):
                    if i + 1 < 8:
                        gm_front(i + 1)
                    gm_back(i)
                w_rel(pl["v"][0])
                w_rel(pl["v"][1])
                if debug is not None and debug["stage"] == "gmlp":
                    dbg_dump([(yaT.rearrange("p a b -> p (a b)").bitcast(F32), 4096, [("yaT", i) for i in range(8)])])
                    break

                P.fence()
                o = GM0 + 16384
                mergedT = sb(o, [16, 1024], BF16); o += 32768
                sg = [sb(o + i * 2048, [512]) for i in range(4)]; o += 8192
                assert o <= PH_END
                mkeys = [("uT", t) for t in range(2)] + [("vg", i) for i in range(2)] + [("vn", i) for i in range(2)] + ["t1", "lnGb", "lnBb", "bSb"]
                for q in range(4):
                    wga, wgak = w_get(pl["gA"][q])
                    wgb, wgbk = w_get(pl["gB"][q])
                    wa, wak = w_get(pl["brA"][q // 2])
                    wb, wbk = w_get(pl["brB"][q // 2])
                    for tt in range(2):
                        for c4 in range(4):
                            dcc = q * 4 + c4
                            st = (tt * 4 + c4) % 2
                            bA, bB, bYa, bYb = st * 4, st * 4 + 1, st * 4 + 2, st * 4 + 3
                            ts_ = slice(tt * 512, (tt + 1) * 512)

                            def emitA(e, c4=c4, ts_=ts_, bA=bA, wga=wga):
                                return mm_acc(e, pbank(bA), [(wga[:, dc, c4 * 128:(c4 + 1) * 128], h1own[:, dc, ts_]) for dc in range(16)])
                            pe_group([wgak] + [("h1own", t) for t in range(tt * 4, tt * 4 + 4)], [pkey(bA)], emitA)

                            def emitB(e, c4=c4, ts_=ts_, bB=bB, wgb=wgb):
                                return mm_acc(e, pbank(bB), [(wgb[:, dc, c4 * 128:(c4 + 1) * 128], h1own[:, dc, ts_]) for dc in range(16)])
                            pe_group([wgbk] + [("h1own", t) for t in range(tt * 4, tt * 4 + 4)], [pkey(bB)], emitB)
                            col = (q % 2) * 512 + c4 * 128

                            def emitYa(e, col=col, ts_=ts_, bYa=bYa, wa=wa):
                                return mm_acc(e, pbank(bYa), [(wa[:, kc, col:col + 128], yaT[:, kc, ts_]) for kc in range(8)])
                            pe_group([wak] + [("yaT", t) for t in range(tt * 4, tt * 4 + 4)], [pkey(bYa)], emitYa)

                            def emitYb(e, col=col, ts_=ts_, bYb=bYb, wb=wb):
                                return mm_acc(e, pbank(bYb), [(wb[:, kc, col:col + 128], ybT[:, kc, ts_]) for kc in range(8)])
                            pe_group([wbk] + [("ybT", t) for t in range(tt * 4, tt * 4 + 4)], [pkey(bYb)], emitYb)
                            sga, sgb = sg[st * 2], sg[st * 2 + 1]
                            act_fn(sga, pbank(bA), AF.Sigmoid, [pkey(bA)], [("sg", st * 2)])
                            act_fn(sgb, pbank(bB), AF.Sigmoid, [pkey(bB)], [("sg", st * 2 + 1)])
                            dve([("sg", st * 2), pkey(bYa)], [("sg", st * 2)], lambda e, sga=sga, bYa=bYa: e.tensor_tensor(
                                out=sga, in0=sga, in1=pbank(bYa), op=ALU.mult))
                            dve([("sg", st * 2 + 1), pkey(bYb)], [("sg", st * 2 + 1)], lambda e, sgb=sgb, bYb=bYb: e.tensor_tensor(
                                out=sgb, in0=sgb, in1=pbank(bYb), op=ALU.mult))
                            dve([("sg", st * 2), ("sg", st * 2 + 1)], [("mergedT", tt)], lambda e, sga=sga, sgb=sgb, dcc=dcc, ts_=ts_: e.tensor_tensor(
                                out=mergedT[:, dcc, ts_], in0=sga, in1=sgb, op=ALU.add))
                    w_rel(pl["gA"][q])
                    w_rel(pl["gB"][q])
                    if q % 2 == 1:
                        w_rel(pl["brA"][q // 2])
                        w_rel(pl["brB"][q // 2])
                if debug is not None and debug["stage"] == "merge":
                    dbg_dump([(mergedT.rearrange("p a b -> p (a b)").bitcast(F32), 8192, [("mergedT", i) for i in range(2)])])
                    break

                P.fence()
                w2effb = sb(PH0, [2048])
                shift2b = sb(PH0 + 8192, [2048])
                h2row = [sb(PH0 + 16384 + i * 4096, [2048], BF16) for i in range(2)]
                g2b = sb(PH0 + 24576, [2048])
                gate1b = sb(PH0 + 32768, [2048])
                h2f = sb(PH0 + 40960, [16, 128])
                xt6 = [sb(PH0 + 49152 + i * 8192, [2048]) for i in range(2)]
                assert PH0 + 65536 == GM0 + 16384
                xn6 = sb(PH0 + 98304, [2048])
                junk6 = sb(PH0 + 106496, [2048], BF16)
                tmpm = sb(PH0 + 110592, [512])
                dead = [("h1own", t) for t in range(8)] + [("h1halo", t) for t in range(4)] + [("ybT", t) for t in range(8)] + \
                    [("yaT", t) for t in range(8)] + [("sg", i) for i in range(4)]
                sp_load(gate1b, modd[0, 2 * D:3 * D].partition_broadcast(128), ["gate1b"], cpool.nxt(), reads=modd_keys)
                sp_load(g2b, g2row[0].partition_broadcast(128), ["g2b"], cpool.nxt())
                sp_load(w2effb, modd[0, 4 * D:5 * D].partition_broadcast(128), ["w2effb"], cpool.nxt(), reads=modd_keys)
                sp_load(shift2b, modd[0, 3 * D:4 * D].partition_broadcast(128), ["shift2b"], cpool.nxt(), reads=modd_keys)
                dve(["w2effb", "g2b"], ["w2effb"], lambda e: e.scalar_tensor_tensor(
                    out=w2effb, in0=w2effb, scalar=1.0, in1=g2b, op0=ALU.add, op1=ALU.mult))
                wos = [w_get(u) for u in pl["wo"]]

                def p6_mm_pe(i):
                    b = i % 2
                    P.dma(SP, [], [("xt6", b)], lambda e, b=b, i=i: e.dma_start(
                        out=xt6[b], in_=xe[row0 + HALO + i * 128:row0 + HALO + (i + 1) * 128, :]), xsem[b])
                    for cu in range(4):
                        def emit(e, i=i, cu=cu):
                            return mm_acc(e, pbank(cu), [(mergedT[:, kc, i * 128:(i + 1) * 128], wos[cu][0][:, kc, :]) for kc in range(16)])
                        pe_group([wos[cu][1], ("mergedT", i // 4)], [pkey(cu)], emit)

                def p6_mm_dve(i):
                    b = i % 2
                    for cu in range(4):
                        dve([pkey(cu), "gate1b"], ["tmpm"], lambda e, cu=cu: e.tensor_tensor(
                            out=tmpm, in0=pbank(cu), in1=gate1b[:, cu * 512:(cu + 1) * 512], op=ALU.mult))
                        dve(["tmpm", ("xt6", b)], [("xt6", b)], lambda e, cu=cu, b=b: e.tensor_tensor(
                            out=xt6[b][:, cu * 512:(cu + 1) * 512], in0=xt6[b][:, cu * 512:(cu + 1) * 512], in1=tmpm, op=ALU.add))

                def p6_front(i):
                    b = i % 2
                    xk = ("xt6", b)
                    ssa = small[:, 32 + i:33 + i]
                    rsa = small[:, 48 + i:49 + i]
                    act_fn(junk6, xt6[b], AF.Square, [xk], [("ss2", i)], accum=ssa)
                    rms_rstd(ssa, rsa, ("ss2", i), ("rs2", i))
                    P.dma(SP, [xk], [("x1s", p * 8 + i)], lambda e, b=b, i=i: e.dma_start(
                        out=x1s[p * TP + i * 128:p * TP + (i + 1) * 128, :], in_=xt6[b]), osem[b])
                    dve([xk, ("rs2", i), "w2effb"], ["xn6"], lambda e, b=b, rsa=rsa: e.scalar_tensor_tensor(
                        out=xn6, in0=xt6[b], scalar=rsa, in1=w2effb, op0=ALU.mult, op1=ALU.mult))
                    dve(["xn6", "shift2b"], ["xn6"], lambda e: e.tensor_tensor(out=xn6, in0=xn6, in1=shift2b, op=ALU.add))
                    hb = i % 2
                    act_fn(h2row[hb], xn6, AF.Copy, ["xn6"], [("h2row", hb)])
                    P.dma(SP, [("h2row", hb)], [("h2d", p * 8 + i)], lambda e, hb=hb, i=i: e.dma_start(
                        out=h2d[p * TP + i * 128:p * TP + (i + 1) * 128, :], in_=h2row[hb]), hsem[hb])
                def p6_back(i):
                    b = i % 2
                    for q in range(4):
                        def emit(e, q=q):
                            ins = None
                            for k in range(4):
                                dc = q * 4 + k
                                ins = e.transpose(pbank(4 + q, 128, k * 128), xn6[:, dc * 128:(dc + 1) * 128], identF)
                            return ins
                        pe_group(["xn6", "identF"], [pkey(4 + q)], emit)
                        act_fn(h2f[:, q * 4:(q + 1) * 4, :], pbank(4 + q).rearrange("p (k t) -> p k t", k=4), AF.Copy,
                               [pkey(4 + q)], [("h2f", q)])
                    LB = 0

                    def emit(e):
                        return mm_acc(e, pbank(LB, 36), [(h2f[:, dc, :], wr[:, dc, :]) for dc in range(16)])
                    pe_group([("h2f", q) for q in range(4)] + ["wr"], [pkey(LB)], emit)
                    lg = rt[:, 0:36]
                    dve([pkey(LB)], ["rt"], lambda e: e.tensor_copy(out=lg, in_=pbank(LB, 36)))
                    gl = rt[:, 0:4]
                    el = rt[:, 4:36]
                    gmax = rt[:, 40:41]
                    ngmax = rt[:, 41:42]
                    gsum = rt[:, 42:43]
                    gw = rt[:, 43:44]
                    goh = rt[:, 44:48]
                    gex = rt[:, 48:52]
                    esel = rt[:, 56:64]
                    etmp = rt[:, 64:96]
                    m1 = rt[:, 96:97]
                    m2 = rt[:, 97:98]
                    oh1 = rt[:, 104:112]
                    oh2 = rt[:, 112:120]
                    msk = rt[:, 120:128]
                    dd = rt[:, 128:129]
                    w1_ = rt[:, 129:130]
                    w2_ = rt[:, 130:131]
                    ew = rt[:, 136:144]
                    gwo = rt[:, 144:148]
                    R = ["rt"]
                    dve(R, R, lambda e: e.tensor_reduce(out=gmax, in_=gl, axis=AX.X, op=ALU.max))
                    dve(R, R, lambda e: e.tensor_scalar(out=ngmax, in0=gmax, scalar1=-1.0, scalar2=None, op0=ALU.mult))
                    act_fn(gex, gl, AF.Exp, R, R, bias=ngmax, accum=gsum)
                    dve(R, R, lambda e: e.reciprocal(out=gw, in_=gsum))
                    dve(R, R, lambda e: e.tensor_scalar(out=goh, in0=gl, scalar1=gmax, scalar2=None, op0=ALU.is_equal))
                    dve(R, R, lambda e: e.tensor_scalar(out=esel, in0=el[:, 0:8], scalar1=goh[:, 0:1], scalar2=None, op0=ALU.mult))
                    for g in range(1, 4):
                        dve(R, R, lambda e, g=g: e.scalar_tensor_tensor(
                            out=esel, in0=el[:, g * 8:(g + 1) * 8], scalar=goh[:, g:g + 1], in1=esel, op0=ALU.mult, op1=ALU.add))
                    dve(R, R, lambda e: e.tensor_reduce(out=m1, in_=esel, axis=AX.X, op=ALU.max))
                    dve(R, R, lambda e: e.tensor_scalar(out=oh1, in0=esel, scalar1=m1, scalar2=None, op0=ALU.is_equal))
                    dve(R, R, lambda e: e.scalar_tensor_tensor(out=msk, in0=oh1, scalar=-1e30, in1=esel, op0=ALU.mult, op1=ALU.add))
                    dve(R, R, lambda e: e.tensor_reduce(out=m2, in_=msk, axis=AX.X, op=ALU.max))
                    dve(R, R, lambda e: e.tensor_scalar(out=oh2, in0=msk, scalar1=m2, scalar2=None, op0=ALU.is_equal))
                    dve(R, R, lambda e: e.tensor_tensor(out=dd, in0=m2, in1=m1, op=ALU.subtract))
                    act_fn(dd, dd, AF.Exp, R, R)
                    dve(R, R, lambda e: e.tensor_scalar(out=w1_, in0=dd, scalar1=1.0, scalar2=None, op0=ALU.add))
                    dve(R, R, lambda e: e.reciprocal(out=w1_, in_=w1_))
                    dve(R, R, lambda e: e.tensor_tensor(out=w2_, in0=dd, in1=w1_, op=ALU.mult))
                    dve(R, R, lambda e: e.tensor_scalar(out=ew, in0=oh1, scalar1=w1_, scalar2=None, op0=ALU.mult))
                    dve(R, R, lambda e: e.scalar_tensor_tensor(out=ew, in0=oh2, scalar=w2_, in1=ew, op0=ALU.mult, op1=ALU.add))
                    dve(R, R, lambda e: e.tensor_scalar(out=gwo, in0=goh, scalar1=gw, scalar2=None, op0=ALU.mult))
                    ti = p * 8 + i
                    for g in range(4):
                        dve(R, [("sel", ti)], lambda e, g=g, ti=ti: e.tensor_scalar(
                            out=sel0[:, ti, g * 8:(g + 1) * 8], in0=oh1, scalar1=goh[:, g:g + 1], scalar2=None, op0=ALU.mult))
                        dve(R, [("sel", ti)], lambda e, g=g, ti=ti: e.tensor_scalar(
                            out=sel1[:, ti, g * 8:(g + 1) * 8], in0=oh2, scalar1=goh[:, g:g + 1], scalar2=None, op0=ALU.mult))
                    dve(R, [("wkk", ti)], lambda e, ti=ti: e.tensor_tensor(out=wsl[:, ti, 0:1], in0=w1_, in1=gw, op=ALU.mult))
                    dve(R, [("wkk", ti)], lambda e, ti=ti: e.tensor_tensor(out=wsl[:, ti, 1:2], in0=w2_, in1=gw, op=ALU.mult))

                p6_mm_pe(0)
                p6_mm_dve(0)
                for i in range(8):
                    if i + 1 < 8:
                        p6_mm_pe(i + 1)
                    p6_front(i)
                    if i + 1 < 8:
                        p6_mm_dve(i + 1)
                    p6_back(i)
                for u in pl["wo"]:
                    w_rel(u)
                if debug is not None and debug["stage"] == "p6":
                    break


            class MW:
                units = []
                released = []
                nloaded = 0

            def m_view(slot, a, b):
                return sb(RING0 + slot * SLOT_B, [a, b], BF16)

            def m_pump(limit=None):
                while MW.nloaded < len(MW.units):
                    u = MW.nloaded
                    if limit is not None and u >= limit:
                        break
                    if u >= MSLOT and not MW.released[u - MSLOT]:
                        break
                    src, a, b = MW.units[u]
                    slot = u % MSLOT
                    dst = m_view(slot, a, b)
                    wk_ = [("mring", slot)]
                    if u < NSLOT:
                        wk_.append(("ring", slot))
                    P.dma(POOL, [], wk_, lambda e, dst=dst, src=src: e.dma_start(out=dst, in_=src), mr_sem[slot])
                    MW.nloaded += 1

            def m_get(u):
                m_pump()
                assert u < MW.nloaded
                src, a, b = MW.units[u]
                return m_view(u % MSLOT, a, b), ("mring", u % MSLOT)

            def m_rel(u):
                MW.released[u] = True

            for e_ in range(NE):
                wpat = "(p c) n -> p c n" if CONTIG else "(c p) n -> p c n"
                for src, a, b in ((w1[e_].rearrange(wpat, p=128), 16, 512),
                                  (w3[e_].rearrange(wpat, p=128), 16, 512),
                                  (w2[e_].rearrange("(c p) n -> p c n", p=128), 4, 2048)):
                    MW.units.append((src, a, b))
                    MW.released.append(False)

            if debug is None or debug["stage"] in ("meta", "moe"):
                m_pump(limit=NSLOT)

            if debug is None or debug["stage"] in ("meta", "moe"):
                P.nofloor.discard("pool")
                P.fence()
                o = PH0 + (MSLOT - NSLOT) * SLOT_B
                selA = sb(o, [16, 32]); o += 2048
                cum = sb(o, [16, 32]); o += 2048
                pos = sb(o, [16, 32]); o += 2048
                tmpd = sb(o, [16, 32]); o += 2048
                destf = sb(o, [32]); o += 128
                desti = sb(o, [32], I32); o += 128
                cntf = sb(o, [32]); o += 128
                cnti = sb(o, [32], I32); o += 128
                fillt = sb(o, [2048], I32); o += 8192
                Utri = sb(o, [128]); o += 512
                OnesM = sb(o, [128]); o += 512
                META_END = o
                sp_load(Utri, Utri_d, ["Utri"], cpool.nxt())
                sp_load(OnesM, Ones_d, ["OnesM"], cpool.nxt())
                selk = [("sel", t) for t in range(16)]
                dve(selk, ["selA"], lambda e: e.tensor_tensor(out=selA, in0=sel0, in1=sel1, op=ALU.add))
                dve([], ["cum"], lambda e: e.memset(cum[:, 0, :], 0.0))
                for t in range(1, 16):
                    dve(["cum", "selA"], ["cum"], lambda e, t=t: e.tensor_tensor(
                        out=cum[:, t, :], in0=cum[:, t - 1, :], in1=selA[:, t - 1, :], op=ALU.add))
                def emit(e):
                    ins = None
                    for t in range(16):
                        e.matmul(pbank(0, 32, t * 32), OnesM, cum[:, t, :], start=True, stop=False)
                        ins = e.matmul(pbank(0, 32, t * 32), Utri, selA[:, t, :], start=False, stop=True)
                    return ins
                pe_group(["cum", "selA", "OnesM", "Utri"], [pkey(0)], emit)
                dve([pkey(0)], ["pos"], lambda e: e.tensor_copy(out=pos, in_=pbank(0).rearrange("p (t x) -> p t x", t=16)))
                dve(["cum", "selA"], ["tmpd"], lambda e: e.tensor_tensor(out=tmpd[:, 0, :], in0=cum[:, 15, :], in1=selA[:, 15, :], op=ALU.add))
                pe_group(["tmpd", "OnesM"], [pkey(1)], lambda e: e.matmul(pbank(1, 32), OnesM, tmpd[:, 0, :], start=True, stop=True))
                dve([pkey(1)], ["cntf"], lambda e: e.tensor_copy(out=cntf, in_=pbank(1, 32)))
                dve(["cntf"], ["cnti"], lambda e: e.tensor_copy(out=cnti, in_=cntf))
                P.dma(SP, ["cnti"], ["cntd"], lambda e: e.dma_start(out=cntd, in_=cnti[0:1, :]), cpool.nxt())
                dve(["pos", "ecap"], ["pos"], lambda e: e.tensor_tensor(
                    out=pos, in0=pos, in1=ecap.unsqueeze(1).to_broadcast([128, 16, 32]), op=ALU.add))
                for k, selx in enumerate((sel0, sel1)):
                    dve(["pos"] + selk, ["tmpd"], lambda e, selx=selx: e.tensor_tensor(out=tmpd, in0=pos, in1=selx, op=ALU.mult))
                    dve(["tmpd"], ["destf"], lambda e, k=k: e.tensor_reduce(
                        out=destf.rearrange("p (t k) -> p t k", k=2)[:, :, k], in_=tmpd, axis=AX.X, op=ALU.add))
                dve(["destf"], ["desti"], lambda e: e.tensor_copy(out=desti, in_=destf))
                sp_load(fillt, fillv_d, ["fillt"], cpool.nxt())
                P.dma(SP, ["fillt"], ["table"], lambda e: e.dma_start(
                    out=table.rearrange("(p r) c -> p (r c)", p=128), in_=fillt), cpool.nxt())
                for t in range(16):
                    for k in range(2):
                        P.dma(POOL, ["desti", "vals", "table"], [("tabw", t, k)], lambda e, t=t, k=k: e.indirect_dma_start(
                            out=table, out_offset=bass.IndirectOffsetOnAxis(ap=desti[:, t * 2 + k:t * 2 + k + 1], axis=0),
                            in_=vals[:, (t * 2 + k) * 4:(t * 2 + k) * 4 + 4], in_offset=None,
                            oob_is_err=False), tpool.nxt())
                tabkeys = [("tabw", t, k) for t in range(16) for k in range(2)]
                if debug is not None and debug["stage"] == "meta":
                    dbg_dump([(destf, 32, ["destf"]), (cntf, 32, ["cntf"]), (wsl.rearrange("p a b -> p (a b)"), 32, [("wkk", t) for t in range(16)]),
                              (sel0.rearrange("p a b -> p (a b)"), 512, selk), (sel1.rearrange("p a b -> p (a b)"), 512, selk)])

            if debug is None or debug["stage"] in ("moe",):
                P.fence()
                o = PH0 + (MSLOT - NSLOT) * SLOT_B
                NXG = 5
                idxt = [sb(o + i * 16, [4], I32) for i in range(NXG)]; o += 128
                xg = [sb(o + i * 4096, [2048], BF16) for i in range(NXG)]; o += NXG * 4096
                xgT = sb(o, [16, 128], BF16); o += 4096
                sAm = sb(o, [512]); o += 2048
                actm = sb(o, [512], BF16); o += 1024
                actTm = sb(o, [4, 128], BF16); o += 1024
                NY = 4
                Ysb = [sb(o + i * 8192, [2048]) for i in range(NY)]; o += NY * 8192
                MOE_END = o
                assert o <= PH_END

                regs = {}
                if target is not None:
                    regs[target] = handle.alloc_register("cnt_" + target)
                tcount = [0]

                def gather_ops(ei, t, xb):
                    row0 = ei * CAP + t * 128
                    P.dma(SP, tabkeys + ["table"], [("idxt", xb)], lambda e: e.dma_start(out=idxt[xb], in_=table[row0:row0 + 128, :]), ixsem[xb])
                    P.dma(POOL, [("idxt", xb)] + [("h2d", i) for i in range(16)], [("xg", xb)], lambda e: e.indirect_dma_start(
                        out=xg[xb], out_offset=None, in_=h2d, in_offset=bass.IndirectOffsetOnAxis(ap=idxt[xb][:, 0:1], axis=0),
                        oob_is_err=False), gasem[xb])

                NSTAT = int(os.environ.get('K_NSTAT', '2'))

                def tile_ops(ei, t, wa, wak, wb, wbk, wc, wck):
                    n = tcount[0]
                    tcount[0] += 1
                    b = n % NY
                    if t < NSTAT:
                        xb = (ei % 2) * NSTAT + t
                    else:
                        xb = NXG - 1
                        gather_ops(ei, t, xb)
                    tb0 = PS[0][:, 0:512].bitcast(BF16)
                    tb1 = PS[0][:, 512:1024].bitcast(BF16)
                    for hb_, tb in enumerate((tb0, tb1)):
                        def emit(e, hb_=hb_, tb=tb):
                            ins = None
                            for k in range(8):
                                dc = hb_ * 8 + k
                                src_cols = xg[xb][:, dc:2048:16] if CONTIG else xg[xb][:, dc * 128:(dc + 1) * 128]
                                ins = e.transpose(tb[:, k * 128:(k + 1) * 128], src_cols, identB)
                            return ins
                        pe_group([("xg", xb), "identB"], [pkey(hb_)], emit)
                    act_fn(xgT[:, 0:8, :], tb0.rearrange("p (k t) -> p k t", k=8), AF.Copy, [pkey(0)], [("xgT", 0)])
                    dve([pkey(1)], [("xgT", 1)], lambda e: e.tensor_copy(out=xgT[:, 8:16, :], in_=tb1.rearrange("p (k t) -> p k t", k=8)))
                    pe_group([wak, ("xgT", 0), ("xgT", 1)], [pkey(2)], lambda e: mm_acc(e, pbank(2), [(xgT[:, dc, :], wa[:, dc, :]) for dc in range(16)]))
                    pe_group([wbk, ("xgT", 0), ("xgT", 1)], [pkey(3)], lambda e: mm_acc(e, pbank(3), [(xgT[:, dc, :], wb[:, dc, :]) for dc in range(16)]))
                    act_fn(sAm, pbank(2), AF.Silu, [pkey(2)], ["sAm"])
                    dve(["sAm", pkey(3)], ["actm"], lambda e: e.tensor_tensor(out=actm, in0=sAm, in1=pbank(3), op=ALU.mult))

                    def emit(e):
                        ins = None
                        for k in range(4):
                            ins = e.transpose(tb0[:, k * 128:(k + 1) * 128], actm[:, k * 128:(k + 1) * 128], identB)
                        return ins
                    pe_group(["actm", "identB"], [pkey(0)], emit)
                    act_fn(actTm, tb0[:, 0:512].rearrange("p (k t) -> p k t", k=4), AF.Copy, [pkey(0)], ["actTm"])
                    for nb in range(4):
                        pe_group([wck, "actTm"], [pkey(4 + nb)], lambda e, nb=nb: mm_acc(
                            e, pbank(4 + nb), [(actTm[:, fc, :], wc[:, fc, nb * 512:(nb + 1) * 512]) for fc in range(4)]))
                    act_fn(Ysb[b][:, 0:1024], PS[2][:, :], AF.Copy, [pkey(4), pkey(5)], [("Ysb", b, 0)])
                    dve([pkey(6), pkey(7)], [("Ysb", b, 1)], lambda e: e.tensor_copy(out=Ysb[b][:, 1024:2048], in_=PS[3][:, :]))
                    for hf in range(2):
                        P.dma(POOL, [("Ysb", b, hf), ("idxt", xb)], [("Ybuf", n, hf)], lambda e, hf=hf: e.indirect_dma_start(
                            out=Ybuf4, out_offset=bass.IndirectOffsetOnAxis(ap=idxt[xb][:, 1 + hf:2 + hf], axis=0),
                            in_=Ysb[b][:, hf * 1024:(hf + 1) * 1024], in_offset=None,
                            oob_is_err=False), scsem[b * 2 + hf])

                MAXT = TOKC // 128

                GRP = 4

                def tiles_from(ei, t, tend, rv, ws):
                    if t >= tend:
                        return
                    with P.guard(lambda h: rv > t * 128):
                        tile_ops(ei, t, *ws)
                        tiles_from(ei, t + 1, tend, rv, ws)

                for t_ in range(NSTAT):
                    gather_ops(0, t_, t_)
                for ei in range(NE):
                    if ei + 1 < NE:
                        for t_ in range(NSTAT):
                            gather_ops(ei + 1, t_, ((ei + 1) % 2) * NSTAT + t_)
                    wa, wak = m_get(3 * ei)
                    wb, wbk = m_get(3 * ei + 1)
                    wc, wck = m_get(3 * ei + 2)
                    rv = None
                    for eng in P.engines:
                        r = P.raw(eng, ["cntd"], lambda h, ei=ei: h.reg_load(regs[target], cntd[0:1, ei:ei + 1]))
                    if target is not None:
                        rv = handle.snap(regs[target])
                    for g0 in range(0, MAXT, GRP):
                        tiles_from(ei, g0, g0 + GRP, rv, (wa, wak, wb, wbk, wc, wck))
                    m_rel(3 * ei)
                    m_rel(3 * ei + 1)
                    m_rel(3 * ei + 2)
                    m_pump()

                P.fence()
                o = RING0
                gate2b = sb(o, [2048]); o += 8192
                fgb = sb(o, [2048]); o += 8192
                NF = 3
                xt8 = [sb(o + i * 8192, [2048]) for i in range(NF)]; o += NF * 8192
                y0t = [sb(o + i * 8192, [2048]) for i in range(NF)]; o += NF * 8192
                y1t = [sb(o + i * 8192, [2048]) for i in range(NF)]; o += NF * 8192
                junk8 = sb(o, [2048], BF16); o += 4096
                assert o <= PH_END
                ybkeys = [("Ybuf", n, hf) for n in range(tcount[0]) for hf in range(2)]
                sp_load(gate2b, modd[0, 5 * D:6 * D].partition_broadcast(128), ["gate2b"], cpool.nxt(), reads=modd_keys)
                sp_load(fgb, fg[0].partition_broadcast(128), ["fgb"], cpool.nxt())

                def fin_loads(i):
                    b = i % NF
                    P.dma(SP, [("x1s", i)], [("xt8", b)], lambda e: e.dma_start(
                        out=xt8[b], in_=x1s[i * 128:(i + 1) * 128, :]), fsem[b])
                    P.dma(SP, ybkeys, [("y0t", b)], lambda e: e.dma_start(
                        out=y0t[b], in_=Ybuf[i * 128:(i + 1) * 128, :]), fsem[3 + b])
                    P.dma(SP, ybkeys, [("y1t", b)], lambda e: e.dma_start(
                        out=y1t[b], in_=Ybuf[TOKC + i * 128:TOKC + (i + 1) * 128, :]), fsem[6 + b])

                fin_loads(0)
                fin_loads(1)
                for i in range(16):
                    b = i % NF
                    xk = ("xt8", b)
                    if i + 2 < 16:
                        fin_loads(i + 2)
                    act_fn(y0t[b], y0t[b], AF.Copy, [("y0t", b), ("wkk", i)], [("y0t", b)], scale=wsl[:, i, 0:1])
                    dve([("y0t", b), ("y1t", b), ("wkk", i)], [("y0t", b)], lambda e, b=b, i=i: e.scalar_tensor_tensor(
                        out=y0t[b], in0=y1t[b], scalar=wsl[:, i, 1:2], in1=y0t[b], op0=ALU.mult, op1=ALU.add))
                    dve([("y0t", b), "gate2b"], [("y0t", b)], lambda e, b=b: e.tensor_tensor(out=y0t[b], in0=y0t[b], in1=gate2b, op=ALU.mult))
                    dve([("y0t", b), xk], [xk], lambda e, b=b: e.tensor_tensor(out=xt8[b], in0=xt8[b], in1=y0t[b], op=ALU.add))
                    ssa = small[:, 128 + i:129 + i]
                    rsa = small[:, 144 + i:145 + i]
                    act_fn(junk8, xt8[b], AF.Square, [xk], [("ss8", i)], accum=ssa)
                    rms_rstd(ssa, rsa, ("ss8", i), ("rs8", i))
                    dve([xk, ("rs8", i), "fgb"], [xk], lambda e, b=b, rsa=rsa: e.scalar_tensor_tensor(
                        out=xt8[b], in0=xt8[b], scalar=rsa, in1=fgb, op0=ALU.mult, op1=ALU.mult))
                    P.dma(SP, [xk], [("y", i)], lambda e, b=b, i=i: e.dma_start(
                        out=y[i * 128:(i + 1) * 128, :], in_=xt8[b]), fsem[9 + b])

            outkeys = [("y", i) for i in range(16)] if debug is None else [k for k in P.state.keys() if isinstance(k, tuple) and k[0] in ("dbg", "y")]
            P.final_wait(SP, outkeys)

        with nc.Block() as block:
            @block.tensor
            def _(e):
                program("pe", e)

            @block.scalar
            def _(e):
                program("act", e)

            @block.vector
            def _(e):
                program("dve", e)

            @block.gpsimd
            def _(e):
                program("pool", e)

            @block.sync
            def _(e):
                program("sp", e)
    return nc


_NC_CACHE = {}


def _vals_const():
    v = np.zeros((128, 16, 2, 4), np.int32)
    p = np.arange(128)[:, None]
    t = np.arange(16)[None, :]
    tok = t * 128 + p
    for k in range(2):
        v[:, :, k, 0] = tok
        v[:, :, k, 1] = 2 * (k * TOKC + tok)
        v[:, :, k, 2] = 2 * (k * TOKC + tok) + 1
    return np.ascontiguousarray(v.reshape(128, 128))


def _fill_const():
    r = np.arange(512)
    row = np.zeros((512, 4), np.int32)
    row[:, 1] = 4 * TOKC + (r % 128)
    row[:, 2] = 4 * TOKC + 128 + (r % 128)
    return np.ascontiguousarray(np.broadcast_to(row.reshape(1, 2048), (128, 2048)))


def _host_inputs(inputs):
    f = np.float32
    x = np.asarray(inputs["x"], f)
    c = np.asarray(inputs["c"], f)
    L = 0
    tab = np.asarray(inputs["rel_bias"], f)[L]
    ext = np.concatenate([tab, np.repeat(tab[:, -1:], 400, axis=1)], axis=1)
    k = np.arange(128)[:, None]
    q = np.arange(128)[None, :]
    bt = np.empty((128, 16, 3, 128), f)
    for dl in range(3):
        idx = 256 + dl * 128 + q - k
        bt[:, :, dl, :] = np.transpose(ext[:, idx], (1, 0, 2))
    bt[64:, :, 0, :64] = NEG
    wr = np.concatenate([np.asarray(inputs["w_group"], f)[L]] +
                        [np.asarray(inputs["w_expert"], f)[L, g] for g in range(4)], axis=1)
    shared = {
        "ident": np.eye(128, dtype=f),
        "ada_w": np.ascontiguousarray(np.asarray(inputs["ada_w"], f)[L]),
        "ada_b": np.ascontiguousarray(np.asarray(inputs["ada_b"], f)[L][None, :]),
        "g1T": np.ascontiguousarray(np.asarray(inputs["norm1_g"], f)[L].reshape(16, 128).T),
        "g2T": np.ascontiguousarray(np.asarray(inputs["norm2_g"], f)[L].reshape(16, 128).T),
        "lnG": np.ascontiguousarray(np.asarray(inputs["gmlp_ln_g"], f)[L][None, :]),
        "lnB": np.ascontiguousarray(np.asarray(inputs["gmlp_ln_b"], f)[L][None, :]),
        "bS": np.ascontiguousarray(np.asarray(inputs["gmlp_b_s"], f)[L].reshape(1, 1024)),
        "wS": np.ascontiguousarray(np.asarray(inputs["gmlp_w_s"], f)[L]),
        "biasT": np.ascontiguousarray(bt.reshape(128, 16 * 384)),
        "tabc": np.ascontiguousarray(tab[:, 512][None, :]),
        "w_in": np.ascontiguousarray(np.asarray(inputs["w_in"], f)[L]),
        "wbrA": np.ascontiguousarray(np.asarray(inputs["w_branch_a"], f)[L]),
        "wbrB": np.ascontiguousarray(np.asarray(inputs["w_branch_b"], f)[L]),
        "wout": np.ascontiguousarray(np.asarray(inputs["w_out"], f)[L]),
        "wr": np.ascontiguousarray(wr),
        "w1": np.ascontiguousarray(np.asarray(inputs["w1"], f)[L].reshape(NE, D, 512)),
        "w3": np.ascontiguousarray(np.asarray(inputs["w3"], f)[L].reshape(NE, D, 512)),
        "w2": np.ascontiguousarray(np.asarray(inputs["w2"], f)[L].reshape(NE, 512, D)),
        "fg": np.ascontiguousarray(np.asarray(inputs["final_g"], f)[None, :]),
        "g2row": np.ascontiguousarray(np.asarray(inputs["norm2_g"], f)[L][None, :]),
        "g1row": np.ascontiguousarray(np.asarray(inputs["norm1_g"], f)[L][None, :]),
        "Utri": np.triu(np.ones((128, 128), f), 1),
        "OnesM": np.ones((128, 128), f),
        "ecap": np.ascontiguousarray(np.broadcast_to((np.arange(NE) * CAP).astype(f)[None, :], (128, NE))),
        "vals": _vals_const(),
        "fillv": _fill_const(),
    }
    in_maps = []
    for core in range(8):
        b = core // 4
        s0 = (core % 4) * TOKC
        xe = np.zeros((TOKC + HALO, D), f)
        if s0 == 0:
            xe[HALO:] = x[b, 0:TOKC]
        else:
            xe[:] = x[b, s0 - HALO:s0 + TOKC]
        flags = np.ones((128, 2), f)
        if s0 == 0:
            flags[:, 0] = 0.0
        m = dict(shared)
        m["xe"] = xe
        m["cT"] = np.ascontiguousarray(c[b].reshape(16, 128).T)
        m["flags"] = flags
        in_maps.append(m)
    return in_maps


def kernel(**inputs):
    if "nc" not in _NC_CACHE:
        _NC_CACHE["nc"] = build()
    nc = _NC_CACHE["nc"]
    in_maps = _host_inputs(inputs)
    res = run_bass_kernel_spmd(nc, in_maps, core_ids=list(range(8)))
    out = np.empty((2, 8192, D), np.float32)
    for core in range(8):
        b = core // 4
        s0 = (core % 4) * TOKC
        out[b, s0:s0 + TOKC] = res.results[core]["y"]
    return out
```
